# Optimizing a Trainium2 kernel written in Bass

```python
import jax, jax.numpy as jnp
from jax import lax
import numpy as np

D_MODEL = 1024
BATCH = 2
SEQ = 8192
DEPTH = 1

GLA_HEADS = 4
GLA_VAL_WIDTH = D_MODEL // 2
GLA_DV = GLA_VAL_WIDTH // GLA_HEADS
GLA_DK = GLA_DV // 2
GLA_KEY_WIDTH = GLA_HEADS * GLA_DK
GLA_GATE_RANK = 16
GLA_TAU = 16.0
GLA_CHUNK = 64
ATT_WIDTH = D_MODEL - GLA_VAL_WIDTH
ATT_HEAD_DIM = 64
ATT_HEADS = ATT_WIDTH // ATT_HEAD_DIM
ROT_DIM = ATT_HEAD_DIM // 4
ROPE_THETA = 500000.0
DILATED_PATTERNS = ((128, 1), (512, 4), (2048, 16))
MIX_WIDTH = GLA_VAL_WIDTH + ATT_WIDTH
IN_WIDTH = 2 * GLA_KEY_WIDTH + 2 * GLA_VAL_WIDTH + 2 * GLA_GATE_RANK + 3 * ATT_WIDTH
MOE_GROUPS = 4
MOE_EXPERTS_PER_GROUP = 8
MOE_N_EXPERTS = MOE_GROUPS * MOE_EXPERTS_PER_GROUP
MOE_TOP_K = 2
MOE_D_FF = D_MODEL // 2
MOE_BLOCK = 128
EPS = 1e-6
NEG_INF = -1e30

kernel_name = 'hybrid_gla_dilated_attn_hier_moe'


def _rmsnorm(x, w):
    xf = x.astype(jnp.float32)
    y = xf * lax.rsqrt(jnp.mean(xf * xf, axis=-1, keepdims=True) + EPS)
    return (y * w.astype(jnp.float32)).astype(x.dtype)


def _gla_one_direction(q, k, v, log_a):
    B, H, S, dk = q.shape
    dv = v.shape[-1]
    C = GLA_CHUNK
    n = S // C
    q = q.reshape(B, H, n, C, dk)
    k = k.reshape(B, H, n, C, dk)
    v = v.reshape(B, H, n, C, dv)
    b = jnp.cumsum(log_a.reshape(B, H, n, C, dk), axis=-2)
    b_last = b[..., -1:, :]
    q_dec = q * jnp.exp(b)
    k_inv = k * jnp.exp(-b)
    k_end = k * jnp.exp(b_last - b)
    lower = jnp.tril(jnp.ones((C, C), dtype=bool))
    attn = jnp.where(lower, jnp.einsum('bhncd,bhnsd->bhncs', q_dec, k_inv), 0.0)
    o_intra = jnp.einsum('bhncs,bhnsv->bhncv', attn, v)
    chunk_kv = jnp.einsum('bhncd,bhncv->bhndv', k_end, v)
    chunk_decay = jnp.exp(b_last[..., 0, :])

    def step(state, inp):
        kv_n, dec_n = inp
        return state * dec_n[..., None] + kv_n, state

    init = jnp.zeros((B, H, dk, dv), jnp.float32)
    _, states = lax.scan(step, init, (jnp.moveaxis(chunk_kv, 2, 0), jnp.moveaxis(chunk_decay, 2, 0)))
    states = jnp.moveaxis(states, 0, 2)
    o_inter = jnp.einsum('bhncd,bhndv->bhncv', q_dec, states)
    return (o_intra + o_inter).reshape(B, H, S, dv)


def _gla_mixer(q, k, v, g_out, lr_f, lr_b, wf, bf, wb, bb, norm_w):
    B, S, _ = q.shape
    f32 = jnp.float32

    def heads(t, d):
        return t.reshape(B, S, GLA_HEADS, d).transpose(0, 2, 1, 3).astype(f32)

    qh = heads(q, GLA_DK) * (GLA_DK ** -0.5)
    kh = heads(k, GLA_DK)
    vh = heads(v, GLA_DV)
    log_a_f = heads(jax.nn.log_sigmoid(lr_f.astype(f32) @ wf.astype(f32) + bf.astype(f32)), GLA_DK) / GLA_TAU
    log_a_b = heads(jax.nn.log_sigmoid(lr_b.astype(f32) @ wb.astype(f32) + bb.astype(f32)), GLA_DK) / GLA_TAU

    def flip(t):
        return jnp.flip(t, axis=2)

    o = _gla_one_direction(qh, kh, vh, log_a_f) + flip(
        _gla_one_direction(flip(qh), flip(kh), flip(vh), flip(log_a_b)))
    o = o.transpose(0, 2, 1, 3)
    gate = g_out.reshape(B, S, GLA_HEADS, GLA_DV).astype(f32)
    o = _rmsnorm(o, norm_w) * jax.nn.silu(gate)
    return o.reshape(B, S, GLA_VAL_WIDTH).astype(q.dtype)


def _rope_tables(S):
    inv = ROPE_THETA ** (-(jnp.arange(0, ROT_DIM, 2, dtype=jnp.float32) / ROT_DIM))
    ang = jnp.arange(S, dtype=jnp.float32)[:, None] * inv[None, :]
    return jnp.cos(ang), jnp.sin(ang)


def _apply_partial_rope(t, cos, sin):
    half = ROT_DIM // 2
    t1 = t[..., :half]
    t2 = t[..., half:ROT_DIM]
    c = cos[None, :, None, :]
    s = sin[None, :, None, :]
    return jnp.concatenate([t1 * c - t2 * s, t2 * c + t1 * s, t[..., ROT_DIM:]], axis=-1)


def _banded_attention(q, k, v, radius):
    N, L, hd = q.shape
    R = radius
    nb = -(-L // R)
    Lp = nb * R
    q_p = jnp.pad(q, ((0, 0), (0, Lp - L), (0, 0))).reshape(N, nb, R, hd)
    k_p = jnp.pad(k, ((0, 0), (R, Lp - L + R), (0, 0))).reshape(N, nb + 2, R, hd)
    v_p = jnp.pad(v, ((0, 0), (R, Lp - L + R), (0, 0))).reshape(N, nb + 2, R, hd)
    k_win = jnp.concatenate([k_p[:, :-2], k_p[:, 1:-1], k_p[:, 2:]], axis=2)
    v_win = jnp.concatenate([v_p[:, :-2], v_p[:, 1:-1], v_p[:, 2:]], axis=2)
    s = jnp.einsum('nbqd,nbkd->nbqk', q_p, k_win) * (hd ** -0.5)
    qpos = (jnp.arange(nb)[:, None] * R + jnp.arange(R)[None, :])[:, :, None]
    kpos = (jnp.arange(nb)[:, None] * R - R + jnp.arange(3 * R)[None, :])[:, None, :]
    valid = (jnp.abs(qpos - kpos) <= R) & (kpos >= 0) & (kpos < L)
    s = jnp.where(valid, s, NEG_INF)
    m = jnp.max(s, axis=-1, keepdims=True)
    p = jnp.exp(s - m)
    z = jnp.sum(p, axis=-1, keepdims=True)
    o = jnp.einsum('nbqk,nbkd->nbqd', p / z, v_win)
    lse = (m + jnp.log(z))[..., 0]
    return o.reshape(N, Lp, hd)[:, :L], lse.reshape(N, Lp)[:, :L]


def _dilated_attention(q, k, v, window, dilation):
    B, S, H, hd = q.shape
    L = S // dilation

    def to_classes(t):
        return t.reshape(B, L, dilation, H, hd).transpose(0, 2, 3, 1, 4).reshape(B * dilation * H, L, hd)

    o, lse = _banded_attention(to_classes(q), to_classes(k), to_classes(v), window // (2 * dilation))
    o = o.reshape(B, dilation, H, L, hd).transpose(0, 3, 1, 2, 4).reshape(B, S, H, hd)
    lse = lse.reshape(B, dilation, H, L).transpose(0, 3, 1, 2).reshape(B, S, H)
    return o, lse


def _dilated_mixer(q, k, v):
    B, S, _ = q.shape
    cos, sin = _rope_tables(S)

    def heads(t):
        return t.reshape(B, S, ATT_HEADS, ATT_HEAD_DIM).astype(jnp.float32)

    qh = _apply_partial_rope(heads(q), cos, sin)
    kh = _apply_partial_rope(heads(k), cos, sin)
    vh = heads(v)
    outs, lses = [], []
    for window, dilation in DILATED_PATTERNS:
        o, lse = _dilated_attention(qh, kh, vh, window, dilation)
        outs.append(o)
        lses.append(lse)
    w = jax.nn.softmax(jnp.stack(lses, axis=0), axis=0)
    o = jnp.einsum('pbsh,pbshd->bshd', w, jnp.stack(outs, axis=0))
    return o.reshape(B, S, ATT_WIDTH).astype(q.dtype)


def _hier_moe(h, wg, bg, we, be, w_gate, w_up, w_down):
    T, D = h.shape
    f32 = jnp.float32
    hf = h.astype(f32)
    g_logits = hf @ wg.astype(f32) + bg.astype(f32)
    g_prob = jax.nn.softmax(g_logits, axis=-1)
    g_sel = jnp.argmax(g_logits, axis=-1)
    rows = jnp.arange(T)
    g_w = g_prob[rows, g_sel]
    e_logits = jnp.einsum('td,gde->tge', hf, we.astype(f32)) + be.astype(f32)
    e_sel_logits = e_logits[rows, g_sel]
    top_v, top_i = lax.top_k(e_sel_logits, MOE_TOP_K)
    weights = g_w[:, None] * jax.nn.softmax(top_v, axis=-1)
    expert = g_sel[:, None] * MOE_EXPERTS_PER_GROUP + top_i

    A = T * MOE_TOP_K
    e_flat = expert.reshape(-1)
    tok_flat = jnp.repeat(rows, MOE_TOP_K)
    w_flat = weights.reshape(-1)
    order = jnp.argsort(e_flat)
    e_s, tok_s, w_s = e_flat[order], tok_flat[order], w_flat[order]
    counts = jnp.zeros((MOE_N_EXPERTS,), jnp.int32).at[e_flat].add(1)
    padded = ((counts + MOE_BLOCK - 1) // MOE_BLOCK) * MOE_BLOCK
    start = jnp.cumsum(counts) - counts
    pend = jnp.cumsum(padded)
    pstart = pend - padded
    dest = pstart[e_s] + (jnp.arange(A) - start[e_s])
    n_blocks = -(-A // MOE_BLOCK) + MOE_N_EXPERTS
    cap = n_blocks * MOE_BLOCK
    buf_tok = jnp.full((cap,), T, jnp.int32).at[dest].set(tok_s.astype(jnp.int32))
    h_pad = jnp.concatenate([h, jnp.zeros((1, D), h.dtype)], axis=0)
    xb = h_pad[buf_tok].reshape(n_blocks, MOE_BLOCK, D)
    block_expert = jnp.clip(jnp.searchsorted(pend, jnp.arange(n_blocks) * MOE_BLOCK, side='right'),
                            0, MOE_N_EXPERTS - 1)

    def expert_block(args):
        xblk, eid = args
        return (jax.nn.silu(xblk @ w_gate[eid]) * (xblk @ w_up[eid])) @ w_down[eid]

    yb = lax.map(expert_block, (xb, block_expert)).reshape(cap, D)
    y_s = yb[dest] * w_s[:, None].astype(h.dtype)
    return jnp.zeros((T, D), h.dtype).at[tok_s].add(y_s)


def setup_inputs(seed: int = 0) -> dict:
    key = jax.random.key(seed)
    ks = jax.random.split(key, 18)

    def nrm(k, shape, scale):
        return jax.random.normal(k, shape, jnp.float32) * scale

    return {
        'x': nrm(ks[0], (BATCH, SEQ, D_MODEL), 1.0),
        'norm1_w': 1.0 + nrm(ks[1], (DEPTH, D_MODEL), 0.02),
        'w_in': nrm(ks[2], (DEPTH, D_MODEL, IN_WIDTH), D_MODEL ** -0.5),
        'gla_fwd_gate_w': nrm(ks[3], (DEPTH, GLA_GATE_RANK, GLA_KEY_WIDTH), GLA_GATE_RANK ** -0.5),
        'gla_fwd_gate_b': nrm(ks[4], (DEPTH, GLA_KEY_WIDTH), 0.1),
        'gla_bwd_gate_w': nrm(ks[5], (DEPTH, GLA_GATE_RANK, GLA_KEY_WIDTH), GLA_GATE_RANK ** -0.5),
        'gla_bwd_gate_b': nrm(ks[6], (DEPTH, GLA_KEY_WIDTH), 0.1),
        'gla_norm_w': 1.0 + nrm(ks[7], (DEPTH, GLA_DV), 0.02),
        'w_out': nrm(ks[8], (DEPTH, MIX_WIDTH, D_MODEL), MIX_WIDTH ** -0.5),
        'norm2_w': 1.0 + nrm(ks[9], (DEPTH, D_MODEL), 0.02),
        'router_group_w': nrm(ks[10], (DEPTH, D_MODEL, MOE_GROUPS), D_MODEL ** -0.5),
        'router_group_b': nrm(ks[11], (DEPTH, MOE_GROUPS), 0.01),
        'router_expert_w': nrm(ks[12], (DEPTH, MOE_GROUPS, D_MODEL, MOE_EXPERTS_PER_GROUP), D_MODEL ** -0.5),
        'router_expert_b': nrm(ks[13], (DEPTH, MOE_GROUPS, MOE_EXPERTS_PER_GROUP), 0.01),
        'expert_w_gate': nrm(ks[14], (DEPTH, MOE_N_EXPERTS, D_MODEL, MOE_D_FF), D_MODEL ** -0.5),
        'expert_w_up': nrm(ks[15], (DEPTH, MOE_N_EXPERTS, D_MODEL, MOE_D_FF), D_MODEL ** -0.5),
        'expert_w_down': nrm(ks[16], (DEPTH, MOE_N_EXPERTS, MOE_D_FF, D_MODEL), MOE_D_FF ** -0.5),
        'final_norm_w': 1.0 + nrm(ks[17], (D_MODEL,), 0.02),
    }


def reference(x, norm1_w, w_in, gla_fwd_gate_w, gla_fwd_gate_b, gla_bwd_gate_w, gla_bwd_gate_b,
              gla_norm_w, w_out, norm2_w, router_group_w, router_group_b, router_expert_w,
              router_expert_b, expert_w_gate, expert_w_up, expert_w_down, final_norm_w):
    B, S, D = x.shape
    sizes = [GLA_KEY_WIDTH, GLA_KEY_WIDTH, GLA_VAL_WIDTH, GLA_VAL_WIDTH,
             GLA_GATE_RANK, GLA_GATE_RANK, ATT_WIDTH, ATT_WIDTH]
    split_at = [int(c) for c in np.cumsum(sizes)]
    h = x
    for l in range(DEPTH):
        u = _rmsnorm(h, norm1_w[l])
        proj = u @ w_in[l]
        gq, gk, gv, gg, glf, glb, aq, ak, av = jnp.split(proj, split_at, axis=-1)
        gla_out = _gla_mixer(gq, gk, gv, gg, glf, glb, gla_fwd_gate_w[l], gla_fwd_gate_b[l],
                             gla_bwd_gate_w[l], gla_bwd_gate_b[l], gla_norm_w[l])
        att_out = _dilated_mixer(aq, ak, av)
        mixed = jnp.concatenate([gla_out, att_out], axis=-1)
        h = h + mixed @ w_out[l]
        u = _rmsnorm(h, norm2_w[l])
        moe = _hier_moe(u.reshape(B * S, D), router_group_w[l], router_group_b[l],
                        router_expert_w[l], router_expert_b[l], expert_w_gate[l],
                        expert_w_up[l], expert_w_down[l])
        h = h + moe.reshape(B, S, D)
    return _rmsnorm(h, final_norm_w)
```

```python
import numpy as np
from contextlib import ExitStack
import concourse.bass as bass
import concourse.mybir as mybir
from concourse.bass_utils import run_bass_kernel_spmd

F32 = mybir.dt.float32
BF16 = mybir.dt.bfloat16
AF = mybir.ActivationFunctionType
ALU = mybir.AluOpType
AX = mybir.AxisListType

ENGS = ("pe", "act", "dve", "pool", "sp")
EPOCH = 4096
DMA_SLOTS = 8

D = 1024
S = 8192
OWN = 2048
HALO = 1024
WIN = OWN + 2 * HALO
NTW = WIN // 128
T0 = HALO // 128
NTO = OWN // 128
INW = 3104
NEXP = 32
EPS = 1e-6


class Op:
    __slots__ = ("eng", "fn", "dma", "deps", "sig", "sigcount", "dmaidx", "idx")

    def __init__(self, eng, fn, dma):
        self.eng = eng
        self.fn = fn
        self.dma = dma
        self.deps = []
        self.sig = False
        self.sigcount = 0
        self.dmaidx = -1
        self.idx = -1


class Prog:
    def __init__(self, nc):
        self.nc = nc
        self.ops = []
        self.last_w = {}
        self.readers = {}
        self.ndma = {e: 0 for e in ENGS}
        self.bar = None

    def barrier(self):
        deps = set()
        for e in ENGS:
            last = None
            nd = 0
            for o in reversed(self.ops):
                if o.eng != e:
                    continue
                if o.dma:
                    if nd < DMA_SLOTS:
                        deps.add(o.idx)
                        nd += 1
                elif last is None:
                    last = o.idx
                    deps.add(o.idx)
                if last is not None and nd >= DMA_SLOTS:
                    break
        b = self.op("sp", None)
        b.deps = sorted(deps | set(b.deps))
        self.bar = b.idx
        return b

    def op(self, eng, fn, reads=(), writes=(), dma=False):
        import os as _os
        mx = int(_os.environ.get("DBG_MAXOPS", "0"))
        if mx and len(self.ops) >= mx and fn is not None:
            fn = None
            if dma:
                dma = False
        px = [k_ for k_ in reads if k_[:2] in ("pf", "pb")]
        if px:
            writes = list(writes) + [k_ for k_ in px if k_ not in writes]
            reads = [k_ for k_ in reads if k_ not in px]
        o = Op(eng, fn, dma)
        o.idx = len(self.ops)
        deps = set()
        if self.bar is not None:
            deps.add(self.bar)
        for k in reads:
            w = self.last_w.get(k)
            if w is not None:
                deps.add(w)
        for k in writes:
            w = self.last_w.get(k)
            if w is not None:
                deps.add(w)
            for r in self.readers.get(k, ()):
                deps.add(r)
        deps.discard(o.idx)
        o.deps = sorted(deps)
        for k in writes:
            self.last_w[k] = o.idx
            self.readers[k] = []
        for k in reads:
            if k not in writes:
                self.readers.setdefault(k, []).append(o.idx)
        if dma:
            o.dmaidx = self.ndma[eng]
            self.ndma[eng] += 1
        self.ops.append(o)
        return o

    def emit(self):
        nc = self.nc
        ops = self.ops
        for o in ops:
            for d in o.deps:
                p = ops[d]
                if not p.dma:
                    p.sig = True
        cnt = {e: 0 for e in ENGS}
        for o in ops:
            if o.sig and not o.dma:
                cnt[o.eng] += 1
                o.sigcount = cnt[o.eng]
        nsem = {e: (cnt[e] + EPOCH - 1) // EPOCH for e in ENGS}
        with ExitStack() as es:
            csem = {e: [es.enter_context(nc.semaphore("c_%s_%d" % (e, i))) for i in range(nsem[e])]
                    for e in ENGS}
            dsem = {e: [es.enter_context(nc.semaphore("d_%s_%d" % (e, i)))
                        for i in range(DMA_SLOTS if self.ndma[e] else 0)] for e in ENGS}
            block = es.enter_context(nc.Block())

            def body_for(e):
                def body(eng):
                    waited_c = {x: 0 for x in ENGS}
                    waited_d = {}
                    for o in ops:
                        if o.eng != e:
                            continue
                        need_c = {}
                        need_d = {}
                        for d in o.deps:
                            p = ops[d]
                            if p.dma:
                                slot = p.dmaidx % DMA_SLOTS
                                val = 16 * (p.dmaidx // DMA_SLOTS + 1)
                                key = (p.eng, slot)
                                if waited_d.get(key, 0) < val:
                                    need_d[key] = max(need_d.get(key, 0), val)
                            else:
                                if waited_c[p.eng] < p.sigcount:
                                    need_c[p.eng] = max(need_c.get(p.eng, 0), p.sigcount)
                        if o.dma:
                            slot = o.dmaidx % DMA_SLOTS
                            val = 16 * (o.dmaidx // DMA_SLOTS)
                            key = (e, slot)
                            if val > 0 and waited_d.get(key, 0) < val:
                                need_d[key] = max(need_d.get(key, 0), val)
                        for pe_, c in need_c.items():
                            ep = (c - 1) // EPOCH
                            eng.wait_ge(csem[pe_][ep], (c - 1) % EPOCH + 1)
                            waited_c[pe_] = c
                        for key, val in need_d.items():
                            eng.wait_ge(dsem[key[0]][key[1]], val)
                            waited_d[key] = val
                        ins = o.fn(eng) if o.fn is not None else None
                        if o.dma:
                            ins.then_inc(dsem[e][o.dmaidx % DMA_SLOTS], 16)
                        elif o.sig:
                            if ins is None:
                                ins = eng.nop()
                            ep = (o.sigcount - 1) // EPOCH
                            ins.then_inc(csem[e][ep], 1)
                return body

            block.tensor(body_for("pe"))
            block.scalar(body_for("act"))
            block.vector(body_for("dve"))
            block.gpsimd(body_for("pool"))
            block.sync(body_for("sp"))


class Arena:
    def __init__(self, ap, ncols):
        self.ap = ap
        self.n = ncols
        self.off = 0

    def alloc(self, shape, dt=F32):
        p = shape[0]
        rest = list(shape[1:])
        nel = 1
        for r in rest:
            nel *= r
        ncol = nel if dt == F32 else (nel + 1) // 2
        ncol += ncol % 2
        assert self.off + ncol <= self.n, "arena overflow: need %d have %d" % (ncol, self.n - self.off)
        v = self.ap[0:p, self.off:self.off + ncol]
        self.off += ncol
        if dt != F32:
            v = v.bitcast(dt)
        if v.shape[1] != nel:
            v = v[:, 0:nel]
        if len(rest) == 2:
            v = v.rearrange("p (a b) -> p a b", a=rest[0])
        elif len(rest) == 3:
            v = v.rearrange("p (a b c) -> p a b c", a=rest[0], b=rest[1])
        return v


def build_program(debug=False, stop_after=None, dbg_tiles=None):
    nc = bass.Bass("TRN2", target_bir_lowering=False)
    P = Prog(nc)
    global LASTP
    LASTP = P

    def din(name, shape, dt=F32):
        return nc.dram_tensor(name, list(shape), dt, kind="ExternalInput").ap()

    def dscr(name, shape, dt):
        kind = "ExternalOutput" if debug else "Internal"
        return nc.dram_tensor(name, list(shape), dt, kind=kind).ap()

    xw = din("xw", [WIN, D])
    vcol = din("vcol", [128, NTW])
    cs_t = din("cs_t", [128, NTW, 16])
    cmat = din("cmat", [128, 8, 128])
    band3 = din("band3", [128, 384])
    w_in = din("w_in", [D, INW])
    wz_d = din("wz", [33, 512])
    vecs = din("vecs", [4, D])
    w_out = din("w_out", [D, D])
    wr_d = din("wr", [D, 36])
    rb_d = din("rb", [1, 36])
    ewg = din("ewg", [NEXP, D, 512])
    ewu = din("ewu", [NEXP, D, 512])
    ewd = din("ewd", [NEXP, 512, D])
    out_d = nc.dram_tensor("out", [OWN, D], F32, kind="ExternalOutput").ap()
    QS = dscr("QS", [OWN, 512], BF16)
    KS = dscr("KS", [WIN + 2 * HALO, 512], BF16)
    VS = dscr("VS", [WIN + 2 * HALO, 520], BF16)
    GV = dscr("GV", [OWN, 512], BF16)
    GG = dscr("GG", [OWN, 512], BF16)
    MG = dscr("MG", [OWN, 512], BF16)
    OTS = dscr("OTS", [8, 64, OWN], BF16)
    H2 = dscr("H2", [OWN, D], F32)
    WTD = nc.dram_tensor("WTD", [128, NTO * 32], F32, kind="ExternalOutput").ap() if debug else None

    QSK = ["QS%d" % i for i in range(NTO)]
    KSK = ["KS%d" % i for i in range(48)]
    VSK = ["VS%d" % i for i in range(48)]
    NCOL = 50 * 1024 + 512
    es = ExitStack()
    with es:
        arena_t = es.enter_context(nc.sbuf_tensor("arena", [128, NCOL], F32))
        AR = Arena(arena_t[:], NCOL)
        sb = lambda name, shape, dt=F32: AR.alloc(shape, dt)

        def ps(name, shape, dt=F32):
            return es.enter_context(nc.psum_tensor("p_" + name, list(shape), dt))

        pf = [ps("pf%d" % i, [128, 512]) for i in range(6)]
        pb = [ps("pb%d" % i, [128, 1024], BF16) for i in range(2)]
        pf_rr = [0]
        pb_rr = [0]

        def next_pf():
            i = pf_rr[0] % 6
            pf_rr[0] += 1
            return pf[i], "pf%d" % i

        def next_pb():
            i = pb_rr[0] % 2
            pb_rr[0] += 1
            return pb[i], "pb%d" % i

        def mm_group(out_ap, pairs, okey, rkeys):
            def fn(e):
                ins = None
                n = len(pairs)
                for j, (l, r) in enumerate(pairs):
                    ins = e.matmul(out_ap, lhsT=l, rhs=r, start=(j == 0), stop=(j == n - 1))
                return ins
            P.op("pe", fn, reads=rkeys, writes=[okey])

        def ACT(out, in_, func, reads, writes, **kw):
            P.op("act", lambda e: e.activation(out=out, in_=in_, func=func, **kw), reads=reads, writes=writes)

        def TT(eng, out, in0, in1, op, reads, writes):
            P.op(eng, lambda e: e.tensor_tensor(out=out, in0=in0, in1=in1, op=op), reads=reads, writes=writes)

        def STT(out, in0, scalar, in1, op0, op1, reads, writes):
            P.op("dve", lambda e: e.scalar_tensor_tensor(out=out, in0=in0, scalar=scalar, in1=in1, op0=op0, op1=op1),
                 reads=reads, writes=writes)

        def TS(eng, out, in0, s1, s2, op0, op1, reads, writes):
            if op1 is None:
                P.op(eng, lambda e: e.tensor_scalar(out=out, in0=in0, scalar1=s1, scalar2=None, op0=op0), reads=reads, writes=writes)
            else:
                P.op(eng, lambda e: e.tensor_scalar(out=out, in0=in0, scalar1=s1, scalar2=s2, op0=op0, op1=op1), reads=reads, writes=writes)

        def CP(eng, out, in_, reads, writes):
            if eng == "act":
                ACT(out, in_, AF.Copy, reads, writes)
            else:
                P.op(eng, lambda e: e.tensor_copy(out=out, in_=in_), reads=reads, writes=writes)

        def DMA(q, out, in_, reads, writes):
            return P.op(q, lambda e: e.dma_start(out=out, in_=in_), reads=reads, writes=writes, dma=True)

        def rstd_from_ssq(dst, src, n, rk, wk):
            ACT(dst, src, AF.Ln, [rk, "epsc"], [wk], scale=1.0 / n, bias=epsc[0:dst.shape[0], :])
            ACT(dst, dst, AF.Exp, [wk], [wk], scale=-0.5)

        cm = sb("cm", [128, 8, 128])
        identb = sb("identb", [128, 128], BF16)
        band = sb("band", [128, 384], BF16)
        maskFB = sb("maskFB", [128, 4, 128])
        n16col = sb("n16col", [128, 2])
        epsc = sb("epsc", [128, 2])
        onec = sb("onec", [128, 2])
        negc = sb("negc", [128, 2])
        vc = sb("vc", [128, NTW])
        cst = sb("cst", [128, NTW, 16])
        wz = sb("wz", [33, 512])
        n1bc = sb("n1bc", [128, D])
        n2bc = sb("n2bc", [128, D])
        fnbc = sb("fnbc", [128, D])
        gnbc = sb("gnbc", [128, 512])
        rbbc = sb("rbbc", [128, 36])
        wr = sb("wr", [128, 8, 36])
        nmax = sb("nmax", [128, 16])
        n16col = n16col[:, 0:1]
        epsc = epsc[:, 0:1]
        onec = onec[:, 0:1]
        negc = negc[:, 0:1]

        DMA("sp", cm, cmat, [], ["cm"])
        DMA("pool", identb, cmat[:, 0, :], [], ["identb"])
        DMA("pool", band, band3, [], ["band"])
        DMA("sp", vc, vcol, [], ["vc"])
        DMA("sp", cst, cs_t, [], ["cst"])
        DMA("sp", wz, wz_d, [], ["wz"])
        DMA("sp", n1bc, vecs[0:1, :].partition_broadcast(128), [], ["n1bc"])
        DMA("sp", n2bc, vecs[1:2, :].partition_broadcast(128), [], ["n2bc"])
        DMA("sp", fnbc, vecs[2:3, :].partition_broadcast(128), [], ["fnbc"])
        DMA("sp", gnbc, vecs[3:4, 0:512].partition_broadcast(128), [], ["gnbc"])
        DMA("sp", rbbc, rb_d[0:1, :].partition_broadcast(128), [], ["rbbc"])
        DMA("sp", wr, wr_d.rearrange("(c p) n -> p c n", p=128), [], ["wr"])
        P.op("dve", lambda e: e.memset(n16col, -1.0 / 16.0), writes=["n16col"])
        P.op("dve", lambda e: e.memset(epsc, EPS), writes=["epsc"])
        P.op("dve", lambda e: e.memset(onec, 1.0), writes=["onec"])
        P.op("dve", lambda e: e.memset(nmax, 0.0), writes=["nmax"])
        for h in range(4):
            CP("dve", maskFB[:, h, :], cm[:, 5 + h // 2, :], ["cm"], ["maskFB"])
        M0 = AR.off

        attnT = sb("attnT", [128, NTO, 512], BF16)
        qdT = sb("qdT", [128, NTO, 4, 128], BF16)
        SfT = sb("SfT", [128, NTO, 2, 128], BF16)
        SbT = sb("SbT", [128, NTO, 2, 128], BF16)
        M1 = AR.off
        win = sb("win", [128, 8, INW], BF16)
        for c in range(8):
            DMA("pool", win[:, c, :], w_in[c * 128:(c + 1) * 128, :], [], ["win%d" % c])
        winkeys = ["win%d" % c for c in range(8)]
        kvB = sb("kvB", [128, NTW - T0, 2, 128], BF16)
        decB = sb("decB", [128, NTW - T0, 2])
        Sf = sb("Sf", [128, 2, 128])
        Sb = sb("Sb", [128, 2, 128])
        P.op("dve", lambda e: e.memset(Sf, 0.0), writes=["Sf"])
        P.op("dve", lambda e: e.memset(Sb, 0.0), writes=["Sb"])
        zt = sb("zt", [128, 520], BF16)
        P.op("pool", lambda e: e.memset(zt, 0.0), writes=["zt"])
        for blk in range(HALO // 128):
            for base in (0, HALO + WIN):
                r0 = base + blk * 128
                DMA("sp", KS[r0:r0 + 128, :], zt[:, 0:512], ["zt"], ["KS%d" % (r0 // 128)])
                DMA("sp", VS[r0:r0 + 128, :], zt, ["zt"], ["VS%d" % (r0 // 128)])
        xt = [sb("xt%d" % i, [128, D]) for i in range(2)]
        junk = sb("junk", [128, D], BF16)
        xn = [sb("xn%d" % i, [128, D], BF16) for i in range(2)]
        xnT = [sb("xnT%d" % i, [128, 8, 128], BF16) for i in range(2)]
        ssq = sb("ssq", [128, 2])
        rstd = sb("rstd", [128, 2])
        qk = sb("qk", [128, 512])
        vbf = [sb("vbf%d" % i, [128, 512], BF16) for i in range(2)]
        gbf = [sb("gbf%d" % i, [128, 512], BF16) for i in range(2)]
        lr = sb("lr", [128, 32])
        lrT = sb("lrT", [33, 128])
        ez = sb("ez", [128, 512])
        spl = sb("spl", [128, 512])
        E1 = sb("E1", [128, 512])
        E2 = sb("E2", [128, 512])
        E3 = sb("E3", [128, 512])
        dec = sb("dec", [128, 4])
        qd = sb("qd", [128, 512], BF16)
        ki = sb("ki", [128, 512], BF16)
        ke = sb("ke", [128, 512], BF16)
        kiT = sb("kiT", [128, 4, 128], BF16)
        qr = sb("qr", [128, 8, 64])
        kr = sb("kr", [128, 8, 64])
        qrb = [sb("qrb%d" % i, [128, 512], BF16) for i in range(2)]
        krb = [sb("krb%d" % i, [128, 512], BF16) for i in range(2)]
        vab = [sb("vab%d" % i, [128, 8, 65], BF16) for i in range(2)]
        rt = sb("rt", [128, 8, 8])
        rt2 = sb("rt2", [128, 8, 8])
        sqs = sb("sqs", [128, 8, 64])
        nrm = sb("nrm", [128, 16])
        P.op("dve", lambda e: e.memset(lrT[32:33, :], 1.0), writes=["lrT_one"])

        for i in (range(NTW) if dbg_tiles is None else dbg_tiles):
            own = T0 <= i < T0 + NTO
            left = i < T0
            io = i - T0
            b2 = i % 2
            xtk, xnk, xnTk = "xt%d" % b2, "xn%d" % b2, "xnT%d" % b2
            if i == 0:
                DMA("sp", xt[0], xw[0:128, :], [], ["xt0"])
            if i + 1 < NTW:
                DMA("sp", xt[(i + 1) % 2], xw[(i + 1) * 128:(i + 2) * 128, :], [], ["xt%d" % ((i + 1) % 2)])
            sk, rk = "ssq%d" % b2, "rstd%d" % b2
            P.op("act", lambda e, b2=b2: e.activation(out=junk, in_=xt[b2], func=AF.Square, accum_out=ssq[:, b2:b2 + 1]),
                 reads=[xtk], writes=["junk", sk])
            rstd_from_ssq(rstd[:, b2:b2 + 1], ssq[:, b2:b2 + 1], D, sk, rk)
            STT(xn[b2], xt[b2], rstd[:, b2:b2 + 1], n1bc, ALU.mult, ALU.mult, [xtk, rk, "n1bc"], [xnk])
            pbt, pbk = next_pb()

            def tr_fn(e, b2=b2, pbt=pbt):
                ins = None
                for c in range(8):
                    ins = e.transpose(out=pbt[:, c * 128:(c + 1) * 128], in_=xn[b2][:, c * 128:(c + 1) * 128], identity=identb)
                return ins
            P.op("pe", tr_fn, reads=[xnk, "identb"], writes=[pbk])
            CP("act", xnT[b2].rearrange("p c t -> p (c t)"), pbt[:, :], [pbk], [xnTk])

            def proj(c0, c1, b2=b2, xnTk=xnTk):
                pt, pk = next_pf()
                n = c1 - c0
                mm_group(pt[:, 0:n], [(xnT[b2][:, c, :], win[:, c, c0:c1]) for c in range(8)], pk, [xnTk] + winkeys)
                return pt, pk

            if own:
                pt, pk = proj(0, 512)
                CP("act", qk, pt[:, 0:512], [pk], ["qk"])
            else:
                pt, pk = proj(256, 512)
                CP("act", qk[:, 256:512], pt[:, 0:256], [pk], ["qk"])
            pt, pk = proj(512, 1024)
            vkey = "vbf%d" % b2
            vt = vbf[b2]
            CP("dve", vt, pt[:, 0:512], [pk], [vkey])
            if own:
                DMA("sp", GV[io * 128:(io + 1) * 128, :], vt, [vkey], ["GV%d" % io])
                pt, pk = proj(1024, 1536)
                CP("act", gbf[b2], pt[:, 0:512], [pk], ["gbf%d" % b2])
                DMA("sp", GG[io * 128:(io + 1) * 128, :], gbf[b2], ["gbf%d" % b2], ["GG%d" % io])
            pt, pk = proj(1536, 1568)
            CP("dve", lr, pt[:, 0:32], [pk], ["lr"])
            pt, pk = next_pf()
            P.op("pe", lambda e, pt=pt: e.transpose(out=pt[0:32, 0:128], in_=lr, identity=cm[:, 0, :]), reads=["lr", "cm"], writes=[pk])
            CP("dve", lrT[0:32, :], pt[0:32, 0:128], [pk], ["lrT"])
            pz, pzk = next_pf()
            P.op("pe", lambda e, pz=pz: e.matmul(pz[:, :], lhsT=lrT, rhs=wz, start=True, stop=True),
                 reads=["lrT", "lrT_one", "wz"], writes=[pzk])
            ACT(ez, pz[:, :], AF.Exp, [pzk], ["ez"], scale=-1.0)
            ACT(spl, ez, AF.Ln, ["ez", "onec"], ["spl"], bias=onec, scale=1.0)
            pbb, pbbk = next_pf()
            P.op("pe", lambda e, pbb=pbb: (e.matmul(pbb[:, 0:256], lhsT=cm[:, 1, :], rhs=spl[:, 0:256], start=True, stop=True),
                                           e.matmul(pbb[:, 256:512], lhsT=cm[:, 2, :], rhs=spl[:, 256:512], start=True, stop=True))[1],
                 reads=["cm", "spl"], writes=[pbbk])
            ACT(E1, pbb[:, :], AF.Exp, [pbbk], ["E1"])
            ACT(E2, pbb[:, :], AF.Exp, [pbbk], ["E2"], scale=-1.0)
            pb3, pb3k = next_pf()
            P.op("pe", lambda e, pb3=pb3: (e.matmul(pb3[:, 0:256], lhsT=cm[:, 3, :], rhs=spl[:, 0:256], start=True, stop=True),
                                           e.matmul(pb3[:, 256:512], lhsT=cm[:, 4, :], rhs=spl[:, 256:512], start=True, stop=True))[1],
                 reads=["cm", "spl"], writes=[pb3k])
            ACT(E3, pb3[:, :], AF.Exp, [pb3k], ["E3"])
            pdc, pdck = next_pf()

            def dec_fn(e, pdc=pdc):
                ins = None
                for j in range(4):
                    ins = e.matmul(pdc[:, j:j + 1], lhsT=spl[:, j * 128:(j + 1) * 128], rhs=n16col, start=True, stop=True)
                return ins
            P.op("pe", dec_fn, reads=["spl", "n16col"], writes=[pdck])
            ACT(dec, pdc[:, 0:4], AF.Exp, [pdck], ["dec"])
            if own:
                STT(qd[:, 0:256], qk[:, 0:256], 0.125, E1[:, 0:256], ALU.mult, ALU.mult, ["qk", "E1"], ["qd"])
                STT(qd[:, 256:512], qk[:, 0:256], 0.125, E1[:, 256:512], ALU.mult, ALU.mult, ["qk", "E1"], ["qd"])
                TT("pool", ki[:, 0:256], qk[:, 256:512], E2[:, 0:256], ALU.mult, ["qk", "E2"], ["ki"])
                TT("pool", ki[:, 256:512], qk[:, 256:512], E2[:, 256:512], ALU.mult, ["qk", "E2"], ["ki"])
            TT("pool", ke[:, 0:256], qk[:, 256:512], E3[:, 0:256], ALU.mult, ["qk", "E3"], ["ke"])
            TT("pool", ke[:, 256:512], qk[:, 256:512], E3[:, 256:512], ALU.mult, ["qk", "E3"], ["ke"])
            if own:
                pbt, pbk = next_pb()

                def tr2_fn(e, pbt=pbt):
                    ins = None
                    for j in range(4):
                        ins = e.transpose(out=pbt[:, j * 128:(j + 1) * 128], in_=qd[:, j * 128:(j + 1) * 128], identity=identb)
                    for j in range(4):
                        ins = e.transpose(out=pbt[:, 512 + j * 128:512 + (j + 1) * 128], in_=ki[:, j * 128:(j + 1) * 128], identity=identb)
                    return ins
                P.op("pe", tr2_fn, reads=["qd", "ki", "identb"], writes=[pbk])
                CP("act", qdT[:, io, :, :].rearrange("p c t -> p (c t)"), pbt[:, 0:512], [pbk], ["qdT%d" % io])
                CP("dve", kiT.rearrange("p c t -> p (c t)"), pbt[:, 512:1024], [pbk], ["kiT"])
                paX, paXk = next_pf()
                paY, paYk = next_pf()

                def att_fn(e, pa, par, io=io):
                    ins = None
                    p0 = par * 64
                    for dirn in range(2):
                        for pr in range(2):
                            blk = dirn * 2 + pr
                            sl = dirn * 2 + pr
                            ins = e.matmul(pa[:, sl * 128:(sl + 1) * 128], lhsT=kiT[p0:p0 + 64, blk, :], rhs=qdT[p0:p0 + 64, io, blk, :],
                                           start=True, stop=True)
                    return ins
                P.op("pe", lambda e, pa=paX, f=att_fn: f(e, pa, 0), reads=["kiT", "qdT%d" % io], writes=[paXk])
                P.op("pe", lambda e, pa=paY, f=att_fn: f(e, pa, 1), reads=["kiT", "qdT%d" % io], writes=[paYk])
                TT("dve", ez, paX[:, :], maskFB.rearrange("p h c -> p (h c)"), ALU.mult, [paXk, "maskFB"], ["ez"])
                TT("dve", E1, paY[:, :], maskFB.rearrange("p h c -> p (h c)"), ALU.mult, [paYk, "maskFB"], ["E1"])
                av = attnT[:, io, :].rearrange("p (a b c) -> p a b c", a=2, b=2)
                TT("pool", av[:, :, 0, :], ez[:, 0:256].rearrange("p (a c) -> p a c", a=2), ez[:, 256:512].rearrange("p (a c) -> p a c", a=2),
                   ALU.add, ["ez"], ["attnT%d" % io])
                TT("pool", av[:, :, 1, :], E1[:, 0:256].rearrange("p (a c) -> p a c", a=2), E1[:, 256:512].rearrange("p (a c) -> p a c", a=2),
                   ALU.add, ["E1"], ["attnT%d" % io])
            for dirn in range(2):
                if dirn == 0 and i >= T0 + NTO:
                    continue
                if dirn == 1 and left:
                    continue
                pkv, pkvk = next_pf()

                def kv_fn(e, pkv=pkv, dirn=dirn, vt=vt):
                    ins = None
                    for pr in range(2):
                        ins = e.matmul(pkv[:, pr * 256:(pr + 1) * 256], lhsT=ke[:, dirn * 256 + pr * 128: dirn * 256 + (pr + 1) * 128],
                                       rhs=vt[:, pr * 256:(pr + 1) * 256], start=True, stop=True)
                    return ins
                P.op("pe", kv_fn, reads=["ke", vkey], writes=[pkvk])
                if dirn == 0:
                    if own:
                        CP("act", SfT[:, io, :, :].rearrange("p a b -> p (a b)"), Sf.rearrange("p a b -> p (a b)"), ["Sf"], ["SfT%d" % io])
                    for pr in range(2):
                        for hh in range(2):
                            p0 = hh * 64
                            STT(Sf[p0:p0 + 64, pr, :], Sf[p0:p0 + 64, pr, :], dec[p0:p0 + 64, pr:pr + 1],
                                pkv[p0:p0 + 64, pr * 256 + hh * 128: pr * 256 + (hh + 1) * 128], ALU.mult, ALU.add,
                                ["Sf", "dec", pkvk], ["Sf"])
                else:
                    ib = i - T0
                    for pr in range(2):
                        for hh in range(2):
                            p0 = hh * 64
                            CP("act", kvB[p0:p0 + 64, ib, pr, :], pkv[p0:p0 + 64, pr * 256 + hh * 128: pr * 256 + (hh + 1) * 128],
                               [pkvk], ["kvB%d" % ib])
                    CP("dve", decB[:, ib, :], dec[:, 2:4], ["dec"], ["decB%d" % ib])

            def rope(pt, pk, dst, dkey, i=i):
                src = pt[:, 0:512].rearrange("p (h d) -> p h d", h=8)
                cosb = cst[:, i, 0:8].unsqueeze(1).broadcast_to([128, 8, 8])
                sinb = cst[:, i, 8:16].unsqueeze(1).broadcast_to([128, 8, 8])
                CP("act", dst.rearrange("p h d -> p (h d)"), pt[:, 0:512], [pk], [dkey])
                TT("dve", rt, src[:, :, 8:16], sinb, ALU.mult, [pk, "cst"], ["rt"])
                TT("dve", rt2, src[:, :, 0:8], cosb, ALU.mult, [pk, "cst"], ["rt2"])
                TT("dve", dst[:, :, 0:8], rt2, rt, ALU.subtract, ["rt", "rt2"], [dkey])
                TT("dve", rt, src[:, :, 0:8], sinb, ALU.mult, [pk, "cst"], ["rt"])
                TT("dve", rt2, src[:, :, 8:16], cosb, ALU.mult, [pk, "cst"], ["rt2"])
                TT("dve", dst[:, :, 8:16], rt2, rt, ALU.add, ["rt", "rt2"], [dkey])

            def sqnorm(src, col0, skey):
                TT("pool", sqs, src, src, ALU.mult, [skey], ["sqs"])
                P.op("dve", lambda e: e.tensor_reduce(out=nrm[:, col0:col0 + 8], in_=sqs, axis=AX.X, op=ALU.add), reads=["sqs"], writes=["nrm"])
                TT("dve", nmax[:, col0:col0 + 8], nmax[:, col0:col0 + 8], nrm[:, col0:col0 + 8], ALU.max, ["nrm", "nmax"], ["nmax"])

            r0k = HALO + i * 128
            if own:
                pt, pk = proj(1568, 2080)
                rope(pt, pk, qr, "qr")
                sqnorm(qr, 0, "qr")
                CP("act", qrb[b2], qr.rearrange("p h d -> p (h d)"), ["qr"], ["qrb%d" % b2])
                DMA("sp", QS[io * 128:(io + 1) * 128, :], qrb[b2], ["qrb%d" % b2], ["QS%d" % io])
            pt, pk = proj(2080, 2592)
            rope(pt, pk, kr, "kr")
            sqnorm(kr, 8, "kr")
            CP("act", krb[b2], kr.rearrange("p h d -> p (h d)"), ["kr"], ["krb%d" % b2])
            DMA("sp", KS[r0k:r0k + 128, :], krb[b2], ["krb%d" % b2], ["KS%d" % (r0k // 128)])
            pt, pk = proj(2592, 3104)
            CP("act", vab[b2][:, :, 0:64], pt[:, 0:512].rearrange("p (h d) -> p h d", h=8), [pk], ["vab%d" % b2])
            CP("dve", vab[b2][:, :, 64:65], vc[:, i:i + 1].unsqueeze(1).broadcast_to([128, 8, 1]), ["vc"], ["vab%d" % b2])
            DMA("sp", VS[r0k:r0k + 128, :], vab[b2].rearrange("p h d -> p (h d)"), ["vab%d" % b2], ["VS%d" % (r0k // 128)])

        if stop_after == "A":
            P.op("sp", None, reads=[k_ for k_ in P.last_w.keys() if k_[:2] in ("QS", "KS", "VS", "GV", "GG")], writes=[])
            P.emit()
            return nc
        for i in range(NTW - 1, T0 - 1, -1):
            ib = i - T0
            if ib < NTO:
                CP("act", SbT[:, ib, :, :].rearrange("p a b -> p (a b)"), Sb.rearrange("p a b -> p (a b)"), ["Sb"], ["SbT%d" % ib])
            if i == T0:
                break
            for pr in range(2):
                STT(Sb[:, pr, :], Sb[:, pr, :], decB[:, ib, pr:pr + 1], kvB[:, ib, pr, :], ALU.mult, ALU.add,
                    ["Sb", "decB%d" % ib, "kvB%d" % ib], ["Sb"])

        P.barrier()
        AR.off = M1
        vb2 = [sb("vb2%d" % i, [128, 512], BF16) for i in range(2)]
        gb2 = [sb("gb2%d" % i, [128, 512], BF16) for i in range(2)]
        osb = sb("osb", [128, 512])
        osq = sb("osq", [128, 4, 128])
        oms = sb("oms", [128, 4])
        sgs = sb("sgs", [128, 512])
        ybf = sb("ybf", [128, 512])
        mixb = [sb("mixb%d" % i, [128, 512], BF16) for i in range(2)]
        for io in range(NTO):
            b2 = io % 2
            DMA("sp", vb2[b2], GV[io * 128:(io + 1) * 128, :], ["GV%d" % io], ["vb2%d" % b2])
            DMA("sp", gb2[b2], GG[io * 128:(io + 1) * 128, :], ["GG%d" % io], ["gb2%d" % b2])
            poX, poXk = next_pf()
            poY, poYk = next_pf()

            def o_fn(e, po, par, io=io, b2=b2):
                ins = None
                p0 = par * 64
                for pr in range(2):
                    h = pr * 2 + par
                    oap = po[:, pr * 128:(pr + 1) * 128]
                    e.matmul(oap, lhsT=attnT[:, io, h * 128:(h + 1) * 128], rhs=vb2[b2][:, h * 128:(h + 1) * 128], start=True, stop=False)
                    e.matmul(oap, lhsT=qdT[p0:p0 + 64, io, pr, :], rhs=SfT[p0:p0 + 64, io, pr, :], start=False, stop=False)
                    ins = e.matmul(oap, lhsT=qdT[p0:p0 + 64, io, 2 + pr, :], rhs=SbT[p0:p0 + 64, io, pr, :], start=False, stop=True)
                return ins
            rk_ = ["attnT%d" % io, "qdT%d" % io, "SfT%d" % io, "SbT%d" % io, "vb2%d" % b2]
            P.op("pe", lambda e, po=poX, f=o_fn: f(e, po, 0), reads=rk_, writes=[poXk])
            P.op("pe", lambda e, po=poY, f=o_fn: f(e, po, 1), reads=rk_, writes=[poYk])
            ov = osb.rearrange("p (a b c) -> p a b c", a=2, b=2)
            CP("act", ov[:, :, 0, :], poX[:, 0:256].rearrange("p (a c) -> p a c", a=2), [poXk], ["osb"])
            CP("act", ov[:, :, 1, :], poY[:, 0:256].rearrange("p (a c) -> p a c", a=2), [poYk], ["osb"])
            TT("pool", osq.rearrange("p h d -> p (h d)"), osb, osb, ALU.mult, ["osb"], ["osq"])
            P.op("dve", lambda e: e.tensor_reduce(out=oms, in_=osq, axis=AX.X, op=ALU.add), reads=["osq"], writes=["oms"])
            rstd_from_ssq(oms, oms, 128, "oms", "oms")
            ACT(sgs, gb2[b2], AF.Silu, ["gb2%d" % b2], ["sgs"])
            TT("dve", ybf, osb, gnbc, ALU.mult, ["osb", "gnbc"], ["ybf"])
            for h in range(4):
                STT(mixb[b2][:, h * 128:(h + 1) * 128], ybf[:, h * 128:(h + 1) * 128], oms[:, h:h + 1], sgs[:, h * 128:(h + 1) * 128],
                    ALU.mult, ALU.mult, ["ybf", "oms", "sgs"], ["mixb%d" % b2])
            DMA("sp", MG[io * 128:(io + 1) * 128, :], mixb[b2], ["mixb%d" % b2], ["MG%d" % io])
        MGK = ["MG%d" % i for i in range(NTO)]
        if stop_after == "G2":
            P.op("sp", None, reads=QSK + KSK + VSK + MGK, writes=[])
            P.emit()
            return nc

        P.barrier()
        AR.off = M0
        nm2 = sb("nm2", [128, 2])
        m2 = sb("m2", [2, 2])
        m1 = sb("m1", [1, 4])
        P.op("dve", lambda e: e.tensor_reduce(out=nm2, in_=nmax.rearrange("p (a h) -> p a h", a=2), axis=AX.X, op=ALU.max),
             reads=["nmax"], writes=["nm2"])
        pt, pk = next_pf()
        P.op("pe", lambda e, pt=pt: e.transpose(out=pt[0:2, 0:128], in_=nm2, identity=cm[:, 0, :]), reads=["nm2", "cm"], writes=[pk])
        P.op("dve", lambda e, pt=pt: e.tensor_reduce(out=m2[:, 0:1], in_=pt[0:2, 0:128], axis=AX.X, op=ALU.max), reads=[pk], writes=["m2"])
        pt, pk = next_pf()
        P.op("pe", lambda e, pt=pt: e.transpose(out=pt[0:1, 0:2], in_=m2[:, 0:1], identity=cm[0:2, 0, 0:2]), reads=["m2", "cm"], writes=[pk])
        CP("dve", m1[:, 0:2], pt[0:1, 0:2], [pk], ["m1"])
        TT("dve", m1[:, 2:3], m1[:, 0:1], m1[:, 1:2], ALU.mult, ["m1"], ["m1"])
        ACT(m1[:, 3:4], m1[:, 2:3], AF.Ln, ["m1"], ["m1"])
        ACT(m1[:, 3:4], m1[:, 3:4], AF.Exp, ["m1"], ["m1"], scale=0.5)
        TS("dve", m1[:, 3:4], m1[:, 3:4], -0.125, None, ALU.mult, None, ["m1"], ["m1"])
        pt, pk = next_pf()
        P.op("pe", lambda e, pt=pt: e.matmul(pt[:, 0:1], lhsT=cm[0:1, 7, :], rhs=m1[:, 3:4], start=True, stop=True), reads=["m1", "cm"], writes=[pk])
        CP("dve", negc, pt[:, 0:1], [pk], ["negc"])

        accT = sb("accT", [65, 8, OWN])
        NQ = 8
        qsb = [sb("qsb%d" % i, [128, 512], BF16) for i in range(NQ)]
        ksb = [sb("ksb%d" % i, [128, 512], BF16) for i in range(NQ + 2)]
        vsb = [sb("vsb%d" % i, [128, 8, 65], BF16) for i in range(NQ + 2)]
        qT = [sb("qT%d" % i, [128, 4, 128], BF16) for i in range(NQ)]
        kT = [sb("kT%d" % i, [128, 4, 128], BF16) for i in range(NQ + 2)]
        pex = [sb("pex%d" % i, [128, 384], BF16) for i in range(4)]
        pmk = [sb("pmk%d" % i, [128, 384], BF16) for i in range(4)]
        cnt4 = [0]
        jobs = [(1, 0, 0, 8), (1, 0, 8, 8)] + [(4, r, 0, 4) for r in range(4)] + [(16, r, 0, 1) for r in range(16)]
        for (dd, r, j0, nq) in jobs:
            QSv = QS.rearrange("(n d) c -> d n c", d=dd)
            KSv = KS.rearrange("(n d) c -> d n c", d=dd)
            VSv = VS.rearrange("(n d) c -> d n c", d=dd)
            accv = accT.rearrange("p h (n d) -> p h d n", d=dd)
            for jq in range(nq):
                n0 = 128 * (j0 + jq)
                DMA("sp", qsb[jq], QSv[r, n0:n0 + 128, :], QSK, ["qsb%d" % jq])
            for kk in range(nq + 2):
                n0 = 2048 // dd + 128 * (j0 + kk - 1)
                DMA("sp", ksb[kk], KSv[r, n0:n0 + 128, :], KSK, ["ksb%d" % kk])
                DMA("sp", vsb[kk].rearrange("p h d -> p (h d)"), VSv[r, n0:n0 + 128, :], VSK, ["vsb%d" % kk])
            tl = [(qsb[jq], "qsb%d" % jq, qT[jq], "qT%d" % jq) for jq in range(nq)] + \
                 [(ksb[kk], "ksb%d" % kk, kT[kk], "kT%d" % kk) for kk in range(nq + 2)]
            for t0 in range(0, len(tl), 2):
                grp = tl[t0:t0 + 2]
                pbt, pbk = next_pb()

                def trq_fn(e, grp=grp, pbt=pbt):
                    ins = None
                    for gi, (src, _, _, _) in enumerate(grp):
                        for c in range(4):
                            ins = e.transpose(out=pbt[:, gi * 512 + c * 128: gi * 512 + (c + 1) * 128], in_=src[:, c * 128:(c + 1) * 128], identity=identb)
                    return ins
                P.op("pe", trq_fn, reads=[g[1] for g in grp] + ["identb"], writes=[pbk])
                for gi, (_, _, dst, dk) in enumerate(grp):
                    CP("act" if gi == 0 else "dve", dst.rearrange("p c t -> p (c t)"), pbt[:, gi * 512:(gi + 1) * 512], [pbk], [dk])
            for jq in range(nq):
                for hg in range(2):
                    bufs = []
                    for h in range(hg * 4, hg * 4 + 4):
                        p0 = (h % 2) * 64
                        blk = h // 2
                        pS, pSk = next_pf()

                        def s_fn(e, pS=pS, jq=jq, p0=p0, blk=blk):
                            ins = None
                            for sl in range(3):
                                ins = e.matmul(pS[:, sl * 128:(sl + 1) * 128], lhsT=kT[jq + sl][p0:p0 + 64, blk, :], rhs=qT[jq][p0:p0 + 64, blk, :],
                                               start=True, stop=True)
                            return ins
                        P.op("pe", s_fn, reads=["kT%d" % (jq + sl) for sl in range(3)] + ["qT%d" % jq], writes=[pSk])
                        bi = cnt4[0] % 4
                        cnt4[0] += 1
                        ACT(pex[bi], pS[:, 0:384], AF.Exp, [pSk, "negc"], ["pex%d" % bi], bias=negc, scale=0.125)
                        TT("pool" if (h % 2) else "dve", pmk[bi], pex[bi], band, ALU.mult, ["pex%d" % bi, "band"], ["pmk%d" % bi])
                        bufs.append(bi)
                    pU, pUk = next_pf()

                    def pv_fn(e, pU=pU, jq=jq, hg=hg, bufs=tuple(bufs)):
                        ins = None
                        for hi in range(4):
                            h = hg * 4 + hi
                            for sl in range(3):
                                ins = e.matmul(pU[0:65, hi * 128:(hi + 1) * 128], lhsT=vsb[jq + sl][:, h, :], rhs=pmk[bufs[hi]][:, sl * 128:(sl + 1) * 128],
                                               start=(sl == 0), stop=(sl == 2))
                        return ins
                    P.op("pe", pv_fn, reads=["vsb%d" % (jq + sl) for sl in range(3)] + ["pmk%d" % b for b in bufs], writes=[pUk])
                    n0 = 128 * (j0 + jq)
                    dst = accv[:, hg * 4:hg * 4 + 4, r, n0:n0 + 128]
                    src = pU[0:65, :].rearrange("p (h t) -> p h t", h=4)
                    akey = "accT"
                    if dd == 1:
                        CP("dve", dst, src, [pUk], [akey])
                    else:
                        TT("dve", dst, src, dst, ALU.add, [pUk, akey], [akey])
        rz = sb("rz", [64, 512])
        otb = [sb("otb%d" % i, [64, 512], BF16) for i in range(2)]
        k2 = 0
        for h in range(8):
            for g in range(4):
                pz, pzk = next_pf()
                P.op("pe", lambda e, pz=pz, h=h, g=g: e.matmul(pz[0:64, :], lhsT=cm[64:65, 7, 0:64], rhs=accT[64:65, h, g * 512:(g + 1) * 512],
                                                                start=True, stop=True), reads=["accT", "cm"], writes=[pzk])
                P.op("dve", lambda e, pz=pz: e.reciprocal(out=rz, in_=pz[0:64, :]), reads=[pzk], writes=["rz"])
                b2 = k2 % 2
                k2 += 1
                TT("pool", otb[b2], accT[0:64, h, g * 512:(g + 1) * 512], rz, ALU.mult, ["accT", "rz"], ["otb%d" % b2])
                DMA("sp", OTS[h, :, g * 512:(g + 1) * 512], otb[b2], ["otb%d" % b2], ["OTS%d_%d" % (h, g)])
        OTK = ["OTS%d_%d" % (h, g) for h in range(8) for g in range(4)]
        if stop_after == "B":
            P.op("sp", None, reads=OTK + MGK, writes=[])
            P.emit()
            return nc

        P.barrier()
        AR.off = M0
        u2T = sb("u2T", [128, 8, OWN], BF16)
        Wt = sb("Wt", [128, NTO, 32])
        M2 = AR.off
        woutG = sb("woutG", [128, 4, D], BF16)
        woutA = sb("woutA", [64, 8, D], BF16)
        DMA("pool", woutG, w_out[0:512, :].rearrange("(c p) n -> p c n", p=128), [], ["woutG"])
        DMA("pool", woutA, w_out[512:1024, :].rearrange("(h p) n -> p h n", p=64), [], ["woutA"])
        xo = [sb("xo%d" % i, [128, D]) for i in range(2)]
        mgl = [sb("mgl%d" % i, [128, 512], BF16) for i in range(2)]
        otl = [sb("otl%d" % i, [64, 8, 128], BF16) for i in range(2)]
        mgT = sb("mgT", [128, 4, 128], BF16)
        h2t = [sb("h2t%d" % i, [128, D]) for i in range(2)]
        u2 = sb("u2", [128, D])
        u2Tf = sb("u2Tf", [128, 8, 128])
        junk2 = sb("junk2", [128, D], BF16)
        ss2 = sb("ss2", [128, 2])
        rs2 = sb("rs2", [128, 2])
        lg = sb("lg", [128, 36])
        sm = sb("sm", [128, 64])
        for io in range(NTO):
            b2 = io % 2
            DMA("sp", xo[b2], xw[HALO + io * 128: HALO + (io + 1) * 128, :], [], ["xo%d" % b2])
            DMA("sp", mgl[b2], MG[io * 128:(io + 1) * 128, :], ["MG%d" % io], ["mgl%d" % b2])
            DMA("sp", otl[b2], OTS[:, :, io * 128:(io + 1) * 128].rearrange("h p t -> p h t"), OTK, ["otl%d" % b2])
            pbt, pbk = next_pb()

            def trm_fn(e, pbt=pbt, b2=b2):
                ins = None
                for c in range(4):
                    ins = e.transpose(out=pbt[:, c * 128:(c + 1) * 128], in_=mgl[b2][:, c * 128:(c + 1) * 128], identity=identb)
                return ins
            P.op("pe", trm_fn, reads=["mgl%d" % b2, "identb"], writes=[pbk])
            CP("act", mgT.rearrange("p c t -> p (c t)"), pbt[:, 0:512], [pbk], ["mgT"])
            for cg in range(2):
                pt, pk = next_pf()
                pairs = [(mgT[:, c, :], woutG[:, c, cg * 512:(cg + 1) * 512]) for c in range(4)] + \
                        [(otl[b2][:, h, :], woutA[:, h, cg * 512:(cg + 1) * 512]) for h in range(8)]
                mm_group(pt[:, :], pairs, pk, ["mgT", "otl%d" % b2, "woutG", "woutA"])
                TT("dve", h2t[b2][:, cg * 512:(cg + 1) * 512], pt[:, :], xo[b2][:, cg * 512:(cg + 1) * 512], ALU.add,
                   [pk, "xo%d" % b2], ["h2t%d" % b2])
            DMA("sp", H2[io * 128:(io + 1) * 128, :], h2t[b2], ["h2t%d" % b2], ["H2_%d" % io])
            P.op("act", lambda e, b2=b2: e.activation(out=junk2, in_=h2t[b2], func=AF.Square, accum_out=ss2[:, 0:1]),
                 reads=["h2t%d" % b2], writes=["junk2", "ss2"])
            rstd_from_ssq(rs2[:, 0:1], ss2[:, 0:1], D, "ss2", "rs2")
            STT(u2, h2t[b2], rs2[:, 0:1], n2bc, ALU.mult, ALU.mult, ["h2t%d" % b2, "rs2", "n2bc"], ["u2"])
            for half in range(2):
                pt, pk = next_pf()

                def tru_fn(e, pt=pt, half=half):
                    ins = None
                    for c in range(4):
                        cc = half * 4 + c
                        ins = e.transpose(out=pt[:, c * 128:(c + 1) * 128], in_=u2[:, cc * 128:(cc + 1) * 128], identity=cm[:, 0, :])
                    return ins
                P.op("pe", tru_fn, reads=["u2", "cm"], writes=[pk])
                CP("act", u2Tf[:, half * 4:half * 4 + 4, :].rearrange("p c t -> p (c t)"), pt[:, :], [pk], ["u2Tf%d" % half])
                CP("dve", u2T[:, half * 4:half * 4 + 4, io * 128:(io + 1) * 128], pt[:, :].rearrange("p (c t) -> p c t", c=4), [pk], ["u2T_%d" % io])
            pr_, prk = next_pf()
            mm_group(pr_[:, 0:36], [(u2Tf[:, c, :], wr[:, c, :]) for c in range(8)], prk, ["u2Tf0", "u2Tf1", "wr"])
            TT("dve", lg, pr_[:, 0:36], rbbc, ALU.add, [prk, "rbbc"], ["lg"])
            gmax, ngmax, gsum, gw = sm[:, 0:1], sm[:, 1:2], sm[:, 2:3], sm[:, 3:4]
            oh = sm[:, 4:8]
            ge = sm[:, 8:12]
            esel = sm[:, 16:24]
            top8 = sm[:, 24:32]
            d21, w1g, w2g = sm[:, 32:33], sm[:, 33:34], sm[:, 34:35]
            wa = sm[:, 40:48]
            wb_ = sm[:, 48:56]
            we = sm[:, 56:64]
            P.op("dve", lambda e: e.tensor_reduce(out=gmax, in_=lg[:, 0:4], axis=AX.X, op=ALU.max), reads=["lg"], writes=["sm"])
            TS("dve", oh, lg[:, 0:4], gmax, None, ALU.is_equal, None, ["lg", "sm"], ["sm"])
            TS("dve", ngmax, gmax, -1.0, None, ALU.mult, None, ["sm"], ["sm"])
            ACT(ge, lg[:, 0:4], AF.Exp, ["lg", "sm"], ["sm"], bias=ngmax, scale=1.0)
            P.op("dve", lambda e: e.tensor_reduce(out=gsum, in_=ge, axis=AX.X, op=ALU.add), reads=["sm"], writes=["sm"])
            P.op("dve", lambda e: e.reciprocal(out=gw, in_=gsum), reads=["sm"], writes=["sm"])
            TS("dve", esel, lg[:, 4:12], oh[:, 0:1], None, ALU.mult, None, ["lg", "sm"], ["sm"])
            for g in range(1, 4):
                STT(esel, lg[:, 4 + 8 * g:12 + 8 * g], oh[:, g:g + 1], esel, ALU.mult, ALU.add, ["lg", "sm"], ["sm"])
            P.op("dve", lambda e: e.max(out=top8, in_=esel), reads=["sm"], writes=["sm"])
            TT("dve", d21, top8[:, 1:2], top8[:, 0:1], ALU.subtract, ["sm"], ["sm"])
            ACT(d21, d21, AF.Exp, ["sm"], ["sm"])
            TS("dve", d21, d21, 1.0, None, ALU.add, None, ["sm"], ["sm"])
            P.op("dve", lambda e: e.reciprocal(out=w1g, in_=d21), reads=["sm"], writes=["sm"])
            TT("dve", w1g, w1g, gw, ALU.mult, ["sm"], ["sm"])
            TT("dve", w2g, gw, w1g, ALU.subtract, ["sm"], ["sm"])
            TS("dve", wa, esel, top8[:, 0:1], w1g, ALU.is_equal, ALU.mult, ["sm"], ["sm"])
            TS("dve", wb_, esel, top8[:, 1:2], w2g, ALU.is_equal, ALU.mult, ["sm"], ["sm"])
            TT("dve", we, wa, wb_, ALU.add, ["sm"], ["sm"])
            for g in range(4):
                TS("dve", Wt[:, io, g * 8:(g + 1) * 8], we, oh[:, g:g + 1], None, ALU.mult, None, ["sm"], ["Wt%d" % io])
        H2K = ["H2_%d" % i for i in range(NTO)]
        WTK = ["Wt%d" % i for i in range(NTO)]
        U2K = ["u2T_%d" % i for i in range(NTO)]
        if debug:
            DMA("sp", WTD, Wt.rearrange("p a b -> p (a b)"), WTK, ["WTD"])
        if stop_after == "C1":
            P.op("sp", None, reads=H2K + ["WTD"], writes=[])
            P.emit()
            return nc

        P.barrier()
        AR.off = M2
        hacc = sb("hacc", [128, NTO, D])
        hid = sb("hid", [128, 4, OWN], BF16)
        wgb = [sb("wgb%d" % i, [128, 8, 512], BF16) for i in range(2)]
        wub = [sb("wub%d" % i, [128, 8, 512], BF16) for i in range(2)]
        wdb = [sb("wdb%d" % i, [128, 4, D], BF16) for i in range(2)]
        sgb = [sb("sgb%d" % i, [128, 512]) for i in range(2)]
        for io in range(NTO):
            DMA("sp", hacc[:, io, :], H2[io * 128:(io + 1) * 128, :], ["H2_%d" % io], ["hacc%d" % io])

        def load_expert(ex):
            b = ex % 2
            DMA("pool", wgb[b], ewg[ex].rearrange("(c p) n -> p c n", p=128), [], ["wgb%d" % b])
            DMA("pool", wub[b], ewu[ex].rearrange("(c p) n -> p c n", p=128), [], ["wub%d" % b])
            DMA("pool", wdb[b], ewd[ex].rearrange("(c p) n -> p c n", p=128), [], ["wdb%d" % b])
        load_expert(0)
        kk2 = 0
        for ex in range(NEXP):
            b = ex % 2
            if ex + 1 < NEXP:
                load_expert(ex + 1)
            for tg in range(4):
                for fc in range(4):
                    pg, pgk = next_pf()
                    pu, puk = next_pf()
                    mm_group(pg[:, :], [(wgb[b][:, c, fc * 128:(fc + 1) * 128], u2T[:, c, tg * 512:(tg + 1) * 512]) for c in range(8)], pgk,
                             ["wgb%d" % b] + U2K)
                    mm_group(pu[:, :], [(wub[b][:, c, fc * 128:(fc + 1) * 128], u2T[:, c, tg * 512:(tg + 1) * 512]) for c in range(8)], puk,
                             ["wub%d" % b] + U2K)
                    sb_i = kk2 % 2
                    kk2 += 1
                    ACT(sgb[sb_i], pg[:, :], AF.Silu, [pgk], ["sgb%d" % sb_i])
                    TT("dve", hid[:, fc, tg * 512:(tg + 1) * 512], sgb[sb_i], pu[:, :], ALU.mult, ["sgb%d" % sb_i, puk], ["hid%d_%d" % (fc, tg)])
            for io in range(NTO):
                tg = io // 4
                for cg in range(2):
                    py, pyk = next_pf()
                    mm_group(py[:, :], [(hid[:, fc, io * 128:(io + 1) * 128], wdb[b][:, fc, cg * 512:(cg + 1) * 512]) for fc in range(4)], pyk,
                             ["wdb%d" % b] + ["hid%d_%d" % (fc, tg) for fc in range(4)])
                    STT(hacc[:, io, cg * 512:(cg + 1) * 512], py[:, :], Wt[:, io, ex:ex + 1], hacc[:, io, cg * 512:(cg + 1) * 512],
                        ALU.mult, ALU.add, [pyk, "Wt%d" % io, "hacc%d" % io], ["hacc%d" % io])
        junk3 = sgb[0].bitcast(BF16)
        ss3 = sb("ss3", [128, 2])
        rs3 = sb("rs3", [128, 2])
        ob = [wgb[i].rearrange("p c n -> p (c n)").bitcast(F32)[:, 0:D] for i in range(2)]
        for io in range(NTO):
            b2 = io % 2
            P.op("act", lambda e, io=io: e.activation(out=junk3, in_=hacc[:, io, :], func=AF.Square, accum_out=ss3[:, 0:1]),
                 reads=["hacc%d" % io], writes=["sgb0", "ss3"])
            rstd_from_ssq(rs3[:, 0:1], ss3[:, 0:1], D, "ss3", "rs3")
            STT(ob[b2], hacc[:, io, :], rs3[:, 0:1], fnbc, ALU.mult, ALU.mult, ["hacc%d" % io, "rs3", "fnbc"], ["wgb%d" % b2])
            DMA("sp", out_d[io * 128:(io + 1) * 128, :], ob[b2], ["wgb%d" % b2], ["OUT%d" % io])
        P.op("sp", None, reads=["OUT%d" % i for i in range(NTO)], writes=[])
        P.emit()
    return nc


def _consts():
    s = np.arange(128)[:, None]
    t = np.arange(128)[None, :]
    cm = np.zeros((128, 8, 128), np.float32)
    cm[:, 0] = (s == t)
    cm[:, 1] = (s <= t) / -16.0
    cm[:, 2] = (s >= t) / -16.0
    cm[:, 3] = (s > t) / -16.0
    cm[:, 4] = (s < t) / -16.0
    cm[:, 5] = (s <= t)
    cm[:, 6] = (s >= t)
    cm[:, 7] = 1.0
    band = np.zeros((128, 384), np.float32)
    band[:, 0:128] = (s >= t + 64)
    band[:, 128:256] = (np.abs(s - t) <= 64)
    band[:, 256:384] = (s <= t - 64)
    return cm, band


def make_in_maps(inputs):
    f = lambda a: np.ascontiguousarray(np.asarray(a, dtype=np.float32))
    x = f(inputs["x"])
    cm, band = _consts()
    wz = np.zeros((33, 512), np.float32)
    wz[0:16, 0:256] = f(inputs["gla_fwd_gate_w"])[0]
    wz[16:32, 256:512] = f(inputs["gla_bwd_gate_w"])[0]
    wz[32, 0:256] = f(inputs["gla_fwd_gate_b"])[0]
    wz[32, 256:512] = f(inputs["gla_bwd_gate_b"])[0]
    vecs = np.zeros((4, D), np.float32)
    vecs[0] = f(inputs["norm1_w"])[0]
    vecs[1] = f(inputs["norm2_w"])[0]
    vecs[2] = f(inputs["final_norm_w"])
    vecs[3] = np.tile(f(inputs["gla_norm_w"])[0], 8)
    wr = np.concatenate([f(inputs["router_group_w"])[0]] + [f(inputs["router_expert_w"])[0, g] for g in range(4)], axis=1)
    rb = np.concatenate([f(inputs["router_group_b"])[0], f(inputs["router_expert_b"])[0].reshape(-1)])[None, :]
    inv = (500000.0 ** (-(np.arange(0, 16, 2, dtype=np.float32) / np.float32(16)))).astype(np.float32)
    shared = dict(cmat=cm, band3=band, w_in=f(inputs["w_in"])[0], wz=wz, vecs=vecs, w_out=f(inputs["w_out"])[0],
                  wr=np.ascontiguousarray(wr), rb=np.ascontiguousarray(rb), ewg=f(inputs["expert_w_gate"])[0],
                  ewu=f(inputs["expert_w_up"])[0], ewd=f(inputs["expert_w_down"])[0])
    maps = []
    for c in range(8):
        b, q = c // 4, c % 4
        s0 = q * OWN
        pos = np.arange(s0 - HALO, s0 + OWN + HALO)
        valid = (pos >= 0) & (pos < S)
        xwin = np.zeros((WIN, D), np.float32)
        xwin[valid] = x[b, pos[valid]]
        ang = (pos.astype(np.float32)[:, None] * inv[None, :]).astype(np.float32)
        cs = np.concatenate([np.cos(ang), np.sin(ang)], axis=1).astype(np.float32)
        cs_t = np.ascontiguousarray(cs.reshape(NTW, 128, 16).transpose(1, 0, 2))
        vcol = np.ascontiguousarray(valid.astype(np.float32).reshape(NTW, 128).T)
        m = dict(shared)
        m.update(xw=xwin, vcol=vcol, cs_t=cs_t)
        maps.append(m)
    return maps


_NC_CACHE = {}


def kernel(**inputs):
    maps = make_in_maps(inputs)
    if "nc" not in _NC_CACHE:
        _NC_CACHE["nc"] = build_program()
    nc = _NC_CACHE["nc"]
    res = run_bass_kernel_spmd(nc, maps, core_ids=list(range(8)))
    out = np.zeros((2, S, D), np.float32)
    for c in range(8):
        b, q = c // 4, c % 4
        out[b, q * OWN:(q + 1) * OWN] = res.results[c]["out"]
    return out
```

```python
import numpy as np
from contextlib import ExitStack
import concourse.bass as bass
import concourse.mybir as mybir
from concourse.bass_utils import run_bass_kernel_spmd

F32 = mybir.dt.float32
BF16 = mybir.dt.bfloat16
I32 = mybir.dt.int32
AF = mybir.ActivationFunctionType
ALU = mybir.AluOpType
AX = mybir.AxisListType

ENGS = ("pe", "act", "dve", "pool", "sp")
EPOCH = 4096
DMA_SLOTS = 8

D = 1024
S = 8192
OWN = 2048
HALO = 1024
WIN = OWN + 2 * HALO
NTW = WIN // 128
T0 = HALO // 128
NTO = OWN // 128
INW = 3104
NEXP = 32
EPS = 1e-6


class Op:
    __slots__ = ("eng", "fn", "dma", "deps", "sig", "sigcount", "dmaidx", "idx")

    def __init__(self, eng, fn, dma):
        self.eng = eng
        self.fn = fn
        self.dma = dma
        self.deps = []
        self.sig = False
        self.sigcount = 0
        self.dmaidx = -1
        self.idx = -1


class Prog:
    def __init__(self, nc):
        self.nc = nc
        self.ops = []
        self.last_w = {}
        self.readers = {}
        self.ndma = {e: 0 for e in ENGS}
        self.bar = None

    def barrier(self):
        deps = set()
        for e in ENGS:
            last = None
            nd = 0
            for o in reversed(self.ops):
                if o.eng != e:
                    continue
                if o.dma:
                    if nd < DMA_SLOTS:
                        deps.add(o.idx)
                        nd += 1
                elif last is None:
                    last = o.idx
                    deps.add(o.idx)
                if last is not None and nd >= DMA_SLOTS:
                    break
        b = self.op("sp", None)
        b.deps = sorted(deps | set(b.deps))
        self.bar = b.idx
        return b

    def op(self, eng, fn, reads=(), writes=(), dma=False):
        import os as _os
        mx = int(_os.environ.get("DBG_MAXOPS", "0"))
        if mx and len(self.ops) >= mx and fn is not None:
            fn = None
            if dma:
                dma = False
        px = [k_ for k_ in reads if k_[:2] in ("pf", "pb")]
        if px:
            writes = list(writes) + [k_ for k_ in px if k_ not in writes]
            reads = [k_ for k_ in reads if k_ not in px]
        o = Op(eng, fn, dma)
        o.idx = len(self.ops)
        deps = set()
        if self.bar is not None:
            deps.add(self.bar)
        for k in reads:
            w = self.last_w.get(k)
            if w is not None:
                deps.add(w)
        for k in writes:
            w = self.last_w.get(k)
            if w is not None:
                deps.add(w)
            for r in self.readers.get(k, ()):
                deps.add(r)
        deps.discard(o.idx)
        o.deps = sorted(deps)
        for k in writes:
            self.last_w[k] = o.idx
            self.readers[k] = []
        for k in reads:
            if k not in writes:
                self.readers.setdefault(k, []).append(o.idx)
        if dma:
            o.dmaidx = self.ndma[eng]
            self.ndma[eng] += 1
        self.ops.append(o)
        return o

    def emit(self):
        nc = self.nc
        ops = self.ops
        for o in ops:
            for d in o.deps:
                p = ops[d]
                if not p.dma:
                    p.sig = True
        cnt = {e: 0 for e in ENGS}
        for o in ops:
            if o.sig and not o.dma:
                cnt[o.eng] += 1
                o.sigcount = cnt[o.eng]
        nsem = {e: (cnt[e] + EPOCH - 1) // EPOCH for e in ENGS}
        with ExitStack() as es:
            csem = {e: [es.enter_context(nc.semaphore("c_%s_%d" % (e, i))) for i in range(nsem[e])]
                    for e in ENGS}
            dsem = {e: [es.enter_context(nc.semaphore("d_%s_%d" % (e, i)))
                        for i in range(DMA_SLOTS if self.ndma[e] else 0)] for e in ENGS}
            block = es.enter_context(nc.Block())

            def body_for(e):
                def body(eng):
                    waited_c = {x: 0 for x in ENGS}
                    waited_d = {}
                    for o in ops:
                        if o.eng != e:
                            continue
                        need_c = {}
                        need_d = {}
                        for d in o.deps:
                            p = ops[d]
                            if p.dma:
                                slot = p.dmaidx % DMA_SLOTS
                                val = 16 * (p.dmaidx // DMA_SLOTS + 1)
                                key = (p.eng, slot)
                                if waited_d.get(key, 0) < val:
                                    need_d[key] = max(need_d.get(key, 0), val)
                            else:
                                if waited_c[p.eng] < p.sigcount:
                                    need_c[p.eng] = max(need_c.get(p.eng, 0), p.sigcount)
                        if o.dma:
                            slot = o.dmaidx % DMA_SLOTS
                            val = 16 * (o.dmaidx // DMA_SLOTS)
                            key = (e, slot)
                            if val > 0 and waited_d.get(key, 0) < val:
                                need_d[key] = max(need_d.get(key, 0), val)
                        for pe_, c in need_c.items():
                            ep = (c - 1) // EPOCH
                            eng.wait_ge(csem[pe_][ep], (c - 1) % EPOCH + 1)
                            waited_c[pe_] = c
                        for key, val in need_d.items():
                            eng.wait_ge(dsem[key[0]][key[1]], val)
                            waited_d[key] = val
                        ins = o.fn(eng) if o.fn is not None else None
                        if o.dma:
                            ins.then_inc(dsem[e][o.dmaidx % DMA_SLOTS], 16)
                        elif o.sig:
                            if ins is None:
                                ins = eng.nop()
                            ep = (o.sigcount - 1) // EPOCH
                            ins.then_inc(csem[e][ep], 1)
                return body

            block.tensor(body_for("pe"))
            block.scalar(body_for("act"))
            block.vector(body_for("dve"))
            block.gpsimd(body_for("pool"))
            block.sync(body_for("sp"))


class Arena:
    def __init__(self, ap, ncols):
        self.ap = ap
        self.n = ncols
        self.off = 0

    def alloc(self, shape, dt=F32):
        p = shape[0]
        rest = list(shape[1:])
        nel = 1
        for r in rest:
            nel *= r
        ncol = nel if dt in (F32, I32) else (nel + 1) // 2
        ncol += ncol % 2
        assert self.off + ncol <= self.n, "arena overflow: need %d have %d" % (ncol, self.n - self.off)
        v = self.ap[0:p, self.off:self.off + ncol]
        self.off += ncol
        if dt != F32:
            v = v.bitcast(dt)
        if v.shape[1] != nel:
            v = v[:, 0:nel]
        if len(rest) == 2:
            v = v.rearrange("p (a b) -> p a b", a=rest[0])
        elif len(rest) == 3:
            v = v.rearrange("p (a b c) -> p a b c", a=rest[0], b=rest[1])
        return v


def build_program(debug=False, stop_after=None, dbg_tiles=None):
    nc = bass.Bass("TRN2", target_bir_lowering=False)
    P = Prog(nc)
    global LASTP
    LASTP = P

    def din(name, shape, dt=F32):
        return nc.dram_tensor(name, list(shape), dt, kind="ExternalInput").ap()

    def dscr(name, shape, dt):
        kind = "ExternalOutput" if debug else "Internal"
        return nc.dram_tensor(name, list(shape), dt, kind=kind).ap()

    xw = din("xw", [WIN, D])
    vcol = din("vcol", [128, NTW])
    cs_t = din("cs_t", [128, NTW, 16])
    cmat = din("cmat", [128, 8, 128])
    band3 = din("band3", [128, 384])
    w_in = din("w_in", [D, INW])
    wz_d = din("wz", [33, 512])
    vecs = din("vecs", [4, D])
    w_out = din("w_out", [D, D])
    wr_d = din("wr", [D, 36])
    rb_d = din("rb", [1, 36])
    ewg = din("ewg", [NEXP, D, 512])
    ewu = din("ewu", [NEXP, D, 512])
    ewd = din("ewd", [NEXP, 512, D])
    out_d = nc.dram_tensor("out", [OWN, D], F32, kind="ExternalOutput").ap()
    QS = dscr("QS", [OWN, 512], BF16)
    KS = dscr("KS", [WIN + 2 * HALO, 512], BF16)
    VS = dscr("VS", [WIN + 2 * HALO, 520], BF16)
    GV = dscr("GV", [OWN, 512], BF16)
    GG = dscr("GG", [OWN, 512], BF16)
    MG = dscr("MG", [OWN, 512], BF16)
    OTS = dscr("OTS", [8, 64, OWN], BF16)
    H2 = dscr("H2", [OWN, D], F32)
    XB = dscr("XB", [2560, D], BF16)
    WB = dscr("WB", [2560, 8], F32)
    YB = dscr("YB", [2560, D], F32)
    WTD = nc.dram_tensor("WTD", [128, NTO * 32], F32, kind="ExternalOutput").ap() if debug else None

    QSK = ["QS%d" % i for i in range(NTO)]
    KSK = ["KS%d" % i for i in range(48)]
    VSK = ["VS%d" % i for i in range(48)]
    NCOL = 50 * 1024 + 512
    es = ExitStack()
    with es:
        arena_t = es.enter_context(nc.sbuf_tensor("arena", [128, NCOL], F32))
        AR = Arena(arena_t[:], NCOL)
        sb = lambda name, shape, dt=F32: AR.alloc(shape, dt)

        def ps(name, shape, dt=F32):
            return es.enter_context(nc.psum_tensor("p_" + name, list(shape), dt))

        pf = [ps("pf%d" % i, [128, 512]) for i in range(6)]
        pb = [ps("pb%d" % i, [128, 1024], BF16) for i in range(2)]
        pf_rr = [0]
        pb_rr = [0]

        def next_pf():
            i = pf_rr[0] % 6
            pf_rr[0] += 1
            return pf[i], "pf%d" % i

        def next_pb():
            i = pb_rr[0] % 2
            pb_rr[0] += 1
            return pb[i], "pb%d" % i

        def mm_group(out_ap, pairs, okey, rkeys):
            def fn(e):
                ins = None
                n = len(pairs)
                for j, (l, r) in enumerate(pairs):
                    ins = e.matmul(out_ap, lhsT=l, rhs=r, start=(j == 0), stop=(j == n - 1))
                return ins
            P.op("pe", fn, reads=rkeys, writes=[okey])

        def ACT(out, in_, func, reads, writes, **kw):
            P.op("act", lambda e: e.activation(out=out, in_=in_, func=func, **kw), reads=reads, writes=writes)

        def TT(eng, out, in0, in1, op, reads, writes):
            P.op(eng, lambda e: e.tensor_tensor(out=out, in0=in0, in1=in1, op=op), reads=reads, writes=writes)

        def STT(out, in0, scalar, in1, op0, op1, reads, writes):
            P.op("dve", lambda e: e.scalar_tensor_tensor(out=out, in0=in0, scalar=scalar, in1=in1, op0=op0, op1=op1),
                 reads=reads, writes=writes)

        def TS(eng, out, in0, s1, s2, op0, op1, reads, writes):
            if op1 is None:
                P.op(eng, lambda e: e.tensor_scalar(out=out, in0=in0, scalar1=s1, scalar2=None, op0=op0), reads=reads, writes=writes)
            else:
                P.op(eng, lambda e: e.tensor_scalar(out=out, in0=in0, scalar1=s1, scalar2=s2, op0=op0, op1=op1), reads=reads, writes=writes)

        def CP(eng, out, in_, reads, writes):
            if eng == "act":
                ACT(out, in_, AF.Copy, reads, writes)
            else:
                P.op(eng, lambda e: e.tensor_copy(out=out, in_=in_), reads=reads, writes=writes)

        def DMA(q, out, in_, reads, writes):
            return P.op(q, lambda e: e.dma_start(out=out, in_=in_), reads=reads, writes=writes, dma=True)

        def rstd_from_ssq(dst, src, n, rk, wk):
            ACT(dst, src, AF.Ln, [rk, "epsc"], [wk], scale=1.0 / n, bias=epsc[0:dst.shape[0], :])
            ACT(dst, dst, AF.Exp, [wk], [wk], scale=-0.5)

        cm = sb("cm", [128, 8, 128])
        identb = sb("identb", [128, 128], BF16)
        band = sb("band", [128, 384], BF16)
        maskFB = sb("maskFB", [128, 4, 128])
        n16col = sb("n16col", [128, 2])
        epsc = sb("epsc", [128, 2])
        onec = sb("onec", [128, 2])
        negc = sb("negc", [128, 2])
        vc = sb("vc", [128, NTW])
        cst = sb("cst", [128, NTW, 16])
        wz = sb("wz", [33, 512])
        n1bc = sb("n1bc", [128, D])
        n2bc = sb("n2bc", [128, D])
        fnbc = sb("fnbc", [128, D])
        gnbc = sb("gnbc", [128, 512])
        rbbc = sb("rbbc", [128, 36])
        wr = sb("wr", [128, 8, 36])
        nmax = sb("nmax", [128, 16])
        n16col = n16col[:, 0:1]
        epsc = epsc[:, 0:1]
        onec = onec[:, 0:1]
        negc = negc[:, 0:1]

        DMA("sp", cm, cmat, [], ["cm"])
        DMA("pool", identb, cmat[:, 0, :], [], ["identb"])
        DMA("pool", band, band3, [], ["band"])
        DMA("sp", vc, vcol, [], ["vc"])
        DMA("sp", cst, cs_t, [], ["cst"])
        DMA("sp", wz, wz_d, [], ["wz"])
        DMA("sp", n1bc, vecs[0:1, :].partition_broadcast(128), [], ["n1bc"])
        DMA("sp", n2bc, vecs[1:2, :].partition_broadcast(128), [], ["n2bc"])
        DMA("sp", fnbc, vecs[2:3, :].partition_broadcast(128), [], ["fnbc"])
        DMA("sp", gnbc, vecs[3:4, 0:512].partition_broadcast(128), [], ["gnbc"])
        DMA("sp", rbbc, rb_d[0:1, :].partition_broadcast(128), [], ["rbbc"])
        DMA("sp", wr, wr_d.rearrange("(c p) n -> p c n", p=128), [], ["wr"])
        P.op("dve", lambda e: e.memset(n16col, -1.0 / 16.0), writes=["n16col"])
        P.op("dve", lambda e: e.memset(epsc, EPS), writes=["epsc"])
        P.op("dve", lambda e: e.memset(onec, 1.0), writes=["onec"])
        P.op("dve", lambda e: e.memset(nmax, 0.0), writes=["nmax"])
        for h in range(4):
            CP("dve", maskFB[:, h, :], cm[:, 5 + h // 2, :], ["cm"], ["maskFB"])
        M0 = AR.off

        attnT = sb("attnT", [128, NTO, 512], BF16)
        qdT = sb("qdT", [128, NTO, 4, 128], BF16)
        SfT = sb("SfT", [128, NTO, 2, 128], BF16)
        SbT = sb("SbT", [128, NTO, 2, 128], BF16)
        M1 = AR.off
        win = sb("win", [128, 8, INW], BF16)
        for c in range(8):
            DMA("pool", win[:, c, :], w_in[c * 128:(c + 1) * 128, :], [], ["win%d" % c])
        winkeys = ["win%d" % c for c in range(8)]
        kvB = sb("kvB", [128, NTW - T0, 2, 128], BF16)
        decB = sb("decB", [128, NTW - T0, 2])
        Sf = sb("Sf", [128, 2, 128])
        Sb = sb("Sb", [128, 2, 128])
        P.op("dve", lambda e: e.memset(Sf, 0.0), writes=["Sf"])
        P.op("dve", lambda e: e.memset(Sb, 0.0), writes=["Sb"])
        zt = sb("zt", [128, 520], BF16)
        P.op("pool", lambda e: e.memset(zt, 0.0), writes=["zt"])
        for blk in range(HALO // 128):
            for base in (0, HALO + WIN):
                r0 = base + blk * 128
                DMA("sp", KS[r0:r0 + 128, :], zt[:, 0:512], ["zt"], ["KS%d" % (r0 // 128)])
                DMA("sp", VS[r0:r0 + 128, :], zt, ["zt"], ["VS%d" % (r0 // 128)])
        xt = [sb("xt%d" % i, [128, D]) for i in range(2)]
        junk = sb("junk", [128, D], BF16)
        xn = [sb("xn%d" % i, [128, D], BF16) for i in range(2)]
        xnT = [sb("xnT%d" % i, [128, 8, 128], BF16) for i in range(2)]
        ssq = sb("ssq", [128, 2])
        rstd = sb("rstd", [128, 2])
        qk = sb("qk", [128, 512])
        vbf = [sb("vbf%d" % i, [128, 512], BF16) for i in range(2)]
        gbf = [sb("gbf%d" % i, [128, 512], BF16) for i in range(2)]
        lr = sb("lr", [128, 32])
        lrT = sb("lrT", [33, 128])
        ez = sb("ez", [128, 512])
        spl = sb("spl", [128, 512])
        E1 = sb("E1", [128, 512])
        E2 = sb("E2", [128, 512])
        E3 = sb("E3", [128, 512])
        dec = sb("dec", [128, 4])
        qd = sb("qd", [128, 512], BF16)
        ki = sb("ki", [128, 512], BF16)
        ke = sb("ke", [128, 512], BF16)
        kiT = sb("kiT", [128, 4, 128], BF16)
        qr = sb("qr", [128, 8, 64])
        kr = sb("kr", [128, 8, 64])
        qrb = [sb("qrb%d" % i, [128, 512], BF16) for i in range(2)]
        krb = [sb("krb%d" % i, [128, 512], BF16) for i in range(2)]
        vab = [sb("vab%d" % i, [128, 8, 65], BF16) for i in range(2)]
        rt = sb("rt", [128, 8, 8])
        rt2 = sb("rt2", [128, 8, 8])
        sqs = sb("sqs", [128, 8, 64])
        nrm = sb("nrm", [128, 16])
        P.op("dve", lambda e: e.memset(lrT[32:33, :], 1.0), writes=["lrT_one"])

        for i in (range(NTW) if dbg_tiles is None else dbg_tiles):
            own = T0 <= i < T0 + NTO
            left = i < T0
            io = i - T0
            b2 = i % 2
            xtk, xnk, xnTk = "xt%d" % b2, "xn%d" % b2, "xnT%d" % b2
            if i == 0:
                DMA("sp", xt[0], xw[0:128, :], [], ["xt0"])
            if i + 1 < NTW:
                DMA("sp", xt[(i + 1) % 2], xw[(i + 1) * 128:(i + 2) * 128, :], [], ["xt%d" % ((i + 1) % 2)])
            sk, rk = "ssq%d" % b2, "rstd%d" % b2
            P.op("act", lambda e, b2=b2: e.activation(out=junk, in_=xt[b2], func=AF.Square, accum_out=ssq[:, b2:b2 + 1]),
                 reads=[xtk], writes=["junk", sk])
            rstd_from_ssq(rstd[:, b2:b2 + 1], ssq[:, b2:b2 + 1], D, sk, rk)
            STT(xn[b2], xt[b2], rstd[:, b2:b2 + 1], n1bc, ALU.mult, ALU.mult, [xtk, rk, "n1bc"], [xnk])
            pbt, pbk = next_pb()

            def tr_fn(e, b2=b2, pbt=pbt):
                ins = None
                for c in range(8):
                    ins = e.transpose(out=pbt[:, c * 128:(c + 1) * 128], in_=xn[b2][:, c * 128:(c + 1) * 128], identity=identb)
                return ins
            P.op("pe", tr_fn, reads=[xnk, "identb"], writes=[pbk])
            CP("act", xnT[b2].rearrange("p c t -> p (c t)"), pbt[:, :], [pbk], [xnTk])

            def proj(c0, c1, b2=b2, xnTk=xnTk):
                pt, pk = next_pf()
                n = c1 - c0
                mm_group(pt[:, 0:n], [(xnT[b2][:, c, :], win[:, c, c0:c1]) for c in range(8)], pk, [xnTk] + winkeys)
                return pt, pk

            if own:
                pt, pk = proj(0, 512)
                CP("act", qk, pt[:, 0:512], [pk], ["qk"])
            else:
                pt, pk = proj(256, 512)
                CP("act", qk[:, 256:512], pt[:, 0:256], [pk], ["qk"])
            pt, pk = proj(512, 1024)
            vkey = "vbf%d" % b2
            vt = vbf[b2]
            CP("dve", vt, pt[:, 0:512], [pk], [vkey])
            if own:
                DMA("sp", GV[io * 128:(io + 1) * 128, :], vt, [vkey], ["GV%d" % io])
                pt, pk = proj(1024, 1536)
                CP("act", gbf[b2], pt[:, 0:512], [pk], ["gbf%d" % b2])
                DMA("sp", GG[io * 128:(io + 1) * 128, :], gbf[b2], ["gbf%d" % b2], ["GG%d" % io])
            pt, pk = proj(1536, 1568)
            CP("dve", lr, pt[:, 0:32], [pk], ["lr"])
            pt, pk = next_pf()
            P.op("pe", lambda e, pt=pt: e.transpose(out=pt[0:32, 0:128], in_=lr, identity=cm[:, 0, :]), reads=["lr", "cm"], writes=[pk])
            CP("dve", lrT[0:32, :], pt[0:32, 0:128], [pk], ["lrT"])
            pz, pzk = next_pf()
            P.op("pe", lambda e, pz=pz: e.matmul(pz[:, :], lhsT=lrT, rhs=wz, start=True, stop=True),
                 reads=["lrT", "lrT_one", "wz"], writes=[pzk])
            ACT(ez, pz[:, :], AF.Exp, [pzk], ["ez"], scale=-1.0)
            ACT(spl, ez, AF.Ln, ["ez", "onec"], ["spl"], bias=onec, scale=1.0)
            pbb, pbbk = next_pf()
            P.op("pe", lambda e, pbb=pbb: (e.matmul(pbb[:, 0:256], lhsT=cm[:, 1, :], rhs=spl[:, 0:256], start=True, stop=True),
                                           e.matmul(pbb[:, 256:512], lhsT=cm[:, 2, :], rhs=spl[:, 256:512], start=True, stop=True))[1],
                 reads=["cm", "spl"], writes=[pbbk])
            ACT(E1, pbb[:, :], AF.Exp, [pbbk], ["E1"])
            ACT(E2, pbb[:, :], AF.Exp, [pbbk], ["E2"], scale=-1.0)
            pb3, pb3k = next_pf()
            P.op("pe", lambda e, pb3=pb3: (e.matmul(pb3[:, 0:256], lhsT=cm[:, 3, :], rhs=spl[:, 0:256], start=True, stop=True),
                                           e.matmul(pb3[:, 256:512], lhsT=cm[:, 4, :], rhs=spl[:, 256:512], start=True, stop=True))[1],
                 reads=["cm", "spl"], writes=[pb3k])
            ACT(E3, pb3[:, :], AF.Exp, [pb3k], ["E3"])
            pdc, pdck = next_pf()

            def dec_fn(e, pdc=pdc):
                ins = None
                for j in range(4):
                    ins = e.matmul(pdc[:, j:j + 1], lhsT=spl[:, j * 128:(j + 1) * 128], rhs=n16col, start=True, stop=True)
                return ins
            P.op("pe", dec_fn, reads=["spl", "n16col"], writes=[pdck])
            ACT(dec, pdc[:, 0:4], AF.Exp, [pdck], ["dec"])
            if own:
                STT(qd[:, 0:256], qk[:, 0:256], 0.125, E1[:, 0:256], ALU.mult, ALU.mult, ["qk", "E1"], ["qd"])
                STT(qd[:, 256:512], qk[:, 0:256], 0.125, E1[:, 256:512], ALU.mult, ALU.mult, ["qk", "E1"], ["qd"])
                TT("pool", ki[:, 0:256], qk[:, 256:512], E2[:, 0:256], ALU.mult, ["qk", "E2"], ["ki"])
                TT("pool", ki[:, 256:512], qk[:, 256:512], E2[:, 256:512], ALU.mult, ["qk", "E2"], ["ki"])
            TT("pool", ke[:, 0:256], qk[:, 256:512], E3[:, 0:256], ALU.mult, ["qk", "E3"], ["ke"])
            TT("pool", ke[:, 256:512], qk[:, 256:512], E3[:, 256:512], ALU.mult, ["qk", "E3"], ["ke"])
            if own:
                pbt, pbk = next_pb()

                def tr2_fn(e, pbt=pbt):
                    ins = None
                    for j in range(4):
                        ins = e.transpose(out=pbt[:, j * 128:(j + 1) * 128], in_=qd[:, j * 128:(j + 1) * 128], identity=identb)
                    for j in range(4):
                        ins = e.transpose(out=pbt[:, 512 + j * 128:512 + (j + 1) * 128], in_=ki[:, j * 128:(j + 1) * 128], identity=identb)
                    return ins
                P.op("pe", tr2_fn, reads=["qd", "ki", "identb"], writes=[pbk])
                CP("act", qdT[:, io, :, :].rearrange("p c t -> p (c t)"), pbt[:, 0:512], [pbk], ["qdT%d" % io])
                CP("dve", kiT.rearrange("p c t -> p (c t)"), pbt[:, 512:1024], [pbk], ["kiT"])
                paX, paXk = next_pf()
                paY, paYk = next_pf()

                def att_fn(e, pa, par, io=io):
                    ins = None
                    p0 = par * 64
                    for dirn in range(2):
                        for pr in range(2):
                            blk = dirn * 2 + pr
                            sl = dirn * 2 + pr
                            ins = e.matmul(pa[:, sl * 128:(sl + 1) * 128], lhsT=kiT[p0:p0 + 64, blk, :], rhs=qdT[p0:p0 + 64, io, blk, :],
                                           start=True, stop=True)
                    return ins
                P.op("pe", lambda e, pa=paX, f=att_fn: f(e, pa, 0), reads=["kiT", "qdT%d" % io], writes=[paXk])
                P.op("pe", lambda e, pa=paY, f=att_fn: f(e, pa, 1), reads=["kiT", "qdT%d" % io], writes=[paYk])
                TT("dve", ez, paX[:, :], maskFB.rearrange("p h c -> p (h c)"), ALU.mult, [paXk, "maskFB"], ["ez"])
                TT("dve", E1, paY[:, :], maskFB.rearrange("p h c -> p (h c)"), ALU.mult, [paYk, "maskFB"], ["E1"])
                av = attnT[:, io, :].rearrange("p (a b c) -> p a b c", a=2, b=2)
                TT("pool", av[:, :, 0, :], ez[:, 0:256].rearrange("p (a c) -> p a c", a=2), ez[:, 256:512].rearrange("p (a c) -> p a c", a=2),
                   ALU.add, ["ez"], ["attnT%d" % io])
                TT("pool", av[:, :, 1, :], E1[:, 0:256].rearrange("p (a c) -> p a c", a=2), E1[:, 256:512].rearrange("p (a c) -> p a c", a=2),
                   ALU.add, ["E1"], ["attnT%d" % io])
            for dirn in range(2):
                if dirn == 0 and i >= T0 + NTO:
                    continue
                if dirn == 1 and left:
                    continue
                pkv, pkvk = next_pf()

                def kv_fn(e, pkv=pkv, dirn=dirn, vt=vt):
                    ins = None
                    for pr in range(2):
                        ins = e.matmul(pkv[:, pr * 256:(pr + 1) * 256], lhsT=ke[:, dirn * 256 + pr * 128: dirn * 256 + (pr + 1) * 128],
                                       rhs=vt[:, pr * 256:(pr + 1) * 256], start=True, stop=True)
                    return ins
                P.op("pe", kv_fn, reads=["ke", vkey], writes=[pkvk])
                if dirn == 0:
                    if own:
                        CP("act", SfT[:, io, :, :].rearrange("p a b -> p (a b)"), Sf.rearrange("p a b -> p (a b)"), ["Sf"], ["SfT%d" % io])
                    for pr in range(2):
                        for hh in range(2):
                            p0 = hh * 64
                            STT(Sf[p0:p0 + 64, pr, :], Sf[p0:p0 + 64, pr, :], dec[p0:p0 + 64, pr:pr + 1],
                                pkv[p0:p0 + 64, pr * 256 + hh * 128: pr * 256 + (hh + 1) * 128], ALU.mult, ALU.add,
                                ["Sf", "dec", pkvk], ["Sf"])
                else:
                    ib = i - T0
                    for pr in range(2):
                        for hh in range(2):
                            p0 = hh * 64
                            CP("act", kvB[p0:p0 + 64, ib, pr, :], pkv[p0:p0 + 64, pr * 256 + hh * 128: pr * 256 + (hh + 1) * 128],
                               [pkvk], ["kvB%d" % ib])
                    CP("dve", decB[:, ib, :], dec[:, 2:4], ["dec"], ["decB%d" % ib])

            def rope(pt, pk, dst, dkey, i=i):
                src = pt[:, 0:512].rearrange("p (h d) -> p h d", h=8)
                cosb = cst[:, i, 0:8].unsqueeze(1).broadcast_to([128, 8, 8])
                sinb = cst[:, i, 8:16].unsqueeze(1).broadcast_to([128, 8, 8])
                CP("act", dst.rearrange("p h d -> p (h d)"), pt[:, 0:512], [pk], [dkey])
                TT("dve", rt, src[:, :, 8:16], sinb, ALU.mult, [pk, "cst"], ["rt"])
                TT("dve", rt2, src[:, :, 0:8], cosb, ALU.mult, [pk, "cst"], ["rt2"])
                TT("dve", dst[:, :, 0:8], rt2, rt, ALU.subtract, ["rt", "rt2"], [dkey])
                TT("dve", rt, src[:, :, 0:8], sinb, ALU.mult, [pk, "cst"], ["rt"])
                TT("dve", rt2, src[:, :, 8:16], cosb, ALU.mult, [pk, "cst"], ["rt2"])
                TT("dve", dst[:, :, 8:16], rt2, rt, ALU.add, ["rt", "rt2"], [dkey])

            def sqnorm(src, col0, skey):
                TT("pool", sqs, src, src, ALU.mult, [skey], ["sqs"])
                P.op("dve", lambda e: e.tensor_reduce(out=nrm[:, col0:col0 + 8], in_=sqs, axis=AX.X, op=ALU.add), reads=["sqs"], writes=["nrm"])
                TT("dve", nmax[:, col0:col0 + 8], nmax[:, col0:col0 + 8], nrm[:, col0:col0 + 8], ALU.max, ["nrm", "nmax"], ["nmax"])

            r0k = HALO + i * 128
            if own:
                pt, pk = proj(1568, 2080)
                rope(pt, pk, qr, "qr")
                sqnorm(qr, 0, "qr")
                CP("act", qrb[b2], qr.rearrange("p h d -> p (h d)"), ["qr"], ["qrb%d" % b2])
                DMA("sp", QS[io * 128:(io + 1) * 128, :], qrb[b2], ["qrb%d" % b2], ["QS%d" % io])
            pt, pk = proj(2080, 2592)
            rope(pt, pk, kr, "kr")
            sqnorm(kr, 8, "kr")
            CP("act", krb[b2], kr.rearrange("p h d -> p (h d)"), ["kr"], ["krb%d" % b2])
            DMA("sp", KS[r0k:r0k + 128, :], krb[b2], ["krb%d" % b2], ["KS%d" % (r0k // 128)])
            pt, pk = proj(2592, 3104)
            CP("act", vab[b2][:, :, 0:64], pt[:, 0:512].rearrange("p (h d) -> p h d", h=8), [pk], ["vab%d" % b2])
            CP("dve", vab[b2][:, :, 64:65], vc[:, i:i + 1].unsqueeze(1).broadcast_to([128, 8, 1]), ["vc"], ["vab%d" % b2])
            DMA("sp", VS[r0k:r0k + 128, :], vab[b2].rearrange("p h d -> p (h d)"), ["vab%d" % b2], ["VS%d" % (r0k // 128)])

        if stop_after == "A":
            P.op("sp", None, reads=[k_ for k_ in P.last_w.keys() if k_[:2] in ("QS", "KS", "VS", "GV", "GG")], writes=[])
            P.emit()
            return nc
        for i in range(NTW - 1, T0 - 1, -1):
            ib = i - T0
            if ib < NTO:
                CP("act", SbT[:, ib, :, :].rearrange("p a b -> p (a b)"), Sb.rearrange("p a b -> p (a b)"), ["Sb"], ["SbT%d" % ib])
            if i == T0:
                break
            for pr in range(2):
                STT(Sb[:, pr, :], Sb[:, pr, :], decB[:, ib, pr:pr + 1], kvB[:, ib, pr, :], ALU.mult, ALU.add,
                    ["Sb", "decB%d" % ib, "kvB%d" % ib], ["Sb"])

        P.barrier()
        AR.off = M1
        vb2 = [sb("vb2%d" % i, [128, 512], BF16) for i in range(2)]
        gb2 = [sb("gb2%d" % i, [128, 512], BF16) for i in range(2)]
        osb = sb("osb", [128, 512])
        osq = sb("osq", [128, 4, 128])
        oms = sb("oms", [128, 4])
        sgs = sb("sgs", [128, 512])
        ybf = sb("ybf", [128, 512])
        mixb = [sb("mixb%d" % i, [128, 512], BF16) for i in range(2)]
        for io in range(NTO):
            b2 = io % 2
            DMA("sp", vb2[b2], GV[io * 128:(io + 1) * 128, :], ["GV%d" % io], ["vb2%d" % b2])
            DMA("sp", gb2[b2], GG[io * 128:(io + 1) * 128, :], ["GG%d" % io], ["gb2%d" % b2])
            poX, poXk = next_pf()
            poY, poYk = next_pf()

            def o_fn(e, po, par, io=io, b2=b2):
                ins = None
                p0 = par * 64
                for pr in range(2):
                    h = pr * 2 + par
                    oap = po[:, pr * 128:(pr + 1) * 128]
                    e.matmul(oap, lhsT=attnT[:, io, h * 128:(h + 1) * 128], rhs=vb2[b2][:, h * 128:(h + 1) * 128], start=True, stop=False)
                    e.matmul(oap, lhsT=qdT[p0:p0 + 64, io, pr, :], rhs=SfT[p0:p0 + 64, io, pr, :], start=False, stop=False)
                    ins = e.matmul(oap, lhsT=qdT[p0:p0 + 64, io, 2 + pr, :], rhs=SbT[p0:p0 + 64, io, pr, :], start=False, stop=True)
                return ins
            rk_ = ["attnT%d" % io, "qdT%d" % io, "SfT%d" % io, "SbT%d" % io, "vb2%d" % b2]
            P.op("pe", lambda e, po=poX, f=o_fn: f(e, po, 0), reads=rk_, writes=[poXk])
            P.op("pe", lambda e, po=poY, f=o_fn: f(e, po, 1), reads=rk_, writes=[poYk])
            ov = osb.rearrange("p (a b c) -> p a b c", a=2, b=2)
            CP("act", ov[:, :, 0, :], poX[:, 0:256].rearrange("p (a c) -> p a c", a=2), [poXk], ["osb"])
            CP("act", ov[:, :, 1, :], poY[:, 0:256].rearrange("p (a c) -> p a c", a=2), [poYk], ["osb"])
            TT("pool", osq.rearrange("p h d -> p (h d)"), osb, osb, ALU.mult, ["osb"], ["osq"])
            P.op("dve", lambda e: e.tensor_reduce(out=oms, in_=osq, axis=AX.X, op=ALU.add), reads=["osq"], writes=["oms"])
            rstd_from_ssq(oms, oms, 128, "oms", "oms")
            ACT(sgs, gb2[b2], AF.Silu, ["gb2%d" % b2], ["sgs"])
            TT("dve", ybf, osb, gnbc, ALU.mult, ["osb", "gnbc"], ["ybf"])
            for h in range(4):
                STT(mixb[b2][:, h * 128:(h + 1) * 128], ybf[:, h * 128:(h + 1) * 128], oms[:, h:h + 1], sgs[:, h * 128:(h + 1) * 128],
                    ALU.mult, ALU.mult, ["ybf", "oms", "sgs"], ["mixb%d" % b2])
            DMA("sp", MG[io * 128:(io + 1) * 128, :], mixb[b2], ["mixb%d" % b2], ["MG%d" % io])
        MGK = ["MG%d" % i for i in range(NTO)]
        if stop_after == "G2":
            P.op("sp", None, reads=QSK + KSK + VSK + MGK, writes=[])
            P.emit()
            return nc

        P.barrier()
        AR.off = M0
        nm2 = sb("nm2", [128, 2])
        m2 = sb("m2", [2, 2])
        m1 = sb("m1", [1, 4])
        P.op("dve", lambda e: e.tensor_reduce(out=nm2, in_=nmax.rearrange("p (a h) -> p a h", a=2), axis=AX.X, op=ALU.max),
             reads=["nmax"], writes=["nm2"])
        pt, pk = next_pf()
        P.op("pe", lambda e, pt=pt: e.transpose(out=pt[0:2, 0:128], in_=nm2, identity=cm[:, 0, :]), reads=["nm2", "cm"], writes=[pk])
        P.op("dve", lambda e, pt=pt: e.tensor_reduce(out=m2[:, 0:1], in_=pt[0:2, 0:128], axis=AX.X, op=ALU.max), reads=[pk], writes=["m2"])
        pt, pk = next_pf()
        P.op("pe", lambda e, pt=pt: e.transpose(out=pt[0:1, 0:2], in_=m2[:, 0:1], identity=cm[0:2, 0, 0:2]), reads=["m2", "cm"], writes=[pk])
        CP("dve", m1[:, 0:2], pt[0:1, 0:2], [pk], ["m1"])
        TT("dve", m1[:, 2:3], m1[:, 0:1], m1[:, 1:2], ALU.mult, ["m1"], ["m1"])
        ACT(m1[:, 3:4], m1[:, 2:3], AF.Ln, ["m1"], ["m1"])
        ACT(m1[:, 3:4], m1[:, 3:4], AF.Exp, ["m1"], ["m1"], scale=0.5)
        TS("dve", m1[:, 3:4], m1[:, 3:4], -0.125, None, ALU.mult, None, ["m1"], ["m1"])
        pt, pk = next_pf()
        P.op("pe", lambda e, pt=pt: e.matmul(pt[:, 0:1], lhsT=cm[0:1, 7, :], rhs=m1[:, 3:4], start=True, stop=True), reads=["m1", "cm"], writes=[pk])
        CP("dve", negc, pt[:, 0:1], [pk], ["negc"])

        accT = sb("accT", [65, 8, OWN])
        NQ = 8
        qsb = [sb("qsb%d" % i, [128, 512], BF16) for i in range(NQ)]
        ksb = [sb("ksb%d" % i, [128, 512], BF16) for i in range(NQ + 2)]
        vsb = [sb("vsb%d" % i, [128, 8, 65], BF16) for i in range(NQ + 2)]
        qT = [sb("qT%d" % i, [128, 4, 128], BF16) for i in range(NQ)]
        kT = [sb("kT%d" % i, [128, 4, 128], BF16) for i in range(NQ + 2)]
        pex = [sb("pex%d" % i, [128, 384], BF16) for i in range(4)]
        pmk = [sb("pmk%d" % i, [128, 384], BF16) for i in range(4)]
        cnt4 = [0]
        jobs = [(1, 0, 0, 8), (1, 0, 8, 8)] + [(4, r, 0, 4) for r in range(4)] + [(16, r, 0, 1) for r in range(16)]
        for (dd, r, j0, nq) in jobs:
            QSv = QS.rearrange("(n d) c -> d n c", d=dd)
            KSv = KS.rearrange("(n d) c -> d n c", d=dd)
            VSv = VS.rearrange("(n d) c -> d n c", d=dd)
            accv = accT.rearrange("p h (n d) -> p h d n", d=dd)
            for jq in range(nq):
                n0 = 128 * (j0 + jq)
                DMA("sp", qsb[jq], QSv[r, n0:n0 + 128, :], QSK, ["qsb%d" % jq])
            for kk in range(nq + 2):
                n0 = 2048 // dd + 128 * (j0 + kk - 1)
                DMA("sp", ksb[kk], KSv[r, n0:n0 + 128, :], KSK, ["ksb%d" % kk])
                DMA("sp", vsb[kk].rearrange("p h d -> p (h d)"), VSv[r, n0:n0 + 128, :], VSK, ["vsb%d" % kk])
            tl = [(qsb[jq], "qsb%d" % jq, qT[jq], "qT%d" % jq) for jq in range(nq)] + \
                 [(ksb[kk], "ksb%d" % kk, kT[kk], "kT%d" % kk) for kk in range(nq + 2)]
            for t0 in range(0, len(tl), 2):
                grp = tl[t0:t0 + 2]
                pbt, pbk = next_pb()

                def trq_fn(e, grp=grp, pbt=pbt):
                    ins = None
                    for gi, (src, _, _, _) in enumerate(grp):
                        for c in range(4):
                            ins = e.transpose(out=pbt[:, gi * 512 + c * 128: gi * 512 + (c + 1) * 128], in_=src[:, c * 128:(c + 1) * 128], identity=identb)
                    return ins
                P.op("pe", trq_fn, reads=[g[1] for g in grp] + ["identb"], writes=[pbk])
                for gi, (_, _, dst, dk) in enumerate(grp):
                    CP("act" if gi == 0 else "dve", dst.rearrange("p c t -> p (c t)"), pbt[:, gi * 512:(gi + 1) * 512], [pbk], [dk])
            for jq in range(nq):
                for hg in range(2):
                    bufs = []
                    for h in range(hg * 4, hg * 4 + 4):
                        p0 = (h % 2) * 64
                        blk = h // 2
                        pS, pSk = next_pf()

                        def s_fn(e, pS=pS, jq=jq, p0=p0, blk=blk):
                            ins = None
                            for sl in range(3):
                                ins = e.matmul(pS[:, sl * 128:(sl + 1) * 128], lhsT=kT[jq + sl][p0:p0 + 64, blk, :], rhs=qT[jq][p0:p0 + 64, blk, :],
                                               start=True, stop=True)
                            return ins
                        P.op("pe", s_fn, reads=["kT%d" % (jq + sl) for sl in range(3)] + ["qT%d" % jq], writes=[pSk])
                        bi = cnt4[0] % 4
                        cnt4[0] += 1
                        ACT(pex[bi], pS[:, 0:384], AF.Exp, [pSk, "negc"], ["pex%d" % bi], bias=negc, scale=0.125)
                        TT("pool" if (h % 2) else "dve", pmk[bi], pex[bi], band, ALU.mult, ["pex%d" % bi, "band"], ["pmk%d" % bi])
                        bufs.append(bi)
                    pU, pUk = next_pf()

                    def pv_fn(e, pU=pU, jq=jq, hg=hg, bufs=tuple(bufs)):
                        ins = None
                        for hi in range(4):
                            h = hg * 4 + hi
                            for sl in range(3):
                                ins = e.matmul(pU[0:65, hi * 128:(hi + 1) * 128], lhsT=vsb[jq + sl][:, h, :], rhs=pmk[bufs[hi]][:, sl * 128:(sl + 1) * 128],
                                               start=(sl == 0), stop=(sl == 2))
                        return ins
                    P.op("pe", pv_fn, reads=["vsb%d" % (jq + sl) for sl in range(3)] + ["pmk%d" % b for b in bufs], writes=[pUk])
                    n0 = 128 * (j0 + jq)
                    dst = accv[:, hg * 4:hg * 4 + 4, r, n0:n0 + 128]
                    src = pU[0:65, :].rearrange("p (h t) -> p h t", h=4)
                    akey = "accT"
                    if dd == 1:
                        CP("dve", dst, src, [pUk], [akey])
                    else:
                        TT("dve", dst, src, dst, ALU.add, [pUk, akey], [akey])
        rz = sb("rz", [64, 512])
        otb = [sb("otb%d" % i, [64, 512], BF16) for i in range(2)]
        k2 = 0
        for h in range(8):
            for g in range(4):
                pz, pzk = next_pf()
                P.op("pe", lambda e, pz=pz, h=h, g=g: e.matmul(pz[0:64, :], lhsT=cm[64:65, 7, 0:64], rhs=accT[64:65, h, g * 512:(g + 1) * 512],
                                                                start=True, stop=True), reads=["accT", "cm"], writes=[pzk])
                P.op("dve", lambda e, pz=pz: e.reciprocal(out=rz, in_=pz[0:64, :]), reads=[pzk], writes=["rz"])
                b2 = k2 % 2
                k2 += 1
                TT("pool", otb[b2], accT[0:64, h, g * 512:(g + 1) * 512], rz, ALU.mult, ["accT", "rz"], ["otb%d" % b2])
                DMA("sp", OTS[h, :, g * 512:(g + 1) * 512], otb[b2], ["otb%d" % b2], ["OTS%d_%d" % (h, g)])
        OTK = ["OTS%d_%d" % (h, g) for h in range(8) for g in range(4)]
        if stop_after == "B":
            P.op("sp", None, reads=OTK + MGK, writes=[])
            P.emit()
            return nc

        P.barrier()
        AR.off = M0
        bc_cache = {}

        def bcreg(e):
            if "r" not in bc_cache:
                bc_cache["r"] = e.to_reg(2559)
            return bc_cache["r"]
        CAPG = 640
        NSLOT = 4 * CAPG
        OOB = 4096.0
        u2tok = sb("u2tok", [128, NTO, D], BF16)
        OH = sb("OH", [128, NTO, 4])
        WE = sb("WE", [128, NTO, 8])
        idxf = sb("idxf", [128, NTO])
        idxi = sb("idxi", [128, 2 * NTO], I32)
        goffm = sb("goffm", [128, 4])
        pren = sb("pren", [128, 4])
        M2 = AR.off
        woutG = sb("woutG", [128, 4, D], BF16)
        woutA = sb("woutA", [64, 8, D], BF16)
        DMA("pool", woutG, w_out[0:512, :].rearrange("(c p) n -> p c n", p=128), [], ["woutG"])
        DMA("pool", woutA, w_out[512:1024, :].rearrange("(h p) n -> p h n", p=64), [], ["woutA"])
        for g in range(4):
            P.op("dve", lambda e, g=g: e.memset(goffm[:, g:g + 1], float(g * CAPG) - OOB), writes=["goffm"])
        P.op("dve", lambda e: e.memset(pren, 0.0), writes=["pren"])
        zx = sb("zx", [128, D], BF16)
        zw = sb("zw", [128, 8])
        P.op("pool", lambda e: e.memset(zx, 0.0), writes=["zx"])
        P.op("pool", lambda e: e.memset(zw, 0.0), writes=["zw"])
        for r0 in range(0, NSLOT, 128):
            DMA("sp", XB[r0:r0 + 128, :], zx, ["zx"], ["XB"])
            DMA("sp", WB[r0:r0 + 128, :], zw, ["zw"], ["WB"])
        xo = [sb("xo%d" % i, [128, D]) for i in range(2)]
        mgl = [sb("mgl%d" % i, [128, 512], BF16) for i in range(2)]
        otl = [sb("otl%d" % i, [64, 8, 128], BF16) for i in range(2)]
        mgT = sb("mgT", [128, 4, 128], BF16)
        h2t = [sb("h2t%d" % i, [128, D]) for i in range(2)]
        u2 = sb("u2", [128, D])
        u2Tf = sb("u2Tf", [128, 8, 128])
        junk2 = sb("junk2", [128, D], BF16)
        ss2 = sb("ss2", [128, 2])
        rs2 = sb("rs2", [128, 2])
        lg = sb("lg", [128, 36])
        sm = sb("sm", [128, 64])
        for io in range(NTO):
            b2 = io % 2
            DMA("sp", xo[b2], xw[HALO + io * 128: HALO + (io + 1) * 128, :], [], ["xo%d" % b2])
            DMA("sp", mgl[b2], MG[io * 128:(io + 1) * 128, :], ["MG%d" % io], ["mgl%d" % b2])
            DMA("sp", otl[b2], OTS[:, :, io * 128:(io + 1) * 128].rearrange("h p t -> p h t"), OTK, ["otl%d" % b2])
            pbt, pbk = next_pb()

            def trm_fn(e, pbt=pbt, b2=b2):
                ins = None
                for c in range(4):
                    ins = e.transpose(out=pbt[:, c * 128:(c + 1) * 128], in_=mgl[b2][:, c * 128:(c + 1) * 128], identity=identb)
                return ins
            P.op("pe", trm_fn, reads=["mgl%d" % b2, "identb"], writes=[pbk])
            CP("act", mgT.rearrange("p c t -> p (c t)"), pbt[:, 0:512], [pbk], ["mgT"])
            for cg in range(2):
                pt, pk = next_pf()
                pairs = [(mgT[:, c, :], woutG[:, c, cg * 512:(cg + 1) * 512]) for c in range(4)] + \
                        [(otl[b2][:, h, :], woutA[:, h, cg * 512:(cg + 1) * 512]) for h in range(8)]
                mm_group(pt[:, :], pairs, pk, ["mgT", "otl%d" % b2, "woutG", "woutA"])
                TT("dve", h2t[b2][:, cg * 512:(cg + 1) * 512], pt[:, :], xo[b2][:, cg * 512:(cg + 1) * 512], ALU.add,
                   [pk, "xo%d" % b2], ["h2t%d" % b2])
            DMA("sp", H2[io * 128:(io + 1) * 128, :], h2t[b2], ["h2t%d" % b2], ["H2_%d" % io])
            P.op("act", lambda e, b2=b2: e.activation(out=junk2, in_=h2t[b2], func=AF.Square, accum_out=ss2[:, 0:1]),
                 reads=["h2t%d" % b2], writes=["junk2", "ss2"])
            rstd_from_ssq(rs2[:, 0:1], ss2[:, 0:1], D, "ss2", "rs2")
            STT(u2, h2t[b2], rs2[:, 0:1], n2bc, ALU.mult, ALU.mult, ["h2t%d" % b2, "rs2", "n2bc"], ["u2"])
            CP("pool", u2tok[:, io, :], u2, ["u2"], ["u2tok%d" % io])
            for half in range(2):
                pt, pk = next_pf()

                def tru_fn(e, pt=pt, half=half):
                    ins = None
                    for c in range(4):
                        cc = half * 4 + c
                        ins = e.transpose(out=pt[:, c * 128:(c + 1) * 128], in_=u2[:, cc * 128:(cc + 1) * 128], identity=cm[:, 0, :])
                    return ins
                P.op("pe", tru_fn, reads=["u2", "cm"], writes=[pk])
                CP("act", u2Tf[:, half * 4:half * 4 + 4, :].rearrange("p c t -> p (c t)"), pt[:, :], [pk], ["u2Tf%d" % half])
            pr_, prk = next_pf()
            mm_group(pr_[:, 0:36], [(u2Tf[:, c, :], wr[:, c, :]) for c in range(8)], prk, ["u2Tf0", "u2Tf1", "wr"])
            TT("dve", lg, pr_[:, 0:36], rbbc, ALU.add, [prk, "rbbc"], ["lg"])
            gmax, ngmax, gsum, gw = sm[:, 0:1], sm[:, 1:2], sm[:, 2:3], sm[:, 3:4]
            oh = OH[:, io, :]
            ohk = "OH%d" % io
            ge = sm[:, 8:12]
            esel = sm[:, 16:24]
            top8 = sm[:, 24:32]
            d21, w1g, w2g = sm[:, 32:33], sm[:, 33:34], sm[:, 34:35]
            wa = sm[:, 40:48]
            wb_ = sm[:, 48:56]
            P.op("dve", lambda e: e.tensor_reduce(out=gmax, in_=lg[:, 0:4], axis=AX.X, op=ALU.max), reads=["lg"], writes=["sm"])
            TS("dve", oh, lg[:, 0:4], gmax, None, ALU.is_equal, None, ["lg", "sm"], [ohk])
            TS("dve", ngmax, gmax, -1.0, None, ALU.mult, None, ["sm"], ["sm"])
            ACT(ge, lg[:, 0:4], AF.Exp, ["lg", "sm"], ["sm"], bias=ngmax, scale=1.0)
            P.op("dve", lambda e: e.tensor_reduce(out=gsum, in_=ge, axis=AX.X, op=ALU.add), reads=["sm"], writes=["sm"])
            P.op("dve", lambda e: e.reciprocal(out=gw, in_=gsum), reads=["sm"], writes=["sm"])
            TS("dve", esel, lg[:, 4:12], oh[:, 0:1], None, ALU.mult, None, ["lg", ohk], ["sm"])
            for g in range(1, 4):
                STT(esel, lg[:, 4 + 8 * g:12 + 8 * g], oh[:, g:g + 1], esel, ALU.mult, ALU.add, ["lg", ohk, "sm"], ["sm"])
            P.op("dve", lambda e: e.max(out=top8, in_=esel), reads=["sm"], writes=["sm"])
            TT("dve", d21, top8[:, 1:2], top8[:, 0:1], ALU.subtract, ["sm"], ["sm"])
            ACT(d21, d21, AF.Exp, ["sm"], ["sm"])
            TS("dve", d21, d21, 1.0, None, ALU.add, None, ["sm"], ["sm"])
            P.op("dve", lambda e: e.reciprocal(out=w1g, in_=d21), reads=["sm"], writes=["sm"])
            TT("dve", w1g, w1g, gw, ALU.mult, ["sm"], ["sm"])
            TT("dve", w2g, gw, w1g, ALU.subtract, ["sm"], ["sm"])
            TS("dve", wa, esel, top8[:, 0:1], w1g, ALU.is_equal, ALU.mult, ["sm"], ["sm"])
            TS("dve", wb_, esel, top8[:, 1:2], w2g, ALU.is_equal, ALU.mult, ["sm"], ["sm"])
            TT("dve", WE[:, io, :], wa, wb_, ALU.add, ["sm"], ["WE%d" % io])
            prk_t, prkk = next_pf()
            P.op("pe", lambda e, t=prk_t, io=io: (e.matmul(t[:, 0:4], lhsT=cm[:, 4, :], rhs=OH[:, io, :], start=True, stop=False),
                                                   e.matmul(t[:, 0:4], lhsT=cm[:, 7, :], rhs=pren, start=False, stop=True))[1],
                 reads=[ohk, "pren", "cm"], writes=[prkk])
            rk = sm[:, 56:60]
            okm = sm[:, 60:64]
            TS("dve", rk, prk_t[:, 0:4], -16.0, None, ALU.mult, None, [prkk], ["sm"])
            STT(pren, oh, -1.0 / 16.0, pren, ALU.mult, ALU.add, [ohk, "pren", prkk], ["pren"])
            TS("dve", okm, rk, float(CAPG), None, ALU.is_lt, None, ["sm"], ["sm"])
            TT("dve", okm, okm, oh, ALU.mult, ["sm", ohk], ["sm"])
            TT("dve", rk, rk, goffm, ALU.add, ["sm", "goffm"], ["sm"])
            TT("dve", rk, rk, okm, ALU.mult, ["sm"], ["sm"])
            P.op("dve", lambda e, io=io: e.tensor_reduce(out=idxf[:, io:io + 1], in_=rk, axis=AX.X, op=ALU.add), reads=["sm"], writes=["idxf%d" % io])
            TS("dve", idxf[:, io:io + 1], idxf[:, io:io + 1], OOB, None, ALU.add, None, ["idxf%d" % io], ["idxf%d" % io])
            CP("dve", idxi[:, io:io + 1], idxf[:, io:io + 1], ["idxf%d" % io], ["idxi%d" % io])
            P.op("pool", lambda e, io=io: e.indirect_dma_start(out=XB[:, :], out_offset=bass.IndirectOffsetOnAxis(ap=idxi[:, io:io + 1], axis=0),
                                                               in_=u2tok[:, io, :], in_offset=None, bounds_check=bcreg(e), oob_is_err=False),
                 reads=["u2tok%d" % io, "idxi%d" % io, "XB"], writes=["XBs%d" % io], dma=True)
            P.op("pool", lambda e, io=io: e.indirect_dma_start(out=WB[:, :], out_offset=bass.IndirectOffsetOnAxis(ap=idxi[:, io:io + 1], axis=0),
                                                               in_=WE[:, io, :], in_offset=None, bounds_check=bcreg(e), oob_is_err=False),
                 reads=["WE%d" % io, "idxi%d" % io, "WB"], writes=["WBs%d" % io], dma=True)
        H2K = ["H2_%d" % i for i in range(NTO)]
        XBK = ["XBs%d" % i for i in range(NTO)] + ["XB"]
        WBK = ["WBs%d" % i for i in range(NTO)] + ["WB"]
        if debug:
            DMA("sp", WTD[:, 0:NTO], idxf, ["idxf%d" % i for i in range(NTO)], ["WTD"])
        if stop_after == "C1":
            P.op("sp", None, reads=H2K + XBK + WBK + ["WTD"], writes=[])
            P.emit()
            return nc

        P.barrier()
        AR.off = M2
        NCH = CAPG // 128
        xs = sb("xs", [128, NCH, D], BF16)
        xTg = sb("xTg", [128, 8, CAPG], BF16)
        wsl = sb("wsl", [128, NCH, 8])
        hid = sb("hid", [128, 4, CAPG], BF16)
        yacc = sb("yacc", [128, NCH, D])
        wgb = [sb("wgb%d" % i, [128, 8, 512], BF16) for i in range(2)]
        wub = [sb("wub%d" % i, [128, 8, 512], BF16) for i in range(2)]
        wdb = [sb("wdb%d" % i, [128, 4, D], BF16) for i in range(2)]
        sgb = [sb("sgb%d" % i, [128, 512]) for i in range(2)]

        def load_expert(ex):
            b = ex % 2
            DMA("pool", wgb[b], ewg[ex].rearrange("(c p) n -> p c n", p=128), [], ["wgb%d" % b])
            DMA("pool", wub[b], ewu[ex].rearrange("(c p) n -> p c n", p=128), [], ["wub%d" % b])
            DMA("pool", wdb[b], ewd[ex].rearrange("(c p) n -> p c n", p=128), [], ["wdb%d" % b])
        load_expert(0)
        kk2 = 0
        nsl = [(0, 512), (512, CAPG)]
        for g in range(4):
            DMA("sp", xs, XB[g * CAPG:(g + 1) * CAPG, :].rearrange("(c p) d -> p c d", p=128), XBK, ["xs"])
            DMA("sp", wsl, WB[g * CAPG:(g + 1) * CAPG, :].rearrange("(c p) d -> p c d", p=128), WBK, ["wsl"])
            for ch in range(NCH):
                pbt, pbk = next_pb()

                def trx_fn(e, pbt=pbt, ch=ch):
                    ins = None
                    for c in range(8):
                        ins = e.transpose(out=pbt[:, c * 128:(c + 1) * 128], in_=xs[:, ch, c * 128:(c + 1) * 128], identity=identb)
                    return ins
                P.op("pe", trx_fn, reads=["xs", "identb"], writes=[pbk])
                CP("act" if ch % 2 else "dve", xTg[:, :, ch * 128:(ch + 1) * 128], pbt[:, :].rearrange("p (c t) -> p c t", c=8), [pbk], ["xTg%d" % ch])
            XTK = ["xTg%d" % ch for ch in range(NCH)]
            for el in range(8):
                ex = g * 8 + el
                b = ex % 2
                if ex + 1 < NEXP:
                    load_expert(ex + 1)
                for (n0, n1) in nsl:
                    for fc in range(4):
                        pg, pgk = next_pf()
                        pu, puk = next_pf()
                        mm_group(pg[:, 0:n1 - n0], [(wgb[b][:, c, fc * 128:(fc + 1) * 128], xTg[:, c, n0:n1]) for c in range(8)], pgk,
                                 ["wgb%d" % b] + XTK)
                        mm_group(pu[:, 0:n1 - n0], [(wub[b][:, c, fc * 128:(fc + 1) * 128], xTg[:, c, n0:n1]) for c in range(8)], puk,
                                 ["wub%d" % b] + XTK)
                        sb_i = kk2 % 2
                        kk2 += 1
                        ACT(sgb[sb_i][:, 0:n1 - n0], pg[:, 0:n1 - n0], AF.Silu, [pgk], ["sgb%d" % sb_i])
                        TT("dve", hid[:, fc, n0:n1], sgb[sb_i][:, 0:n1 - n0], pu[:, 0:n1 - n0], ALU.mult, ["sgb%d" % sb_i, puk], ["hid%d_%d" % (fc, n0)])
                HK = ["hid%d_%d" % (fc, n0) for fc in range(4) for (n0, _) in nsl]
                for ch in range(NCH):
                    for cg in range(2):
                        py, pyk = next_pf()
                        mm_group(py[:, :], [(hid[:, fc, ch * 128:(ch + 1) * 128], wdb[b][:, fc, cg * 512:(cg + 1) * 512]) for fc in range(4)], pyk,
                                 ["wdb%d" % b] + HK)
                        ya = yacc[:, ch, cg * 512:(cg + 1) * 512]
                        yk = "yacc%d" % ch
                        if el == 0:
                            TS("dve", ya, py[:, :], wsl[:, ch, el:el + 1], None, ALU.mult, None, [pyk, "wsl"], [yk])
                        else:
                            STT(ya, py[:, :], wsl[:, ch, el:el + 1], ya, ALU.mult, ALU.add, [pyk, "wsl", yk], [yk])
            DMA("sp", YB[g * CAPG:(g + 1) * CAPG, :].rearrange("(c p) d -> p c d", p=128), yacc, ["yacc%d" % ch for ch in range(NCH)], ["YB%d" % g])
        YBK = ["YB%d" % g for g in range(4)]
        P.barrier()
        AR.off = M2
        hl = [sb("hl%d" % i, [128, D]) for i in range(2)]
        yg = [sb("yg%d" % i, [128, D]) for i in range(2)]
        ob = [sb("ob%d" % i, [128, D]) for i in range(2)]
        junk3 = sb("junk3", [128, D], BF16)
        ss3 = sb("ss3", [128, 2])
        rs3 = sb("rs3", [128, 2])
        for io in range(NTO):
            b2 = io % 2
            DMA("sp", hl[b2], H2[io * 128:(io + 1) * 128, :], ["H2_%d" % io], ["hl%d" % b2])
            P.op("pool", lambda e, b2=b2: e.memset(yg[b2], 0.0), writes=["yg%d" % b2])
            P.op("pool", lambda e, io=io, b2=b2: e.indirect_dma_start(out=yg[b2], out_offset=None, in_=YB[:, :],
                                                                       in_offset=bass.IndirectOffsetOnAxis(ap=idxi[:, io:io + 1], axis=0),
                                                                       bounds_check=bcreg(e), oob_is_err=False),
                 reads=YBK + ["idxi%d" % io], writes=["yg%d" % b2], dma=True)
            TT("dve", hl[b2], hl[b2], yg[b2], ALU.add, ["hl%d" % b2, "yg%d" % b2], ["hl%d" % b2])
            P.op("act", lambda e, b2=b2: e.activation(out=junk3, in_=hl[b2], func=AF.Square, accum_out=ss3[:, 0:1]),
                 reads=["hl%d" % b2], writes=["junk3", "ss3"])
            rstd_from_ssq(rs3[:, 0:1], ss3[:, 0:1], D, "ss3", "rs3")
            STT(ob[b2], hl[b2], rs3[:, 0:1], fnbc, ALU.mult, ALU.mult, ["hl%d" % b2, "rs3", "fnbc"], ["ob%d" % b2])
            DMA("sp", out_d[io * 128:(io + 1) * 128, :], ob[b2], ["ob%d" % b2], ["OUT%d" % io])
        P.op("sp", None, reads=["OUT%d" % i for i in range(NTO)], writes=[])
        P.emit()
    return nc


def _consts():
    s = np.arange(128)[:, None]
    t = np.arange(128)[None, :]
    cm = np.zeros((128, 8, 128), np.float32)
    cm[:, 0] = (s == t)
    cm[:, 1] = (s <= t) / -16.0
    cm[:, 2] = (s >= t) / -16.0
    cm[:, 3] = (s > t) / -16.0
    cm[:, 4] = (s < t) / -16.0
    cm[:, 5] = (s <= t)
    cm[:, 6] = (s >= t)
    cm[:, 7] = 1.0
    band = np.zeros((128, 384), np.float32)
    band[:, 0:128] = (s >= t + 64)
    band[:, 128:256] = (np.abs(s - t) <= 64)
    band[:, 256:384] = (s <= t - 64)
    return cm, band


def make_in_maps(inputs):
    f = lambda a: np.ascontiguousarray(np.asarray(a, dtype=np.float32))
    x = f(inputs["x"])
    cm, band = _consts()
    wz = np.zeros((33, 512), np.float32)
    wz[0:16, 0:256] = f(inputs["gla_fwd_gate_w"])[0]
    wz[16:32, 256:512] = f(inputs["gla_bwd_gate_w"])[0]
    wz[32, 0:256] = f(inputs["gla_fwd_gate_b"])[0]
    wz[32, 256:512] = f(inputs["gla_bwd_gate_b"])[0]
    vecs = np.zeros((4, D), np.float32)
    vecs[0] = f(inputs["norm1_w"])[0]
    vecs[1] = f(inputs["norm2_w"])[0]
    vecs[2] = f(inputs["final_norm_w"])
    vecs[3] = np.tile(f(inputs["gla_norm_w"])[0], 8)
    wr = np.concatenate([f(inputs["router_group_w"])[0]] + [f(inputs["router_expert_w"])[0, g] for g in range(4)], axis=1)
    rb = np.concatenate([f(inputs["router_group_b"])[0], f(inputs["router_expert_b"])[0].reshape(-1)])[None, :]
    inv = (500000.0 ** (-(np.arange(0, 16, 2, dtype=np.float32) / np.float32(16)))).astype(np.float32)
    shared = dict(cmat=cm, band3=band, w_in=f(inputs["w_in"])[0], wz=wz, vecs=vecs, w_out=f(inputs["w_out"])[0],
                  wr=np.ascontiguousarray(wr), rb=np.ascontiguousarray(rb), ewg=f(inputs["expert_w_gate"])[0],
                  ewu=f(inputs["expert_w_up"])[0], ewd=f(inputs["expert_w_down"])[0])
    maps = []
    for c in range(8):
        b, q = c // 4, c % 4
        s0 = q * OWN
        pos = np.arange(s0 - HALO, s0 + OWN + HALO)
        valid = (pos >= 0) & (pos < S)
        xwin = np.zeros((WIN, D), np.float32)
        xwin[valid] = x[b, pos[valid]]
        ang = (pos.astype(np.float32)[:, None] * inv[None, :]).astype(np.float32)
        cs = np.concatenate([np.cos(ang), np.sin(ang)], axis=1).astype(np.float32)
        cs_t = np.ascontiguousarray(cs.reshape(NTW, 128, 16).transpose(1, 0, 2))
        vcol = np.ascontiguousarray(valid.astype(np.float32).reshape(NTW, 128).T)
        m = dict(shared)
        m.update(xw=xwin, vcol=vcol, cs_t=cs_t)
        maps.append(m)
    return maps


_NC_CACHE = {}


def kernel(**inputs):
    maps = make_in_maps(inputs)
    if "nc" not in _NC_CACHE:
        _NC_CACHE["nc"] = build_program()
    nc = _NC_CACHE["nc"]
    res = run_bass_kernel_spmd(nc, maps, core_ids=list(range(8)))
    out = np.zeros((2, S, D), np.float32)
    for c in range(8):
        b, q = c // 4, c % 4
        out[b, q * OWN:(q + 1) * OWN] = res.results[c]["out"]
    return out
```

```python
import numpy as np
from contextlib import ExitStack
import concourse.bass as bass
import concourse.mybir as mybir
from concourse.bass_utils import run_bass_kernel_spmd

F32 = mybir.dt.float32
BF16 = mybir.dt.bfloat16
I32 = mybir.dt.int32
AF = mybir.ActivationFunctionType
ALU = mybir.AluOpType
AX = mybir.AxisListType

ENGS = ("pe", "act", "dve", "pool", "sp")
EPOCH = 4096
DMA_SLOTS = 8

D = 1024
S = 8192
OWN = 2048
HALO = 1024
WIN = OWN + 2 * HALO
NTW = WIN // 128
T0 = HALO // 128
NTO = OWN // 128
INW = 3104
NEXP = 32
EPS = 1e-6


class Op:
    __slots__ = ("eng", "fn", "dma", "deps", "sig", "sigcount", "dmaidx", "idx")

    def __init__(self, eng, fn, dma):
        self.eng = eng
        self.fn = fn
        self.dma = dma
        self.deps = []
        self.sig = False
        self.sigcount = 0
        self.dmaidx = -1
        self.idx = -1


class Prog:
    def __init__(self, nc):
        self.nc = nc
        self.ops = []
        self.last_w = {}
        self.readers = {}
        self.ndma = {e: 0 for e in ENGS}
        self.bar = None

    def barrier(self):
        deps = set()
        for e in ENGS:
            last = None
            nd = 0
            for o in reversed(self.ops):
                if o.eng != e:
                    continue
                if o.dma:
                    if nd < DMA_SLOTS:
                        deps.add(o.idx)
                        nd += 1
                elif last is None:
                    last = o.idx
                    deps.add(o.idx)
                if last is not None and nd >= DMA_SLOTS:
                    break
        b = self.op("sp", None)
        b.deps = sorted(deps | set(b.deps))
        self.bar = b.idx
        return b

    def op(self, eng, fn, reads=(), writes=(), dma=False):
        import os as _os
        mx = int(_os.environ.get("DBG_MAXOPS", "0"))
        if mx and len(self.ops) >= mx and fn is not None:
            fn = None
            if dma:
                dma = False
        px = [k_ for k_ in reads if k_[:2] in ("pf", "pb")]
        if px:
            writes = list(writes) + [k_ for k_ in px if k_ not in writes]
            reads = [k_ for k_ in reads if k_ not in px]
        o = Op(eng, fn, dma)
        o.idx = len(self.ops)
        deps = set()
        if self.bar is not None:
            deps.add(self.bar)
        for k in reads:
            w = self.last_w.get(k)
            if w is not None:
                deps.add(w)
        for k in writes:
            w = self.last_w.get(k)
            if w is not None:
                deps.add(w)
            for r in self.readers.get(k, ()):
                deps.add(r)
        deps.discard(o.idx)
        o.deps = sorted(deps)
        for k in writes:
            self.last_w[k] = o.idx
            self.readers[k] = []
        for k in reads:
            if k not in writes:
                self.readers.setdefault(k, []).append(o.idx)
        if dma:
            o.dmaidx = self.ndma[eng]
            self.ndma[eng] += 1
        self.ops.append(o)
        return o

    def emit(self):
        nc = self.nc
        ops = self.ops
        for o in ops:
            for d in o.deps:
                p = ops[d]
                if not p.dma:
                    p.sig = True
        cnt = {e: 0 for e in ENGS}
        for o in ops:
            if o.sig and not o.dma:
                cnt[o.eng] += 1
                o.sigcount = cnt[o.eng]
        nsem = {e: (cnt[e] + EPOCH - 1) // EPOCH for e in ENGS}
        with ExitStack() as es:
            csem = {e: [es.enter_context(nc.semaphore("c_%s_%d" % (e, i))) for i in range(nsem[e])]
                    for e in ENGS}
            dsem = {e: [es.enter_context(nc.semaphore("d_%s_%d" % (e, i)))
                        for i in range(DMA_SLOTS if self.ndma[e] else 0)] for e in ENGS}
            block = es.enter_context(nc.Block())

            def body_for(e):
                def body(eng):
                    waited_c = {x: 0 for x in ENGS}
                    waited_d = {}
                    for o in ops:
                        if o.eng != e:
                            continue
                        need_c = {}
                        need_d = {}
                        for d in o.deps:
                            p = ops[d]
                            if p.dma:
                                slot = p.dmaidx % DMA_SLOTS
                                val = 16 * (p.dmaidx // DMA_SLOTS + 1)
                                key = (p.eng, slot)
                                if waited_d.get(key, 0) < val:
                                    need_d[key] = max(need_d.get(key, 0), val)
                            else:
                                if waited_c[p.eng] < p.sigcount:
                                    need_c[p.eng] = max(need_c.get(p.eng, 0), p.sigcount)
                        if o.dma:
                            slot = o.dmaidx % DMA_SLOTS
                            val = 16 * (o.dmaidx // DMA_SLOTS)
                            key = (e, slot)
                            if val > 0 and waited_d.get(key, 0) < val:
                                need_d[key] = max(need_d.get(key, 0), val)
                        for pe_, c in need_c.items():
                            ep = (c - 1) // EPOCH
                            eng.wait_ge(csem[pe_][ep], (c - 1) % EPOCH + 1)
                            waited_c[pe_] = c
                        for key, val in need_d.items():
                            eng.wait_ge(dsem[key[0]][key[1]], val)
                            waited_d[key] = val
                        ins = o.fn(eng) if o.fn is not None else None
                        if o.dma:
                            ins.then_inc(dsem[e][o.dmaidx % DMA_SLOTS], 16)
                        elif o.sig:
                            if ins is None:
                                ins = eng.nop()
                            ep = (o.sigcount - 1) // EPOCH
                            ins.then_inc(csem[e][ep], 1)
                return body

            block.tensor(body_for("pe"))
            block.scalar(body_for("act"))
            block.vector(body_for("dve"))
            block.gpsimd(body_for("pool"))
            block.sync(body_for("sp"))


class Arena:
    def __init__(self, ap, ncols):
        self.ap = ap
        self.n = ncols
        self.off = 0

    def alloc(self, shape, dt=F32):
        p = shape[0]
        rest = list(shape[1:])
        nel = 1
        for r in rest:
            nel *= r
        ncol = nel if dt in (F32, I32) else (nel + 1) // 2
        ncol += ncol % 2
        assert self.off + ncol <= self.n, "arena overflow: need %d have %d" % (ncol, self.n - self.off)
        v = self.ap[0:p, self.off:self.off + ncol]
        self.off += ncol
        if dt != F32:
            v = v.bitcast(dt)
        if v.shape[1] != nel:
            v = v[:, 0:nel]
        if len(rest) == 2:
            v = v.rearrange("p (a b) -> p a b", a=rest[0])
        elif len(rest) == 3:
            v = v.rearrange("p (a b c) -> p a b c", a=rest[0], b=rest[1])
        return v


def build_program(debug=False, stop_after=None, dbg_tiles=None):
    nc = bass.Bass("TRN2", target_bir_lowering=False)
    P = Prog(nc)
    global LASTP
    LASTP = P

    def din(name, shape, dt=F32):
        return nc.dram_tensor(name, list(shape), dt, kind="ExternalInput").ap()

    def dscr(name, shape, dt):
        kind = "ExternalOutput" if debug else "Internal"
        return nc.dram_tensor(name, list(shape), dt, kind=kind).ap()

    xw = din("xw", [WIN, D])
    vcol = din("vcol", [128, NTW])
    cs_t = din("cs_t", [128, NTW, 16])
    cmat = din("cmat", [128, 8, 128])
    band3 = din("band3", [128, 384])
    w_in = din("w_in", [D, INW])
    wz_d = din("wz", [33, 512])
    vecs = din("vecs", [4, D])
    w_out = din("w_out", [D, D])
    wr_d = din("wr", [D, 36])
    rb_d = din("rb", [1, 36])
    ewg = din("ewg", [NEXP, D, 512])
    ewu = din("ewu", [NEXP, D, 512])
    ewd = din("ewd", [NEXP, 512, D])
    out_d = nc.dram_tensor("out", [OWN, D], F32, kind="ExternalOutput").ap()
    QS = dscr("QS", [OWN, 512], BF16)
    KS = dscr("KS", [WIN + 2 * HALO, 512], BF16)
    VS = dscr("VS", [WIN + 2 * HALO, 520], BF16)
    GV = dscr("GV", [OWN, 512], BF16)
    GG = dscr("GG", [OWN, 512], BF16)
    MG = dscr("MG", [OWN, 512], BF16)
    OTS = dscr("OTS", [8, 64, OWN], BF16)
    H2 = dscr("H2", [OWN, D], F32)
    XB = dscr("XB", [2560, D], BF16)
    WB = dscr("WB", [2560, 8], F32)
    YB = dscr("YB", [2560, D], F32)
    WTD = nc.dram_tensor("WTD", [128, NTO * 32], F32, kind="ExternalOutput").ap() if debug else None

    QSK = ["QS%d" % i for i in range(NTO)]
    KSK = ["KS%d" % i for i in range(48)]
    VSK = ["VS%d" % i for i in range(48)]
    NCOL = 50 * 1024 + 512
    es = ExitStack()
    with es:
        arena_t = es.enter_context(nc.sbuf_tensor("arena", [128, NCOL], F32))
        AR = Arena(arena_t[:], NCOL)
        sb = lambda name, shape, dt=F32: AR.alloc(shape, dt)

        def ps(name, shape, dt=F32):
            return es.enter_context(nc.psum_tensor("p_" + name, list(shape), dt))

        pf = [ps("pf%d" % i, [128, 512]) for i in range(6)]
        pb = [ps("pb%d" % i, [128, 1024], BF16) for i in range(2)]
        pf_rr = [0]
        pb_rr = [0]

        def next_pf():
            i = pf_rr[0] % 6
            pf_rr[0] += 1
            return pf[i], "pf%d" % i

        def next_pb():
            i = pb_rr[0] % 2
            pb_rr[0] += 1
            return pb[i], "pb%d" % i

        def mm_group(out_ap, pairs, okey, rkeys):
            def fn(e):
                ins = None
                n = len(pairs)
                for j, (l, r) in enumerate(pairs):
                    ins = e.matmul(out_ap, lhsT=l, rhs=r, start=(j == 0), stop=(j == n - 1))
                return ins
            P.op("pe", fn, reads=rkeys, writes=[okey])

        def ACT(out, in_, func, reads, writes, **kw):
            P.op("act", lambda e: e.activation(out=out, in_=in_, func=func, **kw), reads=reads, writes=writes)

        def TT(eng, out, in0, in1, op, reads, writes):
            P.op(eng, lambda e: e.tensor_tensor(out=out, in0=in0, in1=in1, op=op), reads=reads, writes=writes)

        def STT(out, in0, scalar, in1, op0, op1, reads, writes):
            P.op("dve", lambda e: e.scalar_tensor_tensor(out=out, in0=in0, scalar=scalar, in1=in1, op0=op0, op1=op1),
                 reads=reads, writes=writes)

        def TS(eng, out, in0, s1, s2, op0, op1, reads, writes):
            if op1 is None:
                P.op(eng, lambda e: e.tensor_scalar(out=out, in0=in0, scalar1=s1, scalar2=None, op0=op0), reads=reads, writes=writes)
            else:
                P.op(eng, lambda e: e.tensor_scalar(out=out, in0=in0, scalar1=s1, scalar2=s2, op0=op0, op1=op1), reads=reads, writes=writes)

        def CP(eng, out, in_, reads, writes):
            if eng == "act":
                ACT(out, in_, AF.Copy, reads, writes)
            else:
                P.op(eng, lambda e: e.tensor_copy(out=out, in_=in_), reads=reads, writes=writes)

        def DMA(q, out, in_, reads, writes):
            return P.op(q, lambda e: e.dma_start(out=out, in_=in_), reads=reads, writes=writes, dma=True)

        def rstd_from_ssq(dst, src, n, rk, wk):
            ACT(dst, src, AF.Ln, [rk, "epsc"], [wk], scale=1.0 / n, bias=epsc[0:dst.shape[0], :])
            ACT(dst, dst, AF.Exp, [wk], [wk], scale=-0.5)

        cm = sb("cm", [128, 8, 128])
        identb = sb("identb", [128, 128], BF16)
        band = sb("band", [128, 384], BF16)
        maskFB = sb("maskFB", [128, 4, 128])
        n16col = sb("n16col", [128, 2])
        epsc = sb("epsc", [128, 2])
        onec = sb("onec", [128, 2])
        negc = sb("negc", [128, 2])
        vc = sb("vc", [128, NTW])
        cst = sb("cst", [128, NTW, 16])
        wz = sb("wz", [33, 512])
        n1bc = sb("n1bc", [128, D])
        n2bc = sb("n2bc", [128, D])
        fnbc = sb("fnbc", [128, D])
        gnbc = sb("gnbc", [128, 512])
        rbbc = sb("rbbc", [128, 36])
        wr = sb("wr", [128, 8, 36])
        nmax = sb("nmax", [128, 16])
        n16col = n16col[:, 0:1]
        epsc = epsc[:, 0:1]
        onec = onec[:, 0:1]
        negc = negc[:, 0:1]

        DMA("sp", cm, cmat, [], ["cm"])
        DMA("pool", identb, cmat[:, 0, :], [], ["identb"])
        DMA("pool", band, band3, [], ["band"])
        DMA("sp", vc, vcol, [], ["vc"])
        DMA("sp", cst, cs_t, [], ["cst"])
        DMA("sp", wz, wz_d, [], ["wz"])
        DMA("sp", n1bc, vecs[0:1, :].partition_broadcast(128), [], ["n1bc"])
        DMA("sp", n2bc, vecs[1:2, :].partition_broadcast(128), [], ["n2bc"])
        DMA("sp", fnbc, vecs[2:3, :].partition_broadcast(128), [], ["fnbc"])
        DMA("sp", gnbc, vecs[3:4, 0:512].partition_broadcast(128), [], ["gnbc"])
        DMA("sp", rbbc, rb_d[0:1, :].partition_broadcast(128), [], ["rbbc"])
        DMA("sp", wr, wr_d.rearrange("(c p) n -> p c n", p=128), [], ["wr"])
        P.op("dve", lambda e: e.memset(n16col, -1.0 / 16.0), writes=["n16col"])
        P.op("dve", lambda e: e.memset(epsc, EPS), writes=["epsc"])
        P.op("dve", lambda e: e.memset(onec, 1.0), writes=["onec"])
        P.op("dve", lambda e: e.memset(nmax, 0.0), writes=["nmax"])
        for h in range(4):
            CP("dve", maskFB[:, h, :], cm[:, 5 + h // 2, :], ["cm"], ["maskFB"])
        M0 = AR.off

        attnT = sb("attnT", [128, NTO, 512], BF16)
        qdT = sb("qdT", [128, NTO, 4, 128], BF16)
        SfT = sb("SfT", [128, NTO, 2, 128], BF16)
        SbT = sb("SbT", [128, NTO, 2, 128], BF16)
        M1 = AR.off
        win = sb("win", [128, 8, INW], BF16)
        for c in range(8):
            DMA("pool", win[:, c, :], w_in[c * 128:(c + 1) * 128, :], [], ["win%d" % c])
        winkeys = ["win%d" % c for c in range(8)]
        kvB = sb("kvB", [128, NTW - T0, 2, 128], BF16)
        decB = sb("decB", [128, NTW - T0, 2])
        Sf = sb("Sf", [128, 2, 128])
        Sb = sb("Sb", [128, 2, 128])
        P.op("dve", lambda e: e.memset(Sf, 0.0), writes=["Sf"])
        P.op("dve", lambda e: e.memset(Sb, 0.0), writes=["Sb"])
        zt = sb("zt", [128, 520], BF16)
        P.op("pool", lambda e: e.memset(zt, 0.0), writes=["zt"])
        for blk in range(HALO // 128):
            for base in (0, HALO + WIN):
                r0 = base + blk * 128
                DMA("sp", KS[r0:r0 + 128, :], zt[:, 0:512], ["zt"], ["KS%d" % (r0 // 128)])
                DMA("sp", VS[r0:r0 + 128, :], zt, ["zt"], ["VS%d" % (r0 // 128)])
        xt = [sb("xt%d" % i, [128, D]) for i in range(2)]
        junk = sb("junk", [128, D], BF16)
        xn = [sb("xn%d" % i, [128, D], BF16) for i in range(2)]
        xnT = [sb("xnT%d" % i, [128, 8, 128], BF16) for i in range(2)]
        ssq = sb("ssq", [128, 2])
        rstd = sb("rstd", [128, 2])
        qk = [sb("qk%d" % i, [128, 512]) for i in range(2)]
        vbf = [sb("vbf%d" % i, [128, 512], BF16) for i in range(2)]
        gbf = [sb("gbf%d" % i, [128, 512], BF16) for i in range(2)]
        lr = [sb("lr%d" % i, [128, 32]) for i in range(2)]
        aqr = [sb("aqr%d" % i, [128, 8, 64]) for i in range(2)]
        akr = [sb("akr%d" % i, [128, 8, 64]) for i in range(2)]
        vab = [sb("vab%d" % i, [128, 8, 65], BF16) for i in range(2)]
        lrT = sb("lrT", [33, 128])
        ez = sb("ez", [128, 512])
        spl = sb("spl", [128, 512])
        E1 = sb("E1", [128, 512])
        E2 = sb("E2", [128, 512])
        E3 = sb("E3", [128, 512])
        dec = sb("dec", [128, 4])
        qd = sb("qd", [128, 512], BF16)
        ki = sb("ki", [128, 512], BF16)
        ke = sb("ke", [128, 512], BF16)
        kiT = sb("kiT", [128, 4, 128], BF16)
        qrb = [sb("qrb%d" % i, [128, 512], BF16) for i in range(2)]
        krb = [sb("krb%d" % i, [128, 512], BF16) for i in range(2)]
        rta = sb("rta", [128, 8, 8])
        rtb = sb("rtb", [128, 8, 8])
        rtc = sb("rtc", [128, 8, 8])
        rtd = sb("rtd", [128, 8, 8])
        sqs = sb("sqs", [128, 8, 64])
        nrm = sb("nrm", [128, 16])
        P.op("dve", lambda e: e.memset(lrT[32:33, :], 1.0), writes=["lrT_one"])
        tiles = list(range(NTW) if dbg_tiles is None else dbg_tiles)

        def S1(i):
            own = T0 <= i < T0 + NTO
            io = i - T0
            b2 = i % 2
            xtk, xnk, xnTk = "xt%d" % b2, "xn%d" % b2, "xnT%d" % b2
            if i == tiles[0]:
                DMA("sp", xt[b2], xw[i * 128:(i + 1) * 128, :], [], [xtk])
            if i + 1 < NTW and (dbg_tiles is None):
                DMA("sp", xt[(i + 1) % 2], xw[(i + 1) * 128:(i + 2) * 128, :], [], ["xt%d" % ((i + 1) % 2)])
            sk, rk = "ssq%d" % b2, "rstd%d" % b2
            P.op("act", lambda e, b2=b2: e.activation(out=junk, in_=xt[b2], func=AF.Square, accum_out=ssq[:, b2:b2 + 1]),
                 reads=[xtk], writes=["junk", sk])
            rstd_from_ssq(rstd[:, b2:b2 + 1], ssq[:, b2:b2 + 1], D, sk, rk)
            STT(xn[b2], xt[b2], rstd[:, b2:b2 + 1], n1bc, ALU.mult, ALU.mult, [xtk, rk, "n1bc"], [xnk])
            pbt, pbk = next_pb()

            def tr_fn(e, b2=b2, pbt=pbt):
                ins = None
                for c in range(8):
                    ins = e.transpose(out=pbt[:, c * 128:(c + 1) * 128], in_=xn[b2][:, c * 128:(c + 1) * 128], identity=identb)
                return ins
            P.op("pe", tr_fn, reads=[xnk, "identb"], writes=[pbk])
            CP("act", xnT[b2].rearrange("p c t -> p (c t)"), pbt[:, :], [pbk], [xnTk])

            def proj(c0, c1):
                pt, pk = next_pf()
                n = c1 - c0
                mm_group(pt[:, 0:n], [(xnT[b2][:, c, :], win[:, c, c0:c1]) for c in range(8)], pk, [xnTk] + winkeys)
                return pt, pk

            if own:
                pt, pk = proj(0, 512)
                CP("act", qk[b2], pt[:, 0:512], [pk], ["qk%d" % b2])
            else:
                pt, pk = proj(256, 512)
                CP("act", qk[b2][:, 256:512], pt[:, 0:256], [pk], ["qk%d" % b2])
            pt, pk = proj(512, 1024)
            CP("dve", vbf[b2], pt[:, 0:512], [pk], ["vbf%d" % b2])
            if own:
                DMA("sp", GV[io * 128:(io + 1) * 128, :], vbf[b2], ["vbf%d" % b2], ["GV%d" % io])
                pt, pk = proj(1024, 1536)
                CP("act", gbf[b2], pt[:, 0:512], [pk], ["gbf%d" % b2])
                DMA("sp", GG[io * 128:(io + 1) * 128, :], gbf[b2], ["gbf%d" % b2], ["GG%d" % io])
            pt, pk = proj(1536, 1568)
            CP("dve", lr[b2], pt[:, 0:32], [pk], ["lr%d" % b2])
            if own:
                pt, pk = proj(1568, 2080)
                CP("dve", aqr[b2].rearrange("p h d -> p (h d)"), pt[:, 0:512], [pk], ["aqr%d" % b2])
            pt, pk = proj(2080, 2592)
            CP("act", akr[b2].rearrange("p h d -> p (h d)"), pt[:, 0:512], [pk], ["akr%d" % b2])
            pt, pk = proj(2592, 3104)
            r0k = HALO + i * 128
            CP("act", vab[b2][:, :, 0:64], pt[:, 0:512].rearrange("p (h d) -> p h d", h=8), [pk], ["vab%d" % b2])
            CP("dve", vab[b2][:, :, 64:65], vc[:, i:i + 1].unsqueeze(1).broadcast_to([128, 8, 1]), ["vc"], ["vab%d" % b2])
            DMA("sp", VS[r0k:r0k + 128, :], vab[b2].rearrange("p h d -> p (h d)"), ["vab%d" % b2], ["VS%d" % (r0k // 128)])

        def S2(i):
            own = T0 <= i < T0 + NTO
            left = i < T0
            io = i - T0
            b2 = i % 2
            qkb = qk[b2]
            qkk = "qk%d" % b2
            vkey = "vbf%d" % b2
            vt = vbf[b2]
            pt, pk = next_pf()
            P.op("pe", lambda e, pt=pt: e.transpose(out=pt[0:32, 0:128], in_=lr[b2], identity=cm[:, 0, :]), reads=["lr%d" % b2, "cm"], writes=[pk])
            CP("dve", lrT[0:32, :], pt[0:32, 0:128], [pk], ["lrT"])
            pz, pzk = next_pf()
            P.op("pe", lambda e, pz=pz: e.matmul(pz[:, :], lhsT=lrT, rhs=wz, start=True, stop=True),
                 reads=["lrT", "lrT_one", "wz"], writes=[pzk])
            ACT(ez, pz[:, :], AF.Exp, [pzk], ["ez"], scale=-1.0)
            ACT(spl, ez, AF.Ln, ["ez", "onec"], ["spl"], bias=onec, scale=1.0)
            pbb, pbbk = next_pf()
            P.op("pe", lambda e, pbb=pbb: (e.matmul(pbb[:, 0:256], lhsT=cm[:, 1, :], rhs=spl[:, 0:256], start=True, stop=True),
                                           e.matmul(pbb[:, 256:512], lhsT=cm[:, 2, :], rhs=spl[:, 256:512], start=True, stop=True))[1],
                 reads=["cm", "spl"], writes=[pbbk])
            ACT(E1, pbb[:, :], AF.Exp, [pbbk], ["E1"])
            ACT(E2, pbb[:, :], AF.Exp, [pbbk], ["E2"], scale=-1.0)
            pb3, pb3k = next_pf()
            P.op("pe", lambda e, pb3=pb3: (e.matmul(pb3[:, 0:256], lhsT=cm[:, 3, :], rhs=spl[:, 0:256], start=True, stop=True),
                                           e.matmul(pb3[:, 256:512], lhsT=cm[:, 4, :], rhs=spl[:, 256:512], start=True, stop=True))[1],
                 reads=["cm", "spl"], writes=[pb3k])
            ACT(E3, pb3[:, :], AF.Exp, [pb3k], ["E3"])
            pdc, pdck = next_pf()

            def dec_fn(e, pdc=pdc):
                ins = None
                for j in range(4):
                    ins = e.matmul(pdc[:, j:j + 1], lhsT=spl[:, j * 128:(j + 1) * 128], rhs=n16col, start=True, stop=True)
                return ins
            P.op("pe", dec_fn, reads=["spl", "n16col"], writes=[pdck])
            ACT(dec, pdc[:, 0:4], AF.Exp, [pdck], ["dec"])
            if own:
                STT(qd[:, 0:256], qkb[:, 0:256], 0.125, E1[:, 0:256], ALU.mult, ALU.mult, [qkk, "E1"], ["qd"])
                STT(qd[:, 256:512], qkb[:, 0:256], 0.125, E1[:, 256:512], ALU.mult, ALU.mult, [qkk, "E1"], ["qd"])
                TT("pool", ki[:, 0:256], qkb[:, 256:512], E2[:, 0:256], ALU.mult, [qkk, "E2"], ["ki"])
                TT("pool", ki[:, 256:512], qkb[:, 256:512], E2[:, 256:512], ALU.mult, [qkk, "E2"], ["ki"])
            TT("pool", ke[:, 0:256], qkb[:, 256:512], E3[:, 0:256], ALU.mult, [qkk, "E3"], ["ke"])
            TT("pool", ke[:, 256:512], qkb[:, 256:512], E3[:, 256:512], ALU.mult, [qkk, "E3"], ["ke"])
            if own:
                pbt, pbk = next_pb()

                def tr2_fn(e, pbt=pbt):
                    ins = None
                    for j in range(4):
                        ins = e.transpose(out=pbt[:, j * 128:(j + 1) * 128], in_=qd[:, j * 128:(j + 1) * 128], identity=identb)
                    for j in range(4):
                        ins = e.transpose(out=pbt[:, 512 + j * 128:512 + (j + 1) * 128], in_=ki[:, j * 128:(j + 1) * 128], identity=identb)
                    return ins
                P.op("pe", tr2_fn, reads=["qd", "ki", "identb"], writes=[pbk])
                CP("act", qdT[:, io, :, :].rearrange("p c t -> p (c t)"), pbt[:, 0:512], [pbk], ["qdT%d" % io])
                CP("dve", kiT.rearrange("p c t -> p (c t)"), pbt[:, 512:1024], [pbk], ["kiT"])
                paX, paXk = next_pf()
                paY, paYk = next_pf()

                def att_fn(e, pa, par, io=io):
                    ins = None
                    p0 = par * 64
                    for dirn in range(2):
                        for pr in range(2):
                            blk = dirn * 2 + pr
                            sl = dirn * 2 + pr
                            ins = e.matmul(pa[:, sl * 128:(sl + 1) * 128], lhsT=kiT[p0:p0 + 64, blk, :], rhs=qdT[p0:p0 + 64, io, blk, :],
                                           start=True, stop=True)
                    return ins
                P.op("pe", lambda e, pa=paX, f=att_fn: f(e, pa, 0), reads=["kiT", "qdT%d" % io], writes=[paXk])
                P.op("pe", lambda e, pa=paY, f=att_fn: f(e, pa, 1), reads=["kiT", "qdT%d" % io], writes=[paYk])
                TT("dve", ez, paX[:, :], maskFB.rearrange("p h c -> p (h c)"), ALU.mult, [paXk, "maskFB"], ["ez"])
                TT("dve", E1, paY[:, :], maskFB.rearrange("p h c -> p (h c)"), ALU.mult, [paYk, "maskFB"], ["E1"])
                av = attnT[:, io, :].rearrange("p (a b c) -> p a b c", a=2, b=2)
                TT("pool", av[:, :, 0, :], ez[:, 0:256].rearrange("p (a c) -> p a c", a=2), ez[:, 256:512].rearrange("p (a c) -> p a c", a=2),
                   ALU.add, ["ez"], ["attnT%d" % io])
                TT("pool", av[:, :, 1, :], E1[:, 0:256].rearrange("p (a c) -> p a c", a=2), E1[:, 256:512].rearrange("p (a c) -> p a c", a=2),
                   ALU.add, ["E1"], ["attnT%d" % io])
            for dirn in range(2):
                if dirn == 0 and i >= T0 + NTO:
                    continue
                if dirn == 1 and left:
                    continue
                pkv, pkvk = next_pf()

                def kv_fn(e, pkv=pkv, dirn=dirn, vt=vt):
                    ins = None
                    for pr in range(2):
                        ins = e.matmul(pkv[:, pr * 256:(pr + 1) * 256], lhsT=ke[:, dirn * 256 + pr * 128: dirn * 256 + (pr + 1) * 128],
                                       rhs=vt[:, pr * 256:(pr + 1) * 256], start=True, stop=True)
                    return ins
                P.op("pe", kv_fn, reads=["ke", vkey], writes=[pkvk])
                if dirn == 0:
                    if own:
                        CP("act", SfT[:, io, :, :].rearrange("p a b -> p (a b)"), Sf.rearrange("p a b -> p (a b)"), ["Sf"], ["SfT%d" % io])
                    for pr in range(2):
                        for hh in range(2):
                            p0 = hh * 64
                            STT(Sf[p0:p0 + 64, pr, :], Sf[p0:p0 + 64, pr, :], dec[p0:p0 + 64, pr:pr + 1],
                                pkv[p0:p0 + 64, pr * 256 + hh * 128: pr * 256 + (hh + 1) * 128], ALU.mult, ALU.add,
                                ["Sf", "dec", pkvk], ["Sf"])
                else:
                    ib = i - T0
                    for pr in range(2):
                        for hh in range(2):
                            p0 = hh * 64
                            CP("act", kvB[p0:p0 + 64, ib, pr, :], pkv[p0:p0 + 64, pr * 256 + hh * 128: pr * 256 + (hh + 1) * 128],
                               [pkvk], ["kvB%d" % ib])
                    CP("dve", decB[:, ib, :], dec[:, 2:4], ["dec"], ["decB%d" % ib])

            def rope(raw, rkey):
                cosb = cst[:, i, 0:8].unsqueeze(1).broadcast_to([128, 8, 8])
                sinb = cst[:, i, 8:16].unsqueeze(1).broadcast_to([128, 8, 8])
                TT("pool", rta, raw[:, :, 0:8], cosb, ALU.mult, [rkey, "cst"], ["rta"])
                TT("pool", rtb, raw[:, :, 8:16], sinb, ALU.mult, [rkey, "cst"], ["rtb"])
                TT("pool", rtc, raw[:, :, 8:16], cosb, ALU.mult, [rkey, "cst"], ["rtc"])
                TT("pool", rtd, raw[:, :, 0:8], sinb, ALU.mult, [rkey, "cst"], ["rtd"])
                TT("dve", raw[:, :, 0:8], rta, rtb, ALU.subtract, ["rta", "rtb"], [rkey])
                TT("dve", raw[:, :, 8:16], rtc, rtd, ALU.add, ["rtc", "rtd"], [rkey])

            def sqnorm(src, col0, skey):
                TT("pool", sqs, src, src, ALU.mult, [skey], ["sqs"])
                P.op("dve", lambda e: e.tensor_reduce(out=nrm[:, col0:col0 + 8], in_=sqs, axis=AX.X, op=ALU.add), reads=["sqs"], writes=["nrm"])
                TT("dve", nmax[:, col0:col0 + 8], nmax[:, col0:col0 + 8], nrm[:, col0:col0 + 8], ALU.max, ["nrm", "nmax"], ["nmax"])

            r0k = HALO + i * 128
            if own:
                rope(aqr[b2], "aqr%d" % b2)
                sqnorm(aqr[b2], 0, "aqr%d" % b2)
                CP("act", qrb[b2], aqr[b2].rearrange("p h d -> p (h d)"), ["aqr%d" % b2], ["qrb%d" % b2])
                DMA("sp", QS[io * 128:(io + 1) * 128, :], qrb[b2], ["qrb%d" % b2], ["QS%d" % io])
            rope(akr[b2], "akr%d" % b2)
            sqnorm(akr[b2], 8, "akr%d" % b2)
            CP("act", krb[b2], akr[b2].rearrange("p h d -> p (h d)"), ["akr%d" % b2], ["krb%d" % b2])
            DMA("sp", KS[r0k:r0k + 128, :], krb[b2], ["krb%d" % b2], ["KS%d" % (r0k // 128)])

        for n_, i in enumerate(tiles):
            S1(i)
            if n_ > 0:
                S2(tiles[n_ - 1])
        if tiles:
            S2(tiles[-1])

        if stop_after == "A":
            P.op("sp", None, reads=[k_ for k_ in P.last_w.keys() if k_[:2] in ("QS", "KS", "VS", "GV", "GG")], writes=[])
            P.emit()
            return nc
        for i in range(NTW - 1, T0 - 1, -1):
            ib = i - T0
            if ib < NTO:
                CP("act", SbT[:, ib, :, :].rearrange("p a b -> p (a b)"), Sb.rearrange("p a b -> p (a b)"), ["Sb"], ["SbT%d" % ib])
            if i == T0:
                break
            for pr in range(2):
                STT(Sb[:, pr, :], Sb[:, pr, :], decB[:, ib, pr:pr + 1], kvB[:, ib, pr, :], ALU.mult, ALU.add,
                    ["Sb", "decB%d" % ib, "kvB%d" % ib], ["Sb"])

        P.barrier()
        AR.off = M1
        vb2 = [sb("vb2%d" % i, [128, 512], BF16) for i in range(2)]
        gb2 = [sb("gb2%d" % i, [128, 512], BF16) for i in range(2)]
        osb = sb("osb", [128, 512])
        osq = sb("osq", [128, 4, 128])
        oms = sb("oms", [128, 4])
        sgs = sb("sgs", [128, 512])
        ybf = sb("ybf", [128, 512])
        mixb = [sb("mixb%d" % i, [128, 512], BF16) for i in range(2)]
        for io in range(NTO):
            b2 = io % 2
            DMA("sp", vb2[b2], GV[io * 128:(io + 1) * 128, :], ["GV%d" % io], ["vb2%d" % b2])
            DMA("sp", gb2[b2], GG[io * 128:(io + 1) * 128, :], ["GG%d" % io], ["gb2%d" % b2])
            poX, poXk = next_pf()
            poY, poYk = next_pf()

            def o_fn(e, po, par, io=io, b2=b2):
                ins = None
                p0 = par * 64
                for pr in range(2):
                    h = pr * 2 + par
                    oap = po[:, pr * 128:(pr + 1) * 128]
                    e.matmul(oap, lhsT=attnT[:, io, h * 128:(h + 1) * 128], rhs=vb2[b2][:, h * 128:(h + 1) * 128], start=True, stop=False)
                    e.matmul(oap, lhsT=qdT[p0:p0 + 64, io, pr, :], rhs=SfT[p0:p0 + 64, io, pr, :], start=False, stop=False)
                    ins = e.matmul(oap, lhsT=qdT[p0:p0 + 64, io, 2 + pr, :], rhs=SbT[p0:p0 + 64, io, pr, :], start=False, stop=True)
                return ins
            rk_ = ["attnT%d" % io, "qdT%d" % io, "SfT%d" % io, "SbT%d" % io, "vb2%d" % b2]
            P.op("pe", lambda e, po=poX, f=o_fn: f(e, po, 0), reads=rk_, writes=[poXk])
            P.op("pe", lambda e, po=poY, f=o_fn: f(e, po, 1), reads=rk_, writes=[poYk])
            ov = osb.rearrange("p (a b c) -> p a b c", a=2, b=2)
            CP("act", ov[:, :, 0, :], poX[:, 0:256].rearrange("p (a c) -> p a c", a=2), [poXk], ["osb"])
            CP("act", ov[:, :, 1, :], poY[:, 0:256].rearrange("p (a c) -> p a c", a=2), [poYk], ["osb"])
            TT("pool", osq.rearrange("p h d -> p (h d)"), osb, osb, ALU.mult, ["osb"], ["osq"])
            P.op("dve", lambda e: e.tensor_reduce(out=oms, in_=osq, axis=AX.X, op=ALU.add), reads=["osq"], writes=["oms"])
            rstd_from_ssq(oms, oms, 128, "oms", "oms")
            ACT(sgs, gb2[b2], AF.Silu, ["gb2%d" % b2], ["sgs"])
            TT("dve", ybf, osb, gnbc, ALU.mult, ["osb", "gnbc"], ["ybf"])
            for h in range(4):
                STT(mixb[b2][:, h * 128:(h + 1) * 128], ybf[:, h * 128:(h + 1) * 128], oms[:, h:h + 1], sgs[:, h * 128:(h + 1) * 128],
                    ALU.mult, ALU.mult, ["ybf", "oms", "sgs"], ["mixb%d" % b2])
            DMA("sp", MG[io * 128:(io + 1) * 128, :], mixb[b2], ["mixb%d" % b2], ["MG%d" % io])
        MGK = ["MG%d" % i for i in range(NTO)]
        if stop_after == "G2":
            P.op("sp", None, reads=QSK + KSK + VSK + MGK, writes=[])
            P.emit()
            return nc

        P.barrier()
        AR.off = M0
        nm2 = sb("nm2", [128, 2])
        m2 = sb("m2", [2, 2])
        m1 = sb("m1", [1, 4])
        P.op("dve", lambda e: e.tensor_reduce(out=nm2, in_=nmax.rearrange("p (a h) -> p a h", a=2), axis=AX.X, op=ALU.max),
             reads=["nmax"], writes=["nm2"])
        pt, pk = next_pf()
        P.op("pe", lambda e, pt=pt: e.transpose(out=pt[0:2, 0:128], in_=nm2, identity=cm[:, 0, :]), reads=["nm2", "cm"], writes=[pk])
        P.op("dve", lambda e, pt=pt: e.tensor_reduce(out=m2[:, 0:1], in_=pt[0:2, 0:128], axis=AX.X, op=ALU.max), reads=[pk], writes=["m2"])
        pt, pk = next_pf()
        P.op("pe", lambda e, pt=pt: e.transpose(out=pt[0:1, 0:2], in_=m2[:, 0:1], identity=cm[0:2, 0, 0:2]), reads=["m2", "cm"], writes=[pk])
        CP("dve", m1[:, 0:2], pt[0:1, 0:2], [pk], ["m1"])
        TT("dve", m1[:, 2:3], m1[:, 0:1], m1[:, 1:2], ALU.mult, ["m1"], ["m1"])
        ACT(m1[:, 3:4], m1[:, 2:3], AF.Ln, ["m1"], ["m1"])
        ACT(m1[:, 3:4], m1[:, 3:4], AF.Exp, ["m1"], ["m1"], scale=0.5)
        TS("dve", m1[:, 3:4], m1[:, 3:4], -0.125, None, ALU.mult, None, ["m1"], ["m1"])
        pt, pk = next_pf()
        P.op("pe", lambda e, pt=pt: e.matmul(pt[:, 0:1], lhsT=cm[0:1, 7, :], rhs=m1[:, 3:4], start=True, stop=True), reads=["m1", "cm"], writes=[pk])
        CP("dve", negc, pt[:, 0:1], [pk], ["negc"])

        accT = sb("accT", [65, 8, OWN])
        NQ = 8
        qsb2 = [[sb("qsb%d" % i, [128, 512], BF16) for i in range(NQ)] for _ in range(2)]
        ksb2 = [[sb("ksb%d" % i, [128, 512], BF16) for i in range(NQ + 2)] for _ in range(2)]
        vsb2 = [[sb("vsb%d" % i, [128, 8, 65], BF16) for i in range(NQ + 2)] for _ in range(2)]
        qT2 = [[sb("qT%d" % i, [128, 4, 128], BF16) for i in range(NQ)] for _ in range(2)]
        kT2 = [[sb("kT%d" % i, [128, 4, 128], BF16) for i in range(NQ + 2)] for _ in range(2)]
        pex = [sb("pex%d" % i, [128, 384], BF16) for i in range(4)]
        pmk = [sb("pmk%d" % i, [128, 384], BF16) for i in range(4)]
        cnt4 = [0]
        jobs = [(1, 0, 0, 8), (1, 0, 8, 8)] + [(4, r, 0, 4) for r in range(4)] + [(16, r, 0, 1) for r in range(16)]
        for jn, (dd, r, j0, nq) in enumerate(jobs):
            js = jn % 2
            qsb, ksb, vsb, qT, kT = qsb2[js], ksb2[js], vsb2[js], qT2[js], kT2[js]
            QSv = QS.rearrange("(n d) c -> d n c", d=dd)
            KSv = KS.rearrange("(n d) c -> d n c", d=dd)
            VSv = VS.rearrange("(n d) c -> d n c", d=dd)
            accv = accT.rearrange("p h (n d) -> p h d n", d=dd)
            for jq in range(nq):
                n0 = 128 * (j0 + jq)
                DMA("sp", qsb[jq], QSv[r, n0:n0 + 128, :], QSK, [("qsb" + str(js) + "_%d") % jq])
            for kk in range(nq + 2):
                n0 = 2048 // dd + 128 * (j0 + kk - 1)
                DMA("sp", ksb[kk], KSv[r, n0:n0 + 128, :], KSK, [("ksb" + str(js) + "_%d") % kk])
                DMA("sp", vsb[kk].rearrange("p h d -> p (h d)"), VSv[r, n0:n0 + 128, :], VSK, [("vsb" + str(js) + "_%d") % kk])
            tl = [(qsb[jq], ("qsb" + str(js) + "_%d") % jq, qT[jq], ("qT" + str(js) + "_%d") % jq) for jq in range(nq)] + \
                 [(ksb[kk], ("ksb" + str(js) + "_%d") % kk, kT[kk], ("kT" + str(js) + "_%d") % kk) for kk in range(nq + 2)]
            for t0 in range(0, len(tl), 2):
                grp = tl[t0:t0 + 2]
                pbt, pbk = next_pb()

                def trq_fn(e, grp=grp, pbt=pbt):
                    ins = None
                    for gi, (src, _, _, _) in enumerate(grp):
                        for c in range(4):
                            ins = e.transpose(out=pbt[:, gi * 512 + c * 128: gi * 512 + (c + 1) * 128], in_=src[:, c * 128:(c + 1) * 128], identity=identb)
                    return ins
                P.op("pe", trq_fn, reads=[g[1] for g in grp] + ["identb"], writes=[pbk])
                for gi, (_, _, dst, dk) in enumerate(grp):
                    CP("act" if gi == 0 else "dve", dst.rearrange("p c t -> p (c t)"), pbt[:, gi * 512:(gi + 1) * 512], [pbk], [dk])
            for jq in range(nq):
                for hg in range(2):
                    bufs = []
                    for h in range(hg * 4, hg * 4 + 4):
                        p0 = (h % 2) * 64
                        blk = h // 2
                        pS, pSk = next_pf()

                        def s_fn(e, pS=pS, jq=jq, p0=p0, blk=blk, kT=kT, qT=qT):
                            ins = None
                            for sl in range(3):
                                ins = e.matmul(pS[:, sl * 128:(sl + 1) * 128], lhsT=kT[jq + sl][p0:p0 + 64, blk, :], rhs=qT[jq][p0:p0 + 64, blk, :],
                                               start=True, stop=True)
                            return ins
                        P.op("pe", s_fn, reads=[("kT" + str(js) + "_%d") % (jq + sl) for sl in range(3)] + [("qT" + str(js) + "_%d") % jq], writes=[pSk])
                        bi = cnt4[0] % 4
                        cnt4[0] += 1
                        ACT(pex[bi], pS[:, 0:384], AF.Exp, [pSk, "negc"], ["pex%d" % bi], bias=negc, scale=0.125)
                        TT("pool" if (h % 2) else "dve", pmk[bi], pex[bi], band, ALU.mult, ["pex%d" % bi, "band"], ["pmk%d" % bi])
                        bufs.append(bi)
                    pU, pUk = next_pf()

                    def pv_fn(e, pU=pU, jq=jq, hg=hg, bufs=tuple(bufs), vsb=vsb):
                        ins = None
                        for hi in range(4):
                            h = hg * 4 + hi
                            for sl in range(3):
                                ins = e.matmul(pU[0:65, hi * 128:(hi + 1) * 128], lhsT=vsb[jq + sl][:, h, :], rhs=pmk[bufs[hi]][:, sl * 128:(sl + 1) * 128],
                                               start=(sl == 0), stop=(sl == 2))
                        return ins
                    P.op("pe", pv_fn, reads=[("vsb" + str(js) + "_%d") % (jq + sl) for sl in range(3)] + ["pmk%d" % b for b in bufs], writes=[pUk])
                    n0 = 128 * (j0 + jq)
                    dst = accv[:, hg * 4:hg * 4 + 4, r, n0:n0 + 128]
                    src = pU[0:65, :].rearrange("p (h t) -> p h t", h=4)
                    akey = "accT"
                    if dd == 1:
                        CP("dve", dst, src, [pUk], [akey])
                    else:
                        TT("dve", dst, src, dst, ALU.add, [pUk, akey], [akey])
        rz = sb("rz", [64, 512])
        otb = [sb("otb%d" % i, [64, 512], BF16) for i in range(2)]
        k2 = 0
        for h in range(8):
            for g in range(4):
                pz, pzk = next_pf()
                P.op("pe", lambda e, pz=pz, h=h, g=g: e.matmul(pz[0:64, :], lhsT=cm[64:65, 7, 0:64], rhs=accT[64:65, h, g * 512:(g + 1) * 512],
                                                                start=True, stop=True), reads=["accT", "cm"], writes=[pzk])
                P.op("dve", lambda e, pz=pz: e.reciprocal(out=rz, in_=pz[0:64, :]), reads=[pzk], writes=["rz"])
                b2 = k2 % 2
                k2 += 1
                TT("pool", otb[b2], accT[0:64, h, g * 512:(g + 1) * 512], rz, ALU.mult, ["accT", "rz"], ["otb%d" % b2])
                DMA("sp", OTS[h, :, g * 512:(g + 1) * 512], otb[b2], ["otb%d" % b2], ["OTS%d_%d" % (h, g)])
        OTK = ["OTS%d_%d" % (h, g) for h in range(8) for g in range(4)]
        if stop_after == "B":
            P.op("sp", None, reads=OTK + MGK, writes=[])
            P.emit()
            return nc

        P.barrier()
        AR.off = M0
        bc_cache = {}

        def bcreg(e):
            if "r" not in bc_cache:
                bc_cache["r"] = e.to_reg(2559)
            return bc_cache["r"]
        CAPG = 640
        NSLOT = 4 * CAPG
        OOB = 4096.0
        u2tok = sb("u2tok", [128, NTO, D], BF16)
        OH = sb("OH", [128, NTO, 4])
        WE = sb("WE", [128, NTO, 8])
        idxf = sb("idxf", [128, NTO])
        idxi = sb("idxi", [128, 2 * NTO], I32)
        goffm = sb("goffm", [128, 4])
        pren = sb("pren", [128, 4])
        M2 = AR.off
        woutG = sb("woutG", [128, 4, D], BF16)
        woutA = sb("woutA", [64, 8, D], BF16)
        DMA("pool", woutG, w_out[0:512, :].rearrange("(c p) n -> p c n", p=128), [], ["woutG"])
        DMA("pool", woutA, w_out[512:1024, :].rearrange("(h p) n -> p h n", p=64), [], ["woutA"])
        for g in range(4):
            P.op("dve", lambda e, g=g: e.memset(goffm[:, g:g + 1], float(g * CAPG) - OOB), writes=["goffm"])
        P.op("dve", lambda e: e.memset(pren, 0.0), writes=["pren"])
        zx = sb("zx", [128, D], BF16)
        zw = sb("zw", [128, 8])
        P.op("pool", lambda e: e.memset(zx, 0.0), writes=["zx"])
        P.op("pool", lambda e: e.memset(zw, 0.0), writes=["zw"])
        for r0 in range(0, NSLOT, 128):
            DMA("sp", XB[r0:r0 + 128, :], zx, ["zx"], ["XB"])
            DMA("sp", WB[r0:r0 + 128, :], zw, ["zw"], ["WB"])
        xo = [sb("xo%d" % i, [128, D]) for i in range(2)]
        mgl = [sb("mgl%d" % i, [128, 512], BF16) for i in range(2)]
        otl = [sb("otl%d" % i, [64, 8, 128], BF16) for i in range(2)]
        mgT = sb("mgT", [128, 4, 128], BF16)
        h2t = [sb("h2t%d" % i, [128, D]) for i in range(2)]
        u2 = sb("u2", [128, D])
        u2Tf = sb("u2Tf", [128, 8, 128])
        junk2 = sb("junk2", [128, D], BF16)
        ss2 = sb("ss2", [128, 2])
        rs2 = sb("rs2", [128, 2])
        lg = sb("lg", [128, 36])
        sm = sb("sm", [128, 64])
        for io in range(NTO):
            b2 = io % 2
            DMA("sp", xo[b2], xw[HALO + io * 128: HALO + (io + 1) * 128, :], [], ["xo%d" % b2])
            DMA("sp", mgl[b2], MG[io * 128:(io + 1) * 128, :], ["MG%d" % io], ["mgl%d" % b2])
            DMA("sp", otl[b2], OTS[:, :, io * 128:(io + 1) * 128].rearrange("h p t -> p h t"), OTK, ["otl%d" % b2])
            pbt, pbk = next_pb()

            def trm_fn(e, pbt=pbt, b2=b2):
                ins = None
                for c in range(4):
                    ins = e.transpose(out=pbt[:, c * 128:(c + 1) * 128], in_=mgl[b2][:, c * 128:(c + 1) * 128], identity=identb)
                return ins
            P.op("pe", trm_fn, reads=["mgl%d" % b2, "identb"], writes=[pbk])
            CP("act", mgT.rearrange("p c t -> p (c t)"), pbt[:, 0:512], [pbk], ["mgT"])
            for cg in range(2):
                pt, pk = next_pf()
                pairs = [(mgT[:, c, :], woutG[:, c, cg * 512:(cg + 1) * 512]) for c in range(4)] + \
                        [(otl[b2][:, h, :], woutA[:, h, cg * 512:(cg + 1) * 512]) for h in range(8)]
                mm_group(pt[:, :], pairs, pk, ["mgT", "otl%d" % b2, "woutG", "woutA"])
                TT("dve", h2t[b2][:, cg * 512:(cg + 1) * 512], pt[:, :], xo[b2][:, cg * 512:(cg + 1) * 512], ALU.add,
                   [pk, "xo%d" % b2], ["h2t%d" % b2])
            DMA("sp", H2[io * 128:(io + 1) * 128, :], h2t[b2], ["h2t%d" % b2], ["H2_%d" % io])
            P.op("act", lambda e, b2=b2: e.activation(out=junk2, in_=h2t[b2], func=AF.Square, accum_out=ss2[:, 0:1]),
                 reads=["h2t%d" % b2], writes=["junk2", "ss2"])
            rstd_from_ssq(rs2[:, 0:1], ss2[:, 0:1], D, "ss2", "rs2")
            STT(u2, h2t[b2], rs2[:, 0:1], n2bc, ALU.mult, ALU.mult, ["h2t%d" % b2, "rs2", "n2bc"], ["u2"])
            CP("pool", u2tok[:, io, :], u2, ["u2"], ["u2tok%d" % io])
            for half in range(2):
                pt, pk = next_pf()

                def tru_fn(e, pt=pt, half=half):
                    ins = None
                    for c in range(4):
                        cc = half * 4 + c
                        ins = e.transpose(out=pt[:, c * 128:(c + 1) * 128], in_=u2[:, cc * 128:(cc + 1) * 128], identity=cm[:, 0, :])
                    return ins
                P.op("pe", tru_fn, reads=["u2", "cm"], writes=[pk])
                CP("act", u2Tf[:, half * 4:half * 4 + 4, :].rearrange("p c t -> p (c t)"), pt[:, :], [pk], ["u2Tf%d" % half])
            pr_, prk = next_pf()
            mm_group(pr_[:, 0:36], [(u2Tf[:, c, :], wr[:, c, :]) for c in range(8)], prk, ["u2Tf0", "u2Tf1", "wr"])
            TT("dve", lg, pr_[:, 0:36], rbbc, ALU.add, [prk, "rbbc"], ["lg"])
            gmax, ngmax, gsum, gw = sm[:, 0:1], sm[:, 1:2], sm[:, 2:3], sm[:, 3:4]
            oh = OH[:, io, :]
            ohk = "OH%d" % io
            ge = sm[:, 8:12]
            esel = sm[:, 16:24]
            top8 = sm[:, 24:32]
            d21, w1g, w2g = sm[:, 32:33], sm[:, 33:34], sm[:, 34:35]
            wa = sm[:, 40:48]
            wb_ = sm[:, 48:56]
            P.op("dve", lambda e: e.tensor_reduce(out=gmax, in_=lg[:, 0:4], axis=AX.X, op=ALU.max), reads=["lg"], writes=["sm"])
            TS("dve", oh, lg[:, 0:4], gmax, None, ALU.is_equal, None, ["lg", "sm"], [ohk])
            TS("dve", ngmax, gmax, -1.0, None, ALU.mult, None, ["sm"], ["sm"])
            ACT(ge, lg[:, 0:4], AF.Exp, ["lg", "sm"], ["sm"], bias=ngmax, scale=1.0)
            P.op("dve", lambda e: e.tensor_reduce(out=gsum, in_=ge, axis=AX.X, op=ALU.add), reads=["sm"], writes=["sm"])
            P.op("dve", lambda e: e.reciprocal(out=gw, in_=gsum), reads=["sm"], writes=["sm"])
            TS("dve", esel, lg[:, 4:12], oh[:, 0:1], None, ALU.mult, None, ["lg", ohk], ["sm"])
            for g in range(1, 4):
                STT(esel, lg[:, 4 + 8 * g:12 + 8 * g], oh[:, g:g + 1], esel, ALU.mult, ALU.add, ["lg", ohk, "sm"], ["sm"])
            P.op("dve", lambda e: e.max(out=top8, in_=esel), reads=["sm"], writes=["sm"])
            TT("dve", d21, top8[:, 1:2], top8[:, 0:1], ALU.subtract, ["sm"], ["sm"])
            ACT(d21, d21, AF.Exp, ["sm"], ["sm"])
            TS("dve", d21, d21, 1.0, None, ALU.add, None, ["sm"], ["sm"])
            P.op("dve", lambda e: e.reciprocal(out=w1g, in_=d21), reads=["sm"], writes=["sm"])
            TT("dve", w1g, w1g, gw, ALU.mult, ["sm"], ["sm"])
            TT("dve", w2g, gw, w1g, ALU.subtract, ["sm"], ["sm"])
            TS("dve", wa, esel, top8[:, 0:1], w1g, ALU.is_equal, ALU.mult, ["sm"], ["sm"])
            TS("dve", wb_, esel, top8[:, 1:2], w2g, ALU.is_equal, ALU.mult, ["sm"], ["sm"])
            TT("dve", WE[:, io, :], wa, wb_, ALU.add, ["sm"], ["WE%d" % io])
            prk_t, prkk = next_pf()
            P.op("pe", lambda e, t=prk_t, io=io: (e.matmul(t[:, 0:4], lhsT=cm[:, 4, :], rhs=OH[:, io, :], start=True, stop=False),
                                                   e.matmul(t[:, 0:4], lhsT=cm[:, 7, :], rhs=pren, start=False, stop=True))[1],
                 reads=[ohk, "pren", "cm"], writes=[prkk])
            rk = sm[:, 56:60]
            okm = sm[:, 60:64]
            TS("dve", rk, prk_t[:, 0:4], -16.0, None, ALU.mult, None, [prkk], ["sm"])
            STT(pren, oh, -1.0 / 16.0, pren, ALU.mult, ALU.add, [ohk, "pren", prkk], ["pren"])
            TS("dve", okm, rk, float(CAPG), None, ALU.is_lt, None, ["sm"], ["sm"])
            TT("dve", okm, okm, oh, ALU.mult, ["sm", ohk], ["sm"])
            TT("dve", rk, rk, goffm, ALU.add, ["sm", "goffm"], ["sm"])
            TT("dve", rk, rk, okm, ALU.mult, ["sm"], ["sm"])
            P.op("dve", lambda e, io=io: e.tensor_reduce(out=idxf[:, io:io + 1], in_=rk, axis=AX.X, op=ALU.add), reads=["sm"], writes=["idxf%d" % io])
            TS("dve", idxf[:, io:io + 1], idxf[:, io:io + 1], OOB, None, ALU.add, None, ["idxf%d" % io], ["idxf%d" % io])
            CP("dve", idxi[:, io:io + 1], idxf[:, io:io + 1], ["idxf%d" % io], ["idxi%d" % io])
            P.op("pool", lambda e, io=io: e.indirect_dma_start(out=XB[:, :], out_offset=bass.IndirectOffsetOnAxis(ap=idxi[:, io:io + 1], axis=0),
                                                               in_=u2tok[:, io, :], in_offset=None, bounds_check=bcreg(e), oob_is_err=False),
                 reads=["u2tok%d" % io, "idxi%d" % io, "XB"], writes=["XBs%d" % io], dma=True)
            P.op("pool", lambda e, io=io: e.indirect_dma_start(out=WB[:, :], out_offset=bass.IndirectOffsetOnAxis(ap=idxi[:, io:io + 1], axis=0),
                                                               in_=WE[:, io, :], in_offset=None, bounds_check=bcreg(e), oob_is_err=False),
                 reads=["WE%d" % io, "idxi%d" % io, "WB"], writes=["WBs%d" % io], dma=True)
        H2K = ["H2_%d" % i for i in range(NTO)]
        XBK = ["XBs%d" % i for i in range(NTO)] + ["XB"]
        WBK = ["WBs%d" % i for i in range(NTO)] + ["WB"]
        if debug:
            DMA("sp", WTD[:, 0:NTO], idxf, ["idxf%d" % i for i in range(NTO)], ["WTD"])
        if stop_after == "C1":
            P.op("sp", None, reads=H2K + XBK + WBK + ["WTD"], writes=[])
            P.emit()
            return nc

        P.barrier()
        AR.off = M2
        NCH = CAPG // 128
        xs = sb("xs", [128, NCH, D], BF16)
        xTg = sb("xTg", [128, 8, CAPG], BF16)
        wsl = sb("wsl", [128, NCH, 8])
        hid = sb("hid", [128, 4, CAPG], BF16)
        yacc = sb("yacc", [128, NCH, D])
        wgb = [sb("wgb%d" % i, [128, 8, 512], BF16) for i in range(2)]
        wub = [sb("wub%d" % i, [128, 8, 512], BF16) for i in range(2)]
        wdb = [sb("wdb%d" % i, [128, 4, D], BF16) for i in range(2)]
        sgb = [sb("sgb%d" % i, [128, 512]) for i in range(2)]

        def load_expert(ex):
            b = ex % 2
            DMA("pool", wgb[b], ewg[ex].rearrange("(c p) n -> p c n", p=128), [], ["wgb%d" % b])
            DMA("pool", wub[b], ewu[ex].rearrange("(c p) n -> p c n", p=128), [], ["wub%d" % b])
            DMA("pool", wdb[b], ewd[ex].rearrange("(c p) n -> p c n", p=128), [], ["wdb%d" % b])
        load_expert(0)
        kk2 = 0
        nsl = [(0, 512), (512, CAPG)]
        for g in range(4):
            DMA("sp", xs, XB[g * CAPG:(g + 1) * CAPG, :].rearrange("(c p) d -> p c d", p=128), XBK, ["xs"])
            DMA("sp", wsl, WB[g * CAPG:(g + 1) * CAPG, :].rearrange("(c p) d -> p c d", p=128), WBK, ["wsl"])
            for ch in range(NCH):
                pbt, pbk = next_pb()

                def trx_fn(e, pbt=pbt, ch=ch):
                    ins = None
                    for c in range(8):
                        ins = e.transpose(out=pbt[:, c * 128:(c + 1) * 128], in_=xs[:, ch, c * 128:(c + 1) * 128], identity=identb)
                    return ins
                P.op("pe", trx_fn, reads=["xs", "identb"], writes=[pbk])
                CP("act" if ch % 2 else "dve", xTg[:, :, ch * 128:(ch + 1) * 128], pbt[:, :].rearrange("p (c t) -> p c t", c=8), [pbk], ["xTg%d" % ch])
            XTK = ["xTg%d" % ch for ch in range(NCH)]
            for el in range(8):
                ex = g * 8 + el
                b = ex % 2
                if ex + 1 < NEXP:
                    load_expert(ex + 1)
                for (n0, n1) in nsl:
                    for fc in range(4):
                        pg, pgk = next_pf()
                        pu, puk = next_pf()
                        mm_group(pg[:, 0:n1 - n0], [(wgb[b][:, c, fc * 128:(fc + 1) * 128], xTg[:, c, n0:n1]) for c in range(8)], pgk,
                                 ["wgb%d" % b] + XTK)
                        mm_group(pu[:, 0:n1 - n0], [(wub[b][:, c, fc * 128:(fc + 1) * 128], xTg[:, c, n0:n1]) for c in range(8)], puk,
                                 ["wub%d" % b] + XTK)
                        sb_i = kk2 % 2
                        kk2 += 1
                        ACT(sgb[sb_i][:, 0:n1 - n0], pg[:, 0:n1 - n0], AF.Silu, [pgk], ["sgb%d" % sb_i])
                        TT("dve", hid[:, fc, n0:n1], sgb[sb_i][:, 0:n1 - n0], pu[:, 0:n1 - n0], ALU.mult, ["sgb%d" % sb_i, puk], ["hid%d_%d" % (fc, n0)])
                HK = ["hid%d_%d" % (fc, n0) for fc in range(4) for (n0, _) in nsl]
                for ch in range(NCH):
                    for cg in range(2):
                        py, pyk = next_pf()
                        mm_group(py[:, :], [(hid[:, fc, ch * 128:(ch + 1) * 128], wdb[b][:, fc, cg * 512:(cg + 1) * 512]) for fc in range(4)], pyk,
                                 ["wdb%d" % b] + HK)
                        ya = yacc[:, ch, cg * 512:(cg + 1) * 512]
                        yk = "yacc%d" % ch
                        if el == 0:
                            TS("dve", ya, py[:, :], wsl[:, ch, el:el + 1], None, ALU.mult, None, [pyk, "wsl"], [yk])
                        else:
                            STT(ya, py[:, :], wsl[:, ch, el:el + 1], ya, ALU.mult, ALU.add, [pyk, "wsl", yk], [yk])
            DMA("sp", YB[g * CAPG:(g + 1) * CAPG, :].rearrange("(c p) d -> p c d", p=128), yacc, ["yacc%d" % ch for ch in range(NCH)], ["YB%d" % g])
        YBK = ["YB%d" % g for g in range(4)]
        P.barrier()
        AR.off = M2
        hl = [sb("hl%d" % i, [128, D]) for i in range(2)]
        yg = [sb("yg%d" % i, [128, D]) for i in range(2)]
        ob = [sb("ob%d" % i, [128, D]) for i in range(2)]
        junk3 = sb("junk3", [128, D], BF16)
        ss3 = sb("ss3", [128, 2])
        rs3 = sb("rs3", [128, 2])
        for io in range(NTO):
            b2 = io % 2
            DMA("sp", hl[b2], H2[io * 128:(io + 1) * 128, :], ["H2_%d" % io], ["hl%d" % b2])
            P.op("pool", lambda e, b2=b2: e.memset(yg[b2], 0.0), writes=["yg%d" % b2])
            P.op("pool", lambda e, io=io, b2=b2: e.indirect_dma_start(out=yg[b2], out_offset=None, in_=YB[:, :],
                                                                       in_offset=bass.IndirectOffsetOnAxis(ap=idxi[:, io:io + 1], axis=0),
                                                                       bounds_check=bcreg(e), oob_is_err=False),
                 reads=YBK + ["idxi%d" % io], writes=["yg%d" % b2], dma=True)
            TT("dve", hl[b2], hl[b2], yg[b2], ALU.add, ["hl%d" % b2, "yg%d" % b2], ["hl%d" % b2])
            P.op("act", lambda e, b2=b2: e.activation(out=junk3, in_=hl[b2], func=AF.Square, accum_out=ss3[:, 0:1]),
                 reads=["hl%d" % b2], writes=["junk3", "ss3"])
            rstd_from_ssq(rs3[:, 0:1], ss3[:, 0:1], D, "ss3", "rs3")
            STT(ob[b2], hl[b2], rs3[:, 0:1], fnbc, ALU.mult, ALU.mult, ["hl%d" % b2, "rs3", "fnbc"], ["ob%d" % b2])
            DMA("sp", out_d[io * 128:(io + 1) * 128, :], ob[b2], ["ob%d" % b2], ["OUT%d" % io])
        P.op("sp", None, reads=["OUT%d" % i for i in range(NTO)], writes=[])
        P.emit()
    return nc


def _consts():
    s = np.arange(128)[:, None]
    t = np.arange(128)[None, :]
    cm = np.zeros((128, 8, 128), np.float32)
    cm[:, 0] = (s == t)
    cm[:, 1] = (s <= t) / -16.0
    cm[:, 2] = (s >= t) / -16.0
    cm[:, 3] = (s > t) / -16.0
    cm[:, 4] = (s < t) / -16.0
    cm[:, 5] = (s <= t)
    cm[:, 6] = (s >= t)
    cm[:, 7] = 1.0
    band = np.zeros((128, 384), np.float32)
    band[:, 0:128] = (s >= t + 64)
    band[:, 128:256] = (np.abs(s - t) <= 64)
    band[:, 256:384] = (s <= t - 64)
    return cm, band


def make_in_maps(inputs):
    f = lambda a: np.ascontiguousarray(np.asarray(a, dtype=np.float32))
    x = f(inputs["x"])
    cm, band = _consts()
    wz = np.zeros((33, 512), np.float32)
    wz[0:16, 0:256] = f(inputs["gla_fwd_gate_w"])[0]
    wz[16:32, 256:512] = f(inputs["gla_bwd_gate_w"])[0]
    wz[32, 0:256] = f(inputs["gla_fwd_gate_b"])[0]
    wz[32, 256:512] = f(inputs["gla_bwd_gate_b"])[0]
    vecs = np.zeros((4, D), np.float32)
    vecs[0] = f(inputs["norm1_w"])[0]
    vecs[1] = f(inputs["norm2_w"])[0]
    vecs[2] = f(inputs["final_norm_w"])
    vecs[3] = np.tile(f(inputs["gla_norm_w"])[0], 8)
    wr = np.concatenate([f(inputs["router_group_w"])[0]] + [f(inputs["router_expert_w"])[0, g] for g in range(4)], axis=1)
    rb = np.concatenate([f(inputs["router_group_b"])[0], f(inputs["router_expert_b"])[0].reshape(-1)])[None, :]
    inv = (500000.0 ** (-(np.arange(0, 16, 2, dtype=np.float32) / np.float32(16)))).astype(np.float32)
    shared = dict(cmat=cm, band3=band, w_in=f(inputs["w_in"])[0], wz=wz, vecs=vecs, w_out=f(inputs["w_out"])[0],
                  wr=np.ascontiguousarray(wr), rb=np.ascontiguousarray(rb), ewg=f(inputs["expert_w_gate"])[0],
                  ewu=f(inputs["expert_w_up"])[0], ewd=f(inputs["expert_w_down"])[0])
    maps = []
    for c in range(8):
        b, q = c // 4, c % 4
        s0 = q * OWN
        pos = np.arange(s0 - HALO, s0 + OWN + HALO)
        valid = (pos >= 0) & (pos < S)
        xwin = np.zeros((WIN, D), np.float32)
        xwin[valid] = x[b, pos[valid]]
        ang = (pos.astype(np.float32)[:, None] * inv[None, :]).astype(np.float32)
        cs = np.concatenate([np.cos(ang), np.sin(ang)], axis=1).astype(np.float32)
        cs_t = np.ascontiguousarray(cs.reshape(NTW, 128, 16).transpose(1, 0, 2))
        vcol = np.ascontiguousarray(valid.astype(np.float32).reshape(NTW, 128).T)
        m = dict(shared)
        m.update(xw=xwin, vcol=vcol, cs_t=cs_t)
        maps.append(m)
    return maps


_NC_CACHE = {}


def kernel(**inputs):
    maps = make_in_maps(inputs)
    if "nc" not in _NC_CACHE:
        _NC_CACHE["nc"] = build_program()
    nc = _NC_CACHE["nc"]
    res = run_bass_kernel_spmd(nc, maps, core_ids=list(range(8)))
    out = np.zeros((2, S, D), np.float32)
    for c in range(8):
        b, q = c // 4, c % 4
        out[b, q * OWN:(q + 1) * OWN] = res.results[c]["out"]
    return out
```

```python
import numpy as np
from contextlib import ExitStack
import concourse.bass as bass
import concourse.mybir as mybir
from concourse.bass_utils import run_bass_kernel_spmd

F32 = mybir.dt.float32
BF16 = mybir.dt.bfloat16
I32 = mybir.dt.int32
AF = mybir.ActivationFunctionType
ALU = mybir.AluOpType
AX = mybir.AxisListType

ENGS = ("pe", "act", "dve", "pool", "sp")
EPOCH = 4096
DMA_SLOTS = 8

D = 1024
S = 8192
OWN = 2048
HALO = 1024
WIN = OWN + 2 * HALO
NTW = WIN // 128
T0 = HALO // 128
NTO = OWN // 128
INW = 3104
NEXP = 32
EPS = 1e-6


class Op:
    __slots__ = ("eng", "fn", "dma", "deps", "sig", "sigcount", "dmaidx", "idx")

    def __init__(self, eng, fn, dma):
        self.eng = eng
        self.fn = fn
        self.dma = dma
        self.deps = []
        self.sig = False
        self.sigcount = 0
        self.dmaidx = -1
        self.idx = -1


class Prog:
    def __init__(self, nc):
        self.nc = nc
        self.ops = []
        self.last_w = {}
        self.readers = {}
        self.ndma = {e: 0 for e in ENGS}
        self.bar = None
        self.rec = None

    def barrier(self):
        deps = set()
        for e in ENGS:
            last = None
            nd = 0
            for o in reversed(self.ops):
                if o.eng != e:
                    continue
                if o.dma:
                    if nd < DMA_SLOTS:
                        deps.add(o.idx)
                        nd += 1
                elif last is None:
                    last = o.idx
                    deps.add(o.idx)
                if last is not None and nd >= DMA_SLOTS:
                    break
        b = self.op("sp", None)
        b.deps = sorted(deps | set(b.deps))
        self.bar = b.idx
        return b

    def replay_merged(self, a, b):
        na, nb = len(a), len(b)
        i = j = 0
        while i < na or j < nb:
            if j >= nb or (i < na and i * nb <= j * na):
                self.op(*a[i])
                i += 1
            else:
                self.op(*b[j])
                j += 1

    def op(self, eng, fn, reads=(), writes=(), dma=False):
        if self.rec is not None:
            self.rec.append((eng, fn, list(reads), list(writes), dma))
            return None
        import os as _os
        mx = int(_os.environ.get("DBG_MAXOPS", "0"))
        if mx and len(self.ops) >= mx and fn is not None:
            fn = None
            if dma:
                dma = False
        px = [k_ for k_ in reads if k_[:2] in ("pf", "pb")]
        if px:
            writes = list(writes) + [k_ for k_ in px if k_ not in writes]
            reads = [k_ for k_ in reads if k_ not in px]
        o = Op(eng, fn, dma)
        o.idx = len(self.ops)
        deps = set()
        if self.bar is not None:
            deps.add(self.bar)
        for k in reads:
            w = self.last_w.get(k)
            if w is not None:
                deps.add(w)
        for k in writes:
            w = self.last_w.get(k)
            if w is not None:
                deps.add(w)
            for r in self.readers.get(k, ()):
                deps.add(r)
        deps.discard(o.idx)
        o.deps = sorted(deps)
        for k in writes:
            self.last_w[k] = o.idx
            self.readers[k] = []
        for k in reads:
            if k not in writes:
                self.readers.setdefault(k, []).append(o.idx)
        if dma:
            o.dmaidx = self.ndma[eng]
            self.ndma[eng] += 1
        self.ops.append(o)
        return o

    def emit(self):
        nc = self.nc
        ops = self.ops
        for o in ops:
            for d in o.deps:
                p = ops[d]
                if not p.dma:
                    p.sig = True
        cnt = {e: 0 for e in ENGS}
        for o in ops:
            if o.sig and not o.dma:
                cnt[o.eng] += 1
                o.sigcount = cnt[o.eng]
        nsem = {e: (cnt[e] + EPOCH - 1) // EPOCH for e in ENGS}
        with ExitStack() as es:
            csem = {e: [es.enter_context(nc.semaphore("c_%s_%d" % (e, i))) for i in range(nsem[e])]
                    for e in ENGS}
            dsem = {e: [es.enter_context(nc.semaphore("d_%s_%d" % (e, i)))
                        for i in range(DMA_SLOTS if self.ndma[e] else 0)] for e in ENGS}
            block = es.enter_context(nc.Block())

            def body_for(e):
                def body(eng):
                    waited_c = {x: 0 for x in ENGS}
                    waited_d = {}
                    for o in ops:
                        if o.eng != e:
                            continue
                        need_c = {}
                        need_d = {}
                        for d in o.deps:
                            p = ops[d]
                            if p.dma:
                                slot = p.dmaidx % DMA_SLOTS
                                val = 16 * (p.dmaidx // DMA_SLOTS + 1)
                                key = (p.eng, slot)
                                if waited_d.get(key, 0) < val:
                                    need_d[key] = max(need_d.get(key, 0), val)
                            else:
                                if waited_c[p.eng] < p.sigcount:
                                    need_c[p.eng] = max(need_c.get(p.eng, 0), p.sigcount)
                        if o.dma:
                            slot = o.dmaidx % DMA_SLOTS
                            val = 16 * (o.dmaidx // DMA_SLOTS)
                            key = (e, slot)
                            if val > 0 and waited_d.get(key, 0) < val:
                                need_d[key] = max(need_d.get(key, 0), val)
                        for pe_, c in need_c.items():
                            ep = (c - 1) // EPOCH
                            eng.wait_ge(csem[pe_][ep], (c - 1) % EPOCH + 1)
                            waited_c[pe_] = c
                        for key, val in need_d.items():
                            eng.wait_ge(dsem[key[0]][key[1]], val)
                            waited_d[key] = val
                        ins = o.fn(eng) if o.fn is not None else None
                        if o.dma:
                            ins.then_inc(dsem[e][o.dmaidx % DMA_SLOTS], 16)
                        elif o.sig:
                            if ins is None:
                                ins = eng.nop()
                            ep = (o.sigcount - 1) // EPOCH
                            ins.then_inc(csem[e][ep], 1)
                return body

            block.tensor(body_for("pe"))
            block.scalar(body_for("act"))
            block.vector(body_for("dve"))
            block.gpsimd(body_for("pool"))
            block.sync(body_for("sp"))


class Arena:
    def __init__(self, ap, ncols):
        self.ap = ap
        self.n = ncols
        self.off = 0

    def alloc(self, shape, dt=F32):
        p = shape[0]
        rest = list(shape[1:])
        nel = 1
        for r in rest:
            nel *= r
        ncol = nel if dt in (F32, I32) else (nel + 1) // 2
        ncol += ncol % 2
        assert self.off + ncol <= self.n, "arena overflow: need %d have %d" % (ncol, self.n - self.off)
        v = self.ap[0:p, self.off:self.off + ncol]
        self.off += ncol
        if dt != F32:
            v = v.bitcast(dt)
        if v.shape[1] != nel:
            v = v[:, 0:nel]
        if len(rest) == 2:
            v = v.rearrange("p (a b) -> p a b", a=rest[0])
        elif len(rest) == 3:
            v = v.rearrange("p (a b c) -> p a b c", a=rest[0], b=rest[1])
        return v


def build_program(debug=False, stop_after=None, dbg_tiles=None):
    nc = bass.Bass("TRN2", target_bir_lowering=False)
    P = Prog(nc)
    global LASTP
    LASTP = P

    def din(name, shape, dt=F32):
        return nc.dram_tensor(name, list(shape), dt, kind="ExternalInput").ap()

    def dscr(name, shape, dt):
        kind = "ExternalOutput" if debug else "Internal"
        return nc.dram_tensor(name, list(shape), dt, kind=kind).ap()

    xw = din("xw", [WIN, D])
    vcol = din("vcol", [128, NTW])
    cs_t = din("cs_t", [128, NTW, 16])
    cmat = din("cmat", [128, 8, 128])
    band3 = din("band3", [128, 384])
    w_in = din("w_in", [D, INW])
    wz_d = din("wz", [33, 512])
    vecs = din("vecs", [4, D])
    w_out = din("w_out", [D, D])
    wr_d = din("wr", [D, 36])
    rb_d = din("rb", [1, 36])
    ewg = din("ewg", [NEXP, D, 512])
    ewu = din("ewu", [NEXP, D, 512])
    ewd = din("ewd", [NEXP, 512, D])
    out_d = nc.dram_tensor("out", [OWN, D], F32, kind="ExternalOutput").ap()
    QS = dscr("QS", [OWN, 512], BF16)
    KS = dscr("KS", [WIN + 2 * HALO, 512], BF16)
    VS = dscr("VS", [WIN + 2 * HALO, 520], BF16)
    GV = dscr("GV", [OWN, 512], BF16)
    GG = dscr("GG", [OWN, 512], BF16)
    MG = dscr("MG", [OWN, 512], BF16)
    OTS = dscr("OTS", [8, 64, OWN], BF16)
    H2 = dscr("H2", [OWN, D], F32)
    XB = dscr("XB", [2560, D], BF16)
    WB = dscr("WB", [2560, 8], F32)
    YB = dscr("YB", [2560, D], F32)
    WTD = nc.dram_tensor("WTD", [128, NTO * 32], F32, kind="ExternalOutput").ap() if debug else None

    QSK = ["QS%d" % i for i in range(NTO)]
    KSK = ["KS%d" % i for i in range(48)]
    VSK = ["VS%d" % i for i in range(48)]
    NCOL = 50 * 1024 + 512
    es = ExitStack()
    with es:
        arena_t = es.enter_context(nc.sbuf_tensor("arena", [128, NCOL], F32))
        AR = Arena(arena_t[:], NCOL)
        sb = lambda name, shape, dt=F32: AR.alloc(shape, dt)

        def ps(name, shape, dt=F32):
            return es.enter_context(nc.psum_tensor("p_" + name, list(shape), dt))

        pf = [ps("pf%d" % i, [128, 512]) for i in range(6)]
        pb = [ps("pb%d" % i, [128, 1024], BF16) for i in range(2)]
        pf_rr = [0]
        pb_rr = [0]

        stream = [None]
        srr = [0, 0]

        def next_pf():
            if stream[0] is None:
                i = pf_rr[0] % 6
                pf_rr[0] += 1
            else:
                s_ = stream[0]
                i = 3 * s_ + srr[s_] % 3
                srr[s_] += 1
            return pf[i], "pf%d" % i

        def next_pb():
            if stream[0] is None:
                i = pb_rr[0] % 2
                pb_rr[0] += 1
            else:
                i = stream[0]
            return pb[i], "pb%d" % i

        def mm_group(out_ap, pairs, okey, rkeys):
            def fn(e):
                ins = None
                n = len(pairs)
                for j, (l, r) in enumerate(pairs):
                    ins = e.matmul(out_ap, lhsT=l, rhs=r, start=(j == 0), stop=(j == n - 1))
                return ins
            P.op("pe", fn, reads=rkeys, writes=[okey])

        def ACT(out, in_, func, reads, writes, **kw):
            P.op("act", lambda e: e.activation(out=out, in_=in_, func=func, **kw), reads=reads, writes=writes)

        def TT(eng, out, in0, in1, op, reads, writes):
            P.op(eng, lambda e: e.tensor_tensor(out=out, in0=in0, in1=in1, op=op), reads=reads, writes=writes)

        def STT(out, in0, scalar, in1, op0, op1, reads, writes):
            P.op("dve", lambda e: e.scalar_tensor_tensor(out=out, in0=in0, scalar=scalar, in1=in1, op0=op0, op1=op1),
                 reads=reads, writes=writes)

        def TS(eng, out, in0, s1, s2, op0, op1, reads, writes):
            if op1 is None:
                P.op(eng, lambda e: e.tensor_scalar(out=out, in0=in0, scalar1=s1, scalar2=None, op0=op0), reads=reads, writes=writes)
            else:
                P.op(eng, lambda e: e.tensor_scalar(out=out, in0=in0, scalar1=s1, scalar2=s2, op0=op0, op1=op1), reads=reads, writes=writes)

        def CP(eng, out, in_, reads, writes):
            if eng == "act":
                ACT(out, in_, AF.Copy, reads, writes)
            else:
                P.op(eng, lambda e: e.tensor_copy(out=out, in_=in_), reads=reads, writes=writes)

        def DMA(q, out, in_, reads, writes):
            return P.op(q, lambda e: e.dma_start(out=out, in_=in_), reads=reads, writes=writes, dma=True)

        def rstd_from_ssq(dst, src, n, rk, wk):
            ACT(dst, src, AF.Ln, [rk, "epsc"], [wk], scale=1.0 / n, bias=epsc[0:dst.shape[0], :])
            ACT(dst, dst, AF.Exp, [wk], [wk], scale=-0.5)

        cm = sb("cm", [128, 8, 128])
        identb = sb("identb", [128, 128], BF16)
        band = sb("band", [128, 384], BF16)
        maskFB = sb("maskFB", [128, 4, 128])
        n16col = sb("n16col", [128, 2])
        epsc = sb("epsc", [128, 2])
        onec = sb("onec", [128, 2])
        negc = sb("negc", [128, 2])
        vc = sb("vc", [128, NTW])
        cst = sb("cst", [128, NTW, 16])
        wz = sb("wz", [33, 512])
        n1bc = sb("n1bc", [128, D])
        n2bc = sb("n2bc", [128, D])
        fnbc = sb("fnbc", [128, D])
        gnbc = sb("gnbc", [128, 512])
        rbbc = sb("rbbc", [128, 36])
        wr = sb("wr", [128, 8, 36])
        nmax = sb("nmax", [128, 16])
        n16col = n16col[:, 0:1]
        epsc = epsc[:, 0:1]
        onec = onec[:, 0:1]
        negc = negc[:, 0:1]

        DMA("sp", cm, cmat, [], ["cm"])
        DMA("pool", identb, cmat[:, 0, :], [], ["identb"])
        DMA("pool", band, band3, [], ["band"])
        DMA("sp", vc, vcol, [], ["vc"])
        DMA("sp", cst, cs_t, [], ["cst"])
        DMA("sp", wz, wz_d, [], ["wz"])
        DMA("sp", n1bc, vecs[0:1, :].partition_broadcast(128), [], ["n1bc"])
        DMA("sp", n2bc, vecs[1:2, :].partition_broadcast(128), [], ["n2bc"])
        DMA("sp", fnbc, vecs[2:3, :].partition_broadcast(128), [], ["fnbc"])
        DMA("sp", gnbc, vecs[3:4, 0:512].partition_broadcast(128), [], ["gnbc"])
        DMA("sp", rbbc, rb_d[0:1, :].partition_broadcast(128), [], ["rbbc"])
        DMA("sp", wr, wr_d.rearrange("(c p) n -> p c n", p=128), [], ["wr"])
        P.op("dve", lambda e: e.memset(n16col, -1.0 / 16.0), writes=["n16col"])
        P.op("dve", lambda e: e.memset(epsc, EPS), writes=["epsc"])
        P.op("dve", lambda e: e.memset(onec, 1.0), writes=["onec"])
        P.op("dve", lambda e: e.memset(nmax, 0.0), writes=["nmax"])
        for h in range(4):
            CP("dve", maskFB[:, h, :], cm[:, 5 + h // 2, :], ["cm"], ["maskFB"])
        M0 = AR.off

        attnT = sb("attnT", [128, NTO, 512], BF16)
        qdT = sb("qdT", [128, NTO, 4, 128], BF16)
        SfT = sb("SfT", [128, NTO, 2, 128], BF16)
        SbT = sb("SbT", [128, NTO, 2, 128], BF16)
        M1 = AR.off
        win = sb("win", [128, 8, INW], BF16)
        for c in range(8):
            DMA("pool", win[:, c, :], w_in[c * 128:(c + 1) * 128, :], [], ["win%d" % c])
        winkeys = ["win%d" % c for c in range(8)]
        kvB = sb("kvB", [128, NTW - T0, 2, 128], BF16)
        decB = sb("decB", [128, NTW - T0, 2])
        Sf = sb("Sf", [128, 2, 128])
        Sb = sb("Sb", [128, 2, 128])
        P.op("dve", lambda e: e.memset(Sf, 0.0), writes=["Sf"])
        P.op("dve", lambda e: e.memset(Sb, 0.0), writes=["Sb"])
        zt = sb("zt", [128, 520], BF16)
        P.op("pool", lambda e: e.memset(zt, 0.0), writes=["zt"])
        for blk in range(HALO // 128):
            for base in (0, HALO + WIN):
                r0 = base + blk * 128
                DMA("sp", KS[r0:r0 + 128, :], zt[:, 0:512], ["zt"], ["KS%d" % (r0 // 128)])
                DMA("sp", VS[r0:r0 + 128, :], zt, ["zt"], ["VS%d" % (r0 // 128)])
        xt = [sb("xt%d" % i, [128, D]) for i in range(2)]
        junk = sb("junk", [128, D], BF16)
        xn = [sb("xn%d" % i, [128, D], BF16) for i in range(2)]
        xnT = [sb("xnT%d" % i, [128, 8, 128], BF16) for i in range(2)]
        ssq = sb("ssq", [128, 2])
        rstd = sb("rstd", [128, 2])
        qk = [sb("qk%d" % i, [128, 512]) for i in range(2)]
        vbf = [sb("vbf%d" % i, [128, 512], BF16) for i in range(2)]
        gbf = [sb("gbf%d" % i, [128, 512], BF16) for i in range(2)]
        lr = [sb("lr%d" % i, [128, 32]) for i in range(2)]
        aqr = [sb("aqr%d" % i, [128, 8, 64]) for i in range(2)]
        akr = [sb("akr%d" % i, [128, 8, 64]) for i in range(2)]
        vab = [sb("vab%d" % i, [128, 8, 65], BF16) for i in range(2)]
        lrT = sb("lrT", [33, 128])
        ez = sb("ez", [128, 512])
        spl = sb("spl", [128, 512])
        E1 = sb("E1", [128, 512])
        E2 = sb("E2", [128, 512])
        E3 = sb("E3", [128, 512])
        dec = sb("dec", [128, 4])
        qd = sb("qd", [128, 512], BF16)
        ki = sb("ki", [128, 512], BF16)
        ke = sb("ke", [128, 512], BF16)
        kiT = sb("kiT", [128, 4, 128], BF16)
        qrb = [sb("qrb%d" % i, [128, 512], BF16) for i in range(2)]
        krb = [sb("krb%d" % i, [128, 512], BF16) for i in range(2)]
        rta = sb("rta", [128, 8, 8])
        rtb = sb("rtb", [128, 8, 8])
        rtc = sb("rtc", [128, 8, 8])
        rtd = sb("rtd", [128, 8, 8])
        sqs = sb("sqs", [128, 8, 64])
        nrm = sb("nrm", [128, 16])
        P.op("dve", lambda e: e.memset(lrT[32:33, :], 1.0), writes=["lrT_one"])
        tiles = list(range(NTW) if dbg_tiles is None else dbg_tiles)

        def S1(i):
            own = T0 <= i < T0 + NTO
            io = i - T0
            b2 = i % 2
            xtk, xnk, xnTk = "xt%d" % b2, "xn%d" % b2, "xnT%d" % b2
            if i == tiles[0]:
                DMA("sp", xt[b2], xw[i * 128:(i + 1) * 128, :], [], [xtk])
            if i + 1 < NTW and (dbg_tiles is None):
                DMA("sp", xt[(i + 1) % 2], xw[(i + 1) * 128:(i + 2) * 128, :], [], ["xt%d" % ((i + 1) % 2)])
            sk, rk = "ssq%d" % b2, "rstd%d" % b2
            P.op("act", lambda e, b2=b2: e.activation(out=junk, in_=xt[b2], func=AF.Square, accum_out=ssq[:, b2:b2 + 1]),
                 reads=[xtk], writes=["junk", sk])
            rstd_from_ssq(rstd[:, b2:b2 + 1], ssq[:, b2:b2 + 1], D, sk, rk)
            STT(xn[b2], xt[b2], rstd[:, b2:b2 + 1], n1bc, ALU.mult, ALU.mult, [xtk, rk, "n1bc"], [xnk])
            pbt, pbk = next_pb()

            def tr_fn(e, b2=b2, pbt=pbt):
                ins = None
                for c in range(8):
                    ins = e.transpose(out=pbt[:, c * 128:(c + 1) * 128], in_=xn[b2][:, c * 128:(c + 1) * 128], identity=identb)
                return ins
            P.op("pe", tr_fn, reads=[xnk, "identb"], writes=[pbk])
            CP("act", xnT[b2].rearrange("p c t -> p (c t)"), pbt[:, :], [pbk], [xnTk])

            def proj(c0, c1):
                pt, pk = next_pf()
                n = c1 - c0
                mm_group(pt[:, 0:n], [(xnT[b2][:, c, :], win[:, c, c0:c1]) for c in range(8)], pk, [xnTk] + winkeys)
                return pt, pk

            if own:
                pt, pk = proj(0, 512)
                CP("act", qk[b2], pt[:, 0:512], [pk], ["qk%d" % b2])
            else:
                pt, pk = proj(256, 512)
                CP("act", qk[b2][:, 256:512], pt[:, 0:256], [pk], ["qk%d" % b2])
            pt, pk = proj(512, 1024)
            CP("dve", vbf[b2], pt[:, 0:512], [pk], ["vbf%d" % b2])
            if own:
                DMA("sp", GV[io * 128:(io + 1) * 128, :], vbf[b2], ["vbf%d" % b2], ["GV%d" % io])
                pt, pk = proj(1024, 1536)
                CP("act", gbf[b2], pt[:, 0:512], [pk], ["gbf%d" % b2])
                DMA("sp", GG[io * 128:(io + 1) * 128, :], gbf[b2], ["gbf%d" % b2], ["GG%d" % io])
            pt, pk = proj(1536, 1568)
            CP("dve", lr[b2], pt[:, 0:32], [pk], ["lr%d" % b2])
            if own:
                pt, pk = proj(1568, 2080)
                CP("dve", aqr[b2].rearrange("p h d -> p (h d)"), pt[:, 0:512], [pk], ["aqr%d" % b2])
            pt, pk = proj(2080, 2592)
            CP("act", akr[b2].rearrange("p h d -> p (h d)"), pt[:, 0:512], [pk], ["akr%d" % b2])
            pt, pk = proj(2592, 3104)
            r0k = HALO + i * 128
            CP("act", vab[b2][:, :, 0:64], pt[:, 0:512].rearrange("p (h d) -> p h d", h=8), [pk], ["vab%d" % b2])
            CP("dve", vab[b2][:, :, 64:65], vc[:, i:i + 1].unsqueeze(1).broadcast_to([128, 8, 1]), ["vc"], ["vab%d" % b2])
            DMA("sp", VS[r0k:r0k + 128, :], vab[b2].rearrange("p h d -> p (h d)"), ["vab%d" % b2], ["VS%d" % (r0k // 128)])

        def S2(i):
            own = T0 <= i < T0 + NTO
            left = i < T0
            io = i - T0
            b2 = i % 2
            qkb = qk[b2]
            qkk = "qk%d" % b2
            vkey = "vbf%d" % b2
            vt = vbf[b2]
            pt, pk = next_pf()
            P.op("pe", lambda e, pt=pt: e.transpose(out=pt[0:32, 0:128], in_=lr[b2], identity=cm[:, 0, :]), reads=["lr%d" % b2, "cm"], writes=[pk])
            CP("dve", lrT[0:32, :], pt[0:32, 0:128], [pk], ["lrT"])
            pz, pzk = next_pf()
            P.op("pe", lambda e, pz=pz: e.matmul(pz[:, :], lhsT=lrT, rhs=wz, start=True, stop=True),
                 reads=["lrT", "lrT_one", "wz"], writes=[pzk])
            ACT(ez, pz[:, :], AF.Exp, [pzk], ["ez"], scale=-1.0)
            ACT(spl, ez, AF.Ln, ["ez", "onec"], ["spl"], bias=onec, scale=1.0)
            pbb, pbbk = next_pf()
            P.op("pe", lambda e, pbb=pbb: (e.matmul(pbb[:, 0:256], lhsT=cm[:, 1, :], rhs=spl[:, 0:256], start=True, stop=True),
                                           e.matmul(pbb[:, 256:512], lhsT=cm[:, 2, :], rhs=spl[:, 256:512], start=True, stop=True))[1],
                 reads=["cm", "spl"], writes=[pbbk])
            ACT(E1, pbb[:, :], AF.Exp, [pbbk], ["E1"])
            ACT(E2, pbb[:, :], AF.Exp, [pbbk], ["E2"], scale=-1.0)
            pb3, pb3k = next_pf()
            P.op("pe", lambda e, pb3=pb3: (e.matmul(pb3[:, 0:256], lhsT=cm[:, 3, :], rhs=spl[:, 0:256], start=True, stop=True),
                                           e.matmul(pb3[:, 256:512], lhsT=cm[:, 4, :], rhs=spl[:, 256:512], start=True, stop=True))[1],
                 reads=["cm", "spl"], writes=[pb3k])
            ACT(E3, pb3[:, :], AF.Exp, [pb3k], ["E3"])
            pdc, pdck = next_pf()

            def dec_fn(e, pdc=pdc):
                ins = None
                for j in range(4):
                    ins = e.matmul(pdc[:, j:j + 1], lhsT=spl[:, j * 128:(j + 1) * 128], rhs=n16col, start=True, stop=True)
                return ins
            P.op("pe", dec_fn, reads=["spl", "n16col"], writes=[pdck])
            ACT(dec, pdc[:, 0:4], AF.Exp, [pdck], ["dec"])
            if own:
                STT(qd[:, 0:256], qkb[:, 0:256], 0.125, E1[:, 0:256], ALU.mult, ALU.mult, [qkk, "E1"], ["qd"])
                STT(qd[:, 256:512], qkb[:, 0:256], 0.125, E1[:, 256:512], ALU.mult, ALU.mult, [qkk, "E1"], ["qd"])
                TT("pool", ki[:, 0:256], qkb[:, 256:512], E2[:, 0:256], ALU.mult, [qkk, "E2"], ["ki"])
                TT("pool", ki[:, 256:512], qkb[:, 256:512], E2[:, 256:512], ALU.mult, [qkk, "E2"], ["ki"])
            TT("pool", ke[:, 0:256], qkb[:, 256:512], E3[:, 0:256], ALU.mult, [qkk, "E3"], ["ke"])
            TT("pool", ke[:, 256:512], qkb[:, 256:512], E3[:, 256:512], ALU.mult, [qkk, "E3"], ["ke"])
            if own:
                pbt, pbk = next_pb()

                def tr2_fn(e, pbt=pbt):
                    ins = None
                    for j in range(4):
                        ins = e.transpose(out=pbt[:, j * 128:(j + 1) * 128], in_=qd[:, j * 128:(j + 1) * 128], identity=identb)
                    for j in range(4):
                        ins = e.transpose(out=pbt[:, 512 + j * 128:512 + (j + 1) * 128], in_=ki[:, j * 128:(j + 1) * 128], identity=identb)
                    return ins
                P.op("pe", tr2_fn, reads=["qd", "ki", "identb"], writes=[pbk])
                CP("act", qdT[:, io, :, :].rearrange("p c t -> p (c t)"), pbt[:, 0:512], [pbk], ["qdT%d" % io])
                CP("dve", kiT.rearrange("p c t -> p (c t)"), pbt[:, 512:1024], [pbk], ["kiT"])
                paX, paXk = next_pf()
                paY, paYk = next_pf()

                def att_fn(e, pa, par, io=io):
                    ins = None
                    p0 = par * 64
                    for dirn in range(2):
                        for pr in range(2):
                            blk = dirn * 2 + pr
                            sl = dirn * 2 + pr
                            ins = e.matmul(pa[:, sl * 128:(sl + 1) * 128], lhsT=kiT[p0:p0 + 64, blk, :], rhs=qdT[p0:p0 + 64, io, blk, :],
                                           start=True, stop=True)
                    return ins
                P.op("pe", lambda e, pa=paX, f=att_fn: f(e, pa, 0), reads=["kiT", "qdT%d" % io], writes=[paXk])
                P.op("pe", lambda e, pa=paY, f=att_fn: f(e, pa, 1), reads=["kiT", "qdT%d" % io], writes=[paYk])
                TT("dve", ez, paX[:, :], maskFB.rearrange("p h c -> p (h c)"), ALU.mult, [paXk, "maskFB"], ["ez"])
                TT("dve", E1, paY[:, :], maskFB.rearrange("p h c -> p (h c)"), ALU.mult, [paYk, "maskFB"], ["E1"])
                av = attnT[:, io, :].rearrange("p (a b c) -> p a b c", a=2, b=2)
                TT("pool", av[:, :, 0, :], ez[:, 0:256].rearrange("p (a c) -> p a c", a=2), ez[:, 256:512].rearrange("p (a c) -> p a c", a=2),
                   ALU.add, ["ez"], ["attnT%d" % io])
                TT("pool", av[:, :, 1, :], E1[:, 0:256].rearrange("p (a c) -> p a c", a=2), E1[:, 256:512].rearrange("p (a c) -> p a c", a=2),
                   ALU.add, ["E1"], ["attnT%d" % io])
            for dirn in range(2):
                if dirn == 0 and i >= T0 + NTO:
                    continue
                if dirn == 1 and left:
                    continue
                pkv, pkvk = next_pf()

                def kv_fn(e, pkv=pkv, dirn=dirn, vt=vt):
                    ins = None
                    for pr in range(2):
                        ins = e.matmul(pkv[:, pr * 256:(pr + 1) * 256], lhsT=ke[:, dirn * 256 + pr * 128: dirn * 256 + (pr + 1) * 128],
                                       rhs=vt[:, pr * 256:(pr + 1) * 256], start=True, stop=True)
                    return ins
                P.op("pe", kv_fn, reads=["ke", vkey], writes=[pkvk])
                if dirn == 0:
                    if own:
                        CP("act", SfT[:, io, :, :].rearrange("p a b -> p (a b)"), Sf.rearrange("p a b -> p (a b)"), ["Sf"], ["SfT%d" % io])
                    for pr in range(2):
                        for hh in range(2):
                            p0 = hh * 64
                            STT(Sf[p0:p0 + 64, pr, :], Sf[p0:p0 + 64, pr, :], dec[p0:p0 + 64, pr:pr + 1],
                                pkv[p0:p0 + 64, pr * 256 + hh * 128: pr * 256 + (hh + 1) * 128], ALU.mult, ALU.add,
                                ["Sf", "dec", pkvk], ["Sf"])
                else:
                    ib = i - T0
                    for pr in range(2):
                        for hh in range(2):
                            p0 = hh * 64
                            CP("act", kvB[p0:p0 + 64, ib, pr, :], pkv[p0:p0 + 64, pr * 256 + hh * 128: pr * 256 + (hh + 1) * 128],
                               [pkvk], ["kvB%d" % ib])
                    CP("dve", decB[:, ib, :], dec[:, 2:4], ["dec"], ["decB%d" % ib])

            def rope(raw, rkey):
                cosb = cst[:, i, 0:8].unsqueeze(1).broadcast_to([128, 8, 8])
                sinb = cst[:, i, 8:16].unsqueeze(1).broadcast_to([128, 8, 8])
                TT("pool", rta, raw[:, :, 0:8], cosb, ALU.mult, [rkey, "cst"], ["rta"])
                TT("pool", rtb, raw[:, :, 8:16], sinb, ALU.mult, [rkey, "cst"], ["rtb"])
                TT("pool", rtc, raw[:, :, 8:16], cosb, ALU.mult, [rkey, "cst"], ["rtc"])
                TT("pool", rtd, raw[:, :, 0:8], sinb, ALU.mult, [rkey, "cst"], ["rtd"])
                TT("dve", raw[:, :, 0:8], rta, rtb, ALU.subtract, ["rta", "rtb"], [rkey])
                TT("dve", raw[:, :, 8:16], rtc, rtd, ALU.add, ["rtc", "rtd"], [rkey])

            def sqnorm(src, col0, skey):
                TT("pool", sqs, src, src, ALU.mult, [skey], ["sqs"])
                P.op("dve", lambda e: e.tensor_reduce(out=nrm[:, col0:col0 + 8], in_=sqs, axis=AX.X, op=ALU.add), reads=["sqs"], writes=["nrm"])
                TT("dve", nmax[:, col0:col0 + 8], nmax[:, col0:col0 + 8], nrm[:, col0:col0 + 8], ALU.max, ["nrm", "nmax"], ["nmax"])

            r0k = HALO + i * 128
            if own:
                rope(aqr[b2], "aqr%d" % b2)
                sqnorm(aqr[b2], 0, "aqr%d" % b2)
                CP("act", qrb[b2], aqr[b2].rearrange("p h d -> p (h d)"), ["aqr%d" % b2], ["qrb%d" % b2])
                DMA("sp", QS[io * 128:(io + 1) * 128, :], qrb[b2], ["qrb%d" % b2], ["QS%d" % io])
            rope(akr[b2], "akr%d" % b2)
            sqnorm(akr[b2], 8, "akr%d" % b2)
            CP("act", krb[b2], akr[b2].rearrange("p h d -> p (h d)"), ["akr%d" % b2], ["krb%d" % b2])
            DMA("sp", KS[r0k:r0k + 128, :], krb[b2], ["krb%d" % b2], ["KS%d" % (r0k // 128)])

        for n_ in range(len(tiles) + 1):
            ra, rb = [], []
            if n_ < len(tiles):
                P.rec = ra
                stream[0] = 0
                S1(tiles[n_])
            if n_ > 0:
                P.rec = rb
                stream[0] = 1
                S2(tiles[n_ - 1])
            P.rec = None
            stream[0] = None
            P.replay_merged(ra, rb)

        if stop_after == "A":
            P.op("sp", None, reads=[k_ for k_ in P.last_w.keys() if k_[:2] in ("QS", "KS", "VS", "GV", "GG")], writes=[])
            P.emit()
            return nc
        for i in range(NTW - 1, T0 - 1, -1):
            ib = i - T0
            if ib < NTO:
                CP("act", SbT[:, ib, :, :].rearrange("p a b -> p (a b)"), Sb.rearrange("p a b -> p (a b)"), ["Sb"], ["SbT%d" % ib])
            if i == T0:
                break
            for pr in range(2):
                STT(Sb[:, pr, :], Sb[:, pr, :], decB[:, ib, pr:pr + 1], kvB[:, ib, pr, :], ALU.mult, ALU.add,
                    ["Sb", "decB%d" % ib, "kvB%d" % ib], ["Sb"])

        P.barrier()
        AR.off = M1
        vb2 = [sb("vb2%d" % i, [128, 512], BF16) for i in range(2)]
        gb2 = [sb("gb2%d" % i, [128, 512], BF16) for i in range(2)]
        osb = sb("osb", [128, 512])
        osq = sb("osq", [128, 4, 128])
        oms = sb("oms", [128, 4])
        sgs = sb("sgs", [128, 512])
        ybf = sb("ybf", [128, 512])
        mixb = [sb("mixb%d" % i, [128, 512], BF16) for i in range(2)]
        for io in range(NTO):
            b2 = io % 2
            DMA("sp", vb2[b2], GV[io * 128:(io + 1) * 128, :], ["GV%d" % io], ["vb2%d" % b2])
            DMA("sp", gb2[b2], GG[io * 128:(io + 1) * 128, :], ["GG%d" % io], ["gb2%d" % b2])
            poX, poXk = next_pf()
            poY, poYk = next_pf()

            def o_fn(e, po, par, io=io, b2=b2):
                ins = None
                p0 = par * 64
                for pr in range(2):
                    h = pr * 2 + par
                    oap = po[:, pr * 128:(pr + 1) * 128]
                    e.matmul(oap, lhsT=attnT[:, io, h * 128:(h + 1) * 128], rhs=vb2[b2][:, h * 128:(h + 1) * 128], start=True, stop=False)
                    e.matmul(oap, lhsT=qdT[p0:p0 + 64, io, pr, :], rhs=SfT[p0:p0 + 64, io, pr, :], start=False, stop=False)
                    ins = e.matmul(oap, lhsT=qdT[p0:p0 + 64, io, 2 + pr, :], rhs=SbT[p0:p0 + 64, io, pr, :], start=False, stop=True)
                return ins
            rk_ = ["attnT%d" % io, "qdT%d" % io, "SfT%d" % io, "SbT%d" % io, "vb2%d" % b2]
            P.op("pe", lambda e, po=poX, f=o_fn: f(e, po, 0), reads=rk_, writes=[poXk])
            P.op("pe", lambda e, po=poY, f=o_fn: f(e, po, 1), reads=rk_, writes=[poYk])
            ov = osb.rearrange("p (a b c) -> p a b c", a=2, b=2)
            CP("act", ov[:, :, 0, :], poX[:, 0:256].rearrange("p (a c) -> p a c", a=2), [poXk], ["osb"])
            CP("act", ov[:, :, 1, :], poY[:, 0:256].rearrange("p (a c) -> p a c", a=2), [poYk], ["osb"])
            TT("pool", osq.rearrange("p h d -> p (h d)"), osb, osb, ALU.mult, ["osb"], ["osq"])
            P.op("dve", lambda e: e.tensor_reduce(out=oms, in_=osq, axis=AX.X, op=ALU.add), reads=["osq"], writes=["oms"])
            rstd_from_ssq(oms, oms, 128, "oms", "oms")
            ACT(sgs, gb2[b2], AF.Silu, ["gb2%d" % b2], ["sgs"])
            TT("dve", ybf, osb, gnbc, ALU.mult, ["osb", "gnbc"], ["ybf"])
            for h in range(4):
                STT(mixb[b2][:, h * 128:(h + 1) * 128], ybf[:, h * 128:(h + 1) * 128], oms[:, h:h + 1], sgs[:, h * 128:(h + 1) * 128],
                    ALU.mult, ALU.mult, ["ybf", "oms", "sgs"], ["mixb%d" % b2])
            DMA("sp", MG[io * 128:(io + 1) * 128, :], mixb[b2], ["mixb%d" % b2], ["MG%d" % io])
        MGK = ["MG%d" % i for i in range(NTO)]
        if stop_after == "G2":
            P.op("sp", None, reads=QSK + KSK + VSK + MGK, writes=[])
            P.emit()
            return nc

        P.barrier()
        AR.off = M0
        nm2 = sb("nm2", [128, 2])
        m2 = sb("m2", [2, 2])
        m1 = sb("m1", [1, 4])
        P.op("dve", lambda e: e.tensor_reduce(out=nm2, in_=nmax.rearrange("p (a h) -> p a h", a=2), axis=AX.X, op=ALU.max),
             reads=["nmax"], writes=["nm2"])
        pt, pk = next_pf()
        P.op("pe", lambda e, pt=pt: e.transpose(out=pt[0:2, 0:128], in_=nm2, identity=cm[:, 0, :]), reads=["nm2", "cm"], writes=[pk])
        P.op("dve", lambda e, pt=pt: e.tensor_reduce(out=m2[:, 0:1], in_=pt[0:2, 0:128], axis=AX.X, op=ALU.max), reads=[pk], writes=["m2"])
        pt, pk = next_pf()
        P.op("pe", lambda e, pt=pt: e.transpose(out=pt[0:1, 0:2], in_=m2[:, 0:1], identity=cm[0:2, 0, 0:2]), reads=["m2", "cm"], writes=[pk])
        CP("dve", m1[:, 0:2], pt[0:1, 0:2], [pk], ["m1"])
        TT("dve", m1[:, 2:3], m1[:, 0:1], m1[:, 1:2], ALU.mult, ["m1"], ["m1"])
        ACT(m1[:, 3:4], m1[:, 2:3], AF.Ln, ["m1"], ["m1"])
        ACT(m1[:, 3:4], m1[:, 3:4], AF.Exp, ["m1"], ["m1"], scale=0.5)
        TS("dve", m1[:, 3:4], m1[:, 3:4], -0.125, None, ALU.mult, None, ["m1"], ["m1"])
        pt, pk = next_pf()
        P.op("pe", lambda e, pt=pt: e.matmul(pt[:, 0:1], lhsT=cm[0:1, 7, :], rhs=m1[:, 3:4], start=True, stop=True), reads=["m1", "cm"], writes=[pk])
        CP("dve", negc, pt[:, 0:1], [pk], ["negc"])

        accT = sb("accT", [65, 8, OWN])
        NQ = 8
        qsb2 = [[sb("qsb%d" % i, [128, 512], BF16) for i in range(NQ)] for _ in range(2)]
        ksb2 = [[sb("ksb%d" % i, [128, 512], BF16) for i in range(NQ + 2)] for _ in range(2)]
        vsb2 = [[sb("vsb%d" % i, [128, 8, 65], BF16) for i in range(NQ + 2)] for _ in range(2)]
        qT2 = [[sb("qT%d" % i, [128, 4, 128], BF16) for i in range(NQ)] for _ in range(2)]
        kT2 = [[sb("kT%d" % i, [128, 4, 128], BF16) for i in range(NQ + 2)] for _ in range(2)]
        pex = [sb("pex%d" % i, [128, 384], BF16) for i in range(4)]
        pmk = [sb("pmk%d" % i, [128, 384], BF16) for i in range(4)]
        cnt4 = [0]
        jobs = [(1, 0, 0, 8), (1, 0, 8, 8)] + [(4, r, 0, 4) for r in range(4)] + [(16, r, 0, 1) for r in range(16)]
        for jn, (dd, r, j0, nq) in enumerate(jobs):
            js = jn % 2
            qsb, ksb, vsb, qT, kT = qsb2[js], ksb2[js], vsb2[js], qT2[js], kT2[js]
            QSv = QS.rearrange("(n d) c -> d n c", d=dd)
            KSv = KS.rearrange("(n d) c -> d n c", d=dd)
            VSv = VS.rearrange("(n d) c -> d n c", d=dd)
            accv = accT.rearrange("p h (n d) -> p h d n", d=dd)
            for jq in range(nq):
                n0 = 128 * (j0 + jq)
                DMA("sp", qsb[jq], QSv[r, n0:n0 + 128, :], QSK, [("qsb" + str(js) + "_%d") % jq])
            for kk in range(nq + 2):
                n0 = 2048 // dd + 128 * (j0 + kk - 1)
                DMA("sp", ksb[kk], KSv[r, n0:n0 + 128, :], KSK, [("ksb" + str(js) + "_%d") % kk])
                DMA("sp", vsb[kk].rearrange("p h d -> p (h d)"), VSv[r, n0:n0 + 128, :], VSK, [("vsb" + str(js) + "_%d") % kk])
            tl = [(qsb[jq], ("qsb" + str(js) + "_%d") % jq, qT[jq], ("qT" + str(js) + "_%d") % jq) for jq in range(nq)] + \
                 [(ksb[kk], ("ksb" + str(js) + "_%d") % kk, kT[kk], ("kT" + str(js) + "_%d") % kk) for kk in range(nq + 2)]
            for t0 in range(0, len(tl), 2):
                grp = tl[t0:t0 + 2]
                pbt, pbk = next_pb()

                def trq_fn(e, grp=grp, pbt=pbt):
                    ins = None
                    for gi, (src, _, _, _) in enumerate(grp):
                        for c in range(4):
                            ins = e.transpose(out=pbt[:, gi * 512 + c * 128: gi * 512 + (c + 1) * 128], in_=src[:, c * 128:(c + 1) * 128], identity=identb)
                    return ins
                P.op("pe", trq_fn, reads=[g[1] for g in grp] + ["identb"], writes=[pbk])
                for gi, (_, _, dst, dk) in enumerate(grp):
                    CP("act" if gi == 0 else "dve", dst.rearrange("p c t -> p (c t)"), pbt[:, gi * 512:(gi + 1) * 512], [pbk], [dk])
            for jq in range(nq):
                for hg in range(2):
                    bufs = []
                    for h in range(hg * 4, hg * 4 + 4):
                        p0 = (h % 2) * 64
                        blk = h // 2
                        pS, pSk = next_pf()

                        def s_fn(e, pS=pS, jq=jq, p0=p0, blk=blk, kT=kT, qT=qT):
                            ins = None
                            for sl in range(3):
                                ins = e.matmul(pS[:, sl * 128:(sl + 1) * 128], lhsT=kT[jq + sl][p0:p0 + 64, blk, :], rhs=qT[jq][p0:p0 + 64, blk, :],
                                               start=True, stop=True)
                            return ins
                        P.op("pe", s_fn, reads=[("kT" + str(js) + "_%d") % (jq + sl) for sl in range(3)] + [("qT" + str(js) + "_%d") % jq], writes=[pSk])
                        bi = cnt4[0] % 4
                        cnt4[0] += 1
                        ACT(pex[bi], pS[:, 0:384], AF.Exp, [pSk, "negc"], ["pex%d" % bi], bias=negc, scale=0.125)
                        TT("pool" if (h % 2) else "dve", pmk[bi], pex[bi], band, ALU.mult, ["pex%d" % bi, "band"], ["pmk%d" % bi])
                        bufs.append(bi)
                    pU, pUk = next_pf()

                    def pv_fn(e, pU=pU, jq=jq, hg=hg, bufs=tuple(bufs), vsb=vsb):
                        ins = None
                        for hi in range(4):
                            h = hg * 4 + hi
                            for sl in range(3):
                                ins = e.matmul(pU[0:65, hi * 128:(hi + 1) * 128], lhsT=vsb[jq + sl][:, h, :], rhs=pmk[bufs[hi]][:, sl * 128:(sl + 1) * 128],
                                               start=(sl == 0), stop=(sl == 2))
                        return ins
                    P.op("pe", pv_fn, reads=[("vsb" + str(js) + "_%d") % (jq + sl) for sl in range(3)] + ["pmk%d" % b for b in bufs], writes=[pUk])
                    n0 = 128 * (j0 + jq)
                    dst = accv[:, hg * 4:hg * 4 + 4, r, n0:n0 + 128]
                    src = pU[0:65, :].rearrange("p (h t) -> p h t", h=4)
                    akey = "accT"
                    if dd == 1:
                        CP("dve", dst, src, [pUk], [akey])
                    else:
                        TT("dve", dst, src, dst, ALU.add, [pUk, akey], [akey])
        rz = sb("rz", [64, 512])
        otb = [sb("otb%d" % i, [64, 512], BF16) for i in range(2)]
        k2 = 0
        for h in range(8):
            for g in range(4):
                pz, pzk = next_pf()
                P.op("pe", lambda e, pz=pz, h=h, g=g: e.matmul(pz[0:64, :], lhsT=cm[64:65, 7, 0:64], rhs=accT[64:65, h, g * 512:(g + 1) * 512],
                                                                start=True, stop=True), reads=["accT", "cm"], writes=[pzk])
                P.op("dve", lambda e, pz=pz: e.reciprocal(out=rz, in_=pz[0:64, :]), reads=[pzk], writes=["rz"])
                b2 = k2 % 2
                k2 += 1
                TT("pool", otb[b2], accT[0:64, h, g * 512:(g + 1) * 512], rz, ALU.mult, ["accT", "rz"], ["otb%d" % b2])
                DMA("sp", OTS[h, :, g * 512:(g + 1) * 512], otb[b2], ["otb%d" % b2], ["OTS%d_%d" % (h, g)])
        OTK = ["OTS%d_%d" % (h, g) for h in range(8) for g in range(4)]
        if stop_after == "B":
            P.op("sp", None, reads=OTK + MGK, writes=[])
            P.emit()
            return nc

        P.barrier()
        AR.off = M0
        bc_cache = {}

        def bcreg(e):
            if "r" not in bc_cache:
                bc_cache["r"] = e.to_reg(2559)
            return bc_cache["r"]
        CAPG = 640
        NSLOT = 4 * CAPG
        OOB = 4096.0
        u2tok = sb("u2tok", [128, NTO, D], BF16)
        OH = sb("OH", [128, NTO, 4])
        WE = sb("WE", [128, NTO, 8])
        idxf = sb("idxf", [128, NTO])
        idxi = sb("idxi", [128, 2 * NTO], I32)
        goffm = sb("goffm", [128, 4])
        pren = sb("pren", [128, 4])
        M2 = AR.off
        woutG = sb("woutG", [128, 4, D], BF16)
        woutA = sb("woutA", [64, 8, D], BF16)
        DMA("pool", woutG, w_out[0:512, :].rearrange("(c p) n -> p c n", p=128), [], ["woutG"])
        DMA("pool", woutA, w_out[512:1024, :].rearrange("(h p) n -> p h n", p=64), [], ["woutA"])
        for g in range(4):
            P.op("dve", lambda e, g=g: e.memset(goffm[:, g:g + 1], float(g * CAPG) - OOB), writes=["goffm"])
        P.op("dve", lambda e: e.memset(pren, 0.0), writes=["pren"])
        zx = sb("zx", [128, D], BF16)
        zw = sb("zw", [128, 8])
        P.op("pool", lambda e: e.memset(zx, 0.0), writes=["zx"])
        P.op("pool", lambda e: e.memset(zw, 0.0), writes=["zw"])
        for r0 in range(0, NSLOT, 128):
            DMA("sp", XB[r0:r0 + 128, :], zx, ["zx"], ["XB"])
            DMA("sp", WB[r0:r0 + 128, :], zw, ["zw"], ["WB"])
        xo = [sb("xo%d" % i, [128, D]) for i in range(2)]
        mgl = [sb("mgl%d" % i, [128, 512], BF16) for i in range(2)]
        otl = [sb("otl%d" % i, [64, 8, 128], BF16) for i in range(2)]
        mgT = sb("mgT", [128, 4, 128], BF16)
        h2t = [sb("h2t%d" % i, [128, D]) for i in range(2)]
        u2 = sb("u2", [128, D])
        u2Tf = sb("u2Tf", [128, 8, 128])
        junk2 = sb("junk2", [128, D], BF16)
        ss2 = sb("ss2", [128, 2])
        rs2 = sb("rs2", [128, 2])
        lg = sb("lg", [128, 36])
        sm = sb("sm", [128, 64])
        for io in range(NTO):
            b2 = io % 2
            DMA("sp", xo[b2], xw[HALO + io * 128: HALO + (io + 1) * 128, :], [], ["xo%d" % b2])
            DMA("sp", mgl[b2], MG[io * 128:(io + 1) * 128, :], ["MG%d" % io], ["mgl%d" % b2])
            DMA("sp", otl[b2], OTS[:, :, io * 128:(io + 1) * 128].rearrange("h p t -> p h t"), OTK, ["otl%d" % b2])
            pbt, pbk = next_pb()

            def trm_fn(e, pbt=pbt, b2=b2):
                ins = None
                for c in range(4):
                    ins = e.transpose(out=pbt[:, c * 128:(c + 1) * 128], in_=mgl[b2][:, c * 128:(c + 1) * 128], identity=identb)
                return ins
            P.op("pe", trm_fn, reads=["mgl%d" % b2, "identb"], writes=[pbk])
            CP("act", mgT.rearrange("p c t -> p (c t)"), pbt[:, 0:512], [pbk], ["mgT"])
            for cg in range(2):
                pt, pk = next_pf()
                pairs = [(mgT[:, c, :], woutG[:, c, cg * 512:(cg + 1) * 512]) for c in range(4)] + \
                        [(otl[b2][:, h, :], woutA[:, h, cg * 512:(cg + 1) * 512]) for h in range(8)]
                mm_group(pt[:, :], pairs, pk, ["mgT", "otl%d" % b2, "woutG", "woutA"])
                TT("dve", h2t[b2][:, cg * 512:(cg + 1) * 512], pt[:, :], xo[b2][:, cg * 512:(cg + 1) * 512], ALU.add,
                   [pk, "xo%d" % b2], ["h2t%d" % b2])
            DMA("sp", H2[io * 128:(io + 1) * 128, :], h2t[b2], ["h2t%d" % b2], ["H2_%d" % io])
            P.op("act", lambda e, b2=b2: e.activation(out=junk2, in_=h2t[b2], func=AF.Square, accum_out=ss2[:, 0:1]),
                 reads=["h2t%d" % b2], writes=["junk2", "ss2"])
            rstd_from_ssq(rs2[:, 0:1], ss2[:, 0:1], D, "ss2", "rs2")
            STT(u2, h2t[b2], rs2[:, 0:1], n2bc, ALU.mult, ALU.mult, ["h2t%d" % b2, "rs2", "n2bc"], ["u2"])
            CP("pool", u2tok[:, io, :], u2, ["u2"], ["u2tok%d" % io])
            for half in range(2):
                pt, pk = next_pf()

                def tru_fn(e, pt=pt, half=half):
                    ins = None
                    for c in range(4):
                        cc = half * 4 + c
                        ins = e.transpose(out=pt[:, c * 128:(c + 1) * 128], in_=u2[:, cc * 128:(cc + 1) * 128], identity=cm[:, 0, :])
                    return ins
                P.op("pe", tru_fn, reads=["u2", "cm"], writes=[pk])
                CP("act", u2Tf[:, half * 4:half * 4 + 4, :].rearrange("p c t -> p (c t)"), pt[:, :], [pk], ["u2Tf%d" % half])
            pr_, prk = next_pf()
            mm_group(pr_[:, 0:36], [(u2Tf[:, c, :], wr[:, c, :]) for c in range(8)], prk, ["u2Tf0", "u2Tf1", "wr"])
            TT("dve", lg, pr_[:, 0:36], rbbc, ALU.add, [prk, "rbbc"], ["lg"])
            gmax, ngmax, gsum, gw = sm[:, 0:1], sm[:, 1:2], sm[:, 2:3], sm[:, 3:4]
            oh = OH[:, io, :]
            ohk = "OH%d" % io
            ge = sm[:, 8:12]
            esel = sm[:, 16:24]
            top8 = sm[:, 24:32]
            d21, w1g, w2g = sm[:, 32:33], sm[:, 33:34], sm[:, 34:35]
            wa = sm[:, 40:48]
            wb_ = sm[:, 48:56]
            P.op("dve", lambda e: e.tensor_reduce(out=gmax, in_=lg[:, 0:4], axis=AX.X, op=ALU.max), reads=["lg"], writes=["sm"])
            TS("dve", oh, lg[:, 0:4], gmax, None, ALU.is_equal, None, ["lg", "sm"], [ohk])
            TS("dve", ngmax, gmax, -1.0, None, ALU.mult, None, ["sm"], ["sm"])
            ACT(ge, lg[:, 0:4], AF.Exp, ["lg", "sm"], ["sm"], bias=ngmax, scale=1.0)
            P.op("dve", lambda e: e.tensor_reduce(out=gsum, in_=ge, axis=AX.X, op=ALU.add), reads=["sm"], writes=["sm"])
            P.op("dve", lambda e: e.reciprocal(out=gw, in_=gsum), reads=["sm"], writes=["sm"])
            TS("dve", esel, lg[:, 4:12], oh[:, 0:1], None, ALU.mult, None, ["lg", ohk], ["sm"])
            for g in range(1, 4):
                STT(esel, lg[:, 4 + 8 * g:12 + 8 * g], oh[:, g:g + 1], esel, ALU.mult, ALU.add, ["lg", ohk, "sm"], ["sm"])
            P.op("dve", lambda e: e.max(out=top8, in_=esel), reads=["sm"], writes=["sm"])
            TT("dve", d21, top8[:, 1:2], top8[:, 0:1], ALU.subtract, ["sm"], ["sm"])
            ACT(d21, d21, AF.Exp, ["sm"], ["sm"])
            TS("dve", d21, d21, 1.0, None, ALU.add, None, ["sm"], ["sm"])
            P.op("dve", lambda e: e.reciprocal(out=w1g, in_=d21), reads=["sm"], writes=["sm"])
            TT("dve", w1g, w1g, gw, ALU.mult, ["sm"], ["sm"])
            TT("dve", w2g, gw, w1g, ALU.subtract, ["sm"], ["sm"])
            TS("dve", wa, esel, top8[:, 0:1], w1g, ALU.is_equal, ALU.mult, ["sm"], ["sm"])
            TS("dve", wb_, esel, top8[:, 1:2], w2g, ALU.is_equal, ALU.mult, ["sm"], ["sm"])
            TT("dve", WE[:, io, :], wa, wb_, ALU.add, ["sm"], ["WE%d" % io])
            prk_t, prkk = next_pf()
            P.op("pe", lambda e, t=prk_t, io=io: (e.matmul(t[:, 0:4], lhsT=cm[:, 4, :], rhs=OH[:, io, :], start=True, stop=False),
                                                   e.matmul(t[:, 0:4], lhsT=cm[:, 7, :], rhs=pren, start=False, stop=True))[1],
                 reads=[ohk, "pren", "cm"], writes=[prkk])
            rk = sm[:, 56:60]
            okm = sm[:, 60:64]
            TS("dve", rk, prk_t[:, 0:4], -16.0, None, ALU.mult, None, [prkk], ["sm"])
            STT(pren, oh, -1.0 / 16.0, pren, ALU.mult, ALU.add, [ohk, "pren", prkk], ["pren"])
            TS("dve", okm, rk, float(CAPG), None, ALU.is_lt, None, ["sm"], ["sm"])
            TT("dve", okm, okm, oh, ALU.mult, ["sm", ohk], ["sm"])
            TT("dve", rk, rk, goffm, ALU.add, ["sm", "goffm"], ["sm"])
            TT("dve", rk, rk, okm, ALU.mult, ["sm"], ["sm"])
            P.op("dve", lambda e, io=io: e.tensor_reduce(out=idxf[:, io:io + 1], in_=rk, axis=AX.X, op=ALU.add), reads=["sm"], writes=["idxf%d" % io])
            TS("dve", idxf[:, io:io + 1], idxf[:, io:io + 1], OOB, None, ALU.add, None, ["idxf%d" % io], ["idxf%d" % io])
            CP("dve", idxi[:, io:io + 1], idxf[:, io:io + 1], ["idxf%d" % io], ["idxi%d" % io])
            P.op("pool", lambda e, io=io: e.indirect_dma_start(out=XB[:, :], out_offset=bass.IndirectOffsetOnAxis(ap=idxi[:, io:io + 1], axis=0),
                                                               in_=u2tok[:, io, :], in_offset=None, bounds_check=bcreg(e), oob_is_err=False),
                 reads=["u2tok%d" % io, "idxi%d" % io, "XB"], writes=["XBs%d" % io], dma=True)
            P.op("pool", lambda e, io=io: e.indirect_dma_start(out=WB[:, :], out_offset=bass.IndirectOffsetOnAxis(ap=idxi[:, io:io + 1], axis=0),
                                                               in_=WE[:, io, :], in_offset=None, bounds_check=bcreg(e), oob_is_err=False),
                 reads=["WE%d" % io, "idxi%d" % io, "WB"], writes=["WBs%d" % io], dma=True)
        H2K = ["H2_%d" % i for i in range(NTO)]
        XBK = ["XBs%d" % i for i in range(NTO)] + ["XB"]
        WBK = ["WBs%d" % i for i in range(NTO)] + ["WB"]
        if debug:
            DMA("sp", WTD[:, 0:NTO], idxf, ["idxf%d" % i for i in range(NTO)], ["WTD"])
        if stop_after == "C1":
            P.op("sp", None, reads=H2K + XBK + WBK + ["WTD"], writes=[])
            P.emit()
            return nc

        P.barrier()
        AR.off = M2
        NCH = CAPG // 128
        xs = sb("xs", [128, NCH, D], BF16)
        xTg = sb("xTg", [128, 8, CAPG], BF16)
        wsl = sb("wsl", [128, NCH, 8])
        hid = sb("hid", [128, 4, CAPG], BF16)
        yacc = sb("yacc", [128, NCH, D])
        wgb = [sb("wgb%d" % i, [128, 8, 512], BF16) for i in range(2)]
        wub = [sb("wub%d" % i, [128, 8, 512], BF16) for i in range(2)]
        wdb = [sb("wdb%d" % i, [128, 4, D], BF16) for i in range(2)]
        sgb = [sb("sgb%d" % i, [128, 512]) for i in range(2)]

        def load_expert(ex):
            b = ex % 2
            DMA("pool", wgb[b], ewg[ex].rearrange("(c p) n -> p c n", p=128), [], ["wgb%d" % b])
            DMA("pool", wub[b], ewu[ex].rearrange("(c p) n -> p c n", p=128), [], ["wub%d" % b])
            DMA("pool", wdb[b], ewd[ex].rearrange("(c p) n -> p c n", p=128), [], ["wdb%d" % b])
        load_expert(0)
        kk2 = 0
        nsl = [(0, 512), (512, CAPG)]
        for g in range(4):
            DMA("sp", xs, XB[g * CAPG:(g + 1) * CAPG, :].rearrange("(c p) d -> p c d", p=128), XBK, ["xs"])
            DMA("sp", wsl, WB[g * CAPG:(g + 1) * CAPG, :].rearrange("(c p) d -> p c d", p=128), WBK, ["wsl"])
            for ch in range(NCH):
                pbt, pbk = next_pb()

                def trx_fn(e, pbt=pbt, ch=ch):
                    ins = None
                    for c in range(8):
                        ins = e.transpose(out=pbt[:, c * 128:(c + 1) * 128], in_=xs[:, ch, c * 128:(c + 1) * 128], identity=identb)
                    return ins
                P.op("pe", trx_fn, reads=["xs", "identb"], writes=[pbk])
                CP("act" if ch % 2 else "dve", xTg[:, :, ch * 128:(ch + 1) * 128], pbt[:, :].rearrange("p (c t) -> p c t", c=8), [pbk], ["xTg%d" % ch])
            XTK = ["xTg%d" % ch for ch in range(NCH)]
            for el in range(8):
                ex = g * 8 + el
                b = ex % 2
                if ex + 1 < NEXP:
                    load_expert(ex + 1)
                for (n0, n1) in nsl:
                    for fc in range(4):
                        pg, pgk = next_pf()
                        pu, puk = next_pf()
                        mm_group(pg[:, 0:n1 - n0], [(wgb[b][:, c, fc * 128:(fc + 1) * 128], xTg[:, c, n0:n1]) for c in range(8)], pgk,
                                 ["wgb%d" % b] + XTK)
                        mm_group(pu[:, 0:n1 - n0], [(wub[b][:, c, fc * 128:(fc + 1) * 128], xTg[:, c, n0:n1]) for c in range(8)], puk,
                                 ["wub%d" % b] + XTK)
                        sb_i = kk2 % 2
                        kk2 += 1
                        ACT(sgb[sb_i][:, 0:n1 - n0], pg[:, 0:n1 - n0], AF.Silu, [pgk], ["sgb%d" % sb_i])
                        TT("dve", hid[:, fc, n0:n1], sgb[sb_i][:, 0:n1 - n0], pu[:, 0:n1 - n0], ALU.mult, ["sgb%d" % sb_i, puk], ["hid%d_%d" % (fc, n0)])
                HK = ["hid%d_%d" % (fc, n0) for fc in range(4) for (n0, _) in nsl]
                for ch in range(NCH):
                    for cg in range(2):
                        py, pyk = next_pf()
                        mm_group(py[:, :], [(hid[:, fc, ch * 128:(ch + 1) * 128], wdb[b][:, fc, cg * 512:(cg + 1) * 512]) for fc in range(4)], pyk,
                                 ["wdb%d" % b] + HK)
                        ya = yacc[:, ch, cg * 512:(cg + 1) * 512]
                        yk = "yacc%d" % ch
                        if el == 0:
                            TS("dve", ya, py[:, :], wsl[:, ch, el:el + 1], None, ALU.mult, None, [pyk, "wsl"], [yk])
                        else:
                            STT(ya, py[:, :], wsl[:, ch, el:el + 1], ya, ALU.mult, ALU.add, [pyk, "wsl", yk], [yk])
            DMA("sp", YB[g * CAPG:(g + 1) * CAPG, :].rearrange("(c p) d -> p c d", p=128), yacc, ["yacc%d" % ch for ch in range(NCH)], ["YB%d" % g])
        YBK = ["YB%d" % g for g in range(4)]
        P.barrier()
        AR.off = M2
        hl = [sb("hl%d" % i, [128, D]) for i in range(2)]
        yg = [sb("yg%d" % i, [128, D]) for i in range(2)]
        ob = [sb("ob%d" % i, [128, D]) for i in range(2)]
        junk3 = sb("junk3", [128, D], BF16)
        ss3 = sb("ss3", [128, 2])
        rs3 = sb("rs3", [128, 2])
        for io in range(NTO):
            b2 = io % 2
            DMA("sp", hl[b2], H2[io * 128:(io + 1) * 128, :], ["H2_%d" % io], ["hl%d" % b2])
            P.op("pool", lambda e, b2=b2: e.memset(yg[b2], 0.0), writes=["yg%d" % b2])
            P.op("pool", lambda e, io=io, b2=b2: e.indirect_dma_start(out=yg[b2], out_offset=None, in_=YB[:, :],
                                                                       in_offset=bass.IndirectOffsetOnAxis(ap=idxi[:, io:io + 1], axis=0),
                                                                       bounds_check=bcreg(e), oob_is_err=False),
                 reads=YBK + ["idxi%d" % io], writes=["yg%d" % b2], dma=True)
            TT("dve", hl[b2], hl[b2], yg[b2], ALU.add, ["hl%d" % b2, "yg%d" % b2], ["hl%d" % b2])
            P.op("act", lambda e, b2=b2: e.activation(out=junk3, in_=hl[b2], func=AF.Square, accum_out=ss3[:, 0:1]),
                 reads=["hl%d" % b2], writes=["junk3", "ss3"])
            rstd_from_ssq(rs3[:, 0:1], ss3[:, 0:1], D, "ss3", "rs3")
            STT(ob[b2], hl[b2], rs3[:, 0:1], fnbc, ALU.mult, ALU.mult, ["hl%d" % b2, "rs3", "fnbc"], ["ob%d" % b2])
            DMA("sp", out_d[io * 128:(io + 1) * 128, :], ob[b2], ["ob%d" % b2], ["OUT%d" % io])
        P.op("sp", None, reads=["OUT%d" % i for i in range(NTO)], writes=[])
        P.emit()
    return nc


def _consts():
    s = np.arange(128)[:, None]
    t = np.arange(128)[None, :]
    cm = np.zeros((128, 8, 128), np.float32)
    cm[:, 0] = (s == t)
    cm[:, 1] = (s <= t) / -16.0
    cm[:, 2] = (s >= t) / -16.0
    cm[:, 3] = (s > t) / -16.0
    cm[:, 4] = (s < t) / -16.0
    cm[:, 5] = (s <= t)
    cm[:, 6] = (s >= t)
    cm[:, 7] = 1.0
    band = np.zeros((128, 384), np.float32)
    band[:, 0:128] = (s >= t + 64)
    band[:, 128:256] = (np.abs(s - t) <= 64)
    band[:, 256:384] = (s <= t - 64)
    return cm, band


def make_in_maps(inputs):
    f = lambda a: np.ascontiguousarray(np.asarray(a, dtype=np.float32))
    x = f(inputs["x"])
    cm, band = _consts()
    wz = np.zeros((33, 512), np.float32)
    wz[0:16, 0:256] = f(inputs["gla_fwd_gate_w"])[0]
    wz[16:32, 256:512] = f(inputs["gla_bwd_gate_w"])[0]
    wz[32, 0:256] = f(inputs["gla_fwd_gate_b"])[0]
    wz[32, 256:512] = f(inputs["gla_bwd_gate_b"])[0]
    vecs = np.zeros((4, D), np.float32)
    vecs[0] = f(inputs["norm1_w"])[0]
    vecs[1] = f(inputs["norm2_w"])[0]
    vecs[2] = f(inputs["final_norm_w"])
    vecs[3] = np.tile(f(inputs["gla_norm_w"])[0], 8)
    wr = np.concatenate([f(inputs["router_group_w"])[0]] + [f(inputs["router_expert_w"])[0, g] for g in range(4)], axis=1)
    rb = np.concatenate([f(inputs["router_group_b"])[0], f(inputs["router_expert_b"])[0].reshape(-1)])[None, :]
    inv = (500000.0 ** (-(np.arange(0, 16, 2, dtype=np.float32) / np.float32(16)))).astype(np.float32)
    shared = dict(cmat=cm, band3=band, w_in=f(inputs["w_in"])[0], wz=wz, vecs=vecs, w_out=f(inputs["w_out"])[0],
                  wr=np.ascontiguousarray(wr), rb=np.ascontiguousarray(rb), ewg=f(inputs["expert_w_gate"])[0],
                  ewu=f(inputs["expert_w_up"])[0], ewd=f(inputs["expert_w_down"])[0])
    maps = []
    for c in range(8):
        b, q = c // 4, c % 4
        s0 = q * OWN
        pos = np.arange(s0 - HALO, s0 + OWN + HALO)
        valid = (pos >= 0) & (pos < S)
        xwin = np.zeros((WIN, D), np.float32)
        xwin[valid] = x[b, pos[valid]]
        ang = (pos.astype(np.float32)[:, None] * inv[None, :]).astype(np.float32)
        cs = np.concatenate([np.cos(ang), np.sin(ang)], axis=1).astype(np.float32)
        cs_t = np.ascontiguousarray(cs.reshape(NTW, 128, 16).transpose(1, 0, 2))
        vcol = np.ascontiguousarray(valid.astype(np.float32).reshape(NTW, 128).T)
        m = dict(shared)
        m.update(xw=xwin, vcol=vcol, cs_t=cs_t)
        maps.append(m)
    return maps


_NC_CACHE = {}


def kernel(**inputs):
    maps = make_in_maps(inputs)
    if "nc" not in _NC_CACHE:
        _NC_CACHE["nc"] = build_program()
    nc = _NC_CACHE["nc"]
    res = run_bass_kernel_spmd(nc, maps, core_ids=list(range(8)))
    out = np.zeros((2, S, D), np.float32)
    for c in range(8):
        b, q = c // 4, c % 4
        out[b, q * OWN:(q + 1) * OWN] = res.results[c]["out"]
    return out
```

```python
import numpy as np
from contextlib import ExitStack
import concourse.bass as bass
import concourse.mybir as mybir
from concourse.bass_utils import run_bass_kernel_spmd

F32 = mybir.dt.float32
BF16 = mybir.dt.bfloat16
I32 = mybir.dt.int32
AF = mybir.ActivationFunctionType
ALU = mybir.AluOpType
AX = mybir.AxisListType

ENGS = ("pe", "act", "dve", "pool", "sp")
EPOCH = 4096
DMA_SLOTS = 8

D = 1024
S = 8192
OWN = 2048
HALO = 1024
WIN = OWN + 2 * HALO
NTW = WIN // 128
T0 = HALO // 128
NTO = OWN // 128
INW = 3104
NEXP = 32
EPS = 1e-6


class Op:
    __slots__ = ("eng", "fn", "dma", "deps", "sig", "sigcount", "dmaidx", "idx")

    def __init__(self, eng, fn, dma):
        self.eng = eng
        self.fn = fn
        self.dma = dma
        self.deps = []
        self.sig = False
        self.sigcount = 0
        self.dmaidx = -1
        self.idx = -1


class Prog:
    def __init__(self, nc):
        self.nc = nc
        self.ops = []
        self.last_w = {}
        self.readers = {}
        self.ndma = {e: 0 for e in ENGS}
        self.bar = None
        self.rec = None

    def barrier(self):
        deps = set()
        for e in ENGS:
            last = None
            nd = 0
            for o in reversed(self.ops):
                if o.eng != e:
                    continue
                if o.dma:
                    if nd < DMA_SLOTS:
                        deps.add(o.idx)
                        nd += 1
                elif last is None:
                    last = o.idx
                    deps.add(o.idx)
                if last is not None and nd >= DMA_SLOTS:
                    break
        b = self.op("sp", None)
        b.deps = sorted(deps | set(b.deps))
        self.bar = b.idx
        return b

    def replay_merged(self, a, b):
        na, nb = len(a), len(b)
        i = j = 0
        while i < na or j < nb:
            if j >= nb or (i < na and i * nb <= j * na):
                self.op(*a[i])
                i += 1
            else:
                self.op(*b[j])
                j += 1

    def op(self, eng, fn, reads=(), writes=(), dma=False):
        if self.rec is not None:
            self.rec.append((eng, fn, list(reads), list(writes), dma))
            return None
        import os as _os
        mx = int(_os.environ.get("DBG_MAXOPS", "0"))
        if mx and len(self.ops) >= mx and fn is not None:
            fn = None
            if dma:
                dma = False
        px = [k_ for k_ in reads if k_[:2] in ("pf", "pb")]
        if px:
            writes = list(writes) + [k_ for k_ in px if k_ not in writes]
            reads = [k_ for k_ in reads if k_ not in px]
        o = Op(eng, fn, dma)
        o.idx = len(self.ops)
        deps = set()
        if self.bar is not None:
            deps.add(self.bar)
        for k in reads:
            w = self.last_w.get(k)
            if w is not None:
                deps.add(w)
        for k in writes:
            w = self.last_w.get(k)
            if w is not None:
                deps.add(w)
            for r in self.readers.get(k, ()):
                deps.add(r)
        deps.discard(o.idx)
        o.deps = sorted(deps)
        for k in writes:
            self.last_w[k] = o.idx
            self.readers[k] = []
        for k in reads:
            if k not in writes:
                self.readers.setdefault(k, []).append(o.idx)
        if dma:
            o.dmaidx = self.ndma[eng]
            self.ndma[eng] += 1
        self.ops.append(o)
        return o

    def emit(self):
        nc = self.nc
        ops = self.ops
        for o in ops:
            for d in o.deps:
                p = ops[d]
                if not p.dma:
                    p.sig = True
        cnt = {e: 0 for e in ENGS}
        for o in ops:
            if o.sig and not o.dma:
                cnt[o.eng] += 1
                o.sigcount = cnt[o.eng]
        nsem = {e: (cnt[e] + EPOCH - 1) // EPOCH for e in ENGS}
        with ExitStack() as es:
            csem = {e: [es.enter_context(nc.semaphore("c_%s_%d" % (e, i))) for i in range(nsem[e])]
                    for e in ENGS}
            dsem = {e: [es.enter_context(nc.semaphore("d_%s_%d" % (e, i)))
                        for i in range(DMA_SLOTS if self.ndma[e] else 0)] for e in ENGS}
            block = es.enter_context(nc.Block())

            def body_for(e):
                def body(eng):
                    waited_c = {x: 0 for x in ENGS}
                    waited_d = {}
                    for o in ops:
                        if o.eng != e:
                            continue
                        need_c = {}
                        need_d = {}
                        for d in o.deps:
                            p = ops[d]
                            if p.dma:
                                slot = p.dmaidx % DMA_SLOTS
                                val = 16 * (p.dmaidx // DMA_SLOTS + 1)
                                key = (p.eng, slot)
                                if waited_d.get(key, 0) < val:
                                    need_d[key] = max(need_d.get(key, 0), val)
                            else:
                                if waited_c[p.eng] < p.sigcount:
                                    need_c[p.eng] = max(need_c.get(p.eng, 0), p.sigcount)
                        if o.dma:
                            slot = o.dmaidx % DMA_SLOTS
                            val = 16 * (o.dmaidx // DMA_SLOTS)
                            key = (e, slot)
                            if val > 0 and waited_d.get(key, 0) < val:
                                need_d[key] = max(need_d.get(key, 0), val)
                        for pe_, c in need_c.items():
                            ep = (c - 1) // EPOCH
                            eng.wait_ge(csem[pe_][ep], (c - 1) % EPOCH + 1)
                            waited_c[pe_] = c
                        for key, val in need_d.items():
                            eng.wait_ge(dsem[key[0]][key[1]], val)
                            waited_d[key] = val
                        ins = o.fn(eng) if o.fn is not None else None
                        if o.dma:
                            ins.then_inc(dsem[e][o.dmaidx % DMA_SLOTS], 16)
                        elif o.sig:
                            if ins is None:
                                ins = eng.nop()
                            ep = (o.sigcount - 1) // EPOCH
                            ins.then_inc(csem[e][ep], 1)
                return body

            block.tensor(body_for("pe"))
            block.scalar(body_for("act"))
            block.vector(body_for("dve"))
            block.gpsimd(body_for("pool"))
            block.sync(body_for("sp"))


class Arena:
    def __init__(self, ap, ncols):
        self.ap = ap
        self.n = ncols
        self.off = 0

    def alloc(self, shape, dt=F32):
        p = shape[0]
        rest = list(shape[1:])
        nel = 1
        for r in rest:
            nel *= r
        ncol = nel if dt in (F32, I32) else (nel + 1) // 2
        ncol += ncol % 2
        assert self.off + ncol <= self.n, "arena overflow: need %d have %d" % (ncol, self.n - self.off)
        v = self.ap[0:p, self.off:self.off + ncol]
        self.off += ncol
        if dt != F32:
            v = v.bitcast(dt)
        if v.shape[1] != nel:
            v = v[:, 0:nel]
        if len(rest) == 2:
            v = v.rearrange("p (a b) -> p a b", a=rest[0])
        elif len(rest) == 3:
            v = v.rearrange("p (a b c) -> p a b c", a=rest[0], b=rest[1])
        return v


def build_program(debug=False, stop_after=None, dbg_tiles=None):
    nc = bass.Bass("TRN2", target_bir_lowering=False)
    P = Prog(nc)
    global LASTP
    LASTP = P

    def din(name, shape, dt=F32):
        return nc.dram_tensor(name, list(shape), dt, kind="ExternalInput").ap()

    def dscr(name, shape, dt):
        kind = "ExternalOutput" if debug else "Internal"
        return nc.dram_tensor(name, list(shape), dt, kind=kind).ap()

    xw = din("xw", [WIN, D])
    vcol = din("vcol", [128, NTW])
    cs_t = din("cs_t", [128, NTW, 16])
    cmat = din("cmat", [128, 8, 128])
    band3 = din("band3", [128, 384])
    w_in = din("w_in", [D, INW])
    wz_d = din("wz", [33, 512])
    vecs = din("vecs", [4, D])
    w_out = din("w_out", [D, D])
    wr_d = din("wr", [D, 36])
    rb_d = din("rb", [1, 36])
    ewg = din("ewg", [NEXP, D, 512])
    ewu = din("ewu", [NEXP, D, 512])
    ewd = din("ewd", [NEXP, 512, D])
    out_d = nc.dram_tensor("out", [OWN, D], F32, kind="ExternalOutput").ap()
    QS = dscr("QS", [OWN, 512], BF16)
    KS = dscr("KS", [WIN + 2 * HALO, 512], BF16)
    VS = dscr("VS", [WIN + 2 * HALO, 520], BF16)
    GV = dscr("GV", [OWN, 512], BF16)
    GG = dscr("GG", [OWN, 512], BF16)
    MG = dscr("MG", [OWN, 512], BF16)
    OTS = dscr("OTS", [8, 64, OWN], BF16)
    H2 = dscr("H2", [OWN, D], F32)
    XB = dscr("XB", [2560, D], BF16)
    WB = dscr("WB", [2560, 8], F32)
    YB = dscr("YB", [2560, D], F32)
    WTD = nc.dram_tensor("WTD", [128, NTO * 32], F32, kind="ExternalOutput").ap() if debug else None

    QSK = ["QS%d" % i for i in range(NTO)]
    KSK = ["KS%d" % i for i in range(48)]
    VSK = ["VS%d" % i for i in range(48)]
    NCOL = 50 * 1024 + 512
    es = ExitStack()
    with es:
        arena_t = es.enter_context(nc.sbuf_tensor("arena", [128, NCOL], F32))
        AR = Arena(arena_t[:], NCOL)
        sb = lambda name, shape, dt=F32: AR.alloc(shape, dt)

        def ps(name, shape, dt=F32):
            return es.enter_context(nc.psum_tensor("p_" + name, list(shape), dt))

        pf = [ps("pf%d" % i, [128, 512]) for i in range(6)]
        pb = [ps("pb%d" % i, [128, 1024], BF16) for i in range(2)]
        pf_rr = [0]
        pb_rr = [0]

        stream = [None]
        srr = [0, 0]

        def next_pf():
            if stream[0] is None:
                i = pf_rr[0] % 6
                pf_rr[0] += 1
            else:
                s_ = stream[0]
                i = 3 * s_ + srr[s_] % 3
                srr[s_] += 1
            return pf[i], "pf%d" % i

        def next_pb():
            if stream[0] is None:
                i = pb_rr[0] % 2
                pb_rr[0] += 1
            else:
                i = stream[0]
            return pb[i], "pb%d" % i

        def mm_group(out_ap, pairs, okey, rkeys):
            def fn(e):
                ins = None
                n = len(pairs)
                for j, (l, r) in enumerate(pairs):
                    ins = e.matmul(out_ap, lhsT=l, rhs=r, start=(j == 0), stop=(j == n - 1))
                return ins
            P.op("pe", fn, reads=rkeys, writes=[okey])

        def ACT(out, in_, func, reads, writes, **kw):
            P.op("act", lambda e: e.activation(out=out, in_=in_, func=func, **kw), reads=reads, writes=writes)

        def TT(eng, out, in0, in1, op, reads, writes):
            P.op(eng, lambda e: e.tensor_tensor(out=out, in0=in0, in1=in1, op=op), reads=reads, writes=writes)

        def STT(out, in0, scalar, in1, op0, op1, reads, writes):
            P.op("dve", lambda e: e.scalar_tensor_tensor(out=out, in0=in0, scalar=scalar, in1=in1, op0=op0, op1=op1),
                 reads=reads, writes=writes)

        def TS(eng, out, in0, s1, s2, op0, op1, reads, writes):
            if op1 is None:
                P.op(eng, lambda e: e.tensor_scalar(out=out, in0=in0, scalar1=s1, scalar2=None, op0=op0), reads=reads, writes=writes)
            else:
                P.op(eng, lambda e: e.tensor_scalar(out=out, in0=in0, scalar1=s1, scalar2=s2, op0=op0, op1=op1), reads=reads, writes=writes)

        def CP(eng, out, in_, reads, writes):
            if eng == "act":
                ACT(out, in_, AF.Copy, reads, writes)
            else:
                P.op(eng, lambda e: e.tensor_copy(out=out, in_=in_), reads=reads, writes=writes)

        def DMA(q, out, in_, reads, writes):
            return P.op(q, lambda e: e.dma_start(out=out, in_=in_), reads=reads, writes=writes, dma=True)

        def rstd_from_ssq(dst, src, n, rk, wk):
            ACT(dst, src, AF.Ln, [rk, "epsc"], [wk], scale=1.0 / n, bias=epsc[0:dst.shape[0], :])
            ACT(dst, dst, AF.Exp, [wk], [wk], scale=-0.5)

        cm = sb("cm", [128, 8, 128])
        identb = sb("identb", [128, 128], BF16)
        band = sb("band", [128, 384], BF16)
        maskFB = sb("maskFB", [128, 4, 128])
        n16col = sb("n16col", [128, 2])
        epsc = sb("epsc", [128, 2])
        onec = sb("onec", [128, 2])
        negc = sb("negc", [128, 2])
        vc = sb("vc", [128, NTW])
        cst = sb("cst", [128, NTW, 16])
        wz = sb("wz", [33, 512])
        n1bc = sb("n1bc", [128, D])
        n2bc = sb("n2bc", [128, D])
        fnbc = sb("fnbc", [128, D])
        gnbc = sb("gnbc", [128, 512])
        rbbc = sb("rbbc", [128, 36])
        wr = sb("wr", [128, 8, 36])
        nmax = sb("nmax", [128, 16])
        n16col = n16col[:, 0:1]
        epsc = epsc[:, 0:1]
        onec = onec[:, 0:1]
        negc = negc[:, 0:1]

        DMA("sp", cm, cmat, [], ["cm"])
        DMA("pool", identb, cmat[:, 0, :], [], ["identb"])
        DMA("pool", band, band3, [], ["band"])
        DMA("sp", vc, vcol, [], ["vc"])
        DMA("sp", cst, cs_t, [], ["cst"])
        DMA("sp", wz, wz_d, [], ["wz"])
        DMA("sp", n1bc, vecs[0:1, :].partition_broadcast(128), [], ["n1bc"])
        DMA("sp", n2bc, vecs[1:2, :].partition_broadcast(128), [], ["n2bc"])
        DMA("sp", fnbc, vecs[2:3, :].partition_broadcast(128), [], ["fnbc"])
        DMA("sp", gnbc, vecs[3:4, 0:512].partition_broadcast(128), [], ["gnbc"])
        DMA("sp", rbbc, rb_d[0:1, :].partition_broadcast(128), [], ["rbbc"])
        DMA("sp", wr, wr_d.rearrange("(c p) n -> p c n", p=128), [], ["wr"])
        P.op("dve", lambda e: e.memset(n16col, -1.0 / 16.0), writes=["n16col"])
        P.op("dve", lambda e: e.memset(epsc, EPS), writes=["epsc"])
        P.op("dve", lambda e: e.memset(onec, 1.0), writes=["onec"])
        P.op("dve", lambda e: e.memset(nmax, 0.0), writes=["nmax"])
        for h in range(4):
            CP("dve", maskFB[:, h, :], cm[:, 5 + h // 2, :], ["cm"], ["maskFB"])
        M0 = AR.off

        attnT = sb("attnT", [128, NTO, 512], BF16)
        qdT = sb("qdT", [128, NTO, 4, 128], BF16)
        SfT = sb("SfT", [128, NTO, 2, 128], BF16)
        SbT = sb("SbT", [128, NTO, 2, 128], BF16)
        M1 = AR.off
        win = sb("win", [128, 8, INW], BF16)
        for c in range(8):
            DMA("pool", win[:, c, :], w_in[c * 128:(c + 1) * 128, :], [], ["win%d" % c])
        winkeys = ["win%d" % c for c in range(8)]
        kvB = sb("kvB", [128, NTW - T0, 2, 128], BF16)
        decB = sb("decB", [128, NTW - T0, 2])
        Sf = sb("Sf", [128, 2, 128])
        Sb = sb("Sb", [128, 2, 128])
        P.op("dve", lambda e: e.memset(Sf, 0.0), writes=["Sf"])
        P.op("dve", lambda e: e.memset(Sb, 0.0), writes=["Sb"])
        zt = sb("zt", [128, 520], BF16)
        P.op("pool", lambda e: e.memset(zt, 0.0), writes=["zt"])
        for blk in range(HALO // 128):
            for base in (0, HALO + WIN):
                r0 = base + blk * 128
                DMA("sp", KS[r0:r0 + 128, :], zt[:, 0:512], ["zt"], ["KS%d" % (r0 // 128)])
                DMA("sp", VS[r0:r0 + 128, :], zt, ["zt"], ["VS%d" % (r0 // 128)])
        xt = [sb("xt%d" % i, [128, D]) for i in range(2)]
        junk = sb("junk", [128, D], BF16)
        xn = [sb("xn%d" % i, [128, D], BF16) for i in range(2)]
        xnT = [sb("xnT%d" % i, [128, 8, 128], BF16) for i in range(2)]
        ssq = sb("ssq", [128, 2])
        rstd = sb("rstd", [128, 2])
        qk = [sb("qk%d" % i, [128, 512]) for i in range(2)]
        vbf = [sb("vbf%d" % i, [128, 512], BF16) for i in range(2)]
        gbf = [sb("gbf%d" % i, [128, 512], BF16) for i in range(2)]
        lr = [sb("lr%d" % i, [128, 32]) for i in range(2)]
        aqr = [sb("aqr%d" % i, [128, 8, 64]) for i in range(2)]
        akr = [sb("akr%d" % i, [128, 8, 64]) for i in range(2)]
        vab = [sb("vab%d" % i, [128, 8, 65], BF16) for i in range(2)]
        lrT = sb("lrT", [33, 128])
        ez = sb("ez", [128, 512])
        spl = sb("spl", [128, 512])
        E1 = sb("E1", [128, 512])
        E2 = sb("E2", [128, 512])
        E3 = sb("E3", [128, 512])
        dec = sb("dec", [128, 4])
        qd = sb("qd", [128, 512], BF16)
        ki = sb("ki", [128, 512], BF16)
        ke = sb("ke", [128, 512], BF16)
        kiT = sb("kiT", [128, 4, 128], BF16)
        qrb = [sb("qrb%d" % i, [128, 512], BF16) for i in range(2)]
        krb = [sb("krb%d" % i, [128, 512], BF16) for i in range(2)]
        rta = sb("rta", [128, 8, 8])
        rtb = sb("rtb", [128, 8, 8])
        rtc = sb("rtc", [128, 8, 8])
        rtd = sb("rtd", [128, 8, 8])
        sqs = sb("sqs", [128, 8, 64])
        nrm = sb("nrm", [128, 16])
        P.op("dve", lambda e: e.memset(lrT[32:33, :], 1.0), writes=["lrT_one"])
        tiles = list(range(NTW) if dbg_tiles is None else dbg_tiles)

        def S1(i):
            own = T0 <= i < T0 + NTO
            io = i - T0
            b2 = i % 2
            xtk, xnk, xnTk = "xt%d" % b2, "xn%d" % b2, "xnT%d" % b2
            if i == tiles[0]:
                DMA("sp", xt[b2], xw[i * 128:(i + 1) * 128, :], [], [xtk])
            if i + 1 < NTW and (dbg_tiles is None):
                DMA("sp", xt[(i + 1) % 2], xw[(i + 1) * 128:(i + 2) * 128, :], [], ["xt%d" % ((i + 1) % 2)])
            sk, rk = "ssq%d" % b2, "rstd%d" % b2
            P.op("act", lambda e, b2=b2: e.activation(out=junk, in_=xt[b2], func=AF.Square, accum_out=ssq[:, b2:b2 + 1]),
                 reads=[xtk], writes=["junk", sk])
            rstd_from_ssq(rstd[:, b2:b2 + 1], ssq[:, b2:b2 + 1], D, sk, rk)
            STT(xn[b2], xt[b2], rstd[:, b2:b2 + 1], n1bc, ALU.mult, ALU.mult, [xtk, rk, "n1bc"], [xnk])
            pbt, pbk = next_pb()

            def tr_fn(e, b2=b2, pbt=pbt):
                ins = None
                for c in range(8):
                    ins = e.transpose(out=pbt[:, c * 128:(c + 1) * 128], in_=xn[b2][:, c * 128:(c + 1) * 128], identity=identb)
                return ins
            P.op("pe", tr_fn, reads=[xnk, "identb"], writes=[pbk])
            CP("act", xnT[b2].rearrange("p c t -> p (c t)"), pbt[:, :], [pbk], [xnTk])

            def proj(c0, c1):
                pt, pk = next_pf()
                n = c1 - c0
                mm_group(pt[:, 0:n], [(xnT[b2][:, c, :], win[:, c, c0:c1]) for c in range(8)], pk, [xnTk] + winkeys)
                return pt, pk

            if own:
                pt, pk = proj(0, 512)
                CP("act", qk[b2], pt[:, 0:512], [pk], ["qk%d" % b2])
            else:
                pt, pk = proj(256, 512)
                CP("act", qk[b2][:, 256:512], pt[:, 0:256], [pk], ["qk%d" % b2])
            pt, pk = proj(512, 1024)
            CP("dve", vbf[b2], pt[:, 0:512], [pk], ["vbf%d" % b2])
            if own:
                DMA("sp", GV[io * 128:(io + 1) * 128, :], vbf[b2], ["vbf%d" % b2], ["GV%d" % io])
                pt, pk = proj(1024, 1536)
                CP("act", gbf[b2], pt[:, 0:512], [pk], ["gbf%d" % b2])
                DMA("sp", GG[io * 128:(io + 1) * 128, :], gbf[b2], ["gbf%d" % b2], ["GG%d" % io])
            pt, pk = proj(1536, 1568)
            CP("dve", lr[b2], pt[:, 0:32], [pk], ["lr%d" % b2])
            if own:
                pt, pk = proj(1568, 2080)
                CP("dve", aqr[b2].rearrange("p h d -> p (h d)"), pt[:, 0:512], [pk], ["aqr%d" % b2])
            pt, pk = proj(2080, 2592)
            CP("act", akr[b2].rearrange("p h d -> p (h d)"), pt[:, 0:512], [pk], ["akr%d" % b2])
            pt, pk = proj(2592, 3104)
            r0k = HALO + i * 128
            CP("act", vab[b2][:, :, 0:64], pt[:, 0:512].rearrange("p (h d) -> p h d", h=8), [pk], ["vab%d" % b2])
            CP("dve", vab[b2][:, :, 64:65], vc[:, i:i + 1].unsqueeze(1).broadcast_to([128, 8, 1]), ["vc"], ["vab%d" % b2])
            DMA("sp", VS[r0k:r0k + 128, :], vab[b2].rearrange("p h d -> p (h d)"), ["vab%d" % b2], ["VS%d" % (r0k // 128)])

        def S2(i):
            own = T0 <= i < T0 + NTO
            left = i < T0
            io = i - T0
            b2 = i % 2
            qkb = qk[b2]
            qkk = "qk%d" % b2
            vkey = "vbf%d" % b2
            vt = vbf[b2]
            pt, pk = next_pf()
            P.op("pe", lambda e, pt=pt: e.transpose(out=pt[0:32, 0:128], in_=lr[b2], identity=cm[:, 0, :]), reads=["lr%d" % b2, "cm"], writes=[pk])
            CP("dve", lrT[0:32, :], pt[0:32, 0:128], [pk], ["lrT"])
            pz, pzk = next_pf()
            P.op("pe", lambda e, pz=pz: e.matmul(pz[:, :], lhsT=lrT, rhs=wz, start=True, stop=True),
                 reads=["lrT", "lrT_one", "wz"], writes=[pzk])
            ACT(ez, pz[:, :], AF.Exp, [pzk], ["ez"], scale=-1.0)
            ACT(spl, ez, AF.Ln, ["ez", "onec"], ["spl"], bias=onec, scale=1.0)
            pbb, pbbk = next_pf()
            P.op("pe", lambda e, pbb=pbb: (e.matmul(pbb[:, 0:256], lhsT=cm[:, 1, :], rhs=spl[:, 0:256], start=True, stop=True),
                                           e.matmul(pbb[:, 256:512], lhsT=cm[:, 2, :], rhs=spl[:, 256:512], start=True, stop=True))[1],
                 reads=["cm", "spl"], writes=[pbbk])
            ACT(E1, pbb[:, :], AF.Exp, [pbbk], ["E1"])
            ACT(E2, pbb[:, :], AF.Exp, [pbbk], ["E2"], scale=-1.0)
            pb3, pb3k = next_pf()
            P.op("pe", lambda e, pb3=pb3: (e.matmul(pb3[:, 0:256], lhsT=cm[:, 3, :], rhs=spl[:, 0:256], start=True, stop=True),
                                           e.matmul(pb3[:, 256:512], lhsT=cm[:, 4, :], rhs=spl[:, 256:512], start=True, stop=True))[1],
                 reads=["cm", "spl"], writes=[pb3k])
            ACT(E3, pb3[:, :], AF.Exp, [pb3k], ["E3"])
            pdc, pdck = next_pf()

            def dec_fn(e, pdc=pdc):
                ins = None
                for j in range(4):
                    ins = e.matmul(pdc[:, j:j + 1], lhsT=spl[:, j * 128:(j + 1) * 128], rhs=n16col, start=True, stop=True)
                return ins
            P.op("pe", dec_fn, reads=["spl", "n16col"], writes=[pdck])
            ACT(dec, pdc[:, 0:4], AF.Exp, [pdck], ["dec"])
            if own:
                STT(qd[:, 0:256], qkb[:, 0:256], 0.125, E1[:, 0:256], ALU.mult, ALU.mult, [qkk, "E1"], ["qd"])
                STT(qd[:, 256:512], qkb[:, 0:256], 0.125, E1[:, 256:512], ALU.mult, ALU.mult, [qkk, "E1"], ["qd"])
                TT("pool", ki[:, 0:256], qkb[:, 256:512], E2[:, 0:256], ALU.mult, [qkk, "E2"], ["ki"])
                TT("pool", ki[:, 256:512], qkb[:, 256:512], E2[:, 256:512], ALU.mult, [qkk, "E2"], ["ki"])
            TT("pool", ke[:, 0:256], qkb[:, 256:512], E3[:, 0:256], ALU.mult, [qkk, "E3"], ["ke"])
            TT("pool", ke[:, 256:512], qkb[:, 256:512], E3[:, 256:512], ALU.mult, [qkk, "E3"], ["ke"])
            if own:
                pbt, pbk = next_pb()

                def tr2_fn(e, pbt=pbt):
                    ins = None
                    for j in range(4):
                        ins = e.transpose(out=pbt[:, j * 128:(j + 1) * 128], in_=qd[:, j * 128:(j + 1) * 128], identity=identb)
                    for j in range(4):
                        ins = e.transpose(out=pbt[:, 512 + j * 128:512 + (j + 1) * 128], in_=ki[:, j * 128:(j + 1) * 128], identity=identb)
                    return ins
                P.op("pe", tr2_fn, reads=["qd", "ki", "identb"], writes=[pbk])
                CP("act", qdT[:, io, :, :].rearrange("p c t -> p (c t)"), pbt[:, 0:512], [pbk], ["qdT%d" % io])
                CP("dve", kiT.rearrange("p c t -> p (c t)"), pbt[:, 512:1024], [pbk], ["kiT"])
                paX, paXk = next_pf()
                paY, paYk = next_pf()

                def att_fn(e, pa, par, io=io):
                    ins = None
                    p0 = par * 64
                    for dirn in range(2):
                        for pr in range(2):
                            blk = dirn * 2 + pr
                            sl = dirn * 2 + pr
                            ins = e.matmul(pa[:, sl * 128:(sl + 1) * 128], lhsT=kiT[p0:p0 + 64, blk, :], rhs=qdT[p0:p0 + 64, io, blk, :],
                                           start=True, stop=True)
                    return ins
                P.op("pe", lambda e, pa=paX, f=att_fn: f(e, pa, 0), reads=["kiT", "qdT%d" % io], writes=[paXk])
                P.op("pe", lambda e, pa=paY, f=att_fn: f(e, pa, 1), reads=["kiT", "qdT%d" % io], writes=[paYk])
                TT("dve", ez, paX[:, :], maskFB.rearrange("p h c -> p (h c)"), ALU.mult, [paXk, "maskFB"], ["ez"])
                TT("dve", E1, paY[:, :], maskFB.rearrange("p h c -> p (h c)"), ALU.mult, [paYk, "maskFB"], ["E1"])
                av = attnT[:, io, :].rearrange("p (a b c) -> p a b c", a=2, b=2)
                TT("pool", av[:, :, 0, :], ez[:, 0:256].rearrange("p (a c) -> p a c", a=2), ez[:, 256:512].rearrange("p (a c) -> p a c", a=2),
                   ALU.add, ["ez"], ["attnT%d" % io])
                TT("pool", av[:, :, 1, :], E1[:, 0:256].rearrange("p (a c) -> p a c", a=2), E1[:, 256:512].rearrange("p (a c) -> p a c", a=2),
                   ALU.add, ["E1"], ["attnT%d" % io])
            for dirn in range(2):
                if dirn == 0 and i >= T0 + NTO:
                    continue
                if dirn == 1 and left:
                    continue
                pkv, pkvk = next_pf()

                def kv_fn(e, pkv=pkv, dirn=dirn, vt=vt):
                    ins = None
                    for pr in range(2):
                        ins = e.matmul(pkv[:, pr * 256:(pr + 1) * 256], lhsT=ke[:, dirn * 256 + pr * 128: dirn * 256 + (pr + 1) * 128],
                                       rhs=vt[:, pr * 256:(pr + 1) * 256], start=True, stop=True)
                    return ins
                P.op("pe", kv_fn, reads=["ke", vkey], writes=[pkvk])
                if dirn == 0:
                    if own:
                        CP("act", SfT[:, io, :, :].rearrange("p a b -> p (a b)"), Sf.rearrange("p a b -> p (a b)"), ["Sf"], ["SfT%d" % io])
                    for pr in range(2):
                        for hh in range(2):
                            p0 = hh * 64
                            STT(Sf[p0:p0 + 64, pr, :], Sf[p0:p0 + 64, pr, :], dec[p0:p0 + 64, pr:pr + 1],
                                pkv[p0:p0 + 64, pr * 256 + hh * 128: pr * 256 + (hh + 1) * 128], ALU.mult, ALU.add,
                                ["Sf", "dec", pkvk], ["Sf"])
                else:
                    ib = i - T0
                    for pr in range(2):
                        for hh in range(2):
                            p0 = hh * 64
                            CP("act", kvB[p0:p0 + 64, ib, pr, :], pkv[p0:p0 + 64, pr * 256 + hh * 128: pr * 256 + (hh + 1) * 128],
                               [pkvk], ["kvB%d" % ib])
                    CP("dve", decB[:, ib, :], dec[:, 2:4], ["dec"], ["decB%d" % ib])

            def rope(raw, rkey):
                cosb = cst[:, i, 0:8].unsqueeze(1).broadcast_to([128, 8, 8])
                sinb = cst[:, i, 8:16].unsqueeze(1).broadcast_to([128, 8, 8])
                TT("pool", rta, raw[:, :, 0:8], cosb, ALU.mult, [rkey, "cst"], ["rta"])
                TT("pool", rtb, raw[:, :, 8:16], sinb, ALU.mult, [rkey, "cst"], ["rtb"])
                TT("pool", rtc, raw[:, :, 8:16], cosb, ALU.mult, [rkey, "cst"], ["rtc"])
                TT("pool", rtd, raw[:, :, 0:8], sinb, ALU.mult, [rkey, "cst"], ["rtd"])
                TT("dve", raw[:, :, 0:8], rta, rtb, ALU.subtract, ["rta", "rtb"], [rkey])
                TT("dve", raw[:, :, 8:16], rtc, rtd, ALU.add, ["rtc", "rtd"], [rkey])

            def sqnorm(src, col0, skey):
                TT("pool", sqs, src, src, ALU.mult, [skey], ["sqs"])
                P.op("dve", lambda e: e.tensor_reduce(out=nrm[:, col0:col0 + 8], in_=sqs, axis=AX.X, op=ALU.add), reads=["sqs"], writes=["nrm"])
                TT("dve", nmax[:, col0:col0 + 8], nmax[:, col0:col0 + 8], nrm[:, col0:col0 + 8], ALU.max, ["nrm", "nmax"], ["nmax"])

            r0k = HALO + i * 128
            if own:
                rope(aqr[b2], "aqr%d" % b2)
                sqnorm(aqr[b2], 0, "aqr%d" % b2)
                CP("act", qrb[b2], aqr[b2].rearrange("p h d -> p (h d)"), ["aqr%d" % b2], ["qrb%d" % b2])
                DMA("sp", QS[io * 128:(io + 1) * 128, :], qrb[b2], ["qrb%d" % b2], ["QS%d" % io])
            rope(akr[b2], "akr%d" % b2)
            sqnorm(akr[b2], 8, "akr%d" % b2)
            CP("act", krb[b2], akr[b2].rearrange("p h d -> p (h d)"), ["akr%d" % b2], ["krb%d" % b2])
            DMA("sp", KS[r0k:r0k + 128, :], krb[b2], ["krb%d" % b2], ["KS%d" % (r0k // 128)])

        for n_ in range(len(tiles) + 1):
            ra, rb = [], []
            if n_ < len(tiles):
                P.rec = ra
                stream[0] = 0
                S1(tiles[n_])
            if n_ > 0:
                P.rec = rb
                stream[0] = 1
                S2(tiles[n_ - 1])
            P.rec = None
            stream[0] = None
            P.replay_merged(ra, rb)

        if stop_after == "A":
            P.op("sp", None, reads=[k_ for k_ in P.last_w.keys() if k_[:2] in ("QS", "KS", "VS", "GV", "GG")], writes=[])
            P.emit()
            return nc
        for i in range(NTW - 1, T0 - 1, -1):
            ib = i - T0
            if ib < NTO:
                CP("act", SbT[:, ib, :, :].rearrange("p a b -> p (a b)"), Sb.rearrange("p a b -> p (a b)"), ["Sb"], ["SbT%d" % ib])
            if i == T0:
                break
            for pr in range(2):
                STT(Sb[:, pr, :], Sb[:, pr, :], decB[:, ib, pr:pr + 1], kvB[:, ib, pr, :], ALU.mult, ALU.add,
                    ["Sb", "decB%d" % ib, "kvB%d" % ib], ["Sb"])

        P.barrier()
        AR.off = M1
        vb2 = [sb("vb2%d" % i, [128, 512], BF16) for i in range(2)]
        gb2 = [sb("gb2%d" % i, [128, 512], BF16) for i in range(2)]
        osb = sb("osb", [128, 512])
        osq = sb("osq", [128, 4, 128])
        oms = sb("oms", [128, 4])
        sgs = sb("sgs", [128, 512])
        ybf = sb("ybf", [128, 512])
        mixb = [sb("mixb%d" % i, [128, 512], BF16) for i in range(2)]
        for io in range(NTO):
            b2 = io % 2
            DMA("sp", vb2[b2], GV[io * 128:(io + 1) * 128, :], ["GV%d" % io], ["vb2%d" % b2])
            DMA("sp", gb2[b2], GG[io * 128:(io + 1) * 128, :], ["GG%d" % io], ["gb2%d" % b2])
            poX, poXk = next_pf()
            poY, poYk = next_pf()

            def o_fn(e, po, par, io=io, b2=b2):
                ins = None
                p0 = par * 64
                for pr in range(2):
                    h = pr * 2 + par
                    oap = po[:, pr * 128:(pr + 1) * 128]
                    e.matmul(oap, lhsT=attnT[:, io, h * 128:(h + 1) * 128], rhs=vb2[b2][:, h * 128:(h + 1) * 128], start=True, stop=False)
                    e.matmul(oap, lhsT=qdT[p0:p0 + 64, io, pr, :], rhs=SfT[p0:p0 + 64, io, pr, :], start=False, stop=False)
                    ins = e.matmul(oap, lhsT=qdT[p0:p0 + 64, io, 2 + pr, :], rhs=SbT[p0:p0 + 64, io, pr, :], start=False, stop=True)
                return ins
            rk_ = ["attnT%d" % io, "qdT%d" % io, "SfT%d" % io, "SbT%d" % io, "vb2%d" % b2]
            P.op("pe", lambda e, po=poX, f=o_fn: f(e, po, 0), reads=rk_, writes=[poXk])
            P.op("pe", lambda e, po=poY, f=o_fn: f(e, po, 1), reads=rk_, writes=[poYk])
            ov = osb.rearrange("p (a b c) -> p a b c", a=2, b=2)
            CP("act", ov[:, :, 0, :], poX[:, 0:256].rearrange("p (a c) -> p a c", a=2), [poXk], ["osb"])
            CP("act", ov[:, :, 1, :], poY[:, 0:256].rearrange("p (a c) -> p a c", a=2), [poYk], ["osb"])
            TT("pool", osq.rearrange("p h d -> p (h d)"), osb, osb, ALU.mult, ["osb"], ["osq"])
            P.op("dve", lambda e: e.tensor_reduce(out=oms, in_=osq, axis=AX.X, op=ALU.add), reads=["osq"], writes=["oms"])
            rstd_from_ssq(oms, oms, 128, "oms", "oms")
            ACT(sgs, gb2[b2], AF.Silu, ["gb2%d" % b2], ["sgs"])
            TT("dve", ybf, osb, gnbc, ALU.mult, ["osb", "gnbc"], ["ybf"])
            for h in range(4):
                STT(mixb[b2][:, h * 128:(h + 1) * 128], ybf[:, h * 128:(h + 1) * 128], oms[:, h:h + 1], sgs[:, h * 128:(h + 1) * 128],
                    ALU.mult, ALU.mult, ["ybf", "oms", "sgs"], ["mixb%d" % b2])
            DMA("sp", MG[io * 128:(io + 1) * 128, :], mixb[b2], ["mixb%d" % b2], ["MG%d" % io])
        MGK = ["MG%d" % i for i in range(NTO)]
        if stop_after == "G2":
            P.op("sp", None, reads=QSK + KSK + VSK + MGK, writes=[])
            P.emit()
            return nc

        P.barrier()
        AR.off = M0
        nm2 = sb("nm2", [128, 2])
        m2 = sb("m2", [2, 2])
        m1 = sb("m1", [1, 4])
        P.op("dve", lambda e: e.tensor_reduce(out=nm2, in_=nmax.rearrange("p (a h) -> p a h", a=2), axis=AX.X, op=ALU.max),
             reads=["nmax"], writes=["nm2"])
        pt, pk = next_pf()
        P.op("pe", lambda e, pt=pt: e.transpose(out=pt[0:2, 0:128], in_=nm2, identity=cm[:, 0, :]), reads=["nm2", "cm"], writes=[pk])
        P.op("dve", lambda e, pt=pt: e.tensor_reduce(out=m2[:, 0:1], in_=pt[0:2, 0:128], axis=AX.X, op=ALU.max), reads=[pk], writes=["m2"])
        pt, pk = next_pf()
        P.op("pe", lambda e, pt=pt: e.transpose(out=pt[0:1, 0:2], in_=m2[:, 0:1], identity=cm[0:2, 0, 0:2]), reads=["m2", "cm"], writes=[pk])
        CP("dve", m1[:, 0:2], pt[0:1, 0:2], [pk], ["m1"])
        TT("dve", m1[:, 2:3], m1[:, 0:1], m1[:, 1:2], ALU.mult, ["m1"], ["m1"])
        ACT(m1[:, 3:4], m1[:, 2:3], AF.Ln, ["m1"], ["m1"])
        ACT(m1[:, 3:4], m1[:, 3:4], AF.Exp, ["m1"], ["m1"], scale=0.5)
        TS("dve", m1[:, 3:4], m1[:, 3:4], -0.125, None, ALU.mult, None, ["m1"], ["m1"])
        pt, pk = next_pf()
        P.op("pe", lambda e, pt=pt: e.matmul(pt[:, 0:1], lhsT=cm[0:1, 7, :], rhs=m1[:, 3:4], start=True, stop=True), reads=["m1", "cm"], writes=[pk])
        CP("dve", negc, pt[:, 0:1], [pk], ["negc"])

        accT = sb("accT", [65, 8, OWN])
        NQ = 8
        qsb2 = [[sb("qsb%d" % i, [128, 512], BF16) for i in range(NQ)] for _ in range(2)]
        ksb2 = [[sb("ksb%d" % i, [128, 512], BF16) for i in range(NQ + 2)] for _ in range(2)]
        vsb2 = [[sb("vsb%d" % i, [128, 8, 65], BF16) for i in range(NQ + 2)] for _ in range(2)]
        qT2 = [[sb("qT%d" % i, [128, 4, 128], BF16) for i in range(NQ)] for _ in range(2)]
        kT2 = [[sb("kT%d" % i, [128, 4, 128], BF16) for i in range(NQ + 2)] for _ in range(2)]
        pex = [sb("pex%d" % i, [128, 384], BF16) for i in range(4)]
        pmk = [sb("pmk%d" % i, [128, 384], BF16) for i in range(4)]
        cnt4 = [0, 0]
        jobs = [(1, 0, 0, 8), (1, 0, 8, 8)] + [(4, r, 0, 4) for r in range(4)] + [(16, r, 0, 1) for r in range(16)]
        for jn, (dd, r, j0, nq) in enumerate(jobs):
            js = jn % 2
            qsb, ksb, vsb, qT, kT = qsb2[js], ksb2[js], vsb2[js], qT2[js], kT2[js]
            QSv = QS.rearrange("(n d) c -> d n c", d=dd)
            KSv = KS.rearrange("(n d) c -> d n c", d=dd)
            VSv = VS.rearrange("(n d) c -> d n c", d=dd)
            accv = accT.rearrange("p h (n d) -> p h d n", d=dd)
            for jq in range(nq):
                n0 = 128 * (j0 + jq)
                DMA("sp", qsb[jq], QSv[r, n0:n0 + 128, :], QSK, [("qsb" + str(js) + "_%d") % jq])
            for kk in range(nq + 2):
                n0 = 2048 // dd + 128 * (j0 + kk - 1)
                DMA("sp", ksb[kk], KSv[r, n0:n0 + 128, :], KSK, [("ksb" + str(js) + "_%d") % kk])
                DMA("sp", vsb[kk].rearrange("p h d -> p (h d)"), VSv[r, n0:n0 + 128, :], VSK, [("vsb" + str(js) + "_%d") % kk])
            tl = [(qsb[jq], ("qsb" + str(js) + "_%d") % jq, qT[jq], ("qT" + str(js) + "_%d") % jq) for jq in range(nq)] + \
                 [(ksb[kk], ("ksb" + str(js) + "_%d") % kk, kT[kk], ("kT" + str(js) + "_%d") % kk) for kk in range(nq + 2)]
            for t0 in range(0, len(tl), 2):
                grp = tl[t0:t0 + 2]
                pbt, pbk = next_pb()

                def trq_fn(e, grp=grp, pbt=pbt):
                    ins = None
                    for gi, (src, _, _, _) in enumerate(grp):
                        for c in range(4):
                            ins = e.transpose(out=pbt[:, gi * 512 + c * 128: gi * 512 + (c + 1) * 128], in_=src[:, c * 128:(c + 1) * 128], identity=identb)
                    return ins
                P.op("pe", trq_fn, reads=[g[1] for g in grp] + ["identb"], writes=[pbk])
                for gi, (_, _, dst, dk) in enumerate(grp):
                    CP("act" if gi == 0 else "dve", dst.rearrange("p c t -> p (c t)"), pbt[:, gi * 512:(gi + 1) * 512], [pbk], [dk])
            def it_body(jq, hg, s_, kT=kT, qT=qT, vsb=vsb, js=js, dd=dd, r=r, j0=j0, accv=accv):
                bufs = []
                for h in range(hg * 4, hg * 4 + 4):
                    p0 = (h % 2) * 64
                    blk = h // 2
                    pS, pSk = next_pf()

                    def s_fn(e, pS=pS, jq=jq, p0=p0, blk=blk, kT=kT, qT=qT):
                        ins = None
                        for sl in range(3):
                            ins = e.matmul(pS[:, sl * 128:(sl + 1) * 128], lhsT=kT[jq + sl][p0:p0 + 64, blk, :], rhs=qT[jq][p0:p0 + 64, blk, :],
                                           start=True, stop=True)
                        return ins
                    P.op("pe", s_fn, reads=[("kT" + str(js) + "_%d") % (jq + sl) for sl in range(3)] + [("qT" + str(js) + "_%d") % jq], writes=[pSk])
                    bi = 2 * s_ + cnt4[s_] % 2
                    cnt4[s_] += 1
                    ACT(pex[bi], pS[:, 0:384], AF.Exp, [pSk, "negc"], ["pex%d" % bi], bias=negc, scale=0.125)
                    TT("pool" if (h % 2) else "dve", pmk[bi], pex[bi], band, ALU.mult, ["pex%d" % bi, "band"], ["pmk%d" % bi])
                    bufs.append((bi, h))
                    if len(bufs) == 2:
                        pU, pUk = next_pf()

                        def pv_fn(e, pU=pU, jq=jq, bufs=tuple(bufs), vsb=vsb):
                            ins = None
                            for hi, (b_, h_) in enumerate(bufs):
                                for sl in range(3):
                                    ins = e.matmul(pU[0:65, hi * 128:(hi + 1) * 128], lhsT=vsb[jq + sl][:, h_, :], rhs=pmk[b_][:, sl * 128:(sl + 1) * 128],
                                                   start=(sl == 0), stop=(sl == 2))
                            return ins
                        P.op("pe", pv_fn, reads=[("vsb" + str(js) + "_%d") % (jq + sl) for sl in range(3)] + ["pmk%d" % b_ for (b_, _) in bufs], writes=[pUk])
                        n0 = 128 * (j0 + jq)
                        h0 = bufs[0][1]
                        dst = accv[:, h0:h0 + 2, r, n0:n0 + 128]
                        src = pU[0:65, 0:256].rearrange("p (h t) -> p h t", h=2)
                        if dd == 1:
                            CP("dve", dst, src, [pUk], ["accT"])
                        else:
                            TT("dve", dst, src, dst, ALU.add, [pUk, "accT"], ["accT"])
                        bufs = []
            its = [(jq, hg) for jq in range(nq) for hg in range(2)]
            for m in range(0, len(its), 2):
                ra, rb = [], []
                P.rec = ra
                stream[0] = 0
                it_body(its[m][0], its[m][1], 0)
                if m + 1 < len(its):
                    P.rec = rb
                    stream[0] = 1
                    it_body(its[m + 1][0], its[m + 1][1], 1)
                P.rec = None
                stream[0] = None
                P.replay_merged(ra, rb)
        rz = sb("rz", [64, 512])
        otb = [sb("otb%d" % i, [64, 512], BF16) for i in range(2)]
        k2 = 0
        for h in range(8):
            for g in range(4):
                pz, pzk = next_pf()
                P.op("pe", lambda e, pz=pz, h=h, g=g: e.matmul(pz[0:64, :], lhsT=cm[64:65, 7, 0:64], rhs=accT[64:65, h, g * 512:(g + 1) * 512],
                                                                start=True, stop=True), reads=["accT", "cm"], writes=[pzk])
                P.op("dve", lambda e, pz=pz: e.reciprocal(out=rz, in_=pz[0:64, :]), reads=[pzk], writes=["rz"])
                b2 = k2 % 2
                k2 += 1
                TT("pool", otb[b2], accT[0:64, h, g * 512:(g + 1) * 512], rz, ALU.mult, ["accT", "rz"], ["otb%d" % b2])
                DMA("sp", OTS[h, :, g * 512:(g + 1) * 512], otb[b2], ["otb%d" % b2], ["OTS%d_%d" % (h, g)])
        OTK = ["OTS%d_%d" % (h, g) for h in range(8) for g in range(4)]
        if stop_after == "B":
            P.op("sp", None, reads=OTK + MGK, writes=[])
            P.emit()
            return nc

        P.barrier()
        AR.off = M0
        bc_cache = {}

        def bcreg(e):
            if "r" not in bc_cache:
                bc_cache["r"] = e.to_reg(2559)
            return bc_cache["r"]
        CAPG = 640
        NSLOT = 4 * CAPG
        OOB = 4096.0
        u2tok = sb("u2tok", [128, NTO, D], BF16)
        OH = sb("OH", [128, NTO, 4])
        WE = sb("WE", [128, NTO, 8])
        idxf = sb("idxf", [128, NTO])
        idxi = sb("idxi", [128, 2 * NTO], I32)
        goffm = sb("goffm", [128, 4])
        pren = sb("pren", [128, 4])
        M2 = AR.off
        woutG = sb("woutG", [128, 4, D], BF16)
        woutA = sb("woutA", [64, 8, D], BF16)
        DMA("pool", woutG, w_out[0:512, :].rearrange("(c p) n -> p c n", p=128), [], ["woutG"])
        DMA("pool", woutA, w_out[512:1024, :].rearrange("(h p) n -> p h n", p=64), [], ["woutA"])
        for g in range(4):
            P.op("dve", lambda e, g=g: e.memset(goffm[:, g:g + 1], float(g * CAPG) - OOB), writes=["goffm"])
        P.op("dve", lambda e: e.memset(pren, 0.0), writes=["pren"])
        zx = sb("zx", [128, D], BF16)
        zw = sb("zw", [128, 8])
        P.op("pool", lambda e: e.memset(zx, 0.0), writes=["zx"])
        P.op("pool", lambda e: e.memset(zw, 0.0), writes=["zw"])
        for r0 in range(0, NSLOT, 128):
            DMA("sp", XB[r0:r0 + 128, :], zx, ["zx"], ["XB"])
            DMA("sp", WB[r0:r0 + 128, :], zw, ["zw"], ["WB"])
        xo = [sb("xo%d" % i, [128, D]) for i in range(2)]
        mgl = [sb("mgl%d" % i, [128, 512], BF16) for i in range(2)]
        otl = [sb("otl%d" % i, [64, 8, 128], BF16) for i in range(2)]
        mgT = sb("mgT", [128, 4, 128], BF16)
        h2t = [sb("h2t%d" % i, [128, D]) for i in range(2)]
        u2 = sb("u2", [128, D])
        u2Tf = sb("u2Tf", [128, 8, 128])
        junk2 = sb("junk2", [128, D], BF16)
        ss2 = sb("ss2", [128, 2])
        rs2 = sb("rs2", [128, 2])
        lg = sb("lg", [128, 36])
        sm = sb("sm", [128, 64])
        for io in range(NTO):
            b2 = io % 2
            DMA("sp", xo[b2], xw[HALO + io * 128: HALO + (io + 1) * 128, :], [], ["xo%d" % b2])
            DMA("sp", mgl[b2], MG[io * 128:(io + 1) * 128, :], ["MG%d" % io], ["mgl%d" % b2])
            DMA("sp", otl[b2], OTS[:, :, io * 128:(io + 1) * 128].rearrange("h p t -> p h t"), OTK, ["otl%d" % b2])
            pbt, pbk = next_pb()

            def trm_fn(e, pbt=pbt, b2=b2):
                ins = None
                for c in range(4):
                    ins = e.transpose(out=pbt[:, c * 128:(c + 1) * 128], in_=mgl[b2][:, c * 128:(c + 1) * 128], identity=identb)
                return ins
            P.op("pe", trm_fn, reads=["mgl%d" % b2, "identb"], writes=[pbk])
            CP("act", mgT.rearrange("p c t -> p (c t)"), pbt[:, 0:512], [pbk], ["mgT"])
            for cg in range(2):
                pt, pk = next_pf()
                pairs = [(mgT[:, c, :], woutG[:, c, cg * 512:(cg + 1) * 512]) for c in range(4)] + \
                        [(otl[b2][:, h, :], woutA[:, h, cg * 512:(cg + 1) * 512]) for h in range(8)]
                mm_group(pt[:, :], pairs, pk, ["mgT", "otl%d" % b2, "woutG", "woutA"])
                TT("dve", h2t[b2][:, cg * 512:(cg + 1) * 512], pt[:, :], xo[b2][:, cg * 512:(cg + 1) * 512], ALU.add,
                   [pk, "xo%d" % b2], ["h2t%d" % b2])
            DMA("sp", H2[io * 128:(io + 1) * 128, :], h2t[b2], ["h2t%d" % b2], ["H2_%d" % io])
            P.op("act", lambda e, b2=b2: e.activation(out=junk2, in_=h2t[b2], func=AF.Square, accum_out=ss2[:, 0:1]),
                 reads=["h2t%d" % b2], writes=["junk2", "ss2"])
            rstd_from_ssq(rs2[:, 0:1], ss2[:, 0:1], D, "ss2", "rs2")
            STT(u2, h2t[b2], rs2[:, 0:1], n2bc, ALU.mult, ALU.mult, ["h2t%d" % b2, "rs2", "n2bc"], ["u2"])
            CP("pool", u2tok[:, io, :], u2, ["u2"], ["u2tok%d" % io])
            for half in range(2):
                pt, pk = next_pf()

                def tru_fn(e, pt=pt, half=half):
                    ins = None
                    for c in range(4):
                        cc = half * 4 + c
                        ins = e.transpose(out=pt[:, c * 128:(c + 1) * 128], in_=u2[:, cc * 128:(cc + 1) * 128], identity=cm[:, 0, :])
                    return ins
                P.op("pe", tru_fn, reads=["u2", "cm"], writes=[pk])
                CP("act", u2Tf[:, half * 4:half * 4 + 4, :].rearrange("p c t -> p (c t)"), pt[:, :], [pk], ["u2Tf%d" % half])
            pr_, prk = next_pf()
            mm_group(pr_[:, 0:36], [(u2Tf[:, c, :], wr[:, c, :]) for c in range(8)], prk, ["u2Tf0", "u2Tf1", "wr"])
            TT("dve", lg, pr_[:, 0:36], rbbc, ALU.add, [prk, "rbbc"], ["lg"])
            gmax, ngmax, gsum, gw = sm[:, 0:1], sm[:, 1:2], sm[:, 2:3], sm[:, 3:4]
            oh = OH[:, io, :]
            ohk = "OH%d" % io
            ge = sm[:, 8:12]
            esel = sm[:, 16:24]
            top8 = sm[:, 24:32]
            d21, w1g, w2g = sm[:, 32:33], sm[:, 33:34], sm[:, 34:35]
            wa = sm[:, 40:48]
            wb_ = sm[:, 48:56]
            P.op("dve", lambda e: e.tensor_reduce(out=gmax, in_=lg[:, 0:4], axis=AX.X, op=ALU.max), reads=["lg"], writes=["sm"])
            TS("dve", oh, lg[:, 0:4], gmax, None, ALU.is_equal, None, ["lg", "sm"], [ohk])
            TS("dve", ngmax, gmax, -1.0, None, ALU.mult, None, ["sm"], ["sm"])
            ACT(ge, lg[:, 0:4], AF.Exp, ["lg", "sm"], ["sm"], bias=ngmax, scale=1.0)
            P.op("dve", lambda e: e.tensor_reduce(out=gsum, in_=ge, axis=AX.X, op=ALU.add), reads=["sm"], writes=["sm"])
            P.op("dve", lambda e: e.reciprocal(out=gw, in_=gsum), reads=["sm"], writes=["sm"])
            TS("dve", esel, lg[:, 4:12], oh[:, 0:1], None, ALU.mult, None, ["lg", ohk], ["sm"])
            for g in range(1, 4):
                STT(esel, lg[:, 4 + 8 * g:12 + 8 * g], oh[:, g:g + 1], esel, ALU.mult, ALU.add, ["lg", ohk, "sm"], ["sm"])
            P.op("dve", lambda e: e.max(out=top8, in_=esel), reads=["sm"], writes=["sm"])
            TT("dve", d21, top8[:, 1:2], top8[:, 0:1], ALU.subtract, ["sm"], ["sm"])
            ACT(d21, d21, AF.Exp, ["sm"], ["sm"])
            TS("dve", d21, d21, 1.0, None, ALU.add, None, ["sm"], ["sm"])
            P.op("dve", lambda e: e.reciprocal(out=w1g, in_=d21), reads=["sm"], writes=["sm"])
            TT("dve", w1g, w1g, gw, ALU.mult, ["sm"], ["sm"])
            TT("dve", w2g, gw, w1g, ALU.subtract, ["sm"], ["sm"])
            TS("dve", wa, esel, top8[:, 0:1], w1g, ALU.is_equal, ALU.mult, ["sm"], ["sm"])
            TS("dve", wb_, esel, top8[:, 1:2], w2g, ALU.is_equal, ALU.mult, ["sm"], ["sm"])
            TT("dve", WE[:, io, :], wa, wb_, ALU.add, ["sm"], ["WE%d" % io])
            prk_t, prkk = next_pf()
            P.op("pe", lambda e, t=prk_t, io=io: (e.matmul(t[:, 0:4], lhsT=cm[:, 4, :], rhs=OH[:, io, :], start=True, stop=False),
                                                   e.matmul(t[:, 0:4], lhsT=cm[:, 7, :], rhs=pren, start=False, stop=True))[1],
                 reads=[ohk, "pren", "cm"], writes=[prkk])
            rk = sm[:, 56:60]
            okm = sm[:, 60:64]
            TS("dve", rk, prk_t[:, 0:4], -16.0, None, ALU.mult, None, [prkk], ["sm"])
            STT(pren, oh, -1.0 / 16.0, pren, ALU.mult, ALU.add, [ohk, "pren", prkk], ["pren"])
            TS("dve", okm, rk, float(CAPG), None, ALU.is_lt, None, ["sm"], ["sm"])
            TT("dve", okm, okm, oh, ALU.mult, ["sm", ohk], ["sm"])
            TT("dve", rk, rk, goffm, ALU.add, ["sm", "goffm"], ["sm"])
            TT("dve", rk, rk, okm, ALU.mult, ["sm"], ["sm"])
            P.op("dve", lambda e, io=io: e.tensor_reduce(out=idxf[:, io:io + 1], in_=rk, axis=AX.X, op=ALU.add), reads=["sm"], writes=["idxf%d" % io])
            TS("dve", idxf[:, io:io + 1], idxf[:, io:io + 1], OOB, None, ALU.add, None, ["idxf%d" % io], ["idxf%d" % io])
            CP("dve", idxi[:, io:io + 1], idxf[:, io:io + 1], ["idxf%d" % io], ["idxi%d" % io])
            P.op("pool", lambda e, io=io: e.indirect_dma_start(out=XB[:, :], out_offset=bass.IndirectOffsetOnAxis(ap=idxi[:, io:io + 1], axis=0),
                                                               in_=u2tok[:, io, :], in_offset=None, bounds_check=bcreg(e), oob_is_err=False),
                 reads=["u2tok%d" % io, "idxi%d" % io, "XB"], writes=["XBs%d" % io], dma=True)
            P.op("pool", lambda e, io=io: e.indirect_dma_start(out=WB[:, :], out_offset=bass.IndirectOffsetOnAxis(ap=idxi[:, io:io + 1], axis=0),
                                                               in_=WE[:, io, :], in_offset=None, bounds_check=bcreg(e), oob_is_err=False),
                 reads=["WE%d" % io, "idxi%d" % io, "WB"], writes=["WBs%d" % io], dma=True)
        H2K = ["H2_%d" % i for i in range(NTO)]
        XBK = ["XBs%d" % i for i in range(NTO)] + ["XB"]
        WBK = ["WBs%d" % i for i in range(NTO)] + ["WB"]
        if debug:
            DMA("sp", WTD[:, 0:NTO], idxf, ["idxf%d" % i for i in range(NTO)], ["WTD"])
        if stop_after == "C1":
            P.op("sp", None, reads=H2K + XBK + WBK + ["WTD"], writes=[])
            P.emit()
            return nc

        P.barrier()
        AR.off = M2
        NCH = CAPG // 128
        xs = sb("xs", [128, NCH, D], BF16)
        xTg = sb("xTg", [128, 8, CAPG], BF16)
        wsl = sb("wsl", [128, NCH, 8])
        hid = sb("hid", [128, 4, CAPG], BF16)
        yacc = sb("yacc", [128, NCH, D])
        wgb = [sb("wgb%d" % i, [128, 8, 512], BF16) for i in range(2)]
        wub = [sb("wub%d" % i, [128, 8, 512], BF16) for i in range(2)]
        wdb = [sb("wdb%d" % i, [128, 4, D], BF16) for i in range(2)]
        sgb = [sb("sgb%d" % i, [128, 512]) for i in range(2)]

        def load_expert(ex):
            b = ex % 2
            DMA("pool", wgb[b], ewg[ex].rearrange("(c p) n -> p c n", p=128), [], ["wgb%d" % b])
            DMA("pool", wub[b], ewu[ex].rearrange("(c p) n -> p c n", p=128), [], ["wub%d" % b])
            DMA("pool", wdb[b], ewd[ex].rearrange("(c p) n -> p c n", p=128), [], ["wdb%d" % b])
        load_expert(0)
        kk2 = 0
        nsl = [(0, 512), (512, CAPG)]
        for g in range(4):
            DMA("sp", xs, XB[g * CAPG:(g + 1) * CAPG, :].rearrange("(c p) d -> p c d", p=128), XBK, ["xs"])
            DMA("sp", wsl, WB[g * CAPG:(g + 1) * CAPG, :].rearrange("(c p) d -> p c d", p=128), WBK, ["wsl"])
            for ch in range(NCH):
                pbt, pbk = next_pb()

                def trx_fn(e, pbt=pbt, ch=ch):
                    ins = None
                    for c in range(8):
                        ins = e.transpose(out=pbt[:, c * 128:(c + 1) * 128], in_=xs[:, ch, c * 128:(c + 1) * 128], identity=identb)
                    return ins
                P.op("pe", trx_fn, reads=["xs", "identb"], writes=[pbk])
                CP("act" if ch % 2 else "dve", xTg[:, :, ch * 128:(ch + 1) * 128], pbt[:, :].rearrange("p (c t) -> p c t", c=8), [pbk], ["xTg%d" % ch])
            XTK = ["xTg%d" % ch for ch in range(NCH)]
            for el in range(8):
                ex = g * 8 + el
                b = ex % 2
                if ex + 1 < NEXP:
                    load_expert(ex + 1)
                for (n0, n1) in nsl:
                    for fc in range(4):
                        pg, pgk = next_pf()
                        pu, puk = next_pf()
                        mm_group(pg[:, 0:n1 - n0], [(wgb[b][:, c, fc * 128:(fc + 1) * 128], xTg[:, c, n0:n1]) for c in range(8)], pgk,
                                 ["wgb%d" % b] + XTK)
                        mm_group(pu[:, 0:n1 - n0], [(wub[b][:, c, fc * 128:(fc + 1) * 128], xTg[:, c, n0:n1]) for c in range(8)], puk,
                                 ["wub%d" % b] + XTK)
                        sb_i = kk2 % 2
                        kk2 += 1
                        ACT(sgb[sb_i][:, 0:n1 - n0], pg[:, 0:n1 - n0], AF.Silu, [pgk], ["sgb%d" % sb_i])
                        TT("dve", hid[:, fc, n0:n1], sgb[sb_i][:, 0:n1 - n0], pu[:, 0:n1 - n0], ALU.mult, ["sgb%d" % sb_i, puk], ["hid%d_%d" % (fc, n0)])
                HK = ["hid%d_%d" % (fc, n0) for fc in range(4) for (n0, _) in nsl]
                for ch in range(NCH):
                    for cg in range(2):
                        py, pyk = next_pf()
                        mm_group(py[:, :], [(hid[:, fc, ch * 128:(ch + 1) * 128], wdb[b][:, fc, cg * 512:(cg + 1) * 512]) for fc in range(4)], pyk,
                                 ["wdb%d" % b] + HK)
                        ya = yacc[:, ch, cg * 512:(cg + 1) * 512]
                        yk = "yacc%d" % ch
                        if el == 0:
                            TS("dve", ya, py[:, :], wsl[:, ch, el:el + 1], None, ALU.mult, None, [pyk, "wsl"], [yk])
                        else:
                            STT(ya, py[:, :], wsl[:, ch, el:el + 1], ya, ALU.mult, ALU.add, [pyk, "wsl", yk], [yk])
            DMA("sp", YB[g * CAPG:(g + 1) * CAPG, :].rearrange("(c p) d -> p c d", p=128), yacc, ["yacc%d" % ch for ch in range(NCH)], ["YB%d" % g])
        YBK = ["YB%d" % g for g in range(4)]
        P.barrier()
        AR.off = M2
        hl = [sb("hl%d" % i, [128, D]) for i in range(2)]
        yg = [sb("yg%d" % i, [128, D]) for i in range(2)]
        ob = [sb("ob%d" % i, [128, D]) for i in range(2)]
        junk3 = sb("junk3", [128, D], BF16)
        ss3 = sb("ss3", [128, 2])
        rs3 = sb("rs3", [128, 2])
        for io in range(NTO):
            b2 = io % 2
            DMA("sp", hl[b2], H2[io * 128:(io + 1) * 128, :], ["H2_%d" % io], ["hl%d" % b2])
            P.op("pool", lambda e, b2=b2: e.memset(yg[b2], 0.0), writes=["yg%d" % b2])
            P.op("pool", lambda e, io=io, b2=b2: e.indirect_dma_start(out=yg[b2], out_offset=None, in_=YB[:, :],
                                                                       in_offset=bass.IndirectOffsetOnAxis(ap=idxi[:, io:io + 1], axis=0),
                                                                       bounds_check=bcreg(e), oob_is_err=False),
                 reads=YBK + ["idxi%d" % io], writes=["yg%d" % b2], dma=True)
            TT("dve", hl[b2], hl[b2], yg[b2], ALU.add, ["hl%d" % b2, "yg%d" % b2], ["hl%d" % b2])
            P.op("act", lambda e, b2=b2: e.activation(out=junk3, in_=hl[b2], func=AF.Square, accum_out=ss3[:, 0:1]),
                 reads=["hl%d" % b2], writes=["junk3", "ss3"])
            rstd_from_ssq(rs3[:, 0:1], ss3[:, 0:1], D, "ss3", "rs3")
            STT(ob[b2], hl[b2], rs3[:, 0:1], fnbc, ALU.mult, ALU.mult, ["hl%d" % b2, "rs3", "fnbc"], ["ob%d" % b2])
            DMA("sp", out_d[io * 128:(io + 1) * 128, :], ob[b2], ["ob%d" % b2], ["OUT%d" % io])
        P.op("sp", None, reads=["OUT%d" % i for i in range(NTO)], writes=[])
        P.emit()
    return nc


def _consts():
    s = np.arange(128)[:, None]
    t = np.arange(128)[None, :]
    cm = np.zeros((128, 8, 128), np.float32)
    cm[:, 0] = (s == t)
    cm[:, 1] = (s <= t) / -16.0
    cm[:, 2] = (s >= t) / -16.0
    cm[:, 3] = (s > t) / -16.0
    cm[:, 4] = (s < t) / -16.0
    cm[:, 5] = (s <= t)
    cm[:, 6] = (s >= t)
    cm[:, 7] = 1.0
    band = np.zeros((128, 384), np.float32)
    band[:, 0:128] = (s >= t + 64)
    band[:, 128:256] = (np.abs(s - t) <= 64)
    band[:, 256:384] = (s <= t - 64)
    return cm, band


def make_in_maps(inputs):
    f = lambda a: np.ascontiguousarray(np.asarray(a, dtype=np.float32))
    x = f(inputs["x"])
    cm, band = _consts()
    wz = np.zeros((33, 512), np.float32)
    wz[0:16, 0:256] = f(inputs["gla_fwd_gate_w"])[0]
    wz[16:32, 256:512] = f(inputs["gla_bwd_gate_w"])[0]
    wz[32, 0:256] = f(inputs["gla_fwd_gate_b"])[0]
    wz[32, 256:512] = f(inputs["gla_bwd_gate_b"])[0]
    vecs = np.zeros((4, D), np.float32)
    vecs[0] = f(inputs["norm1_w"])[0]
    vecs[1] = f(inputs["norm2_w"])[0]
    vecs[2] = f(inputs["final_norm_w"])
    vecs[3] = np.tile(f(inputs["gla_norm_w"])[0], 8)
    wr = np.concatenate([f(inputs["router_group_w"])[0]] + [f(inputs["router_expert_w"])[0, g] for g in range(4)], axis=1)
    rb = np.concatenate([f(inputs["router_group_b"])[0], f(inputs["router_expert_b"])[0].reshape(-1)])[None, :]
    inv = (500000.0 ** (-(np.arange(0, 16, 2, dtype=np.float32) / np.float32(16)))).astype(np.float32)
    shared = dict(cmat=cm, band3=band, w_in=f(inputs["w_in"])[0], wz=wz, vecs=vecs, w_out=f(inputs["w_out"])[0],
                  wr=np.ascontiguousarray(wr), rb=np.ascontiguousarray(rb), ewg=f(inputs["expert_w_gate"])[0],
                  ewu=f(inputs["expert_w_up"])[0], ewd=f(inputs["expert_w_down"])[0])
    maps = []
    for c in range(8):
        b, q = c // 4, c % 4
        s0 = q * OWN
        pos = np.arange(s0 - HALO, s0 + OWN + HALO)
        valid = (pos >= 0) & (pos < S)
        xwin = np.zeros((WIN, D), np.float32)
        xwin[valid] = x[b, pos[valid]]
        ang = (pos.astype(np.float32)[:, None] * inv[None, :]).astype(np.float32)
        cs = np.concatenate([np.cos(ang), np.sin(ang)], axis=1).astype(np.float32)
        cs_t = np.ascontiguousarray(cs.reshape(NTW, 128, 16).transpose(1, 0, 2))
        vcol = np.ascontiguousarray(valid.astype(np.float32).reshape(NTW, 128).T)
        m = dict(shared)
        m.update(xw=xwin, vcol=vcol, cs_t=cs_t)
        maps.append(m)
    return maps


_NC_CACHE = {}


def kernel(**inputs):
    maps = make_in_maps(inputs)
    if "nc" not in _NC_CACHE:
        _NC_CACHE["nc"] = build_program()
    nc = _NC_CACHE["nc"]
    res = run_bass_kernel_spmd(nc, maps, core_ids=list(range(8)))
    out = np.zeros((2, S, D), np.float32)
    for c in range(8):
        b, q = c // 4, c % 4
        out[b, q * OWN:(q + 1) * OWN] = res.results[c]["out"]
    return out
```

```python
import numpy as np
from contextlib import ExitStack
import concourse.bass as bass
import concourse.mybir as mybir
from concourse.bass_utils import run_bass_kernel_spmd

F32 = mybir.dt.float32
BF16 = mybir.dt.bfloat16
I32 = mybir.dt.int32
AF = mybir.ActivationFunctionType
ALU = mybir.AluOpType
AX = mybir.AxisListType

ENGS = ("pe", "act", "dve", "pool", "sp")
EPOCH = 4096
DMA_SLOTS = 8

D = 1024
S = 8192
OWN = 2048
HALO = 1024
WIN = OWN + 2 * HALO
NTW = WIN // 128
T0 = HALO // 128
NTO = OWN // 128
INW = 3104
NEXP = 32
EPS = 1e-6


class Op:
    __slots__ = ("eng", "fn", "dma", "deps", "sig", "sigcount", "dmaidx", "idx")

    def __init__(self, eng, fn, dma):
        self.eng = eng
        self.fn = fn
        self.dma = dma
        self.deps = []
        self.sig = False
        self.sigcount = 0
        self.dmaidx = -1
        self.idx = -1


class Prog:
    def __init__(self, nc):
        self.nc = nc
        self.ops = []
        self.last_w = {}
        self.readers = {}
        self.ndma = {e: 0 for e in ENGS}
        self.bar = None
        self.rec = None

    def barrier(self):
        deps = set()
        for e in ENGS:
            last = None
            nd = 0
            for o in reversed(self.ops):
                if o.eng != e:
                    continue
                if o.dma:
                    if nd < DMA_SLOTS:
                        deps.add(o.idx)
                        nd += 1
                elif last is None:
                    last = o.idx
                    deps.add(o.idx)
                if last is not None and nd >= DMA_SLOTS:
                    break
        b = self.op("sp", None)
        b.deps = sorted(deps | set(b.deps))
        self.bar = b.idx
        return b

    def replay_merged(self, a, b):
        na, nb = len(a), len(b)
        i = j = 0
        while i < na or j < nb:
            if j >= nb or (i < na and i * nb <= j * na):
                self.op(*a[i])
                i += 1
            else:
                self.op(*b[j])
                j += 1

    def op(self, eng, fn, reads=(), writes=(), dma=False):
        if self.rec is not None:
            self.rec.append((eng, fn, list(reads), list(writes), dma))
            return None
        import os as _os
        mx = int(_os.environ.get("DBG_MAXOPS", "0"))
        if mx and len(self.ops) >= mx and fn is not None:
            fn = None
            if dma:
                dma = False
        px = [k_ for k_ in reads if k_[:2] in ("pf", "pb")]
        if px:
            writes = list(writes) + [k_ for k_ in px if k_ not in writes]
            reads = [k_ for k_ in reads if k_ not in px]
        o = Op(eng, fn, dma)
        o.idx = len(self.ops)
        deps = set()
        if self.bar is not None:
            deps.add(self.bar)
        for k in reads:
            w = self.last_w.get(k)
            if w is not None:
                deps.add(w)
        for k in writes:
            w = self.last_w.get(k)
            if w is not None:
                deps.add(w)
            for r in self.readers.get(k, ()):
                deps.add(r)
        deps.discard(o.idx)
        o.deps = sorted(deps)
        for k in writes:
            self.last_w[k] = o.idx
            self.readers[k] = []
        for k in reads:
            if k not in writes:
                self.readers.setdefault(k, []).append(o.idx)
        if dma:
            o.dmaidx = self.ndma[eng]
            self.ndma[eng] += 1
        self.ops.append(o)
        return o

    def emit(self):
        nc = self.nc
        ops = self.ops
        for o in ops:
            for d in o.deps:
                p = ops[d]
                if not p.dma:
                    p.sig = True
        cnt = {e: 0 for e in ENGS}
        for o in ops:
            if o.sig and not o.dma:
                cnt[o.eng] += 1
                o.sigcount = cnt[o.eng]
        nsem = {e: (cnt[e] + EPOCH - 1) // EPOCH for e in ENGS}
        with ExitStack() as es:
            csem = {e: [es.enter_context(nc.semaphore("c_%s_%d" % (e, i))) for i in range(nsem[e])]
                    for e in ENGS}
            dsem = {e: [es.enter_context(nc.semaphore("d_%s_%d" % (e, i)))
                        for i in range(DMA_SLOTS if self.ndma[e] else 0)] for e in ENGS}
            block = es.enter_context(nc.Block())

            def body_for(e):
                def body(eng):
                    waited_c = {x: 0 for x in ENGS}
                    waited_d = {}
                    for o in ops:
                        if o.eng != e:
                            continue
                        need_c = {}
                        need_d = {}
                        for d in o.deps:
                            p = ops[d]
                            if p.dma:
                                slot = p.dmaidx % DMA_SLOTS
                                val = 16 * (p.dmaidx // DMA_SLOTS + 1)
                                key = (p.eng, slot)
                                if waited_d.get(key, 0) < val:
                                    need_d[key] = max(need_d.get(key, 0), val)
                            else:
                                if waited_c[p.eng] < p.sigcount:
                                    need_c[p.eng] = max(need_c.get(p.eng, 0), p.sigcount)
                        if o.dma:
                            slot = o.dmaidx % DMA_SLOTS
                            val = 16 * (o.dmaidx // DMA_SLOTS)
                            key = (e, slot)
                            if val > 0 and waited_d.get(key, 0) < val:
                                need_d[key] = max(need_d.get(key, 0), val)
                        for pe_, c in need_c.items():
                            ep = (c - 1) // EPOCH
                            eng.wait_ge(csem[pe_][ep], (c - 1) % EPOCH + 1)
                            waited_c[pe_] = c
                        for key, val in need_d.items():
                            eng.wait_ge(dsem[key[0]][key[1]], val)
                            waited_d[key] = val
                        ins = o.fn(eng) if o.fn is not None else None
                        if o.dma:
                            ins.then_inc(dsem[e][o.dmaidx % DMA_SLOTS], 16)
                        elif o.sig:
                            if ins is None:
                                ins = eng.nop()
                            ep = (o.sigcount - 1) // EPOCH
                            ins.then_inc(csem[e][ep], 1)
                return body

            block.tensor(body_for("pe"))
            block.scalar(body_for("act"))
            block.vector(body_for("dve"))
            block.gpsimd(body_for("pool"))
            block.sync(body_for("sp"))


class Arena:
    def __init__(self, ap, ncols):
        self.ap = ap
        self.n = ncols
        self.off = 0

    def alloc(self, shape, dt=F32):
        p = shape[0]
        rest = list(shape[1:])
        nel = 1
        for r in rest:
            nel *= r
        ncol = nel if dt in (F32, I32) else (nel + 1) // 2
        ncol += ncol % 2
        assert self.off + ncol <= self.n, "arena overflow: need %d have %d" % (ncol, self.n - self.off)
        v = self.ap[0:p, self.off:self.off + ncol]
        self.off += ncol
        if dt != F32:
            v = v.bitcast(dt)
        if v.shape[1] != nel:
            v = v[:, 0:nel]
        if len(rest) == 2:
            v = v.rearrange("p (a b) -> p a b", a=rest[0])
        elif len(rest) == 3:
            v = v.rearrange("p (a b c) -> p a b c", a=rest[0], b=rest[1])
        return v


def build_program(debug=False, stop_after=None, dbg_tiles=None):
    nc = bass.Bass("TRN2", target_bir_lowering=False)
    P = Prog(nc)
    global LASTP
    LASTP = P

    def din(name, shape, dt=F32):
        return nc.dram_tensor(name, list(shape), dt, kind="ExternalInput").ap()

    def dscr(name, shape, dt):
        kind = "ExternalOutput" if debug else "Internal"
        return nc.dram_tensor(name, list(shape), dt, kind=kind).ap()

    xw = din("xw", [WIN, D])
    vcol = din("vcol", [128, NTW])
    cs_t = din("cs_t", [128, NTW, 16])
    cmat = din("cmat", [128, 8, 128])
    band3 = din("band3", [128, 384])
    w_in = din("w_in", [D, INW])
    wz_d = din("wz", [33, 512])
    vecs = din("vecs", [4, D])
    w_out = din("w_out", [D, D])
    wr_d = din("wr", [D, 36])
    rb_d = din("rb", [1, 36])
    ewg = din("ewg", [NEXP, D, 512])
    ewu = din("ewu", [NEXP, D, 512])
    ewd = din("ewd", [NEXP, 512, D])
    out_d = nc.dram_tensor("out", [OWN, D], F32, kind="ExternalOutput").ap()
    QS = dscr("QS", [OWN, 512], BF16)
    KS = dscr("KS", [WIN + 2 * HALO, 512], BF16)
    VS = dscr("VS", [WIN + 2 * HALO, 520], BF16)
    GV = dscr("GV", [OWN, 512], BF16)
    GG = dscr("GG", [OWN, 512], BF16)
    MG = dscr("MG", [OWN, 512], BF16)
    OTS = dscr("OTS", [8, 64, OWN], BF16)
    H2 = dscr("H2", [OWN, D], F32)
    XB = dscr("XB", [2560, D], BF16)
    WB = dscr("WB", [2560, 8], F32)
    YB = dscr("YB", [2560, D], F32)
    WTD = nc.dram_tensor("WTD", [128, NTO * 32], F32, kind="ExternalOutput").ap() if debug else None

    QSK = ["QS%d" % i for i in range(NTO)]
    KSK = ["KS%d" % i for i in range(48)]
    VSK = ["VS%d" % i for i in range(48)]
    NCOL = 50 * 1024 + 512
    es = ExitStack()
    with es:
        arena_t = es.enter_context(nc.sbuf_tensor("arena", [128, NCOL], F32))
        AR = Arena(arena_t[:], NCOL)
        sb = lambda name, shape, dt=F32: AR.alloc(shape, dt)

        def ps(name, shape, dt=F32):
            return es.enter_context(nc.psum_tensor("p_" + name, list(shape), dt))

        pf = [ps("pf%d" % i, [128, 512]) for i in range(6)]
        pb = [ps("pb%d" % i, [128, 1024], BF16) for i in range(2)]
        pf_rr = [0]
        pb_rr = [0]

        stream = [None]
        srr = [0, 0]

        def next_pf():
            if stream[0] is None:
                i = pf_rr[0] % 6
                pf_rr[0] += 1
            else:
                s_ = stream[0]
                i = 3 * s_ + srr[s_] % 3
                srr[s_] += 1
            return pf[i], "pf%d" % i

        def next_pb():
            if stream[0] is None:
                i = pb_rr[0] % 2
                pb_rr[0] += 1
            else:
                i = stream[0]
            return pb[i], "pb%d" % i

        def mm_group(out_ap, pairs, okey, rkeys):
            def fn(e):
                ins = None
                n = len(pairs)
                for j, (l, r) in enumerate(pairs):
                    ins = e.matmul(out_ap, lhsT=l, rhs=r, start=(j == 0), stop=(j == n - 1))
                return ins
            P.op("pe", fn, reads=rkeys, writes=[okey])

        def ACT(out, in_, func, reads, writes, **kw):
            P.op("act", lambda e: e.activation(out=out, in_=in_, func=func, **kw), reads=reads, writes=writes)

        def TT(eng, out, in0, in1, op, reads, writes):
            P.op(eng, lambda e: e.tensor_tensor(out=out, in0=in0, in1=in1, op=op), reads=reads, writes=writes)

        def STT(out, in0, scalar, in1, op0, op1, reads, writes):
            P.op("dve", lambda e: e.scalar_tensor_tensor(out=out, in0=in0, scalar=scalar, in1=in1, op0=op0, op1=op1),
                 reads=reads, writes=writes)

        def TS(eng, out, in0, s1, s2, op0, op1, reads, writes):
            if op1 is None:
                P.op(eng, lambda e: e.tensor_scalar(out=out, in0=in0, scalar1=s1, scalar2=None, op0=op0), reads=reads, writes=writes)
            else:
                P.op(eng, lambda e: e.tensor_scalar(out=out, in0=in0, scalar1=s1, scalar2=s2, op0=op0, op1=op1), reads=reads, writes=writes)

        def CP(eng, out, in_, reads, writes):
            if eng == "act":
                ACT(out, in_, AF.Copy, reads, writes)
            else:
                P.op(eng, lambda e: e.tensor_copy(out=out, in_=in_), reads=reads, writes=writes)

        def DMA(q, out, in_, reads, writes):
            return P.op(q, lambda e: e.dma_start(out=out, in_=in_), reads=reads, writes=writes, dma=True)

        def rstd_from_ssq(dst, src, n, rk, wk):
            ACT(dst, src, AF.Ln, [rk, "epsc"], [wk], scale=1.0 / n, bias=epsc[0:dst.shape[0], :])
            ACT(dst, dst, AF.Exp, [wk], [wk], scale=-0.5)

        cm = sb("cm", [128, 8, 128])
        identb = sb("identb", [128, 128], BF16)
        band = sb("band", [128, 384], BF16)
        maskFB = sb("maskFB", [128, 4, 128])
        n16col = sb("n16col", [128, 2])
        epsc = sb("epsc", [128, 2])
        onec = sb("onec", [128, 2])
        negc = sb("negc", [128, 2])
        vc = sb("vc", [128, NTW])
        cst = sb("cst", [128, NTW, 16])
        wz = sb("wz", [33, 512])
        n1bc = sb("n1bc", [128, D])
        n2bc = sb("n2bc", [128, D])
        fnbc = sb("fnbc", [128, D])
        gnbc = sb("gnbc", [128, 512])
        rbbc = sb("rbbc", [128, 36])
        wr = sb("wr", [128, 8, 36])
        nmax = sb("nmax", [128, 16])
        n16col = n16col[:, 0:1]
        epsc = epsc[:, 0:1]
        onec = onec[:, 0:1]
        negc = negc[:, 0:1]

        DMA("sp", cm, cmat, [], ["cm"])
        DMA("pool", identb, cmat[:, 0, :], [], ["identb"])
        DMA("pool", band, band3, [], ["band"])
        DMA("sp", vc, vcol, [], ["vc"])
        DMA("sp", cst, cs_t, [], ["cst"])
        DMA("sp", wz, wz_d, [], ["wz"])
        DMA("sp", n1bc, vecs[0:1, :].partition_broadcast(128), [], ["n1bc"])
        DMA("sp", n2bc, vecs[1:2, :].partition_broadcast(128), [], ["n2bc"])
        DMA("sp", fnbc, vecs[2:3, :].partition_broadcast(128), [], ["fnbc"])
        DMA("sp", gnbc, vecs[3:4, 0:512].partition_broadcast(128), [], ["gnbc"])
        DMA("sp", rbbc, rb_d[0:1, :].partition_broadcast(128), [], ["rbbc"])
        DMA("sp", wr, wr_d.rearrange("(c p) n -> p c n", p=128), [], ["wr"])
        P.op("dve", lambda e: e.memset(n16col, -1.0 / 16.0), writes=["n16col"])
        P.op("dve", lambda e: e.memset(epsc, EPS), writes=["epsc"])
        P.op("dve", lambda e: e.memset(onec, 1.0), writes=["onec"])
        P.op("dve", lambda e: e.memset(nmax, 0.0), writes=["nmax"])
        for h in range(4):
            CP("dve", maskFB[:, h, :], cm[:, 5 + h // 2, :], ["cm"], ["maskFB"])
        M0 = AR.off

        attnT = sb("attnT", [128, NTO, 512], BF16)
        qdT = sb("qdT", [128, NTO, 4, 128], BF16)
        SfT = sb("SfT", [128, NTO, 2, 128], BF16)
        SbT = sb("SbT", [128, NTO, 2, 128], BF16)
        M1 = AR.off
        win = sb("win", [128, 8, INW], BF16)
        for c in range(8):
            DMA("pool", win[:, c, :], w_in[c * 128:(c + 1) * 128, :], [], ["win%d" % c])
        winkeys = ["win%d" % c for c in range(8)]
        kvB = sb("kvB", [128, NTW - T0, 2, 128], BF16)
        decB = sb("decB", [128, NTW - T0, 2])
        Sf = sb("Sf", [128, 2, 128])
        Sb = sb("Sb", [128, 2, 128])
        P.op("dve", lambda e: e.memset(Sf, 0.0), writes=["Sf"])
        P.op("dve", lambda e: e.memset(Sb, 0.0), writes=["Sb"])
        zt = sb("zt", [128, 520], BF16)
        P.op("pool", lambda e: e.memset(zt, 0.0), writes=["zt"])
        for blk in range(HALO // 128):
            for base in (0, HALO + WIN):
                r0 = base + blk * 128
                DMA("sp", KS[r0:r0 + 128, :], zt[:, 0:512], ["zt"], ["KS%d" % (r0 // 128)])
                DMA("sp", VS[r0:r0 + 128, :], zt, ["zt"], ["VS%d" % (r0 // 128)])
        xt = [sb("xt%d" % i, [128, D]) for i in range(2)]
        junk = sb("junk", [128, D], BF16)
        xn = [sb("xn%d" % i, [128, D], BF16) for i in range(2)]
        xnT = [sb("xnT%d" % i, [128, 8, 128], BF16) for i in range(2)]
        ssq = sb("ssq", [128, 2])
        rstd = sb("rstd", [128, 2])
        qk = [sb("qk%d" % i, [128, 512]) for i in range(2)]
        vbf = [sb("vbf%d" % i, [128, 512], BF16) for i in range(2)]
        gbf = [sb("gbf%d" % i, [128, 512], BF16) for i in range(2)]
        lr = [sb("lr%d" % i, [128, 32]) for i in range(2)]
        aqr = [sb("aqr%d" % i, [128, 8, 64]) for i in range(2)]
        akr = [sb("akr%d" % i, [128, 8, 64]) for i in range(2)]
        vab = [sb("vab%d" % i, [128, 8, 65], BF16) for i in range(2)]
        lrT = sb("lrT", [33, 128])
        ez = sb("ez", [128, 512])
        spl = sb("spl", [128, 512])
        E1 = sb("E1", [128, 512])
        E2 = sb("E2", [128, 512])
        E3 = sb("E3", [128, 512])
        dec = sb("dec", [128, 4])
        qd = sb("qd", [128, 512], BF16)
        ki = sb("ki", [128, 512], BF16)
        ke = sb("ke", [128, 512], BF16)
        kiT = sb("kiT", [128, 4, 128], BF16)
        qrb = [sb("qrb%d" % i, [128, 512], BF16) for i in range(2)]
        krb = [sb("krb%d" % i, [128, 512], BF16) for i in range(2)]
        rta = sb("rta", [128, 8, 8])
        rtb = sb("rtb", [128, 8, 8])
        rtc = sb("rtc", [128, 8, 8])
        rtd = sb("rtd", [128, 8, 8])
        sqs = sb("sqs", [128, 8, 64])
        nrm = sb("nrm", [128, 16])
        P.op("dve", lambda e: e.memset(lrT[32:33, :], 1.0), writes=["lrT_one"])
        tiles = list(range(NTW) if dbg_tiles is None else dbg_tiles)

        def S1(i):
            own = T0 <= i < T0 + NTO
            io = i - T0
            b2 = i % 2
            xtk, xnk, xnTk = "xt%d" % b2, "xn%d" % b2, "xnT%d" % b2
            if i == tiles[0]:
                DMA("sp", xt[b2], xw[i * 128:(i + 1) * 128, :], [], [xtk])
            if i + 1 < NTW and (dbg_tiles is None):
                DMA("sp", xt[(i + 1) % 2], xw[(i + 1) * 128:(i + 2) * 128, :], [], ["xt%d" % ((i + 1) % 2)])
            sk, rk = "ssq%d" % b2, "rstd%d" % b2
            P.op("act", lambda e, b2=b2: e.activation(out=junk, in_=xt[b2], func=AF.Square, accum_out=ssq[:, b2:b2 + 1]),
                 reads=[xtk], writes=["junk", sk])
            rstd_from_ssq(rstd[:, b2:b2 + 1], ssq[:, b2:b2 + 1], D, sk, rk)
            STT(xn[b2], xt[b2], rstd[:, b2:b2 + 1], n1bc, ALU.mult, ALU.mult, [xtk, rk, "n1bc"], [xnk])
            pbt, pbk = next_pb()

            def tr_fn(e, b2=b2, pbt=pbt):
                ins = None
                for c in range(8):
                    ins = e.transpose(out=pbt[:, c * 128:(c + 1) * 128], in_=xn[b2][:, c * 128:(c + 1) * 128], identity=identb)
                return ins
            P.op("pe", tr_fn, reads=[xnk, "identb"], writes=[pbk])
            CP("act", xnT[b2].rearrange("p c t -> p (c t)"), pbt[:, :], [pbk], [xnTk])

            def proj(c0, c1):
                pt, pk = next_pf()
                n = c1 - c0
                mm_group(pt[:, 0:n], [(xnT[b2][:, c, :], win[:, c, c0:c1]) for c in range(8)], pk, [xnTk] + winkeys)
                return pt, pk

            if own:
                pt, pk = proj(0, 512)
                CP("act", qk[b2], pt[:, 0:512], [pk], ["qk%d" % b2])
            else:
                pt, pk = proj(256, 512)
                CP("act", qk[b2][:, 256:512], pt[:, 0:256], [pk], ["qk%d" % b2])
            pt, pk = proj(512, 1024)
            CP("dve", vbf[b2], pt[:, 0:512], [pk], ["vbf%d" % b2])
            if own:
                DMA("sp", GV[io * 128:(io + 1) * 128, :], vbf[b2], ["vbf%d" % b2], ["GV%d" % io])
                pt, pk = proj(1024, 1536)
                CP("act", gbf[b2], pt[:, 0:512], [pk], ["gbf%d" % b2])
                DMA("sp", GG[io * 128:(io + 1) * 128, :], gbf[b2], ["gbf%d" % b2], ["GG%d" % io])
            pt, pk = proj(1536, 1568)
            CP("dve", lr[b2], pt[:, 0:32], [pk], ["lr%d" % b2])
            if own:
                pt, pk = proj(1568, 2080)
                CP("dve", aqr[b2].rearrange("p h d -> p (h d)"), pt[:, 0:512], [pk], ["aqr%d" % b2])
            pt, pk = proj(2080, 2592)
            CP("act", akr[b2].rearrange("p h d -> p (h d)"), pt[:, 0:512], [pk], ["akr%d" % b2])
            pt, pk = proj(2592, 3104)
            r0k = HALO + i * 128
            CP("act", vab[b2][:, :, 0:64], pt[:, 0:512].rearrange("p (h d) -> p h d", h=8), [pk], ["vab%d" % b2])
            CP("dve", vab[b2][:, :, 64:65], vc[:, i:i + 1].unsqueeze(1).broadcast_to([128, 8, 1]), ["vc"], ["vab%d" % b2])
            DMA("sp", VS[r0k:r0k + 128, :], vab[b2].rearrange("p h d -> p (h d)"), ["vab%d" % b2], ["VS%d" % (r0k // 128)])

        def S2(i):
            own = T0 <= i < T0 + NTO
            left = i < T0
            io = i - T0
            b2 = i % 2
            qkb = qk[b2]
            qkk = "qk%d" % b2
            vkey = "vbf%d" % b2
            vt = vbf[b2]
            pt, pk = next_pf()
            P.op("pe", lambda e, pt=pt: e.transpose(out=pt[0:32, 0:128], in_=lr[b2], identity=cm[:, 0, :]), reads=["lr%d" % b2, "cm"], writes=[pk])
            CP("dve", lrT[0:32, :], pt[0:32, 0:128], [pk], ["lrT"])
            pz, pzk = next_pf()
            P.op("pe", lambda e, pz=pz: e.matmul(pz[:, :], lhsT=lrT, rhs=wz, start=True, stop=True),
                 reads=["lrT", "lrT_one", "wz"], writes=[pzk])
            ACT(ez, pz[:, :], AF.Exp, [pzk], ["ez"], scale=-1.0)
            ACT(spl, ez, AF.Ln, ["ez", "onec"], ["spl"], bias=onec, scale=1.0)
            pbb, pbbk = next_pf()
            P.op("pe", lambda e, pbb=pbb: (e.matmul(pbb[:, 0:256], lhsT=cm[:, 1, :], rhs=spl[:, 0:256], start=True, stop=True),
                                           e.matmul(pbb[:, 256:512], lhsT=cm[:, 2, :], rhs=spl[:, 256:512], start=True, stop=True))[1],
                 reads=["cm", "spl"], writes=[pbbk])
            ACT(E1, pbb[:, :], AF.Exp, [pbbk], ["E1"])
            ACT(E2, pbb[:, :], AF.Exp, [pbbk], ["E2"], scale=-1.0)
            pb3, pb3k = next_pf()
            P.op("pe", lambda e, pb3=pb3: (e.matmul(pb3[:, 0:256], lhsT=cm[:, 3, :], rhs=spl[:, 0:256], start=True, stop=True),
                                           e.matmul(pb3[:, 256:512], lhsT=cm[:, 4, :], rhs=spl[:, 256:512], start=True, stop=True))[1],
                 reads=["cm", "spl"], writes=[pb3k])
            ACT(E3, pb3[:, :], AF.Exp, [pb3k], ["E3"])
            pdc, pdck = next_pf()

            def dec_fn(e, pdc=pdc):
                ins = None
                for j in range(4):
                    ins = e.matmul(pdc[:, j:j + 1], lhsT=spl[:, j * 128:(j + 1) * 128], rhs=n16col, start=True, stop=True)
                return ins
            P.op("pe", dec_fn, reads=["spl", "n16col"], writes=[pdck])
            ACT(dec, pdc[:, 0:4], AF.Exp, [pdck], ["dec"])
            if own:
                STT(qd[:, 0:256], qkb[:, 0:256], 0.125, E1[:, 0:256], ALU.mult, ALU.mult, [qkk, "E1"], ["qd"])
                STT(qd[:, 256:512], qkb[:, 0:256], 0.125, E1[:, 256:512], ALU.mult, ALU.mult, [qkk, "E1"], ["qd"])
                TT("pool", ki[:, 0:256], qkb[:, 256:512], E2[:, 0:256], ALU.mult, [qkk, "E2"], ["ki"])
                TT("pool", ki[:, 256:512], qkb[:, 256:512], E2[:, 256:512], ALU.mult, [qkk, "E2"], ["ki"])
            TT("pool", ke[:, 0:256], qkb[:, 256:512], E3[:, 0:256], ALU.mult, [qkk, "E3"], ["ke"])
            TT("pool", ke[:, 256:512], qkb[:, 256:512], E3[:, 256:512], ALU.mult, [qkk, "E3"], ["ke"])
            if own:
                pbt, pbk = next_pb()

                def tr2_fn(e, pbt=pbt):
                    ins = None
                    for j in range(4):
                        ins = e.transpose(out=pbt[:, j * 128:(j + 1) * 128], in_=qd[:, j * 128:(j + 1) * 128], identity=identb)
                    for j in range(4):
                        ins = e.transpose(out=pbt[:, 512 + j * 128:512 + (j + 1) * 128], in_=ki[:, j * 128:(j + 1) * 128], identity=identb)
                    return ins
                P.op("pe", tr2_fn, reads=["qd", "ki", "identb"], writes=[pbk])
                CP("act", qdT[:, io, :, :].rearrange("p c t -> p (c t)"), pbt[:, 0:512], [pbk], ["qdT%d" % io])
                CP("dve", kiT.rearrange("p c t -> p (c t)"), pbt[:, 512:1024], [pbk], ["kiT"])
                paX, paXk = next_pf()
                paY, paYk = next_pf()

                def att_fn(e, pa, par, io=io):
                    ins = None
                    p0 = par * 64
                    for dirn in range(2):
                        for pr in range(2):
                            blk = dirn * 2 + pr
                            sl = dirn * 2 + pr
                            ins = e.matmul(pa[:, sl * 128:(sl + 1) * 128], lhsT=kiT[p0:p0 + 64, blk, :], rhs=qdT[p0:p0 + 64, io, blk, :],
                                           start=True, stop=True)
                    return ins
                P.op("pe", lambda e, pa=paX, f=att_fn: f(e, pa, 0), reads=["kiT", "qdT%d" % io], writes=[paXk])
                P.op("pe", lambda e, pa=paY, f=att_fn: f(e, pa, 1), reads=["kiT", "qdT%d" % io], writes=[paYk])
                TT("dve", ez, paX[:, :], maskFB.rearrange("p h c -> p (h c)"), ALU.mult, [paXk, "maskFB"], ["ez"])
                TT("dve", E1, paY[:, :], maskFB.rearrange("p h c -> p (h c)"), ALU.mult, [paYk, "maskFB"], ["E1"])
                av = attnT[:, io, :].rearrange("p (a b c) -> p a b c", a=2, b=2)
                TT("pool", av[:, :, 0, :], ez[:, 0:256].rearrange("p (a c) -> p a c", a=2), ez[:, 256:512].rearrange("p (a c) -> p a c", a=2),
                   ALU.add, ["ez"], ["attnT%d" % io])
                TT("pool", av[:, :, 1, :], E1[:, 0:256].rearrange("p (a c) -> p a c", a=2), E1[:, 256:512].rearrange("p (a c) -> p a c", a=2),
                   ALU.add, ["E1"], ["attnT%d" % io])
            for dirn in range(2):
                if dirn == 0 and i >= T0 + NTO:
                    continue
                if dirn == 1 and left:
                    continue
                pkv, pkvk = next_pf()

                def kv_fn(e, pkv=pkv, dirn=dirn, vt=vt):
                    ins = None
                    for pr in range(2):
                        ins = e.matmul(pkv[:, pr * 256:(pr + 1) * 256], lhsT=ke[:, dirn * 256 + pr * 128: dirn * 256 + (pr + 1) * 128],
                                       rhs=vt[:, pr * 256:(pr + 1) * 256], start=True, stop=True)
                    return ins
                P.op("pe", kv_fn, reads=["ke", vkey], writes=[pkvk])
                if dirn == 0:
                    if own:
                        CP("act", SfT[:, io, :, :].rearrange("p a b -> p (a b)"), Sf.rearrange("p a b -> p (a b)"), ["Sf"], ["SfT%d" % io])
                    for pr in range(2):
                        for hh in range(2):
                            p0 = hh * 64
                            STT(Sf[p0:p0 + 64, pr, :], Sf[p0:p0 + 64, pr, :], dec[p0:p0 + 64, pr:pr + 1],
                                pkv[p0:p0 + 64, pr * 256 + hh * 128: pr * 256 + (hh + 1) * 128], ALU.mult, ALU.add,
                                ["Sf", "dec", pkvk], ["Sf"])
                else:
                    ib = i - T0
                    for pr in range(2):
                        for hh in range(2):
                            p0 = hh * 64
                            CP("act", kvB[p0:p0 + 64, ib, pr, :], pkv[p0:p0 + 64, pr * 256 + hh * 128: pr * 256 + (hh + 1) * 128],
                               [pkvk], ["kvB%d" % ib])
                    CP("dve", decB[:, ib, :], dec[:, 2:4], ["dec"], ["decB%d" % ib])

            def rope(raw, rkey):
                cosb = cst[:, i, 0:8].unsqueeze(1).broadcast_to([128, 8, 8])
                sinb = cst[:, i, 8:16].unsqueeze(1).broadcast_to([128, 8, 8])
                TT("pool", rta, raw[:, :, 0:8], cosb, ALU.mult, [rkey, "cst"], ["rta"])
                TT("pool", rtb, raw[:, :, 8:16], sinb, ALU.mult, [rkey, "cst"], ["rtb"])
                TT("pool", rtc, raw[:, :, 8:16], cosb, ALU.mult, [rkey, "cst"], ["rtc"])
                TT("pool", rtd, raw[:, :, 0:8], sinb, ALU.mult, [rkey, "cst"], ["rtd"])
                TT("dve", raw[:, :, 0:8], rta, rtb, ALU.subtract, ["rta", "rtb"], [rkey])
                TT("dve", raw[:, :, 8:16], rtc, rtd, ALU.add, ["rtc", "rtd"], [rkey])

            def sqnorm(src, col0, skey):
                TT("pool", sqs, src, src, ALU.mult, [skey], ["sqs"])
                P.op("dve", lambda e: e.tensor_reduce(out=nrm[:, col0:col0 + 8], in_=sqs, axis=AX.X, op=ALU.add), reads=["sqs"], writes=["nrm"])
                TT("dve", nmax[:, col0:col0 + 8], nmax[:, col0:col0 + 8], nrm[:, col0:col0 + 8], ALU.max, ["nrm", "nmax"], ["nmax"])

            r0k = HALO + i * 128
            if own:
                rope(aqr[b2], "aqr%d" % b2)
                sqnorm(aqr[b2], 0, "aqr%d" % b2)
                CP("act", qrb[b2], aqr[b2].rearrange("p h d -> p (h d)"), ["aqr%d" % b2], ["qrb%d" % b2])
                DMA("sp", QS[io * 128:(io + 1) * 128, :], qrb[b2], ["qrb%d" % b2], ["QS%d" % io])
            rope(akr[b2], "akr%d" % b2)
            sqnorm(akr[b2], 8, "akr%d" % b2)
            CP("act", krb[b2], akr[b2].rearrange("p h d -> p (h d)"), ["akr%d" % b2], ["krb%d" % b2])
            DMA("sp", KS[r0k:r0k + 128, :], krb[b2], ["krb%d" % b2], ["KS%d" % (r0k // 128)])

        for n_ in range(len(tiles) + 1):
            ra, rb = [], []
            if n_ < len(tiles):
                P.rec = ra
                stream[0] = 0
                S1(tiles[n_])
            if n_ > 0:
                P.rec = rb
                stream[0] = 1
                S2(tiles[n_ - 1])
            P.rec = None
            stream[0] = None
            P.replay_merged(ra, rb)

        if stop_after == "A":
            P.op("sp", None, reads=[k_ for k_ in P.last_w.keys() if k_[:2] in ("QS", "KS", "VS", "GV", "GG")], writes=[])
            P.emit()
            return nc
        for i in range(NTW - 1, T0 - 1, -1):
            ib = i - T0
            if ib < NTO:
                CP("act", SbT[:, ib, :, :].rearrange("p a b -> p (a b)"), Sb.rearrange("p a b -> p (a b)"), ["Sb"], ["SbT%d" % ib])
            if i == T0:
                break
            for pr in range(2):
                STT(Sb[:, pr, :], Sb[:, pr, :], decB[:, ib, pr:pr + 1], kvB[:, ib, pr, :], ALU.mult, ALU.add,
                    ["Sb", "decB%d" % ib, "kvB%d" % ib], ["Sb"])

        P.barrier()
        AR.off = M1
        vb2 = [sb("vb2%d" % i, [128, 512], BF16) for i in range(2)]
        gb2 = [sb("gb2%d" % i, [128, 512], BF16) for i in range(2)]
        osb = sb("osb", [128, 512])
        osq = sb("osq", [128, 4, 128])
        oms = sb("oms", [128, 4])
        sgs = sb("sgs", [128, 512])
        ybf = sb("ybf", [128, 512])
        mixb = [sb("mixb%d" % i, [128, 512], BF16) for i in range(2)]
        for io in range(NTO):
            b2 = io % 2
            DMA("sp", vb2[b2], GV[io * 128:(io + 1) * 128, :], ["GV%d" % io], ["vb2%d" % b2])
            DMA("sp", gb2[b2], GG[io * 128:(io + 1) * 128, :], ["GG%d" % io], ["gb2%d" % b2])
            poX, poXk = next_pf()
            poY, poYk = next_pf()

            def o_fn(e, po, par, io=io, b2=b2):
                ins = None
                p0 = par * 64
                for pr in range(2):
                    h = pr * 2 + par
                    oap = po[:, pr * 128:(pr + 1) * 128]
                    e.matmul(oap, lhsT=attnT[:, io, h * 128:(h + 1) * 128], rhs=vb2[b2][:, h * 128:(h + 1) * 128], start=True, stop=False)
                    e.matmul(oap, lhsT=qdT[p0:p0 + 64, io, pr, :], rhs=SfT[p0:p0 + 64, io, pr, :], start=False, stop=False)
                    ins = e.matmul(oap, lhsT=qdT[p0:p0 + 64, io, 2 + pr, :], rhs=SbT[p0:p0 + 64, io, pr, :], start=False, stop=True)
                return ins
            rk_ = ["attnT%d" % io, "qdT%d" % io, "SfT%d" % io, "SbT%d" % io, "vb2%d" % b2]
            P.op("pe", lambda e, po=poX, f=o_fn: f(e, po, 0), reads=rk_, writes=[poXk])
            P.op("pe", lambda e, po=poY, f=o_fn: f(e, po, 1), reads=rk_, writes=[poYk])
            ov = osb.rearrange("p (a b c) -> p a b c", a=2, b=2)
            CP("act", ov[:, :, 0, :], poX[:, 0:256].rearrange("p (a c) -> p a c", a=2), [poXk], ["osb"])
            CP("act", ov[:, :, 1, :], poY[:, 0:256].rearrange("p (a c) -> p a c", a=2), [poYk], ["osb"])
            TT("pool", osq.rearrange("p h d -> p (h d)"), osb, osb, ALU.mult, ["osb"], ["osq"])
            P.op("dve", lambda e: e.tensor_reduce(out=oms, in_=osq, axis=AX.X, op=ALU.add), reads=["osq"], writes=["oms"])
            rstd_from_ssq(oms, oms, 128, "oms", "oms")
            ACT(sgs, gb2[b2], AF.Silu, ["gb2%d" % b2], ["sgs"])
            TT("dve", ybf, osb, gnbc, ALU.mult, ["osb", "gnbc"], ["ybf"])
            for h in range(4):
                STT(mixb[b2][:, h * 128:(h + 1) * 128], ybf[:, h * 128:(h + 1) * 128], oms[:, h:h + 1], sgs[:, h * 128:(h + 1) * 128],
                    ALU.mult, ALU.mult, ["ybf", "oms", "sgs"], ["mixb%d" % b2])
            DMA("sp", MG[io * 128:(io + 1) * 128, :], mixb[b2], ["mixb%d" % b2], ["MG%d" % io])
        MGK = ["MG%d" % i for i in range(NTO)]
        if stop_after == "G2":
            P.op("sp", None, reads=QSK + KSK + VSK + MGK, writes=[])
            P.emit()
            return nc

        P.barrier()
        AR.off = M0
        nm2 = sb("nm2", [128, 2])
        m2 = sb("m2", [2, 2])
        m1 = sb("m1", [1, 4])
        P.op("dve", lambda e: e.tensor_reduce(out=nm2, in_=nmax.rearrange("p (a h) -> p a h", a=2), axis=AX.X, op=ALU.max),
             reads=["nmax"], writes=["nm2"])
        pt, pk = next_pf()
        P.op("pe", lambda e, pt=pt: e.transpose(out=pt[0:2, 0:128], in_=nm2, identity=cm[:, 0, :]), reads=["nm2", "cm"], writes=[pk])
        P.op("dve", lambda e, pt=pt: e.tensor_reduce(out=m2[:, 0:1], in_=pt[0:2, 0:128], axis=AX.X, op=ALU.max), reads=[pk], writes=["m2"])
        pt, pk = next_pf()
        P.op("pe", lambda e, pt=pt: e.transpose(out=pt[0:1, 0:2], in_=m2[:, 0:1], identity=cm[0:2, 0, 0:2]), reads=["m2", "cm"], writes=[pk])
        CP("dve", m1[:, 0:2], pt[0:1, 0:2], [pk], ["m1"])
        TT("dve", m1[:, 2:3], m1[:, 0:1], m1[:, 1:2], ALU.mult, ["m1"], ["m1"])
        ACT(m1[:, 3:4], m1[:, 2:3], AF.Ln, ["m1"], ["m1"])
        ACT(m1[:, 3:4], m1[:, 3:4], AF.Exp, ["m1"], ["m1"], scale=0.5)
        TS("dve", m1[:, 3:4], m1[:, 3:4], -0.125, None, ALU.mult, None, ["m1"], ["m1"])
        pt, pk = next_pf()
        P.op("pe", lambda e, pt=pt: e.matmul(pt[:, 0:1], lhsT=cm[0:1, 7, :], rhs=m1[:, 3:4], start=True, stop=True), reads=["m1", "cm"], writes=[pk])
        CP("dve", negc, pt[:, 0:1], [pk], ["negc"])

        accT = sb("accT", [65, 8, OWN])
        NQ = 8
        qsb2 = [[sb("qsb%d" % i, [128, 512], BF16) for i in range(NQ)] for _ in range(2)]
        ksb2 = [[sb("ksb%d" % i, [128, 512], BF16) for i in range(NQ + 2)] for _ in range(2)]
        vsb2 = [[sb("vsb%d" % i, [128, 8, 65], BF16) for i in range(NQ + 2)] for _ in range(2)]
        qT2 = [[sb("qT%d" % i, [128, 4, 128], BF16) for i in range(NQ)] for _ in range(2)]
        kT2 = [[sb("kT%d" % i, [128, 4, 128], BF16) for i in range(NQ + 2)] for _ in range(2)]
        pex = [sb("pex%d" % i, [128, 384], BF16) for i in range(4)]
        pmk = [sb("pmk%d" % i, [128, 384], BF16) for i in range(4)]
        cnt4 = [0, 0]
        jobs = [(1, 0, 0, 8), (1, 0, 8, 8)] + [(4, r, 0, 4) for r in range(4)] + [(16, r, 0, 1) for r in range(16)]
        for jn, (dd, r, j0, nq) in enumerate(jobs):
            js = jn % 2
            qsb, ksb, vsb, qT, kT = qsb2[js], ksb2[js], vsb2[js], qT2[js], kT2[js]
            QSv = QS.rearrange("(n d) c -> d n c", d=dd)
            KSv = KS.rearrange("(n d) c -> d n c", d=dd)
            VSv = VS.rearrange("(n d) c -> d n c", d=dd)
            accv = accT.rearrange("p h (n d) -> p h d n", d=dd)
            for jq in range(nq):
                n0 = 128 * (j0 + jq)
                DMA("sp", qsb[jq], QSv[r, n0:n0 + 128, :], QSK, [("qsb" + str(js) + "_%d") % jq])
            for kk in range(nq + 2):
                n0 = 2048 // dd + 128 * (j0 + kk - 1)
                DMA("sp", ksb[kk], KSv[r, n0:n0 + 128, :], KSK, [("ksb" + str(js) + "_%d") % kk])
                DMA("sp", vsb[kk].rearrange("p h d -> p (h d)"), VSv[r, n0:n0 + 128, :], VSK, [("vsb" + str(js) + "_%d") % kk])
            tl = [(qsb[jq], ("qsb" + str(js) + "_%d") % jq, qT[jq], ("qT" + str(js) + "_%d") % jq) for jq in range(nq)] + \
                 [(ksb[kk], ("ksb" + str(js) + "_%d") % kk, kT[kk], ("kT" + str(js) + "_%d") % kk) for kk in range(nq + 2)]
            for t0 in range(0, len(tl), 2):
                grp = tl[t0:t0 + 2]
                pbt, pbk = next_pb()

                def trq_fn(e, grp=grp, pbt=pbt):
                    ins = None
                    for gi, (src, _, _, _) in enumerate(grp):
                        for c in range(4):
                            ins = e.transpose(out=pbt[:, gi * 512 + c * 128: gi * 512 + (c + 1) * 128], in_=src[:, c * 128:(c + 1) * 128], identity=identb)
                    return ins
                P.op("pe", trq_fn, reads=[g[1] for g in grp] + ["identb"], writes=[pbk])
                for gi, (_, _, dst, dk) in enumerate(grp):
                    CP("act" if gi == 0 else "dve", dst.rearrange("p c t -> p (c t)"), pbt[:, gi * 512:(gi + 1) * 512], [pbk], [dk])
            def it_body(jq, hg, s_, kT=kT, qT=qT, vsb=vsb, js=js, dd=dd, r=r, j0=j0, accv=accv):
                bufs = []
                for h in range(hg * 4, hg * 4 + 4):
                    p0 = (h % 2) * 64
                    blk = h // 2
                    pS, pSk = next_pf()

                    def s_fn(e, pS=pS, jq=jq, p0=p0, blk=blk, kT=kT, qT=qT):
                        ins = None
                        for sl in range(3):
                            ins = e.matmul(pS[:, sl * 128:(sl + 1) * 128], lhsT=kT[jq + sl][p0:p0 + 64, blk, :], rhs=qT[jq][p0:p0 + 64, blk, :],
                                           start=True, stop=True)
                        return ins
                    P.op("pe", s_fn, reads=[("kT" + str(js) + "_%d") % (jq + sl) for sl in range(3)] + [("qT" + str(js) + "_%d") % jq], writes=[pSk])
                    bi = 2 * s_ + cnt4[s_] % 2
                    cnt4[s_] += 1
                    ACT(pex[bi], pS[:, 0:384], AF.Exp, [pSk, "negc"], ["pex%d" % bi], bias=negc, scale=0.125)
                    TT("pool" if (h % 2) else "dve", pmk[bi], pex[bi], band, ALU.mult, ["pex%d" % bi, "band"], ["pmk%d" % bi])
                    bufs.append((bi, h))
                    if len(bufs) == 2:
                        pU, pUk = next_pf()

                        def pv_fn(e, pU=pU, jq=jq, bufs=tuple(bufs), vsb=vsb):
                            ins = None
                            for hi, (b_, h_) in enumerate(bufs):
                                for sl in range(3):
                                    ins = e.matmul(pU[0:65, hi * 128:(hi + 1) * 128], lhsT=vsb[jq + sl][:, h_, :], rhs=pmk[b_][:, sl * 128:(sl + 1) * 128],
                                                   start=(sl == 0), stop=(sl == 2))
                            return ins
                        P.op("pe", pv_fn, reads=[("vsb" + str(js) + "_%d") % (jq + sl) for sl in range(3)] + ["pmk%d" % b_ for (b_, _) in bufs], writes=[pUk])
                        n0 = 128 * (j0 + jq)
                        h0 = bufs[0][1]
                        dst = accv[:, h0:h0 + 2, r, n0:n0 + 128]
                        src = pU[0:65, 0:256].rearrange("p (h t) -> p h t", h=2)
                        if dd == 1:
                            CP("dve", dst, src, [pUk], ["accT"])
                        else:
                            TT("dve", dst, src, dst, ALU.add, [pUk, "accT"], ["accT"])
                        bufs = []
            its = [(jq, hg) for jq in range(nq) for hg in range(2)]
            for m in range(0, len(its), 2):
                ra, rb = [], []
                P.rec = ra
                stream[0] = 0
                it_body(its[m][0], its[m][1], 0)
                if m + 1 < len(its):
                    P.rec = rb
                    stream[0] = 1
                    it_body(its[m + 1][0], its[m + 1][1], 1)
                P.rec = None
                stream[0] = None
                P.replay_merged(ra, rb)
        rz = sb("rz", [64, 512])
        otb = [sb("otb%d" % i, [64, 512], BF16) for i in range(2)]
        k2 = 0
        for h in range(8):
            for g in range(4):
                pz, pzk = next_pf()
                P.op("pe", lambda e, pz=pz, h=h, g=g: e.matmul(pz[0:64, :], lhsT=cm[64:65, 7, 0:64], rhs=accT[64:65, h, g * 512:(g + 1) * 512],
                                                                start=True, stop=True), reads=["accT", "cm"], writes=[pzk])
                P.op("dve", lambda e, pz=pz: e.reciprocal(out=rz, in_=pz[0:64, :]), reads=[pzk], writes=["rz"])
                b2 = k2 % 2
                k2 += 1
                TT("pool", otb[b2], accT[0:64, h, g * 512:(g + 1) * 512], rz, ALU.mult, ["accT", "rz"], ["otb%d" % b2])
                DMA("sp", OTS[h, :, g * 512:(g + 1) * 512], otb[b2], ["otb%d" % b2], ["OTS%d_%d" % (h, g)])
        OTK = ["OTS%d_%d" % (h, g) for h in range(8) for g in range(4)]
        if stop_after == "B":
            P.op("sp", None, reads=OTK + MGK, writes=[])
            P.emit()
            return nc

        P.barrier()
        AR.off = M0
        bc_cache = {}

        def bcreg(e):
            if "r" not in bc_cache:
                bc_cache["r"] = e.to_reg(2559)
            return bc_cache["r"]
        CAPG = 640
        NSLOT = 4 * CAPG
        OOB = 4096.0
        u2tok = sb("u2tok", [128, NTO, D], BF16)
        OH = sb("OH", [128, NTO, 4])
        WE = sb("WE", [128, NTO, 8])
        idxf = sb("idxf", [128, NTO])
        idxi = sb("idxi", [128, 2 * NTO], I32)
        goffm = sb("goffm", [128, 4])
        pren = sb("pren", [128, 4])
        M2 = AR.off
        woutG = sb("woutG", [128, 4, D], BF16)
        woutA = sb("woutA", [64, 8, D], BF16)
        DMA("pool", woutG, w_out[0:512, :].rearrange("(c p) n -> p c n", p=128), [], ["woutG"])
        DMA("pool", woutA, w_out[512:1024, :].rearrange("(h p) n -> p h n", p=64), [], ["woutA"])
        for g in range(4):
            P.op("dve", lambda e, g=g: e.memset(goffm[:, g:g + 1], float(g * CAPG) - OOB), writes=["goffm"])
        P.op("dve", lambda e: e.memset(pren, 0.0), writes=["pren"])
        zx = sb("zx", [128, D], BF16)
        zw = sb("zw", [128, 8])
        P.op("pool", lambda e: e.memset(zx, 0.0), writes=["zx"])
        P.op("pool", lambda e: e.memset(zw, 0.0), writes=["zw"])
        for r0 in range(0, NSLOT, 128):
            DMA("sp", XB[r0:r0 + 128, :], zx, ["zx"], ["XB"])
            DMA("sp", WB[r0:r0 + 128, :], zw, ["zw"], ["WB"])
        xo = [sb("xo%d" % i, [128, D]) for i in range(2)]
        mgl = [sb("mgl%d" % i, [128, 512], BF16) for i in range(2)]
        otl = [sb("otl%d" % i, [64, 8, 128], BF16) for i in range(2)]
        mgT = sb("mgT", [128, 4, 128], BF16)
        h2t = [sb("h2t%d" % i, [128, D]) for i in range(2)]
        u2 = sb("u2", [128, D])
        u2Tf = sb("u2Tf", [128, 8, 128])
        junk2 = sb("junk2", [128, D], BF16)
        ss2 = sb("ss2", [128, 2])
        rs2 = sb("rs2", [128, 2])
        lg = sb("lg", [128, 36])
        sm = sb("sm", [128, 64])
        for io in range(NTO):
            b2 = io % 2
            DMA("sp", xo[b2], xw[HALO + io * 128: HALO + (io + 1) * 128, :], [], ["xo%d" % b2])
            DMA("sp", mgl[b2], MG[io * 128:(io + 1) * 128, :], ["MG%d" % io], ["mgl%d" % b2])
            DMA("sp", otl[b2], OTS[:, :, io * 128:(io + 1) * 128].rearrange("h p t -> p h t"), OTK, ["otl%d" % b2])
            pbt, pbk = next_pb()

            def trm_fn(e, pbt=pbt, b2=b2):
                ins = None
                for c in range(4):
                    ins = e.transpose(out=pbt[:, c * 128:(c + 1) * 128], in_=mgl[b2][:, c * 128:(c + 1) * 128], identity=identb)
                return ins
            P.op("pe", trm_fn, reads=["mgl%d" % b2, "identb"], writes=[pbk])
            CP("act", mgT.rearrange("p c t -> p (c t)"), pbt[:, 0:512], [pbk], ["mgT"])
            for cg in range(2):
                pt, pk = next_pf()
                pairs = [(mgT[:, c, :], woutG[:, c, cg * 512:(cg + 1) * 512]) for c in range(4)] + \
                        [(otl[b2][:, h, :], woutA[:, h, cg * 512:(cg + 1) * 512]) for h in range(8)]
                mm_group(pt[:, :], pairs, pk, ["mgT", "otl%d" % b2, "woutG", "woutA"])
                TT("dve", h2t[b2][:, cg * 512:(cg + 1) * 512], pt[:, :], xo[b2][:, cg * 512:(cg + 1) * 512], ALU.add,
                   [pk, "xo%d" % b2], ["h2t%d" % b2])
            DMA("sp", H2[io * 128:(io + 1) * 128, :], h2t[b2], ["h2t%d" % b2], ["H2_%d" % io])
            P.op("act", lambda e, b2=b2: e.activation(out=junk2, in_=h2t[b2], func=AF.Square, accum_out=ss2[:, 0:1]),
                 reads=["h2t%d" % b2], writes=["junk2", "ss2"])
            rstd_from_ssq(rs2[:, 0:1], ss2[:, 0:1], D, "ss2", "rs2")
            STT(u2, h2t[b2], rs2[:, 0:1], n2bc, ALU.mult, ALU.mult, ["h2t%d" % b2, "rs2", "n2bc"], ["u2"])
            CP("pool", u2tok[:, io, :], u2, ["u2"], ["u2tok%d" % io])
            for half in range(2):
                pt, pk = next_pf()

                def tru_fn(e, pt=pt, half=half):
                    ins = None
                    for c in range(4):
                        cc = half * 4 + c
                        ins = e.transpose(out=pt[:, c * 128:(c + 1) * 128], in_=u2[:, cc * 128:(cc + 1) * 128], identity=cm[:, 0, :])
                    return ins
                P.op("pe", tru_fn, reads=["u2", "cm"], writes=[pk])
                CP("act", u2Tf[:, half * 4:half * 4 + 4, :].rearrange("p c t -> p (c t)"), pt[:, :], [pk], ["u2Tf%d" % half])
            pr_, prk = next_pf()
            mm_group(pr_[:, 0:36], [(u2Tf[:, c, :], wr[:, c, :]) for c in range(8)], prk, ["u2Tf0", "u2Tf1", "wr"])
            TT("dve", lg, pr_[:, 0:36], rbbc, ALU.add, [prk, "rbbc"], ["lg"])
            gmax, ngmax, gsum, gw = sm[:, 0:1], sm[:, 1:2], sm[:, 2:3], sm[:, 3:4]
            oh = OH[:, io, :]
            ohk = "OH%d" % io
            ge = sm[:, 8:12]
            esel = sm[:, 16:24]
            top8 = sm[:, 24:32]
            d21, w1g, w2g = sm[:, 32:33], sm[:, 33:34], sm[:, 34:35]
            wa = sm[:, 40:48]
            wb_ = sm[:, 48:56]
            P.op("dve", lambda e: e.tensor_reduce(out=gmax, in_=lg[:, 0:4], axis=AX.X, op=ALU.max), reads=["lg"], writes=["sm"])
            TS("dve", oh, lg[:, 0:4], gmax, None, ALU.is_equal, None, ["lg", "sm"], [ohk])
            TS("dve", ngmax, gmax, -1.0, None, ALU.mult, None, ["sm"], ["sm"])
            ACT(ge, lg[:, 0:4], AF.Exp, ["lg", "sm"], ["sm"], bias=ngmax, scale=1.0)
            P.op("dve", lambda e: e.tensor_reduce(out=gsum, in_=ge, axis=AX.X, op=ALU.add), reads=["sm"], writes=["sm"])
            P.op("dve", lambda e: e.reciprocal(out=gw, in_=gsum), reads=["sm"], writes=["sm"])
            TS("dve", esel, lg[:, 4:12], oh[:, 0:1], None, ALU.mult, None, ["lg", ohk], ["sm"])
            for g in range(1, 4):
                STT(esel, lg[:, 4 + 8 * g:12 + 8 * g], oh[:, g:g + 1], esel, ALU.mult, ALU.add, ["lg", ohk, "sm"], ["sm"])
            P.op("dve", lambda e: e.max(out=top8, in_=esel), reads=["sm"], writes=["sm"])
            TT("dve", d21, top8[:, 1:2], top8[:, 0:1], ALU.subtract, ["sm"], ["sm"])
            ACT(d21, d21, AF.Exp, ["sm"], ["sm"])
            TS("dve", d21, d21, 1.0, None, ALU.add, None, ["sm"], ["sm"])
            P.op("dve", lambda e: e.reciprocal(out=w1g, in_=d21), reads=["sm"], writes=["sm"])
            TT("dve", w1g, w1g, gw, ALU.mult, ["sm"], ["sm"])
            TT("dve", w2g, gw, w1g, ALU.subtract, ["sm"], ["sm"])
            TS("dve", wa, esel, top8[:, 0:1], w1g, ALU.is_equal, ALU.mult, ["sm"], ["sm"])
            TS("dve", wb_, esel, top8[:, 1:2], w2g, ALU.is_equal, ALU.mult, ["sm"], ["sm"])
            TT("dve", WE[:, io, :], wa, wb_, ALU.add, ["sm"], ["WE%d" % io])
            prk_t, prkk = next_pf()
            P.op("pe", lambda e, t=prk_t, io=io: (e.matmul(t[:, 0:4], lhsT=cm[:, 4, :], rhs=OH[:, io, :], start=True, stop=False),
                                                   e.matmul(t[:, 0:4], lhsT=cm[:, 7, :], rhs=pren, start=False, stop=True))[1],
                 reads=[ohk, "pren", "cm"], writes=[prkk])
            rk = sm[:, 56:60]
            okm = sm[:, 60:64]
            TS("dve", rk, prk_t[:, 0:4], -16.0, None, ALU.mult, None, [prkk], ["sm"])
            STT(pren, oh, -1.0 / 16.0, pren, ALU.mult, ALU.add, [ohk, "pren", prkk], ["pren"])
            TS("dve", okm, rk, float(CAPG), None, ALU.is_lt, None, ["sm"], ["sm"])
            TT("dve", okm, okm, oh, ALU.mult, ["sm", ohk], ["sm"])
            TT("dve", rk, rk, goffm, ALU.add, ["sm", "goffm"], ["sm"])
            TT("dve", rk, rk, okm, ALU.mult, ["sm"], ["sm"])
            P.op("dve", lambda e, io=io: e.tensor_reduce(out=idxf[:, io:io + 1], in_=rk, axis=AX.X, op=ALU.add), reads=["sm"], writes=["idxf%d" % io])
            TS("dve", idxf[:, io:io + 1], idxf[:, io:io + 1], OOB, None, ALU.add, None, ["idxf%d" % io], ["idxf%d" % io])
            CP("dve", idxi[:, io:io + 1], idxf[:, io:io + 1], ["idxf%d" % io], ["idxi%d" % io])
            P.op("pool", lambda e, io=io: e.indirect_dma_start(out=XB[:, :], out_offset=bass.IndirectOffsetOnAxis(ap=idxi[:, io:io + 1], axis=0),
                                                               in_=u2tok[:, io, :], in_offset=None, bounds_check=bcreg(e), oob_is_err=False),
                 reads=["u2tok%d" % io, "idxi%d" % io, "XB"], writes=["XBs%d" % io], dma=True)
            P.op("pool", lambda e, io=io: e.indirect_dma_start(out=WB[:, :], out_offset=bass.IndirectOffsetOnAxis(ap=idxi[:, io:io + 1], axis=0),
                                                               in_=WE[:, io, :], in_offset=None, bounds_check=bcreg(e), oob_is_err=False),
                 reads=["WE%d" % io, "idxi%d" % io, "WB"], writes=["WBs%d" % io], dma=True)
        H2K = ["H2_%d" % i for i in range(NTO)]
        XBK = ["XBs%d" % i for i in range(NTO)] + ["XB"]
        WBK = ["WBs%d" % i for i in range(NTO)] + ["WB"]
        if debug:
            DMA("sp", WTD[:, 0:NTO], idxf, ["idxf%d" % i for i in range(NTO)], ["WTD"])
        if stop_after == "C1":
            P.op("sp", None, reads=H2K + XBK + WBK + ["WTD"], writes=[])
            P.emit()
            return nc

        P.barrier()
        AR.off = M2
        NCH = CAPG // 128
        xs = sb("xs", [128, NCH, D], BF16)
        xTg = sb("xTg", [128, 8, CAPG], BF16)
        wsl = sb("wsl", [128, NCH, 8])
        hid = sb("hid", [128, 4, CAPG], BF16)
        yacc = sb("yacc", [128, NCH, D])
        wgb = [sb("wgb%d" % i, [128, 8, 512], BF16) for i in range(2)]
        wub = [sb("wub%d" % i, [128, 8, 512], BF16) for i in range(2)]
        wdb = [sb("wdb%d" % i, [128, 4, D], BF16) for i in range(2)]
        sgb = [sb("sgb%d" % i, [128, 512]) for i in range(2)]

        def load_expert(ex):
            b = ex % 2
            DMA("pool", wgb[b], ewg[ex].rearrange("(c p) n -> p c n", p=128), [], ["wgb%d" % b])
            DMA("pool", wub[b], ewu[ex].rearrange("(c p) n -> p c n", p=128), [], ["wub%d" % b])
            DMA("pool", wdb[b], ewd[ex].rearrange("(c p) n -> p c n", p=128), [], ["wdb%d" % b])
        load_expert(0)
        load_expert(1)
        hid2 = [hid, sb("hidB", [128, 4, CAPG], BF16)]
        kk2 = [0]
        nsl = [(0, 512), (512, CAPG)]

        def GU(ex, hb, XTK):
            b = ex % 2
            for (n0, n1) in nsl:
                for fc in range(4):
                    pg, pgk = next_pf()
                    pu, puk = next_pf()
                    mm_group(pg[:, 0:n1 - n0], [(wgb[b][:, c, fc * 128:(fc + 1) * 128], xTg[:, c, n0:n1]) for c in range(8)], pgk,
                             ["wgb%d" % b] + XTK)
                    mm_group(pu[:, 0:n1 - n0], [(wub[b][:, c, fc * 128:(fc + 1) * 128], xTg[:, c, n0:n1]) for c in range(8)], puk,
                             ["wub%d" % b] + XTK)
                    sb_i = kk2[0] % 2
                    kk2[0] += 1
                    ACT(sgb[sb_i][:, 0:n1 - n0], pg[:, 0:n1 - n0], AF.Silu, [pgk], ["sgb%d" % sb_i])
                    TT("dve", hid2[hb][:, fc, n0:n1], sgb[sb_i][:, 0:n1 - n0], pu[:, 0:n1 - n0], ALU.mult, ["sgb%d" % sb_i, puk],
                       ["hid%d_%d_%d" % (hb, fc, n0)])

        def DN(ex, hb, el):
            b = ex % 2
            HK = ["hid%d_%d_%d" % (hb, fc, n0) for fc in range(4) for (n0, _) in nsl]
            for ch in range(NCH):
                for cg in range(2):
                    py, pyk = next_pf()
                    mm_group(py[:, :], [(hid2[hb][:, fc, ch * 128:(ch + 1) * 128], wdb[b][:, fc, cg * 512:(cg + 1) * 512]) for fc in range(4)], pyk,
                             ["wdb%d" % b] + HK)
                    ya = yacc[:, ch, cg * 512:(cg + 1) * 512]
                    yk = "yacc%d" % ch
                    if el == 0:
                        TS("dve", ya, py[:, :], wsl[:, ch, el:el + 1], None, ALU.mult, None, [pyk, "wsl"], [yk])
                    else:
                        STT(ya, py[:, :], wsl[:, ch, el:el + 1], ya, ALU.mult, ALU.add, [pyk, "wsl", yk], [yk])

        for g in range(4):
            DMA("sp", xs, XB[g * CAPG:(g + 1) * CAPG, :].rearrange("(c p) d -> p c d", p=128), XBK, ["xs"])
            DMA("sp", wsl, WB[g * CAPG:(g + 1) * CAPG, :].rearrange("(c p) d -> p c d", p=128), WBK, ["wsl"])
            for ch in range(NCH):
                pbt, pbk = next_pb()

                def trx_fn(e, pbt=pbt, ch=ch):
                    ins = None
                    for c in range(8):
                        ins = e.transpose(out=pbt[:, c * 128:(c + 1) * 128], in_=xs[:, ch, c * 128:(c + 1) * 128], identity=identb)
                    return ins
                P.op("pe", trx_fn, reads=["xs", "identb"], writes=[pbk])
                CP("act" if ch % 2 else "dve", xTg[:, :, ch * 128:(ch + 1) * 128], pbt[:, :].rearrange("p (c t) -> p c t", c=8), [pbk], ["xTg%d" % ch])
            XTK = ["xTg%d" % ch for ch in range(NCH)]
            GU(g * 8, 0, XTK)
            for el in range(8):
                ex = g * 8 + el
                if ex + 2 < NEXP:
                    b_ = ex % 2
                    DMA("pool", wgb[b_], ewg[ex + 2].rearrange("(c p) n -> p c n", p=128), [], ["wgb%d" % b_])
                    DMA("pool", wub[b_], ewu[ex + 2].rearrange("(c p) n -> p c n", p=128), [], ["wub%d" % b_])
                ra, rb = [], []
                if el + 1 < 8:
                    P.rec = ra
                    stream[0] = 0
                    GU(ex + 1, (el + 1) % 2, XTK)
                P.rec = rb
                stream[0] = 1
                DN(ex, el % 2, el)
                P.rec = None
                stream[0] = None
                P.replay_merged(ra, rb)
                if ex + 2 < NEXP:
                    DMA("pool", wdb[ex % 2], ewd[ex + 2].rearrange("(c p) n -> p c n", p=128), [], ["wdb%d" % (ex % 2)])
            DMA("sp", YB[g * CAPG:(g + 1) * CAPG, :].rearrange("(c p) d -> p c d", p=128), yacc, ["yacc%d" % ch for ch in range(NCH)], ["YB%d" % g])
        YBK = ["YB%d" % g for g in range(4)]
        P.barrier()
        AR.off = M2
        hl = [sb("hl%d" % i, [128, D]) for i in range(2)]
        yg = [sb("yg%d" % i, [128, D]) for i in range(2)]
        ob = [sb("ob%d" % i, [128, D]) for i in range(2)]
        junk3 = sb("junk3", [128, D], BF16)
        ss3 = sb("ss3", [128, 2])
        rs3 = sb("rs3", [128, 2])
        for io in range(NTO):
            b2 = io % 2
            DMA("sp", hl[b2], H2[io * 128:(io + 1) * 128, :], ["H2_%d" % io], ["hl%d" % b2])
            P.op("pool", lambda e, b2=b2: e.memset(yg[b2], 0.0), writes=["yg%d" % b2])
            P.op("pool", lambda e, io=io, b2=b2: e.indirect_dma_start(out=yg[b2], out_offset=None, in_=YB[:, :],
                                                                       in_offset=bass.IndirectOffsetOnAxis(ap=idxi[:, io:io + 1], axis=0),
                                                                       bounds_check=bcreg(e), oob_is_err=False),
                 reads=YBK + ["idxi%d" % io], writes=["yg%d" % b2], dma=True)
            TT("dve", hl[b2], hl[b2], yg[b2], ALU.add, ["hl%d" % b2, "yg%d" % b2], ["hl%d" % b2])
            P.op("act", lambda e, b2=b2: e.activation(out=junk3, in_=hl[b2], func=AF.Square, accum_out=ss3[:, 0:1]),
                 reads=["hl%d" % b2], writes=["junk3", "ss3"])
            rstd_from_ssq(rs3[:, 0:1], ss3[:, 0:1], D, "ss3", "rs3")
            STT(ob[b2], hl[b2], rs3[:, 0:1], fnbc, ALU.mult, ALU.mult, ["hl%d" % b2, "rs3", "fnbc"], ["ob%d" % b2])
            DMA("sp", out_d[io * 128:(io + 1) * 128, :], ob[b2], ["ob%d" % b2], ["OUT%d" % io])
        P.op("sp", None, reads=["OUT%d" % i for i in range(NTO)], writes=[])
        P.emit()
    return nc


def _consts():
    s = np.arange(128)[:, None]
    t = np.arange(128)[None, :]
    cm = np.zeros((128, 8, 128), np.float32)
    cm[:, 0] = (s == t)
    cm[:, 1] = (s <= t) / -16.0
    cm[:, 2] = (s >= t) / -16.0
    cm[:, 3] = (s > t) / -16.0
    cm[:, 4] = (s < t) / -16.0
    cm[:, 5] = (s <= t)
    cm[:, 6] = (s >= t)
    cm[:, 7] = 1.0
    band = np.zeros((128, 384), np.float32)
    band[:, 0:128] = (s >= t + 64)
    band[:, 128:256] = (np.abs(s - t) <= 64)
    band[:, 256:384] = (s <= t - 64)
    return cm, band


def make_in_maps(inputs):
    f = lambda a: np.ascontiguousarray(np.asarray(a, dtype=np.float32))
    x = f(inputs["x"])
    cm, band = _consts()
    wz = np.zeros((33, 512), np.float32)
    wz[0:16, 0:256] = f(inputs["gla_fwd_gate_w"])[0]
    wz[16:32, 256:512] = f(inputs["gla_bwd_gate_w"])[0]
    wz[32, 0:256] = f(inputs["gla_fwd_gate_b"])[0]
    wz[32, 256:512] = f(inputs["gla_bwd_gate_b"])[0]
    vecs = np.zeros((4, D), np.float32)
    vecs[0] = f(inputs["norm1_w"])[0]
    vecs[1] = f(inputs["norm2_w"])[0]
    vecs[2] = f(inputs["final_norm_w"])
    vecs[3] = np.tile(f(inputs["gla_norm_w"])[0], 8)
    wr = np.concatenate([f(inputs["router_group_w"])[0]] + [f(inputs["router_expert_w"])[0, g] for g in range(4)], axis=1)
    rb = np.concatenate([f(inputs["router_group_b"])[0], f(inputs["router_expert_b"])[0].reshape(-1)])[None, :]
    inv = (500000.0 ** (-(np.arange(0, 16, 2, dtype=np.float32) / np.float32(16)))).astype(np.float32)
    shared = dict(cmat=cm, band3=band, w_in=f(inputs["w_in"])[0], wz=wz, vecs=vecs, w_out=f(inputs["w_out"])[0],
                  wr=np.ascontiguousarray(wr), rb=np.ascontiguousarray(rb), ewg=f(inputs["expert_w_gate"])[0],
                  ewu=f(inputs["expert_w_up"])[0], ewd=f(inputs["expert_w_down"])[0])
    maps = []
    for c in range(8):
        b, q = c // 4, c % 4
        s0 = q * OWN
        pos = np.arange(s0 - HALO, s0 + OWN + HALO)
        valid = (pos >= 0) & (pos < S)
        xwin = np.zeros((WIN, D), np.float32)
        xwin[valid] = x[b, pos[valid]]
        ang = (pos.astype(np.float32)[:, None] * inv[None, :]).astype(np.float32)
        cs = np.concatenate([np.cos(ang), np.sin(ang)], axis=1).astype(np.float32)
        cs_t = np.ascontiguousarray(cs.reshape(NTW, 128, 16).transpose(1, 0, 2))
        vcol = np.ascontiguousarray(valid.astype(np.float32).reshape(NTW, 128).T)
        m = dict(shared)
        m.update(xw=xwin, vcol=vcol, cs_t=cs_t)
        maps.append(m)
    return maps


_NC_CACHE = {}


def kernel(**inputs):
    maps = make_in_maps(inputs)
    if "nc" not in _NC_CACHE:
        _NC_CACHE["nc"] = build_program()
    nc = _NC_CACHE["nc"]
    res = run_bass_kernel_spmd(nc, maps, core_ids=list(range(8)))
    out = np.zeros((2, S, D), np.float32)
    for c in range(8):
        b, q = c // 4, c % 4
        out[b, q * OWN:(q + 1) * OWN] = res.results[c]["out"]
    return out
```

```python
import numpy as np
from contextlib import ExitStack
import concourse.bass as bass
import concourse.mybir as mybir
from concourse.bass_utils import run_bass_kernel_spmd

F32 = mybir.dt.float32
BF16 = mybir.dt.bfloat16
I32 = mybir.dt.int32
AF = mybir.ActivationFunctionType
ALU = mybir.AluOpType
AX = mybir.AxisListType

ENGS = ("pe", "act", "dve", "pool", "sp")
EPOCH = 4096
DMA_SLOTS = 8

D = 1024
S = 8192
OWN = 2048
HALO = 1024
WIN = OWN + 2 * HALO
NTW = WIN // 128
T0 = HALO // 128
NTO = OWN // 128
INW = 3104
NEXP = 32
EPS = 1e-6


class Op:
    __slots__ = ("eng", "fn", "dma", "deps", "sig", "sigcount", "dmaidx", "idx")

    def __init__(self, eng, fn, dma):
        self.eng = eng
        self.fn = fn
        self.dma = dma
        self.deps = []
        self.sig = False
        self.sigcount = 0
        self.dmaidx = -1
        self.idx = -1


class Prog:
    def __init__(self, nc):
        self.nc = nc
        self.ops = []
        self.last_w = {}
        self.readers = {}
        self.ndma = {e: 0 for e in ENGS}
        self.bar = None
        self.rec = None

    def barrier(self):
        deps = set()
        for e in ENGS:
            last = None
            nd = 0
            for o in reversed(self.ops):
                if o.eng != e:
                    continue
                if o.dma:
                    if nd < DMA_SLOTS:
                        deps.add(o.idx)
                        nd += 1
                elif last is None:
                    last = o.idx
                    deps.add(o.idx)
                if last is not None and nd >= DMA_SLOTS:
                    break
        b = self.op("sp", None)
        b.deps = sorted(deps | set(b.deps))
        self.bar = b.idx
        return b

    def replay_merged(self, a, b):
        na, nb = len(a), len(b)
        i = j = 0
        while i < na or j < nb:
            if j >= nb or (i < na and i * nb <= j * na):
                self.op(*a[i])
                i += 1
            else:
                self.op(*b[j])
                j += 1

    def op(self, eng, fn, reads=(), writes=(), dma=False):
        if self.rec is not None:
            self.rec.append((eng, fn, list(reads), list(writes), dma))
            return None
        import os as _os
        mx = int(_os.environ.get("DBG_MAXOPS", "0"))
        if mx and len(self.ops) >= mx and fn is not None:
            fn = None
            if dma:
                dma = False
        px = [k_ for k_ in reads if k_[:2] in ("pf", "pb")]
        if px:
            writes = list(writes) + [k_ for k_ in px if k_ not in writes]
            reads = [k_ for k_ in reads if k_ not in px]
        o = Op(eng, fn, dma)
        o.idx = len(self.ops)
        deps = set()
        if self.bar is not None:
            deps.add(self.bar)
        for k in reads:
            w = self.last_w.get(k)
            if w is not None:
                deps.add(w)
        for k in writes:
            w = self.last_w.get(k)
            if w is not None:
                deps.add(w)
            for r in self.readers.get(k, ()):
                deps.add(r)
        deps.discard(o.idx)
        o.deps = sorted(deps)
        for k in writes:
            self.last_w[k] = o.idx
            self.readers[k] = []
        for k in reads:
            if k not in writes:
                self.readers.setdefault(k, []).append(o.idx)
        if dma:
            o.dmaidx = self.ndma[eng]
            self.ndma[eng] += 1
        self.ops.append(o)
        return o

    def emit(self):
        nc = self.nc
        ops = self.ops
        for o in ops:
            for d in o.deps:
                p = ops[d]
                if not p.dma:
                    p.sig = True
        cnt = {e: 0 for e in ENGS}
        for o in ops:
            if o.sig and not o.dma:
                cnt[o.eng] += 1
                o.sigcount = cnt[o.eng]
        nsem = {e: (cnt[e] + EPOCH - 1) // EPOCH for e in ENGS}
        with ExitStack() as es:
            csem = {e: [es.enter_context(nc.semaphore("c_%s_%d" % (e, i))) for i in range(nsem[e])]
                    for e in ENGS}
            dsem = {e: [es.enter_context(nc.semaphore("d_%s_%d" % (e, i)))
                        for i in range(DMA_SLOTS if self.ndma[e] else 0)] for e in ENGS}
            block = es.enter_context(nc.Block())

            def body_for(e):
                def body(eng):
                    waited_c = {x: 0 for x in ENGS}
                    waited_d = {}
                    for o in ops:
                        if o.eng != e:
                            continue
                        need_c = {}
                        need_d = {}
                        for d in o.deps:
                            p = ops[d]
                            if p.dma:
                                slot = p.dmaidx % DMA_SLOTS
                                val = 16 * (p.dmaidx // DMA_SLOTS + 1)
                                key = (p.eng, slot)
                                if waited_d.get(key, 0) < val:
                                    need_d[key] = max(need_d.get(key, 0), val)
                            else:
                                if waited_c[p.eng] < p.sigcount:
                                    need_c[p.eng] = max(need_c.get(p.eng, 0), p.sigcount)
                        if o.dma:
                            slot = o.dmaidx % DMA_SLOTS
                            val = 16 * (o.dmaidx // DMA_SLOTS)
                            key = (e, slot)
                            if val > 0 and waited_d.get(key, 0) < val:
                                need_d[key] = max(need_d.get(key, 0), val)
                        for pe_, c in need_c.items():
                            ep = (c - 1) // EPOCH
                            eng.wait_ge(csem[pe_][ep], (c - 1) % EPOCH + 1)
                            waited_c[pe_] = c
                        for key, val in need_d.items():
                            eng.wait_ge(dsem[key[0]][key[1]], val)
                            waited_d[key] = val
                        ins = o.fn(eng) if o.fn is not None else None
                        if o.dma:
                            ins.then_inc(dsem[e][o.dmaidx % DMA_SLOTS], 16)
                        elif o.sig:
                            if ins is None:
                                ins = eng.nop()
                            ep = (o.sigcount - 1) // EPOCH
                            ins.then_inc(csem[e][ep], 1)
                return body

            block.tensor(body_for("pe"))
            block.scalar(body_for("act"))
            block.vector(body_for("dve"))
            block.gpsimd(body_for("pool"))
            block.sync(body_for("sp"))


class Arena:
    def __init__(self, ap, ncols):
        self.ap = ap
        self.n = ncols
        self.off = 0

    def alloc(self, shape, dt=F32):
        p = shape[0]
        rest = list(shape[1:])
        nel = 1
        for r in rest:
            nel *= r
        ncol = nel if dt in (F32, I32) else (nel + 1) // 2
        ncol += ncol % 2
        assert self.off + ncol <= self.n, "arena overflow: need %d have %d" % (ncol, self.n - self.off)
        v = self.ap[0:p, self.off:self.off + ncol]
        self.off += ncol
        if dt != F32:
            v = v.bitcast(dt)
        if v.shape[1] != nel:
            v = v[:, 0:nel]
        if len(rest) == 2:
            v = v.rearrange("p (a b) -> p a b", a=rest[0])
        elif len(rest) == 3:
            v = v.rearrange("p (a b c) -> p a b c", a=rest[0], b=rest[1])
        return v


def build_program(debug=False, stop_after=None, dbg_tiles=None):
    nc = bass.Bass("TRN2", target_bir_lowering=False)
    P = Prog(nc)
    global LASTP
    LASTP = P

    def din(name, shape, dt=F32):
        return nc.dram_tensor(name, list(shape), dt, kind="ExternalInput").ap()

    def dscr(name, shape, dt):
        kind = "ExternalOutput" if debug else "Internal"
        return nc.dram_tensor(name, list(shape), dt, kind=kind).ap()

    xw = din("xw", [WIN, D])
    vcol = din("vcol", [128, NTW])
    cs_t = din("cs_t", [128, NTW, 16])
    cmat = din("cmat", [128, 8, 128])
    band3 = din("band3", [128, 384])
    w_in = din("w_in", [D, INW])
    wz_d = din("wz", [33, 512])
    vecs = din("vecs", [4, D])
    w_out = din("w_out", [D, D])
    wr_d = din("wr", [D, 36])
    rb_d = din("rb", [1, 36])
    ewg = din("ewg", [NEXP, D, 512])
    ewu = din("ewu", [NEXP, D, 512])
    ewd = din("ewd", [NEXP, 512, D])
    out_d = nc.dram_tensor("out", [OWN, D], F32, kind="ExternalOutput").ap()
    QS = dscr("QS", [OWN, 512], BF16)
    KS = dscr("KS", [WIN + 2 * HALO, 512], BF16)
    VS = dscr("VS", [WIN + 2 * HALO, 520], BF16)
    GV = dscr("GV", [OWN, 512], BF16)
    GG = dscr("GG", [OWN, 512], BF16)
    MG = dscr("MG", [OWN, 512], BF16)
    OTS = dscr("OTS", [8, 64, OWN], BF16)
    H2 = dscr("H2", [OWN, D], F32)
    XB = dscr("XB", [2560, D], BF16)
    WB = dscr("WB", [2560, 8], F32)
    YB = dscr("YB", [2560, D], F32)
    WTD = nc.dram_tensor("WTD", [128, NTO * 32], F32, kind="ExternalOutput").ap() if debug else None

    QSK = ["QS%d" % i for i in range(NTO)]
    KSK = ["KS%d" % i for i in range(48)]
    VSK = ["VS%d" % i for i in range(48)]
    NCOL = 50 * 1024 + 512
    es = ExitStack()
    with es:
        arena_t = es.enter_context(nc.sbuf_tensor("arena", [128, NCOL], F32))
        AR = Arena(arena_t[:], NCOL)
        sb = lambda name, shape, dt=F32: AR.alloc(shape, dt)

        def ps(name, shape, dt=F32):
            return es.enter_context(nc.psum_tensor("p_" + name, list(shape), dt))

        pf = [ps("pf%d" % i, [128, 512]) for i in range(6)]
        pb = [ps("pb%d" % i, [128, 1024], BF16) for i in range(2)]
        pf_rr = [0]
        pb_rr = [0]

        stream = [None]
        srr = [0, 0]

        def next_pf():
            if stream[0] is None:
                i = pf_rr[0] % 6
                pf_rr[0] += 1
            else:
                s_ = stream[0]
                i = 3 * s_ + srr[s_] % 3
                srr[s_] += 1
            return pf[i], "pf%d" % i

        def next_pb():
            if stream[0] is None:
                i = pb_rr[0] % 2
                pb_rr[0] += 1
            else:
                i = stream[0]
            return pb[i], "pb%d" % i

        def mm_group(out_ap, pairs, okey, rkeys):
            def fn(e):
                ins = None
                n = len(pairs)
                for j, (l, r) in enumerate(pairs):
                    ins = e.matmul(out_ap, lhsT=l, rhs=r, start=(j == 0), stop=(j == n - 1))
                return ins
            P.op("pe", fn, reads=rkeys, writes=[okey])

        def ACT(out, in_, func, reads, writes, **kw):
            P.op("act", lambda e: e.activation(out=out, in_=in_, func=func, **kw), reads=reads, writes=writes)

        def TT(eng, out, in0, in1, op, reads, writes):
            P.op(eng, lambda e: e.tensor_tensor(out=out, in0=in0, in1=in1, op=op), reads=reads, writes=writes)

        def STT(out, in0, scalar, in1, op0, op1, reads, writes):
            P.op("dve", lambda e: e.scalar_tensor_tensor(out=out, in0=in0, scalar=scalar, in1=in1, op0=op0, op1=op1),
                 reads=reads, writes=writes)

        def TS(eng, out, in0, s1, s2, op0, op1, reads, writes):
            if op1 is None:
                P.op(eng, lambda e: e.tensor_scalar(out=out, in0=in0, scalar1=s1, scalar2=None, op0=op0), reads=reads, writes=writes)
            else:
                P.op(eng, lambda e: e.tensor_scalar(out=out, in0=in0, scalar1=s1, scalar2=s2, op0=op0, op1=op1), reads=reads, writes=writes)

        def CP(eng, out, in_, reads, writes):
            if eng == "act":
                ACT(out, in_, AF.Copy, reads, writes)
            else:
                P.op(eng, lambda e: e.tensor_copy(out=out, in_=in_), reads=reads, writes=writes)

        def DMA(q, out, in_, reads, writes):
            return P.op(q, lambda e: e.dma_start(out=out, in_=in_), reads=reads, writes=writes, dma=True)

        def rstd_from_ssq(dst, src, n, rk, wk):
            ACT(dst, src, AF.Ln, [rk, "epsc"], [wk], scale=1.0 / n, bias=epsc[0:dst.shape[0], :])
            ACT(dst, dst, AF.Exp, [wk], [wk], scale=-0.5)

        cm = sb("cm", [128, 8, 128])
        identb = sb("identb", [128, 128], BF16)
        band = sb("band", [128, 384], BF16)
        maskFB = sb("maskFB", [128, 4, 128])
        n16col = sb("n16col", [128, 2])
        epsc = sb("epsc", [128, 2])
        onec = sb("onec", [128, 2])
        negc = sb("negc", [128, 2])
        vc = sb("vc", [128, NTW])
        cst = sb("cst", [128, NTW, 16])
        wz = sb("wz", [33, 512])
        n1bc = sb("n1bc", [128, D])
        n2bc = sb("n2bc", [128, D])
        fnbc = sb("fnbc", [128, D])
        gnbc = sb("gnbc", [128, 512])
        rbbc = sb("rbbc", [128, 36])
        wr = sb("wr", [128, 8, 36])
        nmax = sb("nmax", [128, 16])
        n16col = n16col[:, 0:1]
        epsc = epsc[:, 0:1]
        onec = onec[:, 0:1]
        negc = negc[:, 0:1]

        DMA("sp", cm, cmat, [], ["cm"])
        DMA("pool", identb, cmat[:, 0, :], [], ["identb"])
        DMA("pool", band, band3, [], ["band"])
        DMA("sp", vc, vcol, [], ["vc"])
        DMA("sp", cst, cs_t, [], ["cst"])
        DMA("sp", wz, wz_d, [], ["wz"])
        DMA("sp", n1bc, vecs[0:1, :].partition_broadcast(128), [], ["n1bc"])
        DMA("sp", n2bc, vecs[1:2, :].partition_broadcast(128), [], ["n2bc"])
        DMA("sp", fnbc, vecs[2:3, :].partition_broadcast(128), [], ["fnbc"])
        DMA("sp", gnbc, vecs[3:4, 0:512].partition_broadcast(128), [], ["gnbc"])
        DMA("sp", rbbc, rb_d[0:1, :].partition_broadcast(128), [], ["rbbc"])
        DMA("sp", wr, wr_d.rearrange("(c p) n -> p c n", p=128), [], ["wr"])
        P.op("dve", lambda e: e.memset(n16col, -1.0 / 16.0), writes=["n16col"])
        P.op("dve", lambda e: e.memset(epsc, EPS), writes=["epsc"])
        P.op("dve", lambda e: e.memset(onec, 1.0), writes=["onec"])
        P.op("dve", lambda e: e.memset(nmax, 0.0), writes=["nmax"])
        for h in range(4):
            CP("dve", maskFB[:, h, :], cm[:, 5 + h // 2, :], ["cm"], ["maskFB"])
        M0 = AR.off

        attnT = sb("attnT", [128, NTO, 512], BF16)
        qdT = sb("qdT", [128, NTO, 4, 128], BF16)
        SfT = sb("SfT", [128, NTO, 2, 128], BF16)
        SbT = sb("SbT", [128, NTO, 2, 128], BF16)
        M1 = AR.off
        win = sb("win", [128, 8, INW], BF16)
        for c in range(8):
            DMA("pool", win[:, c, :], w_in[c * 128:(c + 1) * 128, :], [], ["win%d" % c])
        winkeys = ["win%d" % c for c in range(8)]
        kvB = sb("kvB", [128, NTW - T0, 2, 128], BF16)
        decB = sb("decB", [128, NTW - T0, 2])
        Sf = sb("Sf", [128, 2, 128])
        Sb = sb("Sb", [128, 2, 128])
        P.op("dve", lambda e: e.memset(Sf, 0.0), writes=["Sf"])
        P.op("dve", lambda e: e.memset(Sb, 0.0), writes=["Sb"])
        zt = sb("zt", [128, 520], BF16)
        P.op("pool", lambda e: e.memset(zt, 0.0), writes=["zt"])
        for blk in range(HALO // 128):
            for base in (0, HALO + WIN):
                r0 = base + blk * 128
                DMA("sp", KS[r0:r0 + 128, :], zt[:, 0:512], ["zt"], ["KS%d" % (r0 // 128)])
                DMA("sp", VS[r0:r0 + 128, :], zt, ["zt"], ["VS%d" % (r0 // 128)])
        xt = [sb("xt%d" % i, [128, D]) for i in range(2)]
        junk = sb("junk", [128, D], BF16)
        xn = [sb("xn%d" % i, [128, D], BF16) for i in range(2)]
        xnT = [sb("xnT%d" % i, [128, 8, 128], BF16) for i in range(2)]
        ssq = sb("ssq", [128, 2])
        rstd = sb("rstd", [128, 2])
        qk = [sb("qk%d" % i, [128, 512]) for i in range(2)]
        vbf = [sb("vbf%d" % i, [128, 512], BF16) for i in range(2)]
        gbf = [sb("gbf%d" % i, [128, 512], BF16) for i in range(2)]
        lr = [sb("lr%d" % i, [128, 32]) for i in range(2)]
        aqr = [sb("aqr%d" % i, [128, 8, 64]) for i in range(2)]
        akr = [sb("akr%d" % i, [128, 8, 64]) for i in range(2)]
        vab = [sb("vab%d" % i, [128, 8, 65], BF16) for i in range(2)]
        lrT = sb("lrT", [33, 128])
        ez = sb("ez", [128, 512])
        spl = sb("spl", [128, 512])
        E1 = sb("E1", [128, 512])
        E2 = sb("E2", [128, 512])
        E3 = sb("E3", [128, 512])
        dec = sb("dec", [128, 4])
        qd = sb("qd", [128, 512], BF16)
        ki = sb("ki", [128, 512], BF16)
        ke = sb("ke", [128, 512], BF16)
        kiT = sb("kiT", [128, 4, 128], BF16)
        qrb = [sb("qrb%d" % i, [128, 512], BF16) for i in range(2)]
        krb = [sb("krb%d" % i, [128, 512], BF16) for i in range(2)]
        rta = sb("rta", [128, 8, 8])
        rtb = sb("rtb", [128, 8, 8])
        rtc = sb("rtc", [128, 8, 8])
        rtd = sb("rtd", [128, 8, 8])
        sqs = sb("sqs", [128, 8, 64])
        nrm = sb("nrm", [128, 16])
        P.op("dve", lambda e: e.memset(lrT[32:33, :], 1.0), writes=["lrT_one"])
        tiles = list(range(NTW) if dbg_tiles is None else dbg_tiles)

        def S1(i):
            own = T0 <= i < T0 + NTO
            io = i - T0
            b2 = i % 2
            xtk, xnk, xnTk = "xt%d" % b2, "xn%d" % b2, "xnT%d" % b2
            if i == tiles[0]:
                DMA("sp", xt[b2], xw[i * 128:(i + 1) * 128, :], [], [xtk])
            if i + 1 < NTW and (dbg_tiles is None):
                DMA("sp", xt[(i + 1) % 2], xw[(i + 1) * 128:(i + 2) * 128, :], [], ["xt%d" % ((i + 1) % 2)])
            sk, rk = "ssq%d" % b2, "rstd%d" % b2
            P.op("act", lambda e, b2=b2: e.activation(out=junk, in_=xt[b2], func=AF.Square, accum_out=ssq[:, b2:b2 + 1]),
                 reads=[xtk], writes=["junk", sk])
            rstd_from_ssq(rstd[:, b2:b2 + 1], ssq[:, b2:b2 + 1], D, sk, rk)
            STT(xn[b2], xt[b2], rstd[:, b2:b2 + 1], n1bc, ALU.mult, ALU.mult, [xtk, rk, "n1bc"], [xnk])
            pbt, pbk = next_pb()

            def tr_fn(e, b2=b2, pbt=pbt):
                ins = None
                for c in range(8):
                    ins = e.transpose(out=pbt[:, c * 128:(c + 1) * 128], in_=xn[b2][:, c * 128:(c + 1) * 128], identity=identb)
                return ins
            P.op("pe", tr_fn, reads=[xnk, "identb"], writes=[pbk])
            CP("act", xnT[b2].rearrange("p c t -> p (c t)"), pbt[:, :], [pbk], [xnTk])

            def proj(c0, c1):
                pt, pk = next_pf()
                n = c1 - c0
                mm_group(pt[:, 0:n], [(xnT[b2][:, c, :], win[:, c, c0:c1]) for c in range(8)], pk, [xnTk] + winkeys)
                return pt, pk

            if own:
                pt, pk = proj(0, 512)
                CP("act", qk[b2], pt[:, 0:512], [pk], ["qk%d" % b2])
            else:
                pt, pk = proj(256, 512)
                CP("act", qk[b2][:, 256:512], pt[:, 0:256], [pk], ["qk%d" % b2])
            pt, pk = proj(512, 1024)
            CP("dve", vbf[b2], pt[:, 0:512], [pk], ["vbf%d" % b2])
            if own:
                DMA("sp", GV[io * 128:(io + 1) * 128, :], vbf[b2], ["vbf%d" % b2], ["GV%d" % io])
                pt, pk = proj(1024, 1536)
                CP("act", gbf[b2], pt[:, 0:512], [pk], ["gbf%d" % b2])
                DMA("sp", GG[io * 128:(io + 1) * 128, :], gbf[b2], ["gbf%d" % b2], ["GG%d" % io])
            pt, pk = proj(1536, 1568)
            CP("dve", lr[b2], pt[:, 0:32], [pk], ["lr%d" % b2])
            if own:
                pt, pk = proj(1568, 2080)
                CP("dve", aqr[b2].rearrange("p h d -> p (h d)"), pt[:, 0:512], [pk], ["aqr%d" % b2])
            pt, pk = proj(2080, 2592)
            CP("act", akr[b2].rearrange("p h d -> p (h d)"), pt[:, 0:512], [pk], ["akr%d" % b2])
            pt, pk = proj(2592, 3104)
            r0k = HALO + i * 128
            CP("act", vab[b2][:, :, 0:64], pt[:, 0:512].rearrange("p (h d) -> p h d", h=8), [pk], ["vab%d" % b2])
            CP("dve", vab[b2][:, :, 64:65], vc[:, i:i + 1].unsqueeze(1).broadcast_to([128, 8, 1]), ["vc"], ["vab%d" % b2])
            DMA("sp", VS[r0k:r0k + 128, :], vab[b2].rearrange("p h d -> p (h d)"), ["vab%d" % b2], ["VS%d" % (r0k // 128)])

        def S2(i):
            own = T0 <= i < T0 + NTO
            left = i < T0
            io = i - T0
            b2 = i % 2
            qkb = qk[b2]
            qkk = "qk%d" % b2
            vkey = "vbf%d" % b2
            vt = vbf[b2]
            pt, pk = next_pf()
            P.op("pe", lambda e, pt=pt: e.transpose(out=pt[0:32, 0:128], in_=lr[b2], identity=cm[:, 0, :]), reads=["lr%d" % b2, "cm"], writes=[pk])
            CP("dve", lrT[0:32, :], pt[0:32, 0:128], [pk], ["lrT"])
            pz, pzk = next_pf()
            P.op("pe", lambda e, pz=pz: e.matmul(pz[:, :], lhsT=lrT, rhs=wz, start=True, stop=True),
                 reads=["lrT", "lrT_one", "wz"], writes=[pzk])
            ACT(ez, pz[:, :], AF.Exp, [pzk], ["ez"], scale=-1.0)
            ACT(spl, ez, AF.Ln, ["ez", "onec"], ["spl"], bias=onec, scale=1.0)
            pbb, pbbk = next_pf()
            P.op("pe", lambda e, pbb=pbb: (e.matmul(pbb[:, 0:256], lhsT=cm[:, 1, :], rhs=spl[:, 0:256], start=True, stop=True),
                                           e.matmul(pbb[:, 256:512], lhsT=cm[:, 2, :], rhs=spl[:, 256:512], start=True, stop=True))[1],
                 reads=["cm", "spl"], writes=[pbbk])
            ACT(E1, pbb[:, :], AF.Exp, [pbbk], ["E1"])
            ACT(E2, pbb[:, :], AF.Exp, [pbbk], ["E2"], scale=-1.0)
            pb3, pb3k = next_pf()
            P.op("pe", lambda e, pb3=pb3: (e.matmul(pb3[:, 0:256], lhsT=cm[:, 3, :], rhs=spl[:, 0:256], start=True, stop=True),
                                           e.matmul(pb3[:, 256:512], lhsT=cm[:, 4, :], rhs=spl[:, 256:512], start=True, stop=True))[1],
                 reads=["cm", "spl"], writes=[pb3k])
            ACT(E3, pb3[:, :], AF.Exp, [pb3k], ["E3"])
            pdc, pdck = next_pf()

            def dec_fn(e, pdc=pdc):
                ins = None
                for j in range(4):
                    ins = e.matmul(pdc[:, j:j + 1], lhsT=spl[:, j * 128:(j + 1) * 128], rhs=n16col, start=True, stop=True)
                return ins
            P.op("pe", dec_fn, reads=["spl", "n16col"], writes=[pdck])
            ACT(dec, pdc[:, 0:4], AF.Exp, [pdck], ["dec"])
            if own:
                STT(qd[:, 0:256], qkb[:, 0:256], 0.125, E1[:, 0:256], ALU.mult, ALU.mult, [qkk, "E1"], ["qd"])
                STT(qd[:, 256:512], qkb[:, 0:256], 0.125, E1[:, 256:512], ALU.mult, ALU.mult, [qkk, "E1"], ["qd"])
                TT("pool", ki[:, 0:256], qkb[:, 256:512], E2[:, 0:256], ALU.mult, [qkk, "E2"], ["ki"])
                TT("pool", ki[:, 256:512], qkb[:, 256:512], E2[:, 256:512], ALU.mult, [qkk, "E2"], ["ki"])
            TT("pool", ke[:, 0:256], qkb[:, 256:512], E3[:, 0:256], ALU.mult, [qkk, "E3"], ["ke"])
            TT("pool", ke[:, 256:512], qkb[:, 256:512], E3[:, 256:512], ALU.mult, [qkk, "E3"], ["ke"])
            if own:
                pbt, pbk = next_pb()

                def tr2_fn(e, pbt=pbt):
                    ins = None
                    for j in range(4):
                        ins = e.transpose(out=pbt[:, j * 128:(j + 1) * 128], in_=qd[:, j * 128:(j + 1) * 128], identity=identb)
                    for j in range(4):
                        ins = e.transpose(out=pbt[:, 512 + j * 128:512 + (j + 1) * 128], in_=ki[:, j * 128:(j + 1) * 128], identity=identb)
                    return ins
                P.op("pe", tr2_fn, reads=["qd", "ki", "identb"], writes=[pbk])
                CP("act", qdT[:, io, :, :].rearrange("p c t -> p (c t)"), pbt[:, 0:512], [pbk], ["qdT%d" % io])
                CP("dve", kiT.rearrange("p c t -> p (c t)"), pbt[:, 512:1024], [pbk], ["kiT"])
                paX, paXk = next_pf()
                paY, paYk = next_pf()

                def att_fn(e, pa, par, io=io):
                    ins = None
                    p0 = par * 64
                    for dirn in range(2):
                        for pr in range(2):
                            blk = dirn * 2 + pr
                            sl = dirn * 2 + pr
                            ins = e.matmul(pa[:, sl * 128:(sl + 1) * 128], lhsT=kiT[p0:p0 + 64, blk, :], rhs=qdT[p0:p0 + 64, io, blk, :],
                                           start=True, stop=True)
                    return ins
                P.op("pe", lambda e, pa=paX, f=att_fn: f(e, pa, 0), reads=["kiT", "qdT%d" % io], writes=[paXk])
                P.op("pe", lambda e, pa=paY, f=att_fn: f(e, pa, 1), reads=["kiT", "qdT%d" % io], writes=[paYk])
                TT("dve", ez, paX[:, :], maskFB.rearrange("p h c -> p (h c)"), ALU.mult, [paXk, "maskFB"], ["ez"])
                TT("dve", E1, paY[:, :], maskFB.rearrange("p h c -> p (h c)"), ALU.mult, [paYk, "maskFB"], ["E1"])
                av = attnT[:, io, :].rearrange("p (a b c) -> p a b c", a=2, b=2)
                TT("pool", av[:, :, 0, :], ez[:, 0:256].rearrange("p (a c) -> p a c", a=2), ez[:, 256:512].rearrange("p (a c) -> p a c", a=2),
                   ALU.add, ["ez"], ["attnT%d" % io])
                TT("pool", av[:, :, 1, :], E1[:, 0:256].rearrange("p (a c) -> p a c", a=2), E1[:, 256:512].rearrange("p (a c) -> p a c", a=2),
                   ALU.add, ["E1"], ["attnT%d" % io])
            for dirn in range(2):
                if dirn == 0 and i >= T0 + NTO:
                    continue
                if dirn == 1 and left:
                    continue
                pkv, pkvk = next_pf()

                def kv_fn(e, pkv=pkv, dirn=dirn, vt=vt):
                    ins = None
                    for pr in range(2):
                        ins = e.matmul(pkv[:, pr * 256:(pr + 1) * 256], lhsT=ke[:, dirn * 256 + pr * 128: dirn * 256 + (pr + 1) * 128],
                                       rhs=vt[:, pr * 256:(pr + 1) * 256], start=True, stop=True)
                    return ins
                P.op("pe", kv_fn, reads=["ke", vkey], writes=[pkvk])
                if dirn == 0:
                    if own:
                        CP("act", SfT[:, io, :, :].rearrange("p a b -> p (a b)"), Sf.rearrange("p a b -> p (a b)"), ["Sf"], ["SfT%d" % io])
                    for pr in range(2):
                        for hh in range(2):
                            p0 = hh * 64
                            STT(Sf[p0:p0 + 64, pr, :], Sf[p0:p0 + 64, pr, :], dec[p0:p0 + 64, pr:pr + 1],
                                pkv[p0:p0 + 64, pr * 256 + hh * 128: pr * 256 + (hh + 1) * 128], ALU.mult, ALU.add,
                                ["Sf", "dec", pkvk], ["Sf"])
                else:
                    ib = i - T0
                    for pr in range(2):
                        for hh in range(2):
                            p0 = hh * 64
                            CP("act", kvB[p0:p0 + 64, ib, pr, :], pkv[p0:p0 + 64, pr * 256 + hh * 128: pr * 256 + (hh + 1) * 128],
                               [pkvk], ["kvB%d" % ib])
                    CP("dve", decB[:, ib, :], dec[:, 2:4], ["dec"], ["decB%d" % ib])

            def rope(raw, rkey):
                cosb = cst[:, i, 0:8].unsqueeze(1).broadcast_to([128, 8, 8])
                sinb = cst[:, i, 8:16].unsqueeze(1).broadcast_to([128, 8, 8])
                TT("pool", rta, raw[:, :, 0:8], cosb, ALU.mult, [rkey, "cst"], ["rta"])
                TT("pool", rtb, raw[:, :, 8:16], sinb, ALU.mult, [rkey, "cst"], ["rtb"])
                TT("pool", rtc, raw[:, :, 8:16], cosb, ALU.mult, [rkey, "cst"], ["rtc"])
                TT("pool", rtd, raw[:, :, 0:8], sinb, ALU.mult, [rkey, "cst"], ["rtd"])
                TT("dve", raw[:, :, 0:8], rta, rtb, ALU.subtract, ["rta", "rtb"], [rkey])
                TT("dve", raw[:, :, 8:16], rtc, rtd, ALU.add, ["rtc", "rtd"], [rkey])

            def sqnorm(src, col0, skey):
                TT("pool", sqs, src, src, ALU.mult, [skey], ["sqs"])
                P.op("dve", lambda e: e.tensor_reduce(out=nrm[:, col0:col0 + 8], in_=sqs, axis=AX.X, op=ALU.add), reads=["sqs"], writes=["nrm"])
                TT("dve", nmax[:, col0:col0 + 8], nmax[:, col0:col0 + 8], nrm[:, col0:col0 + 8], ALU.max, ["nrm", "nmax"], ["nmax"])

            r0k = HALO + i * 128
            if own:
                rope(aqr[b2], "aqr%d" % b2)
                sqnorm(aqr[b2], 0, "aqr%d" % b2)
                CP("act", qrb[b2], aqr[b2].rearrange("p h d -> p (h d)"), ["aqr%d" % b2], ["qrb%d" % b2])
                DMA("sp", QS[io * 128:(io + 1) * 128, :], qrb[b2], ["qrb%d" % b2], ["QS%d" % io])
            rope(akr[b2], "akr%d" % b2)
            sqnorm(akr[b2], 8, "akr%d" % b2)
            CP("act", krb[b2], akr[b2].rearrange("p h d -> p (h d)"), ["akr%d" % b2], ["krb%d" % b2])
            DMA("sp", KS[r0k:r0k + 128, :], krb[b2], ["krb%d" % b2], ["KS%d" % (r0k // 128)])

        for n_ in range(len(tiles) + 1):
            ra, rb = [], []
            if n_ < len(tiles):
                P.rec = ra
                stream[0] = 0
                S1(tiles[n_])
            if n_ > 0:
                P.rec = rb
                stream[0] = 1
                S2(tiles[n_ - 1])
            P.rec = None
            stream[0] = None
            P.replay_merged(ra, rb)

        if stop_after == "A":
            P.op("sp", None, reads=[k_ for k_ in P.last_w.keys() if k_[:2] in ("QS", "KS", "VS", "GV", "GG")], writes=[])
            P.emit()
            return nc
        for i in range(NTW - 1, T0 - 1, -1):
            ib = i - T0
            if ib < NTO:
                CP("act", SbT[:, ib, :, :].rearrange("p a b -> p (a b)"), Sb.rearrange("p a b -> p (a b)"), ["Sb"], ["SbT%d" % ib])
            if i == T0:
                break
            for pr in range(2):
                STT(Sb[:, pr, :], Sb[:, pr, :], decB[:, ib, pr:pr + 1], kvB[:, ib, pr, :], ALU.mult, ALU.add,
                    ["Sb", "decB%d" % ib, "kvB%d" % ib], ["Sb"])

        P.barrier()
        AR.off = M1
        vb2 = [sb("vb2%d" % i, [128, 512], BF16) for i in range(2)]
        gb2 = [sb("gb2%d" % i, [128, 512], BF16) for i in range(2)]
        osb = sb("osb", [128, 512])
        osq = sb("osq", [128, 4, 128])
        oms = sb("oms", [128, 4])
        sgs = sb("sgs", [128, 512])
        ybf = sb("ybf", [128, 512])
        mixb = [sb("mixb%d" % i, [128, 512], BF16) for i in range(2)]
        for io in range(NTO):
            b2 = io % 2
            DMA("sp", vb2[b2], GV[io * 128:(io + 1) * 128, :], ["GV%d" % io], ["vb2%d" % b2])
            DMA("sp", gb2[b2], GG[io * 128:(io + 1) * 128, :], ["GG%d" % io], ["gb2%d" % b2])
            poX, poXk = next_pf()
            poY, poYk = next_pf()

            def o_fn(e, po, par, io=io, b2=b2):
                ins = None
                p0 = par * 64
                for pr in range(2):
                    h = pr * 2 + par
                    oap = po[:, pr * 128:(pr + 1) * 128]
                    e.matmul(oap, lhsT=attnT[:, io, h * 128:(h + 1) * 128], rhs=vb2[b2][:, h * 128:(h + 1) * 128], start=True, stop=False)
                    e.matmul(oap, lhsT=qdT[p0:p0 + 64, io, pr, :], rhs=SfT[p0:p0 + 64, io, pr, :], start=False, stop=False)
                    ins = e.matmul(oap, lhsT=qdT[p0:p0 + 64, io, 2 + pr, :], rhs=SbT[p0:p0 + 64, io, pr, :], start=False, stop=True)
                return ins
            rk_ = ["attnT%d" % io, "qdT%d" % io, "SfT%d" % io, "SbT%d" % io, "vb2%d" % b2]
            P.op("pe", lambda e, po=poX, f=o_fn: f(e, po, 0), reads=rk_, writes=[poXk])
            P.op("pe", lambda e, po=poY, f=o_fn: f(e, po, 1), reads=rk_, writes=[poYk])
            ov = osb.rearrange("p (a b c) -> p a b c", a=2, b=2)
            CP("act", ov[:, :, 0, :], poX[:, 0:256].rearrange("p (a c) -> p a c", a=2), [poXk], ["osb"])
            CP("act", ov[:, :, 1, :], poY[:, 0:256].rearrange("p (a c) -> p a c", a=2), [poYk], ["osb"])
            TT("pool", osq.rearrange("p h d -> p (h d)"), osb, osb, ALU.mult, ["osb"], ["osq"])
            P.op("dve", lambda e: e.tensor_reduce(out=oms, in_=osq, axis=AX.X, op=ALU.add), reads=["osq"], writes=["oms"])
            rstd_from_ssq(oms, oms, 128, "oms", "oms")
            ACT(sgs, gb2[b2], AF.Silu, ["gb2%d" % b2], ["sgs"])
            TT("dve", ybf, osb, gnbc, ALU.mult, ["osb", "gnbc"], ["ybf"])
            for h in range(4):
                STT(mixb[b2][:, h * 128:(h + 1) * 128], ybf[:, h * 128:(h + 1) * 128], oms[:, h:h + 1], sgs[:, h * 128:(h + 1) * 128],
                    ALU.mult, ALU.mult, ["ybf", "oms", "sgs"], ["mixb%d" % b2])
            DMA("sp", MG[io * 128:(io + 1) * 128, :], mixb[b2], ["mixb%d" % b2], ["MG%d" % io])
        MGK = ["MG%d" % i for i in range(NTO)]
        if stop_after == "G2":
            P.op("sp", None, reads=QSK + KSK + VSK + MGK, writes=[])
            P.emit()
            return nc

        P.barrier()
        AR.off = M0
        nm2 = sb("nm2", [128, 2])
        m2 = sb("m2", [2, 2])
        m1 = sb("m1", [1, 4])
        P.op("dve", lambda e: e.tensor_reduce(out=nm2, in_=nmax.rearrange("p (a h) -> p a h", a=2), axis=AX.X, op=ALU.max),
             reads=["nmax"], writes=["nm2"])
        pt, pk = next_pf()
        P.op("pe", lambda e, pt=pt: e.transpose(out=pt[0:2, 0:128], in_=nm2, identity=cm[:, 0, :]), reads=["nm2", "cm"], writes=[pk])
        P.op("dve", lambda e, pt=pt: e.tensor_reduce(out=m2[:, 0:1], in_=pt[0:2, 0:128], axis=AX.X, op=ALU.max), reads=[pk], writes=["m2"])
        pt, pk = next_pf()
        P.op("pe", lambda e, pt=pt: e.transpose(out=pt[0:1, 0:2], in_=m2[:, 0:1], identity=cm[0:2, 0, 0:2]), reads=["m2", "cm"], writes=[pk])
        CP("dve", m1[:, 0:2], pt[0:1, 0:2], [pk], ["m1"])
        TT("dve", m1[:, 2:3], m1[:, 0:1], m1[:, 1:2], ALU.mult, ["m1"], ["m1"])
        ACT(m1[:, 3:4], m1[:, 2:3], AF.Ln, ["m1"], ["m1"])
        ACT(m1[:, 3:4], m1[:, 3:4], AF.Exp, ["m1"], ["m1"], scale=0.5)
        TS("dve", m1[:, 3:4], m1[:, 3:4], -0.125, None, ALU.mult, None, ["m1"], ["m1"])
        pt, pk = next_pf()
        P.op("pe", lambda e, pt=pt: e.matmul(pt[:, 0:1], lhsT=cm[0:1, 7, :], rhs=m1[:, 3:4], start=True, stop=True), reads=["m1", "cm"], writes=[pk])
        CP("dve", negc, pt[:, 0:1], [pk], ["negc"])

        accT = sb("accT", [65, 8, OWN])
        NQ = 8
        qsb2 = [[sb("qsb%d" % i, [128, 512], BF16) for i in range(NQ)] for _ in range(2)]
        ksb2 = [[sb("ksb%d" % i, [128, 512], BF16) for i in range(NQ + 2)] for _ in range(2)]
        vsb2 = [[sb("vsb%d" % i, [128, 8, 65], BF16) for i in range(NQ + 2)] for _ in range(2)]
        qT2 = [[sb("qT%d" % i, [128, 4, 128], BF16) for i in range(NQ)] for _ in range(2)]
        kT2 = [[sb("kT%d" % i, [128, 4, 128], BF16) for i in range(NQ + 2)] for _ in range(2)]
        pex = [sb("pex%d" % i, [128, 384], BF16) for i in range(4)]
        pmk = [sb("pmk%d" % i, [128, 384], BF16) for i in range(4)]
        cnt4 = [0, 0]
        jobs = [(1, 0, 0, 8), (1, 0, 8, 8)] + [(4, r, 0, 4) for r in range(4)] + [(16, r, 0, 1) for r in range(16)]
        for jn, (dd, r, j0, nq) in enumerate(jobs):
            js = jn % 2
            qsb, ksb, vsb, qT, kT = qsb2[js], ksb2[js], vsb2[js], qT2[js], kT2[js]
            QSv = QS.rearrange("(n d) c -> d n c", d=dd)
            KSv = KS.rearrange("(n d) c -> d n c", d=dd)
            VSv = VS.rearrange("(n d) c -> d n c", d=dd)
            accv = accT.rearrange("p h (n d) -> p h d n", d=dd)
            for jq in range(nq):
                n0 = 128 * (j0 + jq)
                DMA("sp", qsb[jq], QSv[r, n0:n0 + 128, :], QSK, [("qsb" + str(js) + "_%d") % jq])
            for kk in range(nq + 2):
                n0 = 2048 // dd + 128 * (j0 + kk - 1)
                DMA("sp", ksb[kk], KSv[r, n0:n0 + 128, :], KSK, [("ksb" + str(js) + "_%d") % kk])
                DMA("sp", vsb[kk].rearrange("p h d -> p (h d)"), VSv[r, n0:n0 + 128, :], VSK, [("vsb" + str(js) + "_%d") % kk])
            tl = [(qsb[jq], ("qsb" + str(js) + "_%d") % jq, qT[jq], ("qT" + str(js) + "_%d") % jq) for jq in range(nq)] + \
                 [(ksb[kk], ("ksb" + str(js) + "_%d") % kk, kT[kk], ("kT" + str(js) + "_%d") % kk) for kk in range(nq + 2)]
            for t0 in range(0, len(tl), 2):
                grp = tl[t0:t0 + 2]
                pbt, pbk = next_pb()

                def trq_fn(e, grp=grp, pbt=pbt):
                    ins = None
                    for gi, (src, _, _, _) in enumerate(grp):
                        for c in range(4):
                            ins = e.transpose(out=pbt[:, gi * 512 + c * 128: gi * 512 + (c + 1) * 128], in_=src[:, c * 128:(c + 1) * 128], identity=identb)
                    return ins
                P.op("pe", trq_fn, reads=[g[1] for g in grp] + ["identb"], writes=[pbk])
                for gi, (_, _, dst, dk) in enumerate(grp):
                    CP("act" if gi == 0 else "dve", dst.rearrange("p c t -> p (c t)"), pbt[:, gi * 512:(gi + 1) * 512], [pbk], [dk])
            def it_body(jq, hg, s_, kT=kT, qT=qT, vsb=vsb, js=js, dd=dd, r=r, j0=j0, accv=accv):
                bufs = []
                for h in range(hg * 4, hg * 4 + 4):
                    p0 = (h % 2) * 64
                    blk = h // 2
                    pS, pSk = next_pf()

                    def s_fn(e, pS=pS, jq=jq, p0=p0, blk=blk, kT=kT, qT=qT):
                        ins = None
                        for sl in range(3):
                            ins = e.matmul(pS[:, sl * 128:(sl + 1) * 128], lhsT=kT[jq + sl][p0:p0 + 64, blk, :], rhs=qT[jq][p0:p0 + 64, blk, :],
                                           start=True, stop=True)
                        return ins
                    P.op("pe", s_fn, reads=[("kT" + str(js) + "_%d") % (jq + sl) for sl in range(3)] + [("qT" + str(js) + "_%d") % jq], writes=[pSk])
                    bi = 2 * s_ + cnt4[s_] % 2
                    cnt4[s_] += 1
                    ACT(pex[bi], pS[:, 0:384], AF.Exp, [pSk, "negc"], ["pex%d" % bi], bias=negc, scale=0.125)
                    TT("pool" if (h % 2) else "dve", pmk[bi], pex[bi], band, ALU.mult, ["pex%d" % bi, "band"], ["pmk%d" % bi])
                    bufs.append((bi, h))
                    if len(bufs) == 2:
                        pU, pUk = next_pf()

                        def pv_fn(e, pU=pU, jq=jq, bufs=tuple(bufs), vsb=vsb):
                            ins = None
                            for hi, (b_, h_) in enumerate(bufs):
                                for sl in range(3):
                                    ins = e.matmul(pU[0:65, hi * 128:(hi + 1) * 128], lhsT=vsb[jq + sl][:, h_, :], rhs=pmk[b_][:, sl * 128:(sl + 1) * 128],
                                                   start=(sl == 0), stop=(sl == 2))
                            return ins
                        P.op("pe", pv_fn, reads=[("vsb" + str(js) + "_%d") % (jq + sl) for sl in range(3)] + ["pmk%d" % b_ for (b_, _) in bufs], writes=[pUk])
                        n0 = 128 * (j0 + jq)
                        h0 = bufs[0][1]
                        dst = accv[:, h0:h0 + 2, r, n0:n0 + 128]
                        src = pU[0:65, 0:256].rearrange("p (h t) -> p h t", h=2)
                        if dd == 1:
                            CP("dve", dst, src, [pUk], ["accT"])
                        else:
                            TT("dve", dst, src, dst, ALU.add, [pUk, "accT"], ["accT"])
                        bufs = []
            its = [(jq, hg) for jq in range(nq) for hg in range(2)]
            for m in range(0, len(its), 2):
                ra, rb = [], []
                P.rec = ra
                stream[0] = 0
                it_body(its[m][0], its[m][1], 0)
                if m + 1 < len(its):
                    P.rec = rb
                    stream[0] = 1
                    it_body(its[m + 1][0], its[m + 1][1], 1)
                P.rec = None
                stream[0] = None
                P.replay_merged(ra, rb)
        rz = sb("rz", [64, 512])
        otb = [sb("otb%d" % i, [64, 512], BF16) for i in range(2)]
        k2 = 0
        for h in range(8):
            for g in range(4):
                pz, pzk = next_pf()
                P.op("pe", lambda e, pz=pz, h=h, g=g: e.matmul(pz[0:64, :], lhsT=cm[64:65, 7, 0:64], rhs=accT[64:65, h, g * 512:(g + 1) * 512],
                                                                start=True, stop=True), reads=["accT", "cm"], writes=[pzk])
                P.op("dve", lambda e, pz=pz: e.reciprocal(out=rz, in_=pz[0:64, :]), reads=[pzk], writes=["rz"])
                b2 = k2 % 2
                k2 += 1
                TT("pool", otb[b2], accT[0:64, h, g * 512:(g + 1) * 512], rz, ALU.mult, ["accT", "rz"], ["otb%d" % b2])
                DMA("sp", OTS[h, :, g * 512:(g + 1) * 512], otb[b2], ["otb%d" % b2], ["OTS%d_%d" % (h, g)])
        OTK = ["OTS%d_%d" % (h, g) for h in range(8) for g in range(4)]
        if stop_after == "B":
            P.op("sp", None, reads=OTK + MGK, writes=[])
            P.emit()
            return nc

        P.barrier()
        AR.off = M0
        bc_cache = {}

        def bcreg(e):
            if "r" not in bc_cache:
                bc_cache["r"] = e.to_reg(2559)
            return bc_cache["r"]
        CAPG = 640
        NSLOT = 4 * CAPG
        OOB = 4096.0
        u2tok = sb("u2tok", [128, NTO, D], BF16)
        OH = sb("OH", [128, NTO, 4])
        WE = sb("WE", [128, NTO, 8])
        idxf = sb("idxf", [128, NTO])
        idxi = sb("idxi", [128, 2 * NTO], I32)
        goffm = sb("goffm", [128, 4])
        pren = sb("pren", [128, 4])
        M2 = AR.off
        woutG = sb("woutG", [128, 4, D], BF16)
        woutA = sb("woutA", [64, 8, D], BF16)
        DMA("pool", woutG, w_out[0:512, :].rearrange("(c p) n -> p c n", p=128), [], ["woutG"])
        DMA("pool", woutA, w_out[512:1024, :].rearrange("(h p) n -> p h n", p=64), [], ["woutA"])
        for g in range(4):
            P.op("dve", lambda e, g=g: e.memset(goffm[:, g:g + 1], float(g * CAPG) - OOB), writes=["goffm"])
        P.op("dve", lambda e: e.memset(pren, 0.0), writes=["pren"])
        zx = sb("zx", [128, D], BF16)
        zw = sb("zw", [128, 8])
        P.op("pool", lambda e: e.memset(zx, 0.0), writes=["zx"])
        P.op("pool", lambda e: e.memset(zw, 0.0), writes=["zw"])
        for r0 in range(0, NSLOT, 128):
            DMA("sp", XB[r0:r0 + 128, :], zx, ["zx"], ["XB"])
            DMA("sp", WB[r0:r0 + 128, :], zw, ["zw"], ["WB"])
        xo = [sb("xo%d" % i, [128, D]) for i in range(2)]
        mgl = [sb("mgl%d" % i, [128, 512], BF16) for i in range(2)]
        otl = [sb("otl%d" % i, [64, 8, 128], BF16) for i in range(2)]
        mgT2 = [sb("mgT%d" % i, [128, 4, 128], BF16) for i in range(2)]
        h2t = [sb("h2t%d" % i, [128, D]) for i in range(2)]
        u22 = [sb("u2%d" % i, [128, D]) for i in range(2)]
        u2Tf2 = [sb("u2Tf%d" % i, [128, 8, 128]) for i in range(2)]
        junk22 = [sb("junk2%d" % i, [128, D], BF16) for i in range(2)]
        ss2 = sb("ss2", [128, 2])
        rs2 = sb("rs2", [128, 2])
        lg2 = [sb("lg%d" % i, [128, 36]) for i in range(2)]
        sm2 = [sb("sm%d" % i, [128, 64]) for i in range(2)]
        smr = sb("smr", [128, 16])

        def c1_body(io):
            b2 = io % 2
            mgT, u2, u2Tf, junk2, lg, sm = mgT2[b2], u22[b2], u2Tf2[b2], junk22[b2], lg2[b2], sm2[b2]
            mk, uk, lk, sk_ = "mgT%d" % b2, "u2_%d" % b2, "lg%d" % b2, "sm%d" % b2
            DMA("sp", xo[b2], xw[HALO + io * 128: HALO + (io + 1) * 128, :], [], ["xo%d" % b2])
            DMA("sp", mgl[b2], MG[io * 128:(io + 1) * 128, :], ["MG%d" % io], ["mgl%d" % b2])
            DMA("sp", otl[b2], OTS[:, :, io * 128:(io + 1) * 128].rearrange("h p t -> p h t"), OTK, ["otl%d" % b2])
            pbt, pbk = next_pb()

            def trm_fn(e, pbt=pbt, b2=b2):
                ins = None
                for c in range(4):
                    ins = e.transpose(out=pbt[:, c * 128:(c + 1) * 128], in_=mgl[b2][:, c * 128:(c + 1) * 128], identity=identb)
                return ins
            P.op("pe", trm_fn, reads=["mgl%d" % b2, "identb"], writes=[pbk])
            CP("act", mgT.rearrange("p c t -> p (c t)"), pbt[:, 0:512], [pbk], [mk])
            for cg in range(2):
                pt, pk = next_pf()
                pairs = [(mgT[:, c, :], woutG[:, c, cg * 512:(cg + 1) * 512]) for c in range(4)] + \
                        [(otl[b2][:, h, :], woutA[:, h, cg * 512:(cg + 1) * 512]) for h in range(8)]
                mm_group(pt[:, :], pairs, pk, [mk, "otl%d" % b2, "woutG", "woutA"])
                TT("dve", h2t[b2][:, cg * 512:(cg + 1) * 512], pt[:, :], xo[b2][:, cg * 512:(cg + 1) * 512], ALU.add,
                   [pk, "xo%d" % b2], ["h2t%d" % b2])
            DMA("sp", H2[io * 128:(io + 1) * 128, :], h2t[b2], ["h2t%d" % b2], ["H2_%d" % io])
            P.op("act", lambda e: e.activation(out=junk2, in_=h2t[b2], func=AF.Square, accum_out=ss2[:, b2:b2 + 1]),
                 reads=["h2t%d" % b2], writes=["junk2%d" % b2, "ss2%d" % b2])
            rstd_from_ssq(rs2[:, b2:b2 + 1], ss2[:, b2:b2 + 1], D, "ss2%d" % b2, "rs2%d" % b2)
            STT(u2, h2t[b2], rs2[:, b2:b2 + 1], n2bc, ALU.mult, ALU.mult, ["h2t%d" % b2, "rs2%d" % b2, "n2bc"], [uk])
            CP("pool", u2tok[:, io, :], u2, [uk], ["u2tok%d" % io])
            for half in range(2):
                pt, pk = next_pf()

                def tru_fn(e, pt=pt, half=half):
                    ins = None
                    for c in range(4):
                        cc = half * 4 + c
                        ins = e.transpose(out=pt[:, c * 128:(c + 1) * 128], in_=u2[:, cc * 128:(cc + 1) * 128], identity=cm[:, 0, :])
                    return ins
                P.op("pe", tru_fn, reads=[uk, "cm"], writes=[pk])
                CP("act", u2Tf[:, half * 4:half * 4 + 4, :].rearrange("p c t -> p (c t)"), pt[:, :], [pk], ["u2Tf%d_%d" % (b2, half)])
            pr_, prk = next_pf()
            mm_group(pr_[:, 0:36], [(u2Tf[:, c, :], wr[:, c, :]) for c in range(8)], prk, ["u2Tf%d_0" % b2, "u2Tf%d_1" % b2, "wr"])
            TT("dve", lg, pr_[:, 0:36], rbbc, ALU.add, [prk, "rbbc"], [lk])
            gmax, ngmax, gsum, gw = sm[:, 0:1], sm[:, 1:2], sm[:, 2:3], sm[:, 3:4]
            oh = OH[:, io, :]
            ohk = "OH%d" % io
            ge = sm[:, 8:12]
            esel = sm[:, 16:24]
            top8 = sm[:, 24:32]
            d21, w1g, w2g = sm[:, 32:33], sm[:, 33:34], sm[:, 34:35]
            wa = sm[:, 40:48]
            wb_ = sm[:, 48:56]
            P.op("dve", lambda e: e.tensor_reduce(out=gmax, in_=lg[:, 0:4], axis=AX.X, op=ALU.max), reads=[lk], writes=[sk_])
            TS("dve", oh, lg[:, 0:4], gmax, None, ALU.is_equal, None, [lk, sk_], [ohk])
            TS("dve", ngmax, gmax, -1.0, None, ALU.mult, None, [sk_], [sk_])
            ACT(ge, lg[:, 0:4], AF.Exp, [lk, sk_], [sk_], bias=ngmax, scale=1.0)
            P.op("dve", lambda e: e.tensor_reduce(out=gsum, in_=ge, axis=AX.X, op=ALU.add), reads=[sk_], writes=[sk_])
            P.op("dve", lambda e: e.reciprocal(out=gw, in_=gsum), reads=[sk_], writes=[sk_])
            TS("dve", esel, lg[:, 4:12], oh[:, 0:1], None, ALU.mult, None, [lk, ohk], [sk_])
            for g in range(1, 4):
                STT(esel, lg[:, 4 + 8 * g:12 + 8 * g], oh[:, g:g + 1], esel, ALU.mult, ALU.add, [lk, ohk, sk_], [sk_])
            P.op("dve", lambda e: e.max(out=top8, in_=esel), reads=[sk_], writes=[sk_])
            TT("dve", d21, top8[:, 1:2], top8[:, 0:1], ALU.subtract, [sk_], [sk_])
            ACT(d21, d21, AF.Exp, [sk_], [sk_])
            TS("dve", d21, d21, 1.0, None, ALU.add, None, [sk_], [sk_])
            P.op("dve", lambda e: e.reciprocal(out=w1g, in_=d21), reads=[sk_], writes=[sk_])
            TT("dve", w1g, w1g, gw, ALU.mult, [sk_], [sk_])
            TT("dve", w2g, gw, w1g, ALU.subtract, [sk_], [sk_])
            TS("dve", wa, esel, top8[:, 0:1], w1g, ALU.is_equal, ALU.mult, [sk_], [sk_])
            TS("dve", wb_, esel, top8[:, 1:2], w2g, ALU.is_equal, ALU.mult, [sk_], [sk_])
            TT("dve", WE[:, io, :], wa, wb_, ALU.add, [sk_], ["WE%d" % io])

        for m in range(0, NTO, 2):
            ra, rb = [], []
            P.rec = ra
            stream[0] = 0
            c1_body(m)
            P.rec = rb
            stream[0] = 1
            c1_body(m + 1)
            P.rec = None
            stream[0] = None
            P.replay_merged(ra, rb)

        for io in range(NTO):
            oh = OH[:, io, :]
            ohk = "OH%d" % io
            prk_t, prkk = next_pf()
            P.op("pe", lambda e, t=prk_t, io=io: (e.matmul(t[:, 0:4], lhsT=cm[:, 4, :], rhs=OH[:, io, :], start=True, stop=False),
                                                   e.matmul(t[:, 0:4], lhsT=cm[:, 7, :], rhs=pren, start=False, stop=True))[1],
                 reads=[ohk, "pren", "cm"], writes=[prkk])
            rk = smr[:, 0:4]
            okm = smr[:, 4:8]
            TS("dve", rk, prk_t[:, 0:4], -16.0, None, ALU.mult, None, [prkk], ["smr"])
            STT(pren, oh, -1.0 / 16.0, pren, ALU.mult, ALU.add, [ohk, "pren", prkk], ["pren"])
            TS("dve", okm, rk, float(CAPG), None, ALU.is_lt, None, ["smr"], ["smr"])
            TT("dve", okm, okm, oh, ALU.mult, ["smr", ohk], ["smr"])
            TT("dve", rk, rk, goffm, ALU.add, ["smr", "goffm"], ["smr"])
            TT("dve", rk, rk, okm, ALU.mult, ["smr"], ["smr"])
            P.op("dve", lambda e, io=io, rk=rk: e.tensor_reduce(out=idxf[:, io:io + 1], in_=rk, axis=AX.X, op=ALU.add), reads=["smr"], writes=["idxf%d" % io])
            TS("dve", idxf[:, io:io + 1], idxf[:, io:io + 1], OOB, None, ALU.add, None, ["idxf%d" % io], ["idxf%d" % io])
            CP("dve", idxi[:, io:io + 1], idxf[:, io:io + 1], ["idxf%d" % io], ["idxi%d" % io])
            P.op("pool", lambda e, io=io: e.indirect_dma_start(out=XB[:, :], out_offset=bass.IndirectOffsetOnAxis(ap=idxi[:, io:io + 1], axis=0),
                                                               in_=u2tok[:, io, :], in_offset=None, bounds_check=bcreg(e), oob_is_err=False),
                 reads=["u2tok%d" % io, "idxi%d" % io, "XB"], writes=["XBs%d" % io], dma=True)
            P.op("pool", lambda e, io=io: e.indirect_dma_start(out=WB[:, :], out_offset=bass.IndirectOffsetOnAxis(ap=idxi[:, io:io + 1], axis=0),
                                                               in_=WE[:, io, :], in_offset=None, bounds_check=bcreg(e), oob_is_err=False),
                 reads=["WE%d" % io, "idxi%d" % io, "WB"], writes=["WBs%d" % io], dma=True)
        H2K = ["H2_%d" % i for i in range(NTO)]
        XBK = ["XBs%d" % i for i in range(NTO)] + ["XB"]
        WBK = ["WBs%d" % i for i in range(NTO)] + ["WB"]
        if debug:
            DMA("sp", WTD[:, 0:NTO], idxf, ["idxf%d" % i for i in range(NTO)], ["WTD"])
        if stop_after == "C1":
            P.op("sp", None, reads=H2K + XBK + WBK + ["WTD"], writes=[])
            P.emit()
            return nc

        P.barrier()
        AR.off = M2
        NCH = CAPG // 128
        xs = sb("xs", [128, NCH, D], BF16)
        xTg = sb("xTg", [128, 8, CAPG], BF16)
        wsl = sb("wsl", [128, NCH, 8])
        hid = sb("hid", [128, 4, CAPG], BF16)
        yacc = sb("yacc", [128, NCH, D])
        wgb = [sb("wgb%d" % i, [128, 8, 512], BF16) for i in range(2)]
        wub = [sb("wub%d" % i, [128, 8, 512], BF16) for i in range(2)]
        wdb = [sb("wdb%d" % i, [128, 4, D], BF16) for i in range(2)]
        sgb = [sb("sgb%d" % i, [128, 512]) for i in range(2)]

        def load_expert(ex):
            b = ex % 2
            DMA("pool", wgb[b], ewg[ex].rearrange("(c p) n -> p c n", p=128), [], ["wgb%d" % b])
            DMA("pool", wub[b], ewu[ex].rearrange("(c p) n -> p c n", p=128), [], ["wub%d" % b])
            DMA("pool", wdb[b], ewd[ex].rearrange("(c p) n -> p c n", p=128), [], ["wdb%d" % b])
        load_expert(0)
        load_expert(1)
        hid2 = [hid, sb("hidB", [128, 4, CAPG], BF16)]
        kk2 = [0]
        nsl = [(0, 512), (512, CAPG)]

        def GU(ex, hb, XTK):
            b = ex % 2
            for (n0, n1) in nsl:
                for fc in range(4):
                    pg, pgk = next_pf()
                    pu, puk = next_pf()
                    mm_group(pg[:, 0:n1 - n0], [(wgb[b][:, c, fc * 128:(fc + 1) * 128], xTg[:, c, n0:n1]) for c in range(8)], pgk,
                             ["wgb%d" % b] + XTK)
                    mm_group(pu[:, 0:n1 - n0], [(wub[b][:, c, fc * 128:(fc + 1) * 128], xTg[:, c, n0:n1]) for c in range(8)], puk,
                             ["wub%d" % b] + XTK)
                    sb_i = kk2[0] % 2
                    kk2[0] += 1
                    ACT(sgb[sb_i][:, 0:n1 - n0], pg[:, 0:n1 - n0], AF.Silu, [pgk], ["sgb%d" % sb_i])
                    TT("dve", hid2[hb][:, fc, n0:n1], sgb[sb_i][:, 0:n1 - n0], pu[:, 0:n1 - n0], ALU.mult, ["sgb%d" % sb_i, puk],
                       ["hid%d_%d_%d" % (hb, fc, n0)])

        def DN(ex, hb, el):
            b = ex % 2
            HK = ["hid%d_%d_%d" % (hb, fc, n0) for fc in range(4) for (n0, _) in nsl]
            for ch in range(NCH):
                for cg in range(2):
                    py, pyk = next_pf()
                    mm_group(py[:, :], [(hid2[hb][:, fc, ch * 128:(ch + 1) * 128], wdb[b][:, fc, cg * 512:(cg + 1) * 512]) for fc in range(4)], pyk,
                             ["wdb%d" % b] + HK)
                    ya = yacc[:, ch, cg * 512:(cg + 1) * 512]
                    yk = "yacc%d" % ch
                    if el == 0:
                        TS("dve", ya, py[:, :], wsl[:, ch, el:el + 1], None, ALU.mult, None, [pyk, "wsl"], [yk])
                    else:
                        STT(ya, py[:, :], wsl[:, ch, el:el + 1], ya, ALU.mult, ALU.add, [pyk, "wsl", yk], [yk])

        for g in range(4):
            DMA("sp", xs, XB[g * CAPG:(g + 1) * CAPG, :].rearrange("(c p) d -> p c d", p=128), XBK, ["xs"])
            DMA("sp", wsl, WB[g * CAPG:(g + 1) * CAPG, :].rearrange("(c p) d -> p c d", p=128), WBK, ["wsl"])
            for ch in range(NCH):
                pbt, pbk = next_pb()

                def trx_fn(e, pbt=pbt, ch=ch):
                    ins = None
                    for c in range(8):
                        ins = e.transpose(out=pbt[:, c * 128:(c + 1) * 128], in_=xs[:, ch, c * 128:(c + 1) * 128], identity=identb)
                    return ins
                P.op("pe", trx_fn, reads=["xs", "identb"], writes=[pbk])
                CP("act" if ch % 2 else "dve", xTg[:, :, ch * 128:(ch + 1) * 128], pbt[:, :].rearrange("p (c t) -> p c t", c=8), [pbk], ["xTg%d" % ch])
            XTK = ["xTg%d" % ch for ch in range(NCH)]
            GU(g * 8, 0, XTK)
            for el in range(8):
                ex = g * 8 + el
                if ex + 2 < NEXP:
                    b_ = ex % 2
                    DMA("pool", wgb[b_], ewg[ex + 2].rearrange("(c p) n -> p c n", p=128), [], ["wgb%d" % b_])
                    DMA("pool", wub[b_], ewu[ex + 2].rearrange("(c p) n -> p c n", p=128), [], ["wub%d" % b_])
                ra, rb = [], []
                if el + 1 < 8:
                    P.rec = ra
                    stream[0] = 0
                    GU(ex + 1, (el + 1) % 2, XTK)
                P.rec = rb
                stream[0] = 1
                DN(ex, el % 2, el)
                P.rec = None
                stream[0] = None
                P.replay_merged(ra, rb)
                if ex + 2 < NEXP:
                    DMA("pool", wdb[ex % 2], ewd[ex + 2].rearrange("(c p) n -> p c n", p=128), [], ["wdb%d" % (ex % 2)])
            DMA("sp", YB[g * CAPG:(g + 1) * CAPG, :].rearrange("(c p) d -> p c d", p=128), yacc, ["yacc%d" % ch for ch in range(NCH)], ["YB%d" % g])
        YBK = ["YB%d" % g for g in range(4)]
        P.barrier()
        AR.off = M2
        hl = [sb("hl%d" % i, [128, D]) for i in range(2)]
        yg = [sb("yg%d" % i, [128, D]) for i in range(2)]
        ob = [sb("ob%d" % i, [128, D]) for i in range(2)]
        junk3 = sb("junk3", [128, D], BF16)
        ss3 = sb("ss3", [128, 2])
        rs3 = sb("rs3", [128, 2])
        for io in range(NTO):
            b2 = io % 2
            DMA("sp", hl[b2], H2[io * 128:(io + 1) * 128, :], ["H2_%d" % io], ["hl%d" % b2])
            P.op("pool", lambda e, b2=b2: e.memset(yg[b2], 0.0), writes=["yg%d" % b2])
            P.op("pool", lambda e, io=io, b2=b2: e.indirect_dma_start(out=yg[b2], out_offset=None, in_=YB[:, :],
                                                                       in_offset=bass.IndirectOffsetOnAxis(ap=idxi[:, io:io + 1], axis=0),
                                                                       bounds_check=bcreg(e), oob_is_err=False),
                 reads=YBK + ["idxi%d" % io], writes=["yg%d" % b2], dma=True)
            TT("dve", hl[b2], hl[b2], yg[b2], ALU.add, ["hl%d" % b2, "yg%d" % b2], ["hl%d" % b2])
            P.op("act", lambda e, b2=b2: e.activation(out=junk3, in_=hl[b2], func=AF.Square, accum_out=ss3[:, 0:1]),
                 reads=["hl%d" % b2], writes=["junk3", "ss3"])
            rstd_from_ssq(rs3[:, 0:1], ss3[:, 0:1], D, "ss3", "rs3")
            STT(ob[b2], hl[b2], rs3[:, 0:1], fnbc, ALU.mult, ALU.mult, ["hl%d" % b2, "rs3", "fnbc"], ["ob%d" % b2])
            DMA("sp", out_d[io * 128:(io + 1) * 128, :], ob[b2], ["ob%d" % b2], ["OUT%d" % io])
        P.op("sp", None, reads=["OUT%d" % i for i in range(NTO)], writes=[])
        P.emit()
    return nc


def _consts():
    s = np.arange(128)[:, None]
    t = np.arange(128)[None, :]
    cm = np.zeros((128, 8, 128), np.float32)
    cm[:, 0] = (s == t)
    cm[:, 1] = (s <= t) / -16.0
    cm[:, 2] = (s >= t) / -16.0
    cm[:, 3] = (s > t) / -16.0
    cm[:, 4] = (s < t) / -16.0
    cm[:, 5] = (s <= t)
    cm[:, 6] = (s >= t)
    cm[:, 7] = 1.0
    band = np.zeros((128, 384), np.float32)
    band[:, 0:128] = (s >= t + 64)
    band[:, 128:256] = (np.abs(s - t) <= 64)
    band[:, 256:384] = (s <= t - 64)
    return cm, band


def make_in_maps(inputs):
    f = lambda a: np.ascontiguousarray(np.asarray(a, dtype=np.float32))
    x = f(inputs["x"])
    cm, band = _consts()
    wz = np.zeros((33, 512), np.float32)
    wz[0:16, 0:256] = f(inputs["gla_fwd_gate_w"])[0]
    wz[16:32, 256:512] = f(inputs["gla_bwd_gate_w"])[0]
    wz[32, 0:256] = f(inputs["gla_fwd_gate_b"])[0]
    wz[32, 256:512] = f(inputs["gla_bwd_gate_b"])[0]
    vecs = np.zeros((4, D), np.float32)
    vecs[0] = f(inputs["norm1_w"])[0]
    vecs[1] = f(inputs["norm2_w"])[0]
    vecs[2] = f(inputs["final_norm_w"])
    vecs[3] = np.tile(f(inputs["gla_norm_w"])[0], 8)
    wr = np.concatenate([f(inputs["router_group_w"])[0]] + [f(inputs["router_expert_w"])[0, g] for g in range(4)], axis=1)
    rb = np.concatenate([f(inputs["router_group_b"])[0], f(inputs["router_expert_b"])[0].reshape(-1)])[None, :]
    inv = (500000.0 ** (-(np.arange(0, 16, 2, dtype=np.float32) / np.float32(16)))).astype(np.float32)
    shared = dict(cmat=cm, band3=band, w_in=f(inputs["w_in"])[0], wz=wz, vecs=vecs, w_out=f(inputs["w_out"])[0],
                  wr=np.ascontiguousarray(wr), rb=np.ascontiguousarray(rb), ewg=f(inputs["expert_w_gate"])[0],
                  ewu=f(inputs["expert_w_up"])[0], ewd=f(inputs["expert_w_down"])[0])
    maps = []
    for c in range(8):
        b, q = c // 4, c % 4
        s0 = q * OWN
        pos = np.arange(s0 - HALO, s0 + OWN + HALO)
        valid = (pos >= 0) & (pos < S)
        xwin = np.zeros((WIN, D), np.float32)
        xwin[valid] = x[b, pos[valid]]
        ang = (pos.astype(np.float32)[:, None] * inv[None, :]).astype(np.float32)
        cs = np.concatenate([np.cos(ang), np.sin(ang)], axis=1).astype(np.float32)
        cs_t = np.ascontiguousarray(cs.reshape(NTW, 128, 16).transpose(1, 0, 2))
        vcol = np.ascontiguousarray(valid.astype(np.float32).reshape(NTW, 128).T)
        m = dict(shared)
        m.update(xw=xwin, vcol=vcol, cs_t=cs_t)
        maps.append(m)
    return maps


_NC_CACHE = {}


def kernel(**inputs):
    maps = make_in_maps(inputs)
    if "nc" not in _NC_CACHE:
        _NC_CACHE["nc"] = build_program()
    nc = _NC_CACHE["nc"]
    res = run_bass_kernel_spmd(nc, maps, core_ids=list(range(8)))
    out = np.zeros((2, S, D), np.float32)
    for c in range(8):
        b, q = c // 4, c % 4
        out[b, q * OWN:(q + 1) * OWN] = res.results[c]["out"]
    return out
```

```python
import numpy as np
from contextlib import ExitStack
import concourse.bass as bass
import concourse.mybir as mybir
from concourse.bass_utils import run_bass_kernel_spmd

F32 = mybir.dt.float32
BF16 = mybir.dt.bfloat16
I32 = mybir.dt.int32
AF = mybir.ActivationFunctionType
ALU = mybir.AluOpType
AX = mybir.AxisListType

ENGS = ("pe", "act", "dve", "pool", "sp")
EPOCH = 4096
DMA_SLOTS = 8

D = 1024
S = 8192
OWN = 2048
HALO = 1024
WIN = OWN + 2 * HALO
NTW = WIN // 128
T0 = HALO // 128
NTO = OWN // 128
INW = 3104
NEXP = 32
EPS = 1e-6


class Op:
    __slots__ = ("eng", "fn", "dma", "deps", "sig", "sigcount", "dmaidx", "idx")

    def __init__(self, eng, fn, dma):
        self.eng = eng
        self.fn = fn
        self.dma = dma
        self.deps = []
        self.sig = False
        self.sigcount = 0
        self.dmaidx = -1
        self.idx = -1


class Prog:
    def __init__(self, nc):
        self.nc = nc
        self.ops = []
        self.last_w = {}
        self.readers = {}
        self.ndma = {e: 0 for e in ENGS}
        self.bar = None
        self.rec = None

    def barrier(self):
        deps = set()
        for e in ENGS:
            last = None
            nd = 0
            for o in reversed(self.ops):
                if o.eng != e:
                    continue
                if o.dma:
                    if nd < DMA_SLOTS:
                        deps.add(o.idx)
                        nd += 1
                elif last is None:
                    last = o.idx
                    deps.add(o.idx)
                if last is not None and nd >= DMA_SLOTS:
                    break
        b = self.op("sp", None)
        b.deps = sorted(deps | set(b.deps))
        self.bar = b.idx
        return b

    def replay_merged(self, a, b):
        na, nb = len(a), len(b)
        i = j = 0
        while i < na or j < nb:
            if j >= nb or (i < na and i * nb <= j * na):
                self.op(*a[i])
                i += 1
            else:
                self.op(*b[j])
                j += 1

    def op(self, eng, fn, reads=(), writes=(), dma=False):
        if self.rec is not None:
            self.rec.append((eng, fn, list(reads), list(writes), dma))
            return None
        import os as _os
        mx = int(_os.environ.get("DBG_MAXOPS", "0"))
        if mx and len(self.ops) >= mx and fn is not None:
            fn = None
            if dma:
                dma = False
        px = [k_ for k_ in reads if k_[:2] in ("pf", "pb")]
        if px:
            writes = list(writes) + [k_ for k_ in px if k_ not in writes]
            reads = [k_ for k_ in reads if k_ not in px]
        o = Op(eng, fn, dma)
        o.idx = len(self.ops)
        deps = set()
        if self.bar is not None:
            deps.add(self.bar)
        for k in reads:
            w = self.last_w.get(k)
            if w is not None:
                deps.add(w)
        for k in writes:
            w = self.last_w.get(k)
            if w is not None:
                deps.add(w)
            for r in self.readers.get(k, ()):
                deps.add(r)
        deps.discard(o.idx)
        o.deps = sorted(deps)
        for k in writes:
            self.last_w[k] = o.idx
            self.readers[k] = []
        for k in reads:
            if k not in writes:
                self.readers.setdefault(k, []).append(o.idx)
        if dma:
            o.dmaidx = self.ndma[eng]
            self.ndma[eng] += 1
        self.ops.append(o)
        return o

    def emit(self):
        nc = self.nc
        ops = self.ops
        for o in ops:
            for d in o.deps:
                p = ops[d]
                if not p.dma:
                    p.sig = True
        cnt = {e: 0 for e in ENGS}
        for o in ops:
            if o.sig and not o.dma:
                cnt[o.eng] += 1
                o.sigcount = cnt[o.eng]
        nsem = {e: (cnt[e] + EPOCH - 1) // EPOCH for e in ENGS}
        with ExitStack() as es:
            csem = {e: [es.enter_context(nc.semaphore("c_%s_%d" % (e, i))) for i in range(nsem[e])]
                    for e in ENGS}
            dsem = {e: [es.enter_context(nc.semaphore("d_%s_%d" % (e, i)))
                        for i in range(DMA_SLOTS if self.ndma[e] else 0)] for e in ENGS}
            block = es.enter_context(nc.Block())

            def body_for(e):
                def body(eng):
                    waited_c = {x: 0 for x in ENGS}
                    waited_d = {}
                    for o in ops:
                        if o.eng != e:
                            continue
                        need_c = {}
                        need_d = {}
                        for d in o.deps:
                            p = ops[d]
                            if p.dma:
                                slot = p.dmaidx % DMA_SLOTS
                                val = 16 * (p.dmaidx // DMA_SLOTS + 1)
                                key = (p.eng, slot)
                                if waited_d.get(key, 0) < val:
                                    need_d[key] = max(need_d.get(key, 0), val)
                            else:
                                if waited_c[p.eng] < p.sigcount:
                                    need_c[p.eng] = max(need_c.get(p.eng, 0), p.sigcount)
                        if o.dma:
                            slot = o.dmaidx % DMA_SLOTS
                            val = 16 * (o.dmaidx // DMA_SLOTS)
                            key = (e, slot)
                            if val > 0 and waited_d.get(key, 0) < val:
                                need_d[key] = max(need_d.get(key, 0), val)
                        for pe_, c in need_c.items():
                            ep = (c - 1) // EPOCH
                            eng.wait_ge(csem[pe_][ep], (c - 1) % EPOCH + 1)
                            waited_c[pe_] = c
                        for key, val in need_d.items():
                            eng.wait_ge(dsem[key[0]][key[1]], val)
                            waited_d[key] = val
                        ins = o.fn(eng) if o.fn is not None else None
                        if o.dma:
                            ins.then_inc(dsem[e][o.dmaidx % DMA_SLOTS], 16)
                        elif o.sig:
                            if ins is None:
                                ins = eng.nop()
                            ep = (o.sigcount - 1) // EPOCH
                            ins.then_inc(csem[e][ep], 1)
                return body

            block.tensor(body_for("pe"))
            block.scalar(body_for("act"))
            block.vector(body_for("dve"))
            block.gpsimd(body_for("pool"))
            block.sync(body_for("sp"))


class Arena:
    def __init__(self, ap, ncols):
        self.ap = ap
        self.n = ncols
        self.off = 0

    def alloc(self, shape, dt=F32):
        p = shape[0]
        rest = list(shape[1:])
        nel = 1
        for r in rest:
            nel *= r
        ncol = nel if dt in (F32, I32) else (nel + 1) // 2
        ncol += ncol % 2
        assert self.off + ncol <= self.n, "arena overflow: need %d have %d" % (ncol, self.n - self.off)
        v = self.ap[0:p, self.off:self.off + ncol]
        self.off += ncol
        if dt != F32:
            v = v.bitcast(dt)
        if v.shape[1] != nel:
            v = v[:, 0:nel]
        if len(rest) == 2:
            v = v.rearrange("p (a b) -> p a b", a=rest[0])
        elif len(rest) == 3:
            v = v.rearrange("p (a b c) -> p a b c", a=rest[0], b=rest[1])
        return v


def build_program(debug=False, stop_after=None, dbg_tiles=None):
    nc = bass.Bass("TRN2", target_bir_lowering=False)
    P = Prog(nc)
    global LASTP
    LASTP = P

    def din(name, shape, dt=F32):
        return nc.dram_tensor(name, list(shape), dt, kind="ExternalInput").ap()

    def dscr(name, shape, dt):
        kind = "ExternalOutput" if debug else "Internal"
        return nc.dram_tensor(name, list(shape), dt, kind=kind).ap()

    xw = din("xw", [WIN, D])
    vcol = din("vcol", [128, NTW])
    cs_t = din("cs_t", [128, NTW, 16])
    cmat = din("cmat", [128, 8, 128])
    band3 = din("band3", [128, 384])
    w_in = din("w_in", [D, INW])
    wz_d = din("wz", [33, 512])
    vecs = din("vecs", [4, D])
    w_out = din("w_out", [D, D])
    wr_d = din("wr", [D, 36])
    rb_d = din("rb", [1, 36])
    ewg = din("ewg", [NEXP, D, 512])
    ewu = din("ewu", [NEXP, D, 512])
    ewd = din("ewd", [NEXP, 512, D])
    out_d = nc.dram_tensor("out", [OWN, D], F32, kind="ExternalOutput").ap()
    QS = dscr("QS", [OWN, 512], BF16)
    KS = dscr("KS", [WIN + 2 * HALO, 512], BF16)
    VS = dscr("VS", [WIN + 2 * HALO, 520], BF16)
    GV = dscr("GV", [OWN, 512], BF16)
    GG = dscr("GG", [OWN, 512], BF16)
    MG = dscr("MG", [OWN, 512], BF16)
    OTS = dscr("OTS", [8, 64, OWN], BF16)
    H2 = dscr("H2", [OWN, D], F32)
    XB = dscr("XB", [2560, D], BF16)
    WB = dscr("WB", [2560, 8], F32)
    YB = dscr("YB", [2560, D], F32)
    WTD = nc.dram_tensor("WTD", [128, NTO * 32], F32, kind="ExternalOutput").ap() if debug else None

    QSK = ["QS%d" % i for i in range(NTO)]
    KSK = ["KS%d" % i for i in range(48)]
    VSK = ["VS%d" % i for i in range(48)]
    NCOL = 50 * 1024 + 512
    es = ExitStack()
    with es:
        arena_t = es.enter_context(nc.sbuf_tensor("arena", [128, NCOL], F32))
        AR = Arena(arena_t[:], NCOL)
        sb = lambda name, shape, dt=F32: AR.alloc(shape, dt)

        def ps(name, shape, dt=F32):
            return es.enter_context(nc.psum_tensor("p_" + name, list(shape), dt))

        pf = [ps("pf%d" % i, [128, 512]) for i in range(6)]
        pb = [ps("pb%d" % i, [128, 1024], BF16) for i in range(2)]
        pf_rr = [0]
        pb_rr = [0]

        stream = [None]
        srr = [0, 0]

        def next_pf():
            if stream[0] is None:
                i = pf_rr[0] % 6
                pf_rr[0] += 1
            else:
                s_ = stream[0]
                i = 3 * s_ + srr[s_] % 3
                srr[s_] += 1
            return pf[i], "pf%d" % i

        def next_pb():
            if stream[0] is None:
                i = pb_rr[0] % 2
                pb_rr[0] += 1
            else:
                i = stream[0]
            return pb[i], "pb%d" % i

        def mm_group(out_ap, pairs, okey, rkeys):
            def fn(e):
                ins = None
                n = len(pairs)
                for j, (l, r) in enumerate(pairs):
                    ins = e.matmul(out_ap, lhsT=l, rhs=r, start=(j == 0), stop=(j == n - 1))
                return ins
            P.op("pe", fn, reads=rkeys, writes=[okey])

        def ACT(out, in_, func, reads, writes, **kw):
            P.op("act", lambda e: e.activation(out=out, in_=in_, func=func, **kw), reads=reads, writes=writes)

        def TT(eng, out, in0, in1, op, reads, writes):
            P.op(eng, lambda e: e.tensor_tensor(out=out, in0=in0, in1=in1, op=op), reads=reads, writes=writes)

        def STT(out, in0, scalar, in1, op0, op1, reads, writes):
            P.op("dve", lambda e: e.scalar_tensor_tensor(out=out, in0=in0, scalar=scalar, in1=in1, op0=op0, op1=op1),
                 reads=reads, writes=writes)

        def TS(eng, out, in0, s1, s2, op0, op1, reads, writes):
            if op1 is None:
                P.op(eng, lambda e: e.tensor_scalar(out=out, in0=in0, scalar1=s1, scalar2=None, op0=op0), reads=reads, writes=writes)
            else:
                P.op(eng, lambda e: e.tensor_scalar(out=out, in0=in0, scalar1=s1, scalar2=s2, op0=op0, op1=op1), reads=reads, writes=writes)

        def CP(eng, out, in_, reads, writes):
            if eng == "act":
                ACT(out, in_, AF.Copy, reads, writes)
            else:
                P.op(eng, lambda e: e.tensor_copy(out=out, in_=in_), reads=reads, writes=writes)

        def DMA(q, out, in_, reads, writes):
            return P.op(q, lambda e: e.dma_start(out=out, in_=in_), reads=reads, writes=writes, dma=True)

        def rstd_from_ssq(dst, src, n, rk, wk):
            ACT(dst, src, AF.Ln, [rk, "epsc"], [wk], scale=1.0 / n, bias=epsc[0:dst.shape[0], :])
            ACT(dst, dst, AF.Exp, [wk], [wk], scale=-0.5)

        cm = sb("cm", [128, 8, 128])
        identb = sb("identb", [128, 128], BF16)
        band = sb("band", [128, 384], BF16)
        maskFB = sb("maskFB", [128, 4, 128])
        n16col = sb("n16col", [128, 2])
        epsc = sb("epsc", [128, 2])
        onec = sb("onec", [128, 2])
        negc = sb("negc", [128, 2])
        vc = sb("vc", [128, NTW])
        cst = sb("cst", [128, NTW, 16])
        wz = sb("wz", [33, 512])
        n1bc = sb("n1bc", [128, D])
        n2bc = sb("n2bc", [128, D])
        fnbc = sb("fnbc", [128, D])
        gnbc = sb("gnbc", [128, 512])
        rbbc = sb("rbbc", [128, 36])
        wr = sb("wr", [128, 8, 36])
        nmax = sb("nmax", [128, 16])
        n16col = n16col[:, 0:1]
        epsc = epsc[:, 0:1]
        onec = onec[:, 0:1]
        negc = negc[:, 0:1]

        DMA("sp", cm, cmat, [], ["cm"])
        DMA("pool", identb, cmat[:, 0, :], [], ["identb"])
        DMA("pool", band, band3, [], ["band"])
        DMA("sp", vc, vcol, [], ["vc"])
        DMA("sp", cst, cs_t, [], ["cst"])
        DMA("sp", wz, wz_d, [], ["wz"])
        DMA("sp", n1bc, vecs[0:1, :].partition_broadcast(128), [], ["n1bc"])
        DMA("sp", n2bc, vecs[1:2, :].partition_broadcast(128), [], ["n2bc"])
        DMA("sp", fnbc, vecs[2:3, :].partition_broadcast(128), [], ["fnbc"])
        DMA("sp", gnbc, vecs[3:4, 0:512].partition_broadcast(128), [], ["gnbc"])
        DMA("sp", rbbc, rb_d[0:1, :].partition_broadcast(128), [], ["rbbc"])
        DMA("sp", wr, wr_d.rearrange("(c p) n -> p c n", p=128), [], ["wr"])
        P.op("dve", lambda e: e.memset(n16col, -1.0 / 16.0), writes=["n16col"])
        P.op("dve", lambda e: e.memset(epsc, EPS), writes=["epsc"])
        P.op("dve", lambda e: e.memset(onec, 1.0), writes=["onec"])
        P.op("dve", lambda e: e.memset(nmax, 0.0), writes=["nmax"])
        for h in range(4):
            CP("dve", maskFB[:, h, :], cm[:, 5 + h // 2, :], ["cm"], ["maskFB"])
        M0 = AR.off

        attnT = sb("attnT", [128, NTO, 512], BF16)
        qdT = sb("qdT", [128, NTO, 4, 128], BF16)
        SfT = sb("SfT", [128, NTO, 2, 128], BF16)
        SbT = sb("SbT", [128, NTO, 2, 128], BF16)
        M1 = AR.off
        win = sb("win", [128, 8, INW], BF16)
        for c in range(8):
            DMA("pool", win[:, c, :], w_in[c * 128:(c + 1) * 128, :], [], ["win%d" % c])
        winkeys = ["win%d" % c for c in range(8)]
        kvB = sb("kvB", [128, NTW - T0, 2, 128], BF16)
        decB = sb("decB", [128, NTW - T0, 2])
        Sf = sb("Sf", [128, 2, 128])
        Sb = sb("Sb", [128, 2, 128])
        P.op("dve", lambda e: e.memset(Sf, 0.0), writes=["Sf"])
        P.op("dve", lambda e: e.memset(Sb, 0.0), writes=["Sb"])
        zt = sb("zt", [128, 520], BF16)
        P.op("pool", lambda e: e.memset(zt, 0.0), writes=["zt"])
        for blk in range(HALO // 128):
            for base in (0, HALO + WIN):
                r0 = base + blk * 128
                DMA("sp", KS[r0:r0 + 128, :], zt[:, 0:512], ["zt"], ["KS%d" % (r0 // 128)])
                DMA("sp", VS[r0:r0 + 128, :], zt, ["zt"], ["VS%d" % (r0 // 128)])
        xt = [sb("xt%d" % i, [128, D]) for i in range(2)]
        junk = sb("junk", [128, D], BF16)
        xn = [sb("xn%d" % i, [128, D], BF16) for i in range(2)]
        xnT = [sb("xnT%d" % i, [128, 8, 128], BF16) for i in range(2)]
        ssq = sb("ssq", [128, 2])
        rstd = sb("rstd", [128, 2])
        qk = [sb("qk%d" % i, [128, 512]) for i in range(2)]
        vbf = [sb("vbf%d" % i, [128, 512], BF16) for i in range(2)]
        gbf = [sb("gbf%d" % i, [128, 512], BF16) for i in range(2)]
        lr = [sb("lr%d" % i, [128, 32]) for i in range(2)]
        aqr = [sb("aqr%d" % i, [128, 8, 64]) for i in range(2)]
        akr = [sb("akr%d" % i, [128, 8, 64]) for i in range(2)]
        vab = [sb("vab%d" % i, [128, 8, 65], BF16) for i in range(2)]
        lrT = sb("lrT", [33, 128])
        ez = sb("ez", [128, 512])
        spl = sb("spl", [128, 512])
        E1 = sb("E1", [128, 512])
        E2 = sb("E2", [128, 512])
        E3 = sb("E3", [128, 512])
        dec = sb("dec", [128, 4])
        qd = sb("qd", [128, 512], BF16)
        ki = sb("ki", [128, 512], BF16)
        ke = sb("ke", [128, 512], BF16)
        kiT = sb("kiT", [128, 4, 128], BF16)
        qrb = [sb("qrb%d" % i, [128, 512], BF16) for i in range(2)]
        krb = [sb("krb%d" % i, [128, 512], BF16) for i in range(2)]
        rta = sb("rta", [128, 8, 8])
        rtb = sb("rtb", [128, 8, 8])
        rtc = sb("rtc", [128, 8, 8])
        rtd = sb("rtd", [128, 8, 8])
        sqs = sb("sqs", [128, 8, 64])
        nrm = sb("nrm", [128, 16])
        P.op("dve", lambda e: e.memset(lrT[32:33, :], 1.0), writes=["lrT_one"])
        tiles = list(range(NTW) if dbg_tiles is None else dbg_tiles)

        def S1(i):
            own = T0 <= i < T0 + NTO
            io = i - T0
            b2 = i % 2
            xtk, xnk, xnTk = "xt%d" % b2, "xn%d" % b2, "xnT%d" % b2
            if i == tiles[0]:
                DMA("sp", xt[b2], xw[i * 128:(i + 1) * 128, :], [], [xtk])
            if i + 1 < NTW and (dbg_tiles is None):
                DMA("sp", xt[(i + 1) % 2], xw[(i + 1) * 128:(i + 2) * 128, :], [], ["xt%d" % ((i + 1) % 2)])
            sk, rk = "ssq%d" % b2, "rstd%d" % b2
            P.op("act", lambda e, b2=b2: e.activation(out=junk, in_=xt[b2], func=AF.Square, accum_out=ssq[:, b2:b2 + 1]),
                 reads=[xtk], writes=["junk", sk])
            rstd_from_ssq(rstd[:, b2:b2 + 1], ssq[:, b2:b2 + 1], D, sk, rk)
            STT(xn[b2], xt[b2], rstd[:, b2:b2 + 1], n1bc, ALU.mult, ALU.mult, [xtk, rk, "n1bc"], [xnk])
            pbt, pbk = next_pb()

            def tr_fn(e, b2=b2, pbt=pbt):
                ins = None
                for c in range(8):
                    ins = e.transpose(out=pbt[:, c * 128:(c + 1) * 128], in_=xn[b2][:, c * 128:(c + 1) * 128], identity=identb)
                return ins
            P.op("pe", tr_fn, reads=[xnk, "identb"], writes=[pbk])
            CP("act", xnT[b2].rearrange("p c t -> p (c t)"), pbt[:, :], [pbk], [xnTk])

            def proj(c0, c1):
                pt, pk = next_pf()
                n = c1 - c0
                mm_group(pt[:, 0:n], [(xnT[b2][:, c, :], win[:, c, c0:c1]) for c in range(8)], pk, [xnTk] + winkeys)
                return pt, pk

            if own:
                pt, pk = proj(0, 512)
                CP("act", qk[b2], pt[:, 0:512], [pk], ["qk%d" % b2])
            else:
                pt, pk = proj(256, 512)
                CP("act", qk[b2][:, 256:512], pt[:, 0:256], [pk], ["qk%d" % b2])
            pt, pk = proj(512, 1024)
            CP("dve", vbf[b2], pt[:, 0:512], [pk], ["vbf%d" % b2])
            if own:
                DMA("sp", GV[io * 128:(io + 1) * 128, :], vbf[b2], ["vbf%d" % b2], ["GV%d" % io])
                pt, pk = proj(1024, 1536)
                CP("act", gbf[b2], pt[:, 0:512], [pk], ["gbf%d" % b2])
                DMA("sp", GG[io * 128:(io + 1) * 128, :], gbf[b2], ["gbf%d" % b2], ["GG%d" % io])
            pt, pk = proj(1536, 1568)
            CP("dve", lr[b2], pt[:, 0:32], [pk], ["lr%d" % b2])
            if own:
                pt, pk = proj(1568, 2080)
                CP("dve", aqr[b2].rearrange("p h d -> p (h d)"), pt[:, 0:512], [pk], ["aqr%d" % b2])
            pt, pk = proj(2080, 2592)
            CP("act", akr[b2].rearrange("p h d -> p (h d)"), pt[:, 0:512], [pk], ["akr%d" % b2])
            pt, pk = proj(2592, 3104)
            r0k = HALO + i * 128
            CP("act", vab[b2][:, :, 0:64], pt[:, 0:512].rearrange("p (h d) -> p h d", h=8), [pk], ["vab%d" % b2])
            CP("dve", vab[b2][:, :, 64:65], vc[:, i:i + 1].unsqueeze(1).broadcast_to([128, 8, 1]), ["vc"], ["vab%d" % b2])
            DMA("sp", VS[r0k:r0k + 128, :], vab[b2].rearrange("p h d -> p (h d)"), ["vab%d" % b2], ["VS%d" % (r0k // 128)])

        def S2(i):
            own = T0 <= i < T0 + NTO
            left = i < T0
            io = i - T0
            b2 = i % 2
            qkb = qk[b2]
            qkk = "qk%d" % b2
            vkey = "vbf%d" % b2
            vt = vbf[b2]
            pt, pk = next_pf()
            P.op("pe", lambda e, pt=pt: e.transpose(out=pt[0:32, 0:128], in_=lr[b2], identity=cm[:, 0, :]), reads=["lr%d" % b2, "cm"], writes=[pk])
            CP("dve", lrT[0:32, :], pt[0:32, 0:128], [pk], ["lrT"])
            pz, pzk = next_pf()
            P.op("pe", lambda e, pz=pz: e.matmul(pz[:, :], lhsT=lrT, rhs=wz, start=True, stop=True),
                 reads=["lrT", "lrT_one", "wz"], writes=[pzk])
            ACT(ez, pz[:, :], AF.Exp, [pzk], ["ez"], scale=-1.0)
            ACT(spl, ez, AF.Ln, ["ez", "onec"], ["spl"], bias=onec, scale=1.0)
            pbb, pbbk = next_pf()
            P.op("pe", lambda e, pbb=pbb: (e.matmul(pbb[:, 0:256], lhsT=cm[:, 1, :], rhs=spl[:, 0:256], start=True, stop=True),
                                           e.matmul(pbb[:, 256:512], lhsT=cm[:, 2, :], rhs=spl[:, 256:512], start=True, stop=True))[1],
                 reads=["cm", "spl"], writes=[pbbk])
            ACT(E1, pbb[:, :], AF.Exp, [pbbk], ["E1"])
            ACT(E2, pbb[:, :], AF.Exp, [pbbk], ["E2"], scale=-1.0)
            pb3, pb3k = next_pf()
            P.op("pe", lambda e, pb3=pb3: (e.matmul(pb3[:, 0:256], lhsT=cm[:, 3, :], rhs=spl[:, 0:256], start=True, stop=True),
                                           e.matmul(pb3[:, 256:512], lhsT=cm[:, 4, :], rhs=spl[:, 256:512], start=True, stop=True))[1],
                 reads=["cm", "spl"], writes=[pb3k])
            ACT(E3, pb3[:, :], AF.Exp, [pb3k], ["E3"])
            pdc, pdck = next_pf()

            def dec_fn(e, pdc=pdc):
                ins = None
                for j in range(4):
                    ins = e.matmul(pdc[:, j:j + 1], lhsT=spl[:, j * 128:(j + 1) * 128], rhs=n16col, start=True, stop=True)
                return ins
            P.op("pe", dec_fn, reads=["spl", "n16col"], writes=[pdck])
            ACT(dec, pdc[:, 0:4], AF.Exp, [pdck], ["dec"])
            if own:
                STT(qd[:, 0:256], qkb[:, 0:256], 0.125, E1[:, 0:256], ALU.mult, ALU.mult, [qkk, "E1"], ["qd"])
                STT(qd[:, 256:512], qkb[:, 0:256], 0.125, E1[:, 256:512], ALU.mult, ALU.mult, [qkk, "E1"], ["qd"])
                TT("pool", ki[:, 0:256], qkb[:, 256:512], E2[:, 0:256], ALU.mult, [qkk, "E2"], ["ki"])
                TT("pool", ki[:, 256:512], qkb[:, 256:512], E2[:, 256:512], ALU.mult, [qkk, "E2"], ["ki"])
            TT("pool", ke[:, 0:256], qkb[:, 256:512], E3[:, 0:256], ALU.mult, [qkk, "E3"], ["ke"])
            TT("pool", ke[:, 256:512], qkb[:, 256:512], E3[:, 256:512], ALU.mult, [qkk, "E3"], ["ke"])
            if own:
                pbt, pbk = next_pb()

                def tr2_fn(e, pbt=pbt):
                    ins = None
                    for j in range(4):
                        ins = e.transpose(out=pbt[:, j * 128:(j + 1) * 128], in_=qd[:, j * 128:(j + 1) * 128], identity=identb)
                    for j in range(4):
                        ins = e.transpose(out=pbt[:, 512 + j * 128:512 + (j + 1) * 128], in_=ki[:, j * 128:(j + 1) * 128], identity=identb)
                    return ins
                P.op("pe", tr2_fn, reads=["qd", "ki", "identb"], writes=[pbk])
                CP("act", qdT[:, io, :, :].rearrange("p c t -> p (c t)"), pbt[:, 0:512], [pbk], ["qdT%d" % io])
                CP("dve", kiT.rearrange("p c t -> p (c t)"), pbt[:, 512:1024], [pbk], ["kiT"])
                paX, paXk = next_pf()
                paY, paYk = next_pf()

                def att_fn(e, pa, par, io=io):
                    ins = None
                    p0 = par * 64
                    for dirn in range(2):
                        for pr in range(2):
                            blk = dirn * 2 + pr
                            sl = dirn * 2 + pr
                            ins = e.matmul(pa[:, sl * 128:(sl + 1) * 128], lhsT=kiT[p0:p0 + 64, blk, :], rhs=qdT[p0:p0 + 64, io, blk, :],
                                           start=True, stop=True)
                    return ins
                P.op("pe", lambda e, pa=paX, f=att_fn: f(e, pa, 0), reads=["kiT", "qdT%d" % io], writes=[paXk])
                P.op("pe", lambda e, pa=paY, f=att_fn: f(e, pa, 1), reads=["kiT", "qdT%d" % io], writes=[paYk])
                TT("dve", ez, paX[:, :], maskFB.rearrange("p h c -> p (h c)"), ALU.mult, [paXk, "maskFB"], ["ez"])
                TT("dve", E1, paY[:, :], maskFB.rearrange("p h c -> p (h c)"), ALU.mult, [paYk, "maskFB"], ["E1"])
                av = attnT[:, io, :].rearrange("p (a b c) -> p a b c", a=2, b=2)
                TT("pool", av[:, :, 0, :], ez[:, 0:256].rearrange("p (a c) -> p a c", a=2), ez[:, 256:512].rearrange("p (a c) -> p a c", a=2),
                   ALU.add, ["ez"], ["attnT%d" % io])
                TT("pool", av[:, :, 1, :], E1[:, 0:256].rearrange("p (a c) -> p a c", a=2), E1[:, 256:512].rearrange("p (a c) -> p a c", a=2),
                   ALU.add, ["E1"], ["attnT%d" % io])
            for dirn in range(2):
                if dirn == 0 and i >= T0 + NTO:
                    continue
                if dirn == 1 and left:
                    continue
                pkv, pkvk = next_pf()

                def kv_fn(e, pkv=pkv, dirn=dirn, vt=vt):
                    ins = None
                    for pr in range(2):
                        ins = e.matmul(pkv[:, pr * 256:(pr + 1) * 256], lhsT=ke[:, dirn * 256 + pr * 128: dirn * 256 + (pr + 1) * 128],
                                       rhs=vt[:, pr * 256:(pr + 1) * 256], start=True, stop=True)
                    return ins
                P.op("pe", kv_fn, reads=["ke", vkey], writes=[pkvk])
                if dirn == 0:
                    if own:
                        CP("act", SfT[:, io, :, :].rearrange("p a b -> p (a b)"), Sf.rearrange("p a b -> p (a b)"), ["Sf"], ["SfT%d" % io])
                    for pr in range(2):
                        for hh in range(2):
                            p0 = hh * 64
                            STT(Sf[p0:p0 + 64, pr, :], Sf[p0:p0 + 64, pr, :], dec[p0:p0 + 64, pr:pr + 1],
                                pkv[p0:p0 + 64, pr * 256 + hh * 128: pr * 256 + (hh + 1) * 128], ALU.mult, ALU.add,
                                ["Sf", "dec", pkvk], ["Sf"])
                else:
                    ib = i - T0
                    for pr in range(2):
                        for hh in range(2):
                            p0 = hh * 64
                            CP("act", kvB[p0:p0 + 64, ib, pr, :], pkv[p0:p0 + 64, pr * 256 + hh * 128: pr * 256 + (hh + 1) * 128],
                               [pkvk], ["kvB%d" % ib])
                    CP("dve", decB[:, ib, :], dec[:, 2:4], ["dec"], ["decB%d" % ib])

            def rope(raw, rkey):
                cosb = cst[:, i, 0:8].unsqueeze(1).broadcast_to([128, 8, 8])
                sinb = cst[:, i, 8:16].unsqueeze(1).broadcast_to([128, 8, 8])
                TT("pool", rta, raw[:, :, 0:8], cosb, ALU.mult, [rkey, "cst"], ["rta"])
                TT("pool", rtb, raw[:, :, 8:16], sinb, ALU.mult, [rkey, "cst"], ["rtb"])
                TT("pool", rtc, raw[:, :, 8:16], cosb, ALU.mult, [rkey, "cst"], ["rtc"])
                TT("pool", rtd, raw[:, :, 0:8], sinb, ALU.mult, [rkey, "cst"], ["rtd"])
                TT("dve", raw[:, :, 0:8], rta, rtb, ALU.subtract, ["rta", "rtb"], [rkey])
                TT("dve", raw[:, :, 8:16], rtc, rtd, ALU.add, ["rtc", "rtd"], [rkey])

            def sqnorm(src, col0, skey):
                TT("pool", sqs, src, src, ALU.mult, [skey], ["sqs"])
                P.op("dve", lambda e: e.tensor_reduce(out=nrm[:, col0:col0 + 8], in_=sqs, axis=AX.X, op=ALU.add), reads=["sqs"], writes=["nrm"])
                TT("dve", nmax[:, col0:col0 + 8], nmax[:, col0:col0 + 8], nrm[:, col0:col0 + 8], ALU.max, ["nrm", "nmax"], ["nmax"])

            r0k = HALO + i * 128
            if own:
                rope(aqr[b2], "aqr%d" % b2)
                sqnorm(aqr[b2], 0, "aqr%d" % b2)
                CP("act", qrb[b2], aqr[b2].rearrange("p h d -> p (h d)"), ["aqr%d" % b2], ["qrb%d" % b2])
                DMA("sp", QS[io * 128:(io + 1) * 128, :], qrb[b2], ["qrb%d" % b2], ["QS%d" % io])
            rope(akr[b2], "akr%d" % b2)
            sqnorm(akr[b2], 8, "akr%d" % b2)
            CP("act", krb[b2], akr[b2].rearrange("p h d -> p (h d)"), ["akr%d" % b2], ["krb%d" % b2])
            DMA("sp", KS[r0k:r0k + 128, :], krb[b2], ["krb%d" % b2], ["KS%d" % (r0k // 128)])

        for n_ in range(len(tiles) + 1):
            ra, rb = [], []
            if n_ < len(tiles):
                P.rec = ra
                stream[0] = 0
                S1(tiles[n_])
            if n_ > 0:
                P.rec = rb
                stream[0] = 1
                S2(tiles[n_ - 1])
            P.rec = None
            stream[0] = None
            P.replay_merged(ra, rb)

        if stop_after == "A":
            P.op("sp", None, reads=[k_ for k_ in P.last_w.keys() if k_[:2] in ("QS", "KS", "VS", "GV", "GG")], writes=[])
            P.emit()
            return nc
        for i in range(NTW - 1, T0 - 1, -1):
            ib = i - T0
            if ib < NTO:
                CP("act", SbT[:, ib, :, :].rearrange("p a b -> p (a b)"), Sb.rearrange("p a b -> p (a b)"), ["Sb"], ["SbT%d" % ib])
            if i == T0:
                break
            for pr in range(2):
                STT(Sb[:, pr, :], Sb[:, pr, :], decB[:, ib, pr:pr + 1], kvB[:, ib, pr, :], ALU.mult, ALU.add,
                    ["Sb", "decB%d" % ib, "kvB%d" % ib], ["Sb"])

        P.barrier()
        AR.off = M1
        vb2 = [sb("vb2%d" % i, [128, 512], BF16) for i in range(2)]
        gb2 = [sb("gb2%d" % i, [128, 512], BF16) for i in range(2)]
        osb = sb("osb", [128, 512])
        osq = sb("osq", [128, 4, 128])
        oms = sb("oms", [128, 4])
        sgs = sb("sgs", [128, 512])
        ybf = sb("ybf", [128, 512])
        mixb = [sb("mixb%d" % i, [128, 512], BF16) for i in range(2)]
        for io in range(NTO):
            b2 = io % 2
            DMA("sp", vb2[b2], GV[io * 128:(io + 1) * 128, :], ["GV%d" % io], ["vb2%d" % b2])
            DMA("sp", gb2[b2], GG[io * 128:(io + 1) * 128, :], ["GG%d" % io], ["gb2%d" % b2])
            poX, poXk = next_pf()
            poY, poYk = next_pf()

            def o_fn(e, po, par, io=io, b2=b2):
                ins = None
                p0 = par * 64
                for pr in range(2):
                    h = pr * 2 + par
                    oap = po[:, pr * 128:(pr + 1) * 128]
                    e.matmul(oap, lhsT=attnT[:, io, h * 128:(h + 1) * 128], rhs=vb2[b2][:, h * 128:(h + 1) * 128], start=True, stop=False)
                    e.matmul(oap, lhsT=qdT[p0:p0 + 64, io, pr, :], rhs=SfT[p0:p0 + 64, io, pr, :], start=False, stop=False)
                    ins = e.matmul(oap, lhsT=qdT[p0:p0 + 64, io, 2 + pr, :], rhs=SbT[p0:p0 + 64, io, pr, :], start=False, stop=True)
                return ins
            rk_ = ["attnT%d" % io, "qdT%d" % io, "SfT%d" % io, "SbT%d" % io, "vb2%d" % b2]
            P.op("pe", lambda e, po=poX, f=o_fn: f(e, po, 0), reads=rk_, writes=[poXk])
            P.op("pe", lambda e, po=poY, f=o_fn: f(e, po, 1), reads=rk_, writes=[poYk])
            ov = osb.rearrange("p (a b c) -> p a b c", a=2, b=2)
            CP("act", ov[:, :, 0, :], poX[:, 0:256].rearrange("p (a c) -> p a c", a=2), [poXk], ["osb"])
            CP("act", ov[:, :, 1, :], poY[:, 0:256].rearrange("p (a c) -> p a c", a=2), [poYk], ["osb"])
            TT("pool", osq.rearrange("p h d -> p (h d)"), osb, osb, ALU.mult, ["osb"], ["osq"])
            P.op("dve", lambda e: e.tensor_reduce(out=oms, in_=osq, axis=AX.X, op=ALU.add), reads=["osq"], writes=["oms"])
            rstd_from_ssq(oms, oms, 128, "oms", "oms")
            ACT(sgs, gb2[b2], AF.Silu, ["gb2%d" % b2], ["sgs"])
            TT("dve", ybf, osb, gnbc, ALU.mult, ["osb", "gnbc"], ["ybf"])
            for h in range(4):
                STT(mixb[b2][:, h * 128:(h + 1) * 128], ybf[:, h * 128:(h + 1) * 128], oms[:, h:h + 1], sgs[:, h * 128:(h + 1) * 128],
                    ALU.mult, ALU.mult, ["ybf", "oms", "sgs"], ["mixb%d" % b2])
            DMA("sp", MG[io * 128:(io + 1) * 128, :], mixb[b2], ["mixb%d" % b2], ["MG%d" % io])
        MGK = ["MG%d" % i for i in range(NTO)]
        if stop_after == "G2":
            P.op("sp", None, reads=QSK + KSK + VSK + MGK, writes=[])
            P.emit()
            return nc

        P.barrier()
        AR.off = M0
        nm2 = sb("nm2", [128, 2])
        m2 = sb("m2", [2, 2])
        m1 = sb("m1", [1, 4])
        P.op("dve", lambda e: e.tensor_reduce(out=nm2, in_=nmax.rearrange("p (a h) -> p a h", a=2), axis=AX.X, op=ALU.max),
             reads=["nmax"], writes=["nm2"])
        pt, pk = next_pf()
        P.op("pe", lambda e, pt=pt: e.transpose(out=pt[0:2, 0:128], in_=nm2, identity=cm[:, 0, :]), reads=["nm2", "cm"], writes=[pk])
        P.op("dve", lambda e, pt=pt: e.tensor_reduce(out=m2[:, 0:1], in_=pt[0:2, 0:128], axis=AX.X, op=ALU.max), reads=[pk], writes=["m2"])
        pt, pk = next_pf()
        P.op("pe", lambda e, pt=pt: e.transpose(out=pt[0:1, 0:2], in_=m2[:, 0:1], identity=cm[0:2, 0, 0:2]), reads=["m2", "cm"], writes=[pk])
        CP("dve", m1[:, 0:2], pt[0:1, 0:2], [pk], ["m1"])
        TT("dve", m1[:, 2:3], m1[:, 0:1], m1[:, 1:2], ALU.mult, ["m1"], ["m1"])
        ACT(m1[:, 3:4], m1[:, 2:3], AF.Ln, ["m1"], ["m1"])
        ACT(m1[:, 3:4], m1[:, 3:4], AF.Exp, ["m1"], ["m1"], scale=0.5)
        TS("dve", m1[:, 3:4], m1[:, 3:4], -0.125, None, ALU.mult, None, ["m1"], ["m1"])
        pt, pk = next_pf()
        P.op("pe", lambda e, pt=pt: e.matmul(pt[:, 0:1], lhsT=cm[0:1, 7, :], rhs=m1[:, 3:4], start=True, stop=True), reads=["m1", "cm"], writes=[pk])
        CP("dve", negc, pt[:, 0:1], [pk], ["negc"])

        accT = sb("accT", [65, 8, OWN])
        NQ = 8
        qsb2 = [[sb("qsb%d" % i, [128, 512], BF16) for i in range(NQ)] for _ in range(2)]
        ksb2 = [[sb("ksb%d" % i, [128, 512], BF16) for i in range(NQ + 2)] for _ in range(2)]
        vsb2 = [[sb("vsb%d" % i, [128, 8, 65], BF16) for i in range(NQ + 2)] for _ in range(2)]
        qT2 = [[sb("qT%d" % i, [128, 4, 128], BF16) for i in range(NQ)] for _ in range(2)]
        kT2 = [[sb("kT%d" % i, [128, 4, 128], BF16) for i in range(NQ + 2)] for _ in range(2)]
        pex = [sb("pex%d" % i, [128, 384], BF16) for i in range(4)]
        pmk = [sb("pmk%d" % i, [128, 384], BF16) for i in range(4)]
        cnt4 = [0, 0]
        jobs = [(1, 0, 0, 8), (1, 0, 8, 8)] + [(4, r, 0, 4) for r in range(4)] + [(16, r, 0, 1) for r in range(16)]
        for jn, (dd, r, j0, nq) in enumerate(jobs):
            js = jn % 2
            qsb, ksb, vsb, qT, kT = qsb2[js], ksb2[js], vsb2[js], qT2[js], kT2[js]
            QSv = QS.rearrange("(n d) c -> d n c", d=dd)
            KSv = KS.rearrange("(n d) c -> d n c", d=dd)
            VSv = VS.rearrange("(n d) c -> d n c", d=dd)
            accv = accT.rearrange("p h (n d) -> p h d n", d=dd)
            for jq in range(nq):
                n0 = 128 * (j0 + jq)
                DMA("sp", qsb[jq], QSv[r, n0:n0 + 128, :], QSK, [("qsb" + str(js) + "_%d") % jq])
            for kk in range(nq + 2):
                n0 = 2048 // dd + 128 * (j0 + kk - 1)
                DMA("sp", ksb[kk], KSv[r, n0:n0 + 128, :], KSK, [("ksb" + str(js) + "_%d") % kk])
                DMA("sp", vsb[kk].rearrange("p h d -> p (h d)"), VSv[r, n0:n0 + 128, :], VSK, [("vsb" + str(js) + "_%d") % kk])
            tl = [(qsb[jq], ("qsb" + str(js) + "_%d") % jq, qT[jq], ("qT" + str(js) + "_%d") % jq) for jq in range(nq)] + \
                 [(ksb[kk], ("ksb" + str(js) + "_%d") % kk, kT[kk], ("kT" + str(js) + "_%d") % kk) for kk in range(nq + 2)]
            for t0 in range(0, len(tl), 2):
                grp = tl[t0:t0 + 2]
                pbt, pbk = next_pb()

                def trq_fn(e, grp=grp, pbt=pbt):
                    ins = None
                    for gi, (src, _, _, _) in enumerate(grp):
                        for c in range(4):
                            ins = e.transpose(out=pbt[:, gi * 512 + c * 128: gi * 512 + (c + 1) * 128], in_=src[:, c * 128:(c + 1) * 128], identity=identb)
                    return ins
                P.op("pe", trq_fn, reads=[g[1] for g in grp] + ["identb"], writes=[pbk])
                for gi, (_, _, dst, dk) in enumerate(grp):
                    CP("act" if gi == 0 else "dve", dst.rearrange("p c t -> p (c t)"), pbt[:, gi * 512:(gi + 1) * 512], [pbk], [dk])
            def it_body(jq, hg, s_, kT=kT, qT=qT, vsb=vsb, js=js, dd=dd, r=r, j0=j0, accv=accv):
                bufs = []
                for h in range(hg * 4, hg * 4 + 4):
                    p0 = (h % 2) * 64
                    blk = h // 2
                    pS, pSk = next_pf()

                    def s_fn(e, pS=pS, jq=jq, p0=p0, blk=blk, kT=kT, qT=qT):
                        ins = None
                        for sl in range(3):
                            ins = e.matmul(pS[:, sl * 128:(sl + 1) * 128], lhsT=kT[jq + sl][p0:p0 + 64, blk, :], rhs=qT[jq][p0:p0 + 64, blk, :],
                                           start=True, stop=True)
                        return ins
                    P.op("pe", s_fn, reads=[("kT" + str(js) + "_%d") % (jq + sl) for sl in range(3)] + [("qT" + str(js) + "_%d") % jq], writes=[pSk])
                    bi = 2 * s_ + cnt4[s_] % 2
                    cnt4[s_] += 1
                    ACT(pex[bi], pS[:, 0:384], AF.Exp, [pSk, "negc"], ["pex%d" % bi], bias=negc, scale=0.125)
                    TT("pool" if (h % 4 == 3) else "dve", pmk[bi], pex[bi], band, ALU.mult, ["pex%d" % bi, "band"], ["pmk%d" % bi])
                    bufs.append((bi, h))
                    if len(bufs) == 2:
                        pU, pUk = next_pf()

                        def pv_fn(e, pU=pU, jq=jq, bufs=tuple(bufs), vsb=vsb):
                            ins = None
                            for hi, (b_, h_) in enumerate(bufs):
                                for sl in range(3):
                                    ins = e.matmul(pU[0:65, hi * 128:(hi + 1) * 128], lhsT=vsb[jq + sl][:, h_, :], rhs=pmk[b_][:, sl * 128:(sl + 1) * 128],
                                                   start=(sl == 0), stop=(sl == 2))
                            return ins
                        P.op("pe", pv_fn, reads=[("vsb" + str(js) + "_%d") % (jq + sl) for sl in range(3)] + ["pmk%d" % b_ for (b_, _) in bufs], writes=[pUk])
                        n0 = 128 * (j0 + jq)
                        h0 = bufs[0][1]
                        dst = accv[:, h0:h0 + 2, r, n0:n0 + 128]
                        src = pU[0:65, 0:256].rearrange("p (h t) -> p h t", h=2)
                        if dd == 1:
                            CP("dve", dst, src, [pUk], ["accT"])
                        else:
                            TT("dve", dst, src, dst, ALU.add, [pUk, "accT"], ["accT"])
                        bufs = []
            its = [(jq, hg) for jq in range(nq) for hg in range(2)]
            for m in range(0, len(its), 2):
                ra, rb = [], []
                P.rec = ra
                stream[0] = 0
                it_body(its[m][0], its[m][1], 0)
                if m + 1 < len(its):
                    P.rec = rb
                    stream[0] = 1
                    it_body(its[m + 1][0], its[m + 1][1], 1)
                P.rec = None
                stream[0] = None
                P.replay_merged(ra, rb)
        rz = sb("rz", [64, 512])
        otb = [sb("otb%d" % i, [64, 512], BF16) for i in range(2)]
        k2 = 0
        for h in range(8):
            for g in range(4):
                pz, pzk = next_pf()
                P.op("pe", lambda e, pz=pz, h=h, g=g: e.matmul(pz[0:64, :], lhsT=cm[64:65, 7, 0:64], rhs=accT[64:65, h, g * 512:(g + 1) * 512],
                                                                start=True, stop=True), reads=["accT", "cm"], writes=[pzk])
                P.op("dve", lambda e, pz=pz: e.reciprocal(out=rz, in_=pz[0:64, :]), reads=[pzk], writes=["rz"])
                b2 = k2 % 2
                k2 += 1
                TT("pool", otb[b2], accT[0:64, h, g * 512:(g + 1) * 512], rz, ALU.mult, ["accT", "rz"], ["otb%d" % b2])
                DMA("sp", OTS[h, :, g * 512:(g + 1) * 512], otb[b2], ["otb%d" % b2], ["OTS%d_%d" % (h, g)])
        OTK = ["OTS%d_%d" % (h, g) for h in range(8) for g in range(4)]
        if stop_after == "B":
            P.op("sp", None, reads=OTK + MGK, writes=[])
            P.emit()
            return nc

        P.barrier()
        AR.off = M0
        bc_cache = {}

        def bcreg(e):
            if "r" not in bc_cache:
                bc_cache["r"] = e.to_reg(2559)
            return bc_cache["r"]
        CAPG = 640
        NSLOT = 4 * CAPG
        OOB = 4096.0
        u2tok = sb("u2tok", [128, NTO, D], BF16)
        OH = sb("OH", [128, NTO, 4])
        WE = sb("WE", [128, NTO, 8])
        idxf = sb("idxf", [128, NTO])
        idxi = sb("idxi", [128, 2 * NTO], I32)
        goffm = sb("goffm", [128, 4])
        pren = sb("pren", [128, 4])
        M2 = AR.off
        woutG = sb("woutG", [128, 4, D], BF16)
        woutA = sb("woutA", [64, 8, D], BF16)
        DMA("pool", woutG, w_out[0:512, :].rearrange("(c p) n -> p c n", p=128), [], ["woutG"])
        DMA("pool", woutA, w_out[512:1024, :].rearrange("(h p) n -> p h n", p=64), [], ["woutA"])
        for g in range(4):
            P.op("dve", lambda e, g=g: e.memset(goffm[:, g:g + 1], float(g * CAPG) - OOB), writes=["goffm"])
        P.op("dve", lambda e: e.memset(pren, 0.0), writes=["pren"])
        zx = sb("zx", [128, D], BF16)
        zw = sb("zw", [128, 8])
        P.op("pool", lambda e: e.memset(zx, 0.0), writes=["zx"])
        P.op("pool", lambda e: e.memset(zw, 0.0), writes=["zw"])
        for r0 in range(0, NSLOT, 128):
            DMA("sp", XB[r0:r0 + 128, :], zx, ["zx"], ["XB"])
            DMA("sp", WB[r0:r0 + 128, :], zw, ["zw"], ["WB"])
        xo = [sb("xo%d" % i, [128, D]) for i in range(2)]
        mgl = [sb("mgl%d" % i, [128, 512], BF16) for i in range(2)]
        otl = [sb("otl%d" % i, [64, 8, 128], BF16) for i in range(2)]
        mgT2 = [sb("mgT%d" % i, [128, 4, 128], BF16) for i in range(2)]
        h2t = [sb("h2t%d" % i, [128, D]) for i in range(2)]
        u22 = [sb("u2%d" % i, [128, D]) for i in range(2)]
        u2Tf2 = [sb("u2Tf%d" % i, [128, 8, 128]) for i in range(2)]
        junk22 = [sb("junk2%d" % i, [128, D], BF16) for i in range(2)]
        ss2 = sb("ss2", [128, 2])
        rs2 = sb("rs2", [128, 2])
        lg2 = [sb("lg%d" % i, [128, 36]) for i in range(2)]
        sm2 = [sb("sm%d" % i, [128, 64]) for i in range(2)]
        smr = sb("smr", [128, 16])

        def c1_body(io):
            b2 = io % 2
            mgT, u2, u2Tf, junk2, lg, sm = mgT2[b2], u22[b2], u2Tf2[b2], junk22[b2], lg2[b2], sm2[b2]
            mk, uk, lk, sk_ = "mgT%d" % b2, "u2_%d" % b2, "lg%d" % b2, "sm%d" % b2
            DMA("sp", xo[b2], xw[HALO + io * 128: HALO + (io + 1) * 128, :], [], ["xo%d" % b2])
            DMA("sp", mgl[b2], MG[io * 128:(io + 1) * 128, :], ["MG%d" % io], ["mgl%d" % b2])
            DMA("sp", otl[b2], OTS[:, :, io * 128:(io + 1) * 128].rearrange("h p t -> p h t"), OTK, ["otl%d" % b2])
            pbt, pbk = next_pb()

            def trm_fn(e, pbt=pbt, b2=b2):
                ins = None
                for c in range(4):
                    ins = e.transpose(out=pbt[:, c * 128:(c + 1) * 128], in_=mgl[b2][:, c * 128:(c + 1) * 128], identity=identb)
                return ins
            P.op("pe", trm_fn, reads=["mgl%d" % b2, "identb"], writes=[pbk])
            CP("act", mgT.rearrange("p c t -> p (c t)"), pbt[:, 0:512], [pbk], [mk])
            for cg in range(2):
                pt, pk = next_pf()
                pairs = [(mgT[:, c, :], woutG[:, c, cg * 512:(cg + 1) * 512]) for c in range(4)] + \
                        [(otl[b2][:, h, :], woutA[:, h, cg * 512:(cg + 1) * 512]) for h in range(8)]
                mm_group(pt[:, :], pairs, pk, [mk, "otl%d" % b2, "woutG", "woutA"])
                TT("dve", h2t[b2][:, cg * 512:(cg + 1) * 512], pt[:, :], xo[b2][:, cg * 512:(cg + 1) * 512], ALU.add,
                   [pk, "xo%d" % b2], ["h2t%d" % b2])
            DMA("sp", H2[io * 128:(io + 1) * 128, :], h2t[b2], ["h2t%d" % b2], ["H2_%d" % io])
            P.op("act", lambda e: e.activation(out=junk2, in_=h2t[b2], func=AF.Square, accum_out=ss2[:, b2:b2 + 1]),
                 reads=["h2t%d" % b2], writes=["junk2%d" % b2, "ss2%d" % b2])
            rstd_from_ssq(rs2[:, b2:b2 + 1], ss2[:, b2:b2 + 1], D, "ss2%d" % b2, "rs2%d" % b2)
            STT(u2, h2t[b2], rs2[:, b2:b2 + 1], n2bc, ALU.mult, ALU.mult, ["h2t%d" % b2, "rs2%d" % b2, "n2bc"], [uk])
            CP("pool", u2tok[:, io, :], u2, [uk], ["u2tok%d" % io])
            for half in range(2):
                pt, pk = next_pf()

                def tru_fn(e, pt=pt, half=half):
                    ins = None
                    for c in range(4):
                        cc = half * 4 + c
                        ins = e.transpose(out=pt[:, c * 128:(c + 1) * 128], in_=u2[:, cc * 128:(cc + 1) * 128], identity=cm[:, 0, :])
                    return ins
                P.op("pe", tru_fn, reads=[uk, "cm"], writes=[pk])
                CP("act", u2Tf[:, half * 4:half * 4 + 4, :].rearrange("p c t -> p (c t)"), pt[:, :], [pk], ["u2Tf%d_%d" % (b2, half)])
            pr_, prk = next_pf()
            mm_group(pr_[:, 0:36], [(u2Tf[:, c, :], wr[:, c, :]) for c in range(8)], prk, ["u2Tf%d_0" % b2, "u2Tf%d_1" % b2, "wr"])
            TT("dve", lg, pr_[:, 0:36], rbbc, ALU.add, [prk, "rbbc"], [lk])
            gmax, ngmax, gsum, gw = sm[:, 0:1], sm[:, 1:2], sm[:, 2:3], sm[:, 3:4]
            oh = OH[:, io, :]
            ohk = "OH%d" % io
            ge = sm[:, 8:12]
            esel = sm[:, 16:24]
            top8 = sm[:, 24:32]
            d21, w1g, w2g = sm[:, 32:33], sm[:, 33:34], sm[:, 34:35]
            wa = sm[:, 40:48]
            wb_ = sm[:, 48:56]
            P.op("dve", lambda e: e.tensor_reduce(out=gmax, in_=lg[:, 0:4], axis=AX.X, op=ALU.max), reads=[lk], writes=[sk_])
            TS("dve", oh, lg[:, 0:4], gmax, None, ALU.is_equal, None, [lk, sk_], [ohk])
            TS("dve", ngmax, gmax, -1.0, None, ALU.mult, None, [sk_], [sk_])
            ACT(ge, lg[:, 0:4], AF.Exp, [lk, sk_], [sk_], bias=ngmax, scale=1.0)
            P.op("dve", lambda e: e.tensor_reduce(out=gsum, in_=ge, axis=AX.X, op=ALU.add), reads=[sk_], writes=[sk_])
            P.op("dve", lambda e: e.reciprocal(out=gw, in_=gsum), reads=[sk_], writes=[sk_])
            TS("dve", esel, lg[:, 4:12], oh[:, 0:1], None, ALU.mult, None, [lk, ohk], [sk_])
            for g in range(1, 4):
                STT(esel, lg[:, 4 + 8 * g:12 + 8 * g], oh[:, g:g + 1], esel, ALU.mult, ALU.add, [lk, ohk, sk_], [sk_])
            P.op("dve", lambda e: e.max(out=top8, in_=esel), reads=[sk_], writes=[sk_])
            TT("dve", d21, top8[:, 1:2], top8[:, 0:1], ALU.subtract, [sk_], [sk_])
            ACT(d21, d21, AF.Exp, [sk_], [sk_])
            TS("dve", d21, d21, 1.0, None, ALU.add, None, [sk_], [sk_])
            P.op("dve", lambda e: e.reciprocal(out=w1g, in_=d21), reads=[sk_], writes=[sk_])
            TT("dve", w1g, w1g, gw, ALU.mult, [sk_], [sk_])
            TT("dve", w2g, gw, w1g, ALU.subtract, [sk_], [sk_])
            TS("dve", wa, esel, top8[:, 0:1], w1g, ALU.is_equal, ALU.mult, [sk_], [sk_])
            TS("dve", wb_, esel, top8[:, 1:2], w2g, ALU.is_equal, ALU.mult, [sk_], [sk_])
            TT("dve", WE[:, io, :], wa, wb_, ALU.add, [sk_], ["WE%d" % io])

        for m in range(0, NTO, 2):
            ra, rb = [], []
            P.rec = ra
            stream[0] = 0
            c1_body(m)
            P.rec = rb
            stream[0] = 1
            c1_body(m + 1)
            P.rec = None
            stream[0] = None
            P.replay_merged(ra, rb)

        for io in range(NTO):
            oh = OH[:, io, :]
            ohk = "OH%d" % io
            prk_t, prkk = next_pf()
            P.op("pe", lambda e, t=prk_t, io=io: (e.matmul(t[:, 0:4], lhsT=cm[:, 4, :], rhs=OH[:, io, :], start=True, stop=False),
                                                   e.matmul(t[:, 0:4], lhsT=cm[:, 7, :], rhs=pren, start=False, stop=True))[1],
                 reads=[ohk, "pren", "cm"], writes=[prkk])
            rk = smr[:, 0:4]
            okm = smr[:, 4:8]
            TS("dve", rk, prk_t[:, 0:4], -16.0, None, ALU.mult, None, [prkk], ["smr"])
            STT(pren, oh, -1.0 / 16.0, pren, ALU.mult, ALU.add, [ohk, "pren", prkk], ["pren"])
            TS("dve", okm, rk, float(CAPG), None, ALU.is_lt, None, ["smr"], ["smr"])
            TT("dve", okm, okm, oh, ALU.mult, ["smr", ohk], ["smr"])
            TT("dve", rk, rk, goffm, ALU.add, ["smr", "goffm"], ["smr"])
            TT("dve", rk, rk, okm, ALU.mult, ["smr"], ["smr"])
            P.op("dve", lambda e, io=io, rk=rk: e.tensor_reduce(out=idxf[:, io:io + 1], in_=rk, axis=AX.X, op=ALU.add), reads=["smr"], writes=["idxf%d" % io])
            TS("dve", idxf[:, io:io + 1], idxf[:, io:io + 1], OOB, None, ALU.add, None, ["idxf%d" % io], ["idxf%d" % io])
            CP("dve", idxi[:, io:io + 1], idxf[:, io:io + 1], ["idxf%d" % io], ["idxi%d" % io])
            P.op("pool", lambda e, io=io: e.indirect_dma_start(out=XB[:, :], out_offset=bass.IndirectOffsetOnAxis(ap=idxi[:, io:io + 1], axis=0),
                                                               in_=u2tok[:, io, :], in_offset=None, bounds_check=bcreg(e), oob_is_err=False),
                 reads=["u2tok%d" % io, "idxi%d" % io, "XB"], writes=["XBs%d" % io], dma=True)
            P.op("pool", lambda e, io=io: e.indirect_dma_start(out=WB[:, :], out_offset=bass.IndirectOffsetOnAxis(ap=idxi[:, io:io + 1], axis=0),
                                                               in_=WE[:, io, :], in_offset=None, bounds_check=bcreg(e), oob_is_err=False),
                 reads=["WE%d" % io, "idxi%d" % io, "WB"], writes=["WBs%d" % io], dma=True)
        H2K = ["H2_%d" % i for i in range(NTO)]
        XBK = ["XBs%d" % i for i in range(NTO)] + ["XB"]
        WBK = ["WBs%d" % i for i in range(NTO)] + ["WB"]
        if debug:
            DMA("sp", WTD[:, 0:NTO], idxf, ["idxf%d" % i for i in range(NTO)], ["WTD"])
        if stop_after == "C1":
            P.op("sp", None, reads=H2K + XBK + WBK + ["WTD"], writes=[])
            P.emit()
            return nc

        P.barrier()
        AR.off = M2
        NCH = CAPG // 128
        xs = sb("xs", [128, NCH, D], BF16)
        xTg = sb("xTg", [128, 8, CAPG], BF16)
        wsl = sb("wsl", [128, NCH, 8])
        hid = sb("hid", [128, 4, CAPG], BF16)
        yacc = sb("yacc", [128, NCH, D])
        wgb = [sb("wgb%d" % i, [128, 8, 512], BF16) for i in range(2)]
        wub = [sb("wub%d" % i, [128, 8, 512], BF16) for i in range(2)]
        wdb = [sb("wdb%d" % i, [128, 4, D], BF16) for i in range(2)]
        sgb = [sb("sgb%d" % i, [128, 512]) for i in range(2)]

        def load_expert(ex):
            b = ex % 2
            DMA("pool", wgb[b], ewg[ex].rearrange("(c p) n -> p c n", p=128), [], ["wgb%d" % b])
            DMA("pool", wub[b], ewu[ex].rearrange("(c p) n -> p c n", p=128), [], ["wub%d" % b])
            DMA("pool", wdb[b], ewd[ex].rearrange("(c p) n -> p c n", p=128), [], ["wdb%d" % b])
        load_expert(0)
        load_expert(1)
        hid2 = [hid, sb("hidB", [128, 4, CAPG], BF16)]
        kk2 = [0]
        nsl = [(0, 512), (512, CAPG)]

        def GU(ex, hb, XTK):
            b = ex % 2
            for (n0, n1) in nsl:
                for fc in range(4):
                    pg, pgk = next_pf()
                    pu, puk = next_pf()
                    mm_group(pg[:, 0:n1 - n0], [(wgb[b][:, c, fc * 128:(fc + 1) * 128], xTg[:, c, n0:n1]) for c in range(8)], pgk,
                             ["wgb%d" % b] + XTK)
                    mm_group(pu[:, 0:n1 - n0], [(wub[b][:, c, fc * 128:(fc + 1) * 128], xTg[:, c, n0:n1]) for c in range(8)], puk,
                             ["wub%d" % b] + XTK)
                    sb_i = kk2[0] % 2
                    kk2[0] += 1
                    ACT(sgb[sb_i][:, 0:n1 - n0], pg[:, 0:n1 - n0], AF.Silu, [pgk], ["sgb%d" % sb_i])
                    TT("dve", hid2[hb][:, fc, n0:n1], sgb[sb_i][:, 0:n1 - n0], pu[:, 0:n1 - n0], ALU.mult, ["sgb%d" % sb_i, puk],
                       ["hid%d_%d_%d" % (hb, fc, n0)])

        def DN(ex, hb, el):
            b = ex % 2
            HK = ["hid%d_%d_%d" % (hb, fc, n0) for fc in range(4) for (n0, _) in nsl]
            for ch in range(NCH):
                for cg in range(2):
                    py, pyk = next_pf()
                    mm_group(py[:, :], [(hid2[hb][:, fc, ch * 128:(ch + 1) * 128], wdb[b][:, fc, cg * 512:(cg + 1) * 512]) for fc in range(4)], pyk,
                             ["wdb%d" % b] + HK)
                    ya = yacc[:, ch, cg * 512:(cg + 1) * 512]
                    yk = "yacc%d" % ch
                    if el == 0:
                        TS("dve", ya, py[:, :], wsl[:, ch, el:el + 1], None, ALU.mult, None, [pyk, "wsl"], [yk])
                    else:
                        STT(ya, py[:, :], wsl[:, ch, el:el + 1], ya, ALU.mult, ALU.add, [pyk, "wsl", yk], [yk])

        for g in range(4):
            DMA("sp", xs, XB[g * CAPG:(g + 1) * CAPG, :].rearrange("(c p) d -> p c d", p=128), XBK, ["xs"])
            DMA("sp", wsl, WB[g * CAPG:(g + 1) * CAPG, :].rearrange("(c p) d -> p c d", p=128), WBK, ["wsl"])
            for ch in range(NCH):
                pbt, pbk = next_pb()

                def trx_fn(e, pbt=pbt, ch=ch):
                    ins = None
                    for c in range(8):
                        ins = e.transpose(out=pbt[:, c * 128:(c + 1) * 128], in_=xs[:, ch, c * 128:(c + 1) * 128], identity=identb)
                    return ins
                P.op("pe", trx_fn, reads=["xs", "identb"], writes=[pbk])
                CP("act" if ch % 2 else "dve", xTg[:, :, ch * 128:(ch + 1) * 128], pbt[:, :].rearrange("p (c t) -> p c t", c=8), [pbk], ["xTg%d" % ch])
            XTK = ["xTg%d" % ch for ch in range(NCH)]
            GU(g * 8, 0, XTK)
            for el in range(8):
                ex = g * 8 + el
                if ex + 2 < NEXP:
                    b_ = ex % 2
                    DMA("pool", wgb[b_], ewg[ex + 2].rearrange("(c p) n -> p c n", p=128), [], ["wgb%d" % b_])
                    DMA("pool", wub[b_], ewu[ex + 2].rearrange("(c p) n -> p c n", p=128), [], ["wub%d" % b_])
                ra, rb = [], []
                if el + 1 < 8:
                    P.rec = ra
                    stream[0] = 0
                    GU(ex + 1, (el + 1) % 2, XTK)
                P.rec = rb
                stream[0] = 1
                DN(ex, el % 2, el)
                P.rec = None
                stream[0] = None
                P.replay_merged(ra, rb)
                if ex + 2 < NEXP:
                    DMA("pool", wdb[ex % 2], ewd[ex + 2].rearrange("(c p) n -> p c n", p=128), [], ["wdb%d" % (ex % 2)])
            DMA("sp", YB[g * CAPG:(g + 1) * CAPG, :].rearrange("(c p) d -> p c d", p=128), yacc, ["yacc%d" % ch for ch in range(NCH)], ["YB%d" % g])
        YBK = ["YB%d" % g for g in range(4)]
        P.barrier()
        AR.off = M2
        hl = [sb("hl%d" % i, [128, D]) for i in range(2)]
        yg = [sb("yg%d" % i, [128, D]) for i in range(2)]
        ob = [sb("ob%d" % i, [128, D]) for i in range(2)]
        junk3 = sb("junk3", [128, D], BF16)
        ss3 = sb("ss3", [128, 2])
        rs3 = sb("rs3", [128, 2])
        for io in range(NTO):
            b2 = io % 2
            DMA("sp", hl[b2], H2[io * 128:(io + 1) * 128, :], ["H2_%d" % io], ["hl%d" % b2])
            P.op("pool", lambda e, b2=b2: e.memset(yg[b2], 0.0), writes=["yg%d" % b2])
            P.op("pool", lambda e, io=io, b2=b2: e.indirect_dma_start(out=yg[b2], out_offset=None, in_=YB[:, :],
                                                                       in_offset=bass.IndirectOffsetOnAxis(ap=idxi[:, io:io + 1], axis=0),
                                                                       bounds_check=bcreg(e), oob_is_err=False),
                 reads=YBK + ["idxi%d" % io], writes=["yg%d" % b2], dma=True)
            TT("dve", hl[b2], hl[b2], yg[b2], ALU.add, ["hl%d" % b2, "yg%d" % b2], ["hl%d" % b2])
            P.op("act", lambda e, b2=b2: e.activation(out=junk3, in_=hl[b2], func=AF.Square, accum_out=ss3[:, 0:1]),
                 reads=["hl%d" % b2], writes=["junk3", "ss3"])
            rstd_from_ssq(rs3[:, 0:1], ss3[:, 0:1], D, "ss3", "rs3")
            STT(ob[b2], hl[b2], rs3[:, 0:1], fnbc, ALU.mult, ALU.mult, ["hl%d" % b2, "rs3", "fnbc"], ["ob%d" % b2])
            DMA("sp", out_d[io * 128:(io + 1) * 128, :], ob[b2], ["ob%d" % b2], ["OUT%d" % io])
        P.op("sp", None, reads=["OUT%d" % i for i in range(NTO)], writes=[])
        P.emit()
    return nc


def _consts():
    s = np.arange(128)[:, None]
    t = np.arange(128)[None, :]
    cm = np.zeros((128, 8, 128), np.float32)
    cm[:, 0] = (s == t)
    cm[:, 1] = (s <= t) / -16.0
    cm[:, 2] = (s >= t) / -16.0
    cm[:, 3] = (s > t) / -16.0
    cm[:, 4] = (s < t) / -16.0
    cm[:, 5] = (s <= t)
    cm[:, 6] = (s >= t)
    cm[:, 7] = 1.0
    band = np.zeros((128, 384), np.float32)
    band[:, 0:128] = (s >= t + 64)
    band[:, 128:256] = (np.abs(s - t) <= 64)
    band[:, 256:384] = (s <= t - 64)
    return cm, band


def make_in_maps(inputs):
    f = lambda a: np.ascontiguousarray(np.asarray(a, dtype=np.float32))
    x = f(inputs["x"])
    cm, band = _consts()
    wz = np.zeros((33, 512), np.float32)
    wz[0:16, 0:256] = f(inputs["gla_fwd_gate_w"])[0]
    wz[16:32, 256:512] = f(inputs["gla_bwd_gate_w"])[0]
    wz[32, 0:256] = f(inputs["gla_fwd_gate_b"])[0]
    wz[32, 256:512] = f(inputs["gla_bwd_gate_b"])[0]
    vecs = np.zeros((4, D), np.float32)
    vecs[0] = f(inputs["norm1_w"])[0]
    vecs[1] = f(inputs["norm2_w"])[0]
    vecs[2] = f(inputs["final_norm_w"])
    vecs[3] = np.tile(f(inputs["gla_norm_w"])[0], 8)
    wr = np.concatenate([f(inputs["router_group_w"])[0]] + [f(inputs["router_expert_w"])[0, g] for g in range(4)], axis=1)
    rb = np.concatenate([f(inputs["router_group_b"])[0], f(inputs["router_expert_b"])[0].reshape(-1)])[None, :]
    inv = (500000.0 ** (-(np.arange(0, 16, 2, dtype=np.float32) / np.float32(16)))).astype(np.float32)
    shared = dict(cmat=cm, band3=band, w_in=f(inputs["w_in"])[0], wz=wz, vecs=vecs, w_out=f(inputs["w_out"])[0],
                  wr=np.ascontiguousarray(wr), rb=np.ascontiguousarray(rb), ewg=f(inputs["expert_w_gate"])[0],
                  ewu=f(inputs["expert_w_up"])[0], ewd=f(inputs["expert_w_down"])[0])
    maps = []
    for c in range(8):
        b, q = c // 4, c % 4
        s0 = q * OWN
        pos = np.arange(s0 - HALO, s0 + OWN + HALO)
        valid = (pos >= 0) & (pos < S)
        xwin = np.zeros((WIN, D), np.float32)
        xwin[valid] = x[b, pos[valid]]
        ang = (pos.astype(np.float32)[:, None] * inv[None, :]).astype(np.float32)
        cs = np.concatenate([np.cos(ang), np.sin(ang)], axis=1).astype(np.float32)
        cs_t = np.ascontiguousarray(cs.reshape(NTW, 128, 16).transpose(1, 0, 2))
        vcol = np.ascontiguousarray(valid.astype(np.float32).reshape(NTW, 128).T)
        m = dict(shared)
        m.update(xw=xwin, vcol=vcol, cs_t=cs_t)
        maps.append(m)
    return maps


_NC_CACHE = {}


def kernel(**inputs):
    maps = make_in_maps(inputs)
    if "nc" not in _NC_CACHE:
        _NC_CACHE["nc"] = build_program()
    nc = _NC_CACHE["nc"]
    res = run_bass_kernel_spmd(nc, maps, core_ids=list(range(8)))
    out = np.zeros((2, S, D), np.float32)
    for c in range(8):
        b, q = c // 4, c % 4
        out[b, q * OWN:(q + 1) * OWN] = res.results[c]["out"]
    return out
```

```python
import numpy as np
from contextlib import ExitStack
import concourse.bass as bass
import concourse.mybir as mybir
from concourse.bass_utils import run_bass_kernel_spmd

F32 = mybir.dt.float32
BF16 = mybir.dt.bfloat16
I32 = mybir.dt.int32
AF = mybir.ActivationFunctionType
ALU = mybir.AluOpType
AX = mybir.AxisListType

ENGS = ("pe", "act", "dve", "pool", "sp")
EPOCH = 4096
DMA_SLOTS = 8

D = 1024
S = 8192
OWN = 2048
HALO = 1024
WIN = OWN + 2 * HALO
NTW = WIN // 128
T0 = HALO // 128
NTO = OWN // 128
INW = 3104
NEXP = 32
EPS = 1e-6


class Op:
    __slots__ = ("eng", "fn", "dma", "deps", "sig", "sigcount", "dmaidx", "idx")

    def __init__(self, eng, fn, dma):
        self.eng = eng
        self.fn = fn
        self.dma = dma
        self.deps = []
        self.sig = False
        self.sigcount = 0
        self.dmaidx = -1
        self.idx = -1


class Prog:
    def __init__(self, nc):
        self.nc = nc
        self.ops = []
        self.last_w = {}
        self.readers = {}
        self.ndma = {e: 0 for e in ENGS}
        self.bar = None
        self.rec = None

    def barrier(self):
        deps = set()
        for e in ENGS:
            last = None
            nd = 0
            for o in reversed(self.ops):
                if o.eng != e:
                    continue
                if o.dma:
                    if nd < DMA_SLOTS:
                        deps.add(o.idx)
                        nd += 1
                elif last is None:
                    last = o.idx
                    deps.add(o.idx)
                if last is not None and nd >= DMA_SLOTS:
                    break
        b = self.op("sp", None)
        b.deps = sorted(deps | set(b.deps))
        self.bar = b.idx
        return b

    def replay_merged(self, a, b):
        na, nb = len(a), len(b)
        i = j = 0
        while i < na or j < nb:
            if j >= nb or (i < na and i * nb <= j * na):
                self.op(*a[i])
                i += 1
            else:
                self.op(*b[j])
                j += 1

    def op(self, eng, fn, reads=(), writes=(), dma=False):
        if self.rec is not None:
            self.rec.append((eng, fn, list(reads), list(writes), dma))
            return None
        import os as _os
        mx = int(_os.environ.get("DBG_MAXOPS", "0"))
        if mx and len(self.ops) >= mx and fn is not None:
            fn = None
            if dma:
                dma = False
        px = [k_ for k_ in reads if k_[:2] in ("pf", "pb")]
        if px:
            writes = list(writes) + [k_ for k_ in px if k_ not in writes]
            reads = [k_ for k_ in reads if k_ not in px]
        o = Op(eng, fn, dma)
        o.idx = len(self.ops)
        deps = set()
        if self.bar is not None:
            deps.add(self.bar)
        for k in reads:
            w = self.last_w.get(k)
            if w is not None:
                deps.add(w)
        for k in writes:
            w = self.last_w.get(k)
            if w is not None:
                deps.add(w)
            for r in self.readers.get(k, ()):
                deps.add(r)
        deps.discard(o.idx)
        o.deps = sorted(deps)
        for k in writes:
            self.last_w[k] = o.idx
            self.readers[k] = []
        for k in reads:
            if k not in writes:
                self.readers.setdefault(k, []).append(o.idx)
        if dma:
            o.dmaidx = self.ndma[eng]
            self.ndma[eng] += 1
        self.ops.append(o)
        return o

    def emit(self):
        nc = self.nc
        ops = self.ops
        for o in ops:
            for d in o.deps:
                p = ops[d]
                if not p.dma:
                    p.sig = True
        cnt = {e: 0 for e in ENGS}
        for o in ops:
            if o.sig and not o.dma:
                cnt[o.eng] += 1
                o.sigcount = cnt[o.eng]
        nsem = {e: (cnt[e] + EPOCH - 1) // EPOCH for e in ENGS}
        with ExitStack() as es:
            csem = {e: [es.enter_context(nc.semaphore("c_%s_%d" % (e, i))) for i in range(nsem[e])]
                    for e in ENGS}
            dsem = {e: [es.enter_context(nc.semaphore("d_%s_%d" % (e, i)))
                        for i in range(DMA_SLOTS if self.ndma[e] else 0)] for e in ENGS}
            block = es.enter_context(nc.Block())

            def body_for(e):
                def body(eng):
                    waited_c = {x: 0 for x in ENGS}
                    waited_d = {}
                    for o in ops:
                        if o.eng != e:
                            continue
                        need_c = {}
                        need_d = {}
                        for d in o.deps:
                            p = ops[d]
                            if p.dma:
                                slot = p.dmaidx % DMA_SLOTS
                                val = 16 * (p.dmaidx // DMA_SLOTS + 1)
                                key = (p.eng, slot)
                                if waited_d.get(key, 0) < val:
                                    need_d[key] = max(need_d.get(key, 0), val)
                            else:
                                if waited_c[p.eng] < p.sigcount:
                                    need_c[p.eng] = max(need_c.get(p.eng, 0), p.sigcount)
                        if o.dma:
                            slot = o.dmaidx % DMA_SLOTS
                            val = 16 * (o.dmaidx // DMA_SLOTS)
                            key = (e, slot)
                            if val > 0 and waited_d.get(key, 0) < val:
                                need_d[key] = max(need_d.get(key, 0), val)
                        for pe_, c in need_c.items():
                            ep = (c - 1) // EPOCH
                            eng.wait_ge(csem[pe_][ep], (c - 1) % EPOCH + 1)
                            waited_c[pe_] = c
                        for key, val in need_d.items():
                            eng.wait_ge(dsem[key[0]][key[1]], val)
                            waited_d[key] = val
                        ins = o.fn(eng) if o.fn is not None else None
                        if o.dma:
                            ins.then_inc(dsem[e][o.dmaidx % DMA_SLOTS], 16)
                        elif o.sig:
                            if ins is None:
                                ins = eng.nop()
                            ep = (o.sigcount - 1) // EPOCH
                            ins.then_inc(csem[e][ep], 1)
                return body

            block.tensor(body_for("pe"))
            block.scalar(body_for("act"))
            block.vector(body_for("dve"))
            block.gpsimd(body_for("pool"))
            block.sync(body_for("sp"))


class Arena:
    def __init__(self, ap, ncols):
        self.ap = ap
        self.n = ncols
        self.off = 0

    def alloc(self, shape, dt=F32):
        p = shape[0]
        rest = list(shape[1:])
        nel = 1
        for r in rest:
            nel *= r
        ncol = nel if dt in (F32, I32) else (nel + 1) // 2
        ncol += ncol % 2
        assert self.off + ncol <= self.n, "arena overflow: need %d have %d" % (ncol, self.n - self.off)
        v = self.ap[0:p, self.off:self.off + ncol]
        self.off += ncol
        if dt != F32:
            v = v.bitcast(dt)
        if v.shape[1] != nel:
            v = v[:, 0:nel]
        if len(rest) == 2:
            v = v.rearrange("p (a b) -> p a b", a=rest[0])
        elif len(rest) == 3:
            v = v.rearrange("p (a b c) -> p a b c", a=rest[0], b=rest[1])
        return v


def build_program(debug=False, stop_after=None, dbg_tiles=None):
    nc = bass.Bass("TRN2", target_bir_lowering=False)
    P = Prog(nc)
    global LASTP
    LASTP = P

    def din(name, shape, dt=F32):
        return nc.dram_tensor(name, list(shape), dt, kind="ExternalInput").ap()

    def dscr(name, shape, dt):
        kind = "ExternalOutput" if debug else "Internal"
        return nc.dram_tensor(name, list(shape), dt, kind=kind).ap()

    xw = din("xw", [WIN, D])
    vcol = din("vcol", [128, NTW])
    cs_t = din("cs_t", [128, NTW, 16])
    cmat = din("cmat", [128, 8, 128])
    band3 = din("band3", [128, 384])
    w_in = din("w_in", [D, INW])
    wz_d = din("wz", [33, 512])
    vecs = din("vecs", [4, D])
    w_out = din("w_out", [D, D])
    wr_d = din("wr", [D, 36])
    rb_d = din("rb", [1, 36])
    ewg = din("ewg", [NEXP, D, 512])
    ewu = din("ewu", [NEXP, D, 512])
    ewd = din("ewd", [NEXP, 512, D])
    out_d = nc.dram_tensor("out", [OWN, D], F32, kind="ExternalOutput").ap()
    QS = dscr("QS", [OWN, 512], BF16)
    KS = dscr("KS", [WIN + 2 * HALO, 512], BF16)
    VS = dscr("VS", [WIN + 2 * HALO, 520], BF16)
    GV = dscr("GV", [OWN, 512], BF16)
    GG = dscr("GG", [OWN, 512], BF16)
    MG = dscr("MG", [OWN, 512], BF16)
    OTS = dscr("OTS", [8, 64, OWN], BF16)
    H2 = dscr("H2", [OWN, D], F32)
    XB = dscr("XB", [2560, D], BF16)
    WB = dscr("WB", [2560, 8], F32)
    YB = dscr("YB", [2560, D], F32)
    WTD = nc.dram_tensor("WTD", [128, NTO * 32], F32, kind="ExternalOutput").ap() if debug else None

    QSK = ["QS%d" % i for i in range(NTO)]
    KSK = ["KS%d" % i for i in range(48)]
    VSK = ["VS%d" % i for i in range(48)]
    NCOL = 50 * 1024 + 512
    es = ExitStack()
    with es:
        arena_t = es.enter_context(nc.sbuf_tensor("arena", [128, NCOL], F32))
        AR = Arena(arena_t[:], NCOL)
        sb = lambda name, shape, dt=F32: AR.alloc(shape, dt)

        def ps(name, shape, dt=F32):
            return es.enter_context(nc.psum_tensor("p_" + name, list(shape), dt))

        pf = [ps("pf%d" % i, [128, 512]) for i in range(6)]
        pb = [ps("pb%d" % i, [128, 1024], BF16) for i in range(2)]
        pf_rr = [0]
        pb_rr = [0]

        stream = [None]
        srr = [0, 0]

        def next_pf():
            if stream[0] is None:
                i = pf_rr[0] % 6
                pf_rr[0] += 1
            else:
                s_ = stream[0]
                i = 3 * s_ + srr[s_] % 3
                srr[s_] += 1
            return pf[i], "pf%d" % i

        def next_pb():
            if stream[0] is None:
                i = pb_rr[0] % 2
                pb_rr[0] += 1
            else:
                i = stream[0]
            return pb[i], "pb%d" % i

        def mm_group(out_ap, pairs, okey, rkeys):
            def fn(e):
                ins = None
                n = len(pairs)
                for j, (l, r) in enumerate(pairs):
                    ins = e.matmul(out_ap, lhsT=l, rhs=r, start=(j == 0), stop=(j == n - 1))
                return ins
            P.op("pe", fn, reads=rkeys, writes=[okey])

        def ACT(out, in_, func, reads, writes, **kw):
            P.op("act", lambda e: e.activation(out=out, in_=in_, func=func, **kw), reads=reads, writes=writes)

        def TT(eng, out, in0, in1, op, reads, writes):
            P.op(eng, lambda e: e.tensor_tensor(out=out, in0=in0, in1=in1, op=op), reads=reads, writes=writes)

        def STT(out, in0, scalar, in1, op0, op1, reads, writes):
            P.op("dve", lambda e: e.scalar_tensor_tensor(out=out, in0=in0, scalar=scalar, in1=in1, op0=op0, op1=op1),
                 reads=reads, writes=writes)

        def TS(eng, out, in0, s1, s2, op0, op1, reads, writes):
            if op1 is None:
                P.op(eng, lambda e: e.tensor_scalar(out=out, in0=in0, scalar1=s1, scalar2=None, op0=op0), reads=reads, writes=writes)
            else:
                P.op(eng, lambda e: e.tensor_scalar(out=out, in0=in0, scalar1=s1, scalar2=s2, op0=op0, op1=op1), reads=reads, writes=writes)

        def CP(eng, out, in_, reads, writes):
            if eng == "act":
                ACT(out, in_, AF.Copy, reads, writes)
            else:
                P.op(eng, lambda e: e.tensor_copy(out=out, in_=in_), reads=reads, writes=writes)

        def DMA(q, out, in_, reads, writes):
            return P.op(q, lambda e: e.dma_start(out=out, in_=in_), reads=reads, writes=writes, dma=True)

        def rstd_from_ssq(dst, src, n, rk, wk):
            ACT(dst, src, AF.Ln, [rk, "epsc"], [wk], scale=1.0 / n, bias=epsc[0:dst.shape[0], :])
            ACT(dst, dst, AF.Exp, [wk], [wk], scale=-0.5)

        cm = sb("cm", [128, 8, 128])
        identb = sb("identb", [128, 128], BF16)
        band = sb("band", [128, 384], BF16)
        maskFB = sb("maskFB", [128, 4, 128])
        n16col = sb("n16col", [128, 2])
        epsc = sb("epsc", [128, 2])
        onec = sb("onec", [128, 2])
        negc = sb("negc", [128, 2])
        vc = sb("vc", [128, NTW])
        cst = sb("cst", [128, NTW, 16])
        wz = sb("wz", [33, 512])
        n1bc = sb("n1bc", [128, D])
        n2bc = sb("n2bc", [128, D])
        fnbc = sb("fnbc", [128, D])
        gnbc = sb("gnbc", [128, 512])
        rbbc = sb("rbbc", [128, 36])
        wr = sb("wr", [128, 8, 36])
        nmax = sb("nmax", [128, 16])
        n16col = n16col[:, 0:1]
        epsc = epsc[:, 0:1]
        onec = onec[:, 0:1]
        negc = negc[:, 0:1]

        DMA("sp", cm, cmat, [], ["cm"])
        DMA("pool", identb, cmat[:, 0, :], [], ["identb"])
        DMA("pool", band, band3, [], ["band"])
        DMA("sp", vc, vcol, [], ["vc"])
        DMA("sp", cst, cs_t, [], ["cst"])
        DMA("sp", wz, wz_d, [], ["wz"])
        DMA("sp", n1bc, vecs[0:1, :].partition_broadcast(128), [], ["n1bc"])
        DMA("sp", n2bc, vecs[1:2, :].partition_broadcast(128), [], ["n2bc"])
        DMA("sp", fnbc, vecs[2:3, :].partition_broadcast(128), [], ["fnbc"])
        DMA("sp", gnbc, vecs[3:4, 0:512].partition_broadcast(128), [], ["gnbc"])
        DMA("sp", rbbc, rb_d[0:1, :].partition_broadcast(128), [], ["rbbc"])
        DMA("sp", wr, wr_d.rearrange("(c p) n -> p c n", p=128), [], ["wr"])
        P.op("dve", lambda e: e.memset(n16col, -1.0 / 16.0), writes=["n16col"])
        P.op("dve", lambda e: e.memset(epsc, EPS), writes=["epsc"])
        P.op("dve", lambda e: e.memset(onec, 1.0), writes=["onec"])
        P.op("dve", lambda e: e.memset(nmax, 0.0), writes=["nmax"])
        for h in range(4):
            CP("dve", maskFB[:, h, :], cm[:, 5 + h // 2, :], ["cm"], ["maskFB"])
        M0 = AR.off

        attnT = sb("attnT", [128, NTO, 512], BF16)
        qdT = sb("qdT", [128, NTO, 4, 128], BF16)
        SfT = sb("SfT", [128, NTO, 2, 128], BF16)
        SbT = sb("SbT", [128, NTO, 2, 128], BF16)
        M1 = AR.off
        win = sb("win", [128, 8, INW], BF16)
        for c in range(8):
            DMA("pool", win[:, c, :], w_in[c * 128:(c + 1) * 128, :], [], ["win%d" % c])
        winkeys = ["win%d" % c for c in range(8)]
        kvB = sb("kvB", [128, NTW - T0, 2, 128], BF16)
        decB = sb("decB", [128, NTW - T0, 2])
        Sf = sb("Sf", [128, 2, 128])
        Sb = sb("Sb", [128, 2, 128])
        P.op("dve", lambda e: e.memset(Sf, 0.0), writes=["Sf"])
        P.op("dve", lambda e: e.memset(Sb, 0.0), writes=["Sb"])
        zt = sb("zt", [128, 520], BF16)
        P.op("pool", lambda e: e.memset(zt, 0.0), writes=["zt"])
        for blk in range(HALO // 128):
            for base in (0, HALO + WIN):
                r0 = base + blk * 128
                DMA("sp", KS[r0:r0 + 128, :], zt[:, 0:512], ["zt"], ["KS%d" % (r0 // 128)])
                DMA("sp", VS[r0:r0 + 128, :], zt, ["zt"], ["VS%d" % (r0 // 128)])
        xt = [sb("xt%d" % i, [128, D]) for i in range(2)]
        junk = sb("junk", [128, D], BF16)
        xn = [sb("xn%d" % i, [128, D], BF16) for i in range(2)]
        xnT = [sb("xnT%d" % i, [128, 8, 128], BF16) for i in range(2)]
        ssq = sb("ssq", [128, 2])
        rstd = sb("rstd", [128, 2])
        qk = [sb("qk%d" % i, [128, 512]) for i in range(2)]
        vbf = [sb("vbf%d" % i, [128, 512], BF16) for i in range(2)]
        gbf = [sb("gbf%d" % i, [128, 512], BF16) for i in range(2)]
        lr = [sb("lr%d" % i, [128, 32]) for i in range(2)]
        aqr = [sb("aqr%d" % i, [128, 8, 64]) for i in range(2)]
        akr = [sb("akr%d" % i, [128, 8, 64]) for i in range(2)]
        vab = [sb("vab%d" % i, [128, 8, 65], BF16) for i in range(2)]
        lrT = sb("lrT", [33, 128])
        ez = sb("ez", [128, 512])
        spl = sb("spl", [128, 512])
        E1 = sb("E1", [128, 512])
        E2 = sb("E2", [128, 512])
        E3 = sb("E3", [128, 512])
        dec = sb("dec", [128, 4])
        qd = sb("qd", [128, 512], BF16)
        ki = sb("ki", [128, 512], BF16)
        ke = sb("ke", [128, 512], BF16)
        kiT = sb("kiT", [128, 4, 128], BF16)
        qrb = [sb("qrb%d" % i, [128, 512], BF16) for i in range(2)]
        krb = [sb("krb%d" % i, [128, 512], BF16) for i in range(2)]
        rta = sb("rta", [128, 8, 8])
        rtb = sb("rtb", [128, 8, 8])
        rtc = sb("rtc", [128, 8, 8])
        rtd = sb("rtd", [128, 8, 8])
        sqs = sb("sqs", [128, 8, 64])
        nrm = sb("nrm", [128, 16])
        P.op("dve", lambda e: e.memset(lrT[32:33, :], 1.0), writes=["lrT_one"])
        tiles = list(range(NTW) if dbg_tiles is None else dbg_tiles)

        def S1(i):
            own = T0 <= i < T0 + NTO
            io = i - T0
            b2 = i % 2
            xtk, xnk, xnTk = "xt%d" % b2, "xn%d" % b2, "xnT%d" % b2
            if i == tiles[0]:
                DMA("sp", xt[b2], xw[i * 128:(i + 1) * 128, :], [], [xtk])
            if i + 1 < NTW and (dbg_tiles is None):
                DMA("sp", xt[(i + 1) % 2], xw[(i + 1) * 128:(i + 2) * 128, :], [], ["xt%d" % ((i + 1) % 2)])
            sk, rk = "ssq%d" % b2, "rstd%d" % b2
            P.op("act", lambda e, b2=b2: e.activation(out=junk, in_=xt[b2], func=AF.Square, accum_out=ssq[:, b2:b2 + 1]),
                 reads=[xtk], writes=["junk", sk])
            rstd_from_ssq(rstd[:, b2:b2 + 1], ssq[:, b2:b2 + 1], D, sk, rk)
            STT(xn[b2], xt[b2], rstd[:, b2:b2 + 1], n1bc, ALU.mult, ALU.mult, [xtk, rk, "n1bc"], [xnk])
            pbt, pbk = next_pb()

            def tr_fn(e, b2=b2, pbt=pbt):
                ins = None
                for c in range(8):
                    ins = e.transpose(out=pbt[:, c * 128:(c + 1) * 128], in_=xn[b2][:, c * 128:(c + 1) * 128], identity=identb)
                return ins
            P.op("pe", tr_fn, reads=[xnk, "identb"], writes=[pbk])
            CP("act", xnT[b2].rearrange("p c t -> p (c t)"), pbt[:, :], [pbk], [xnTk])

            def proj(c0, c1):
                pt, pk = next_pf()
                n = c1 - c0
                mm_group(pt[:, 0:n], [(xnT[b2][:, c, :], win[:, c, c0:c1]) for c in range(8)], pk, [xnTk] + winkeys)
                return pt, pk

            if own:
                pt, pk = proj(0, 512)
                CP("act", qk[b2], pt[:, 0:512], [pk], ["qk%d" % b2])
            else:
                pt, pk = proj(256, 512)
                CP("act", qk[b2][:, 256:512], pt[:, 0:256], [pk], ["qk%d" % b2])
            pt, pk = proj(512, 1024)
            CP("dve", vbf[b2], pt[:, 0:512], [pk], ["vbf%d" % b2])
            if own:
                DMA("sp", GV[io * 128:(io + 1) * 128, :], vbf[b2], ["vbf%d" % b2], ["GV%d" % io])
                pt, pk = proj(1024, 1536)
                CP("act", gbf[b2], pt[:, 0:512], [pk], ["gbf%d" % b2])
                DMA("sp", GG[io * 128:(io + 1) * 128, :], gbf[b2], ["gbf%d" % b2], ["GG%d" % io])
            pt, pk = proj(1536, 1568)
            CP("dve", lr[b2], pt[:, 0:32], [pk], ["lr%d" % b2])
            if own:
                pt, pk = proj(1568, 2080)
                CP("dve", aqr[b2].rearrange("p h d -> p (h d)"), pt[:, 0:512], [pk], ["aqr%d" % b2])
            pt, pk = proj(2080, 2592)
            CP("act", akr[b2].rearrange("p h d -> p (h d)"), pt[:, 0:512], [pk], ["akr%d" % b2])
            pt, pk = proj(2592, 3104)
            r0k = HALO + i * 128
            CP("act", vab[b2][:, :, 0:64], pt[:, 0:512].rearrange("p (h d) -> p h d", h=8), [pk], ["vab%d" % b2])
            CP("dve", vab[b2][:, :, 64:65], vc[:, i:i + 1].unsqueeze(1).broadcast_to([128, 8, 1]), ["vc"], ["vab%d" % b2])
            DMA("sp", VS[r0k:r0k + 128, :], vab[b2].rearrange("p h d -> p (h d)"), ["vab%d" % b2], ["VS%d" % (r0k // 128)])

        def S2(i):
            own = T0 <= i < T0 + NTO
            left = i < T0
            io = i - T0
            b2 = i % 2
            qkb = qk[b2]
            qkk = "qk%d" % b2
            vkey = "vbf%d" % b2
            vt = vbf[b2]
            pt, pk = next_pf()
            P.op("pe", lambda e, pt=pt: e.transpose(out=pt[0:32, 0:128], in_=lr[b2], identity=cm[:, 0, :]), reads=["lr%d" % b2, "cm"], writes=[pk])
            CP("dve", lrT[0:32, :], pt[0:32, 0:128], [pk], ["lrT"])
            pz, pzk = next_pf()
            P.op("pe", lambda e, pz=pz: e.matmul(pz[:, :], lhsT=lrT, rhs=wz, start=True, stop=True),
                 reads=["lrT", "lrT_one", "wz"], writes=[pzk])
            ACT(ez, pz[:, :], AF.Exp, [pzk], ["ez"], scale=-1.0)
            ACT(spl, ez, AF.Ln, ["ez", "onec"], ["spl"], bias=onec, scale=1.0)
            pbb, pbbk = next_pf()
            P.op("pe", lambda e, pbb=pbb: (e.matmul(pbb[:, 0:256], lhsT=cm[:, 1, :], rhs=spl[:, 0:256], start=True, stop=True),
                                           e.matmul(pbb[:, 256:512], lhsT=cm[:, 2, :], rhs=spl[:, 256:512], start=True, stop=True))[1],
                 reads=["cm", "spl"], writes=[pbbk])
            ACT(E1, pbb[:, :], AF.Exp, [pbbk], ["E1"])
            ACT(E2, pbb[:, :], AF.Exp, [pbbk], ["E2"], scale=-1.0)
            pb3, pb3k = next_pf()
            P.op("pe", lambda e, pb3=pb3: (e.matmul(pb3[:, 0:256], lhsT=cm[:, 3, :], rhs=spl[:, 0:256], start=True, stop=True),
                                           e.matmul(pb3[:, 256:512], lhsT=cm[:, 4, :], rhs=spl[:, 256:512], start=True, stop=True))[1],
                 reads=["cm", "spl"], writes=[pb3k])
            ACT(E3, pb3[:, :], AF.Exp, [pb3k], ["E3"])
            pdc, pdck = next_pf()

            def dec_fn(e, pdc=pdc):
                ins = None
                for j in range(4):
                    ins = e.matmul(pdc[:, j:j + 1], lhsT=spl[:, j * 128:(j + 1) * 128], rhs=n16col, start=True, stop=True)
                return ins
            P.op("pe", dec_fn, reads=["spl", "n16col"], writes=[pdck])
            ACT(dec, pdc[:, 0:4], AF.Exp, [pdck], ["dec"])
            if own:
                STT(qd[:, 0:256], qkb[:, 0:256], 0.125, E1[:, 0:256], ALU.mult, ALU.mult, [qkk, "E1"], ["qd"])
                STT(qd[:, 256:512], qkb[:, 0:256], 0.125, E1[:, 256:512], ALU.mult, ALU.mult, [qkk, "E1"], ["qd"])
                TT("pool", ki[:, 0:256], qkb[:, 256:512], E2[:, 0:256], ALU.mult, [qkk, "E2"], ["ki"])
                TT("pool", ki[:, 256:512], qkb[:, 256:512], E2[:, 256:512], ALU.mult, [qkk, "E2"], ["ki"])
            TT("pool", ke[:, 0:256], qkb[:, 256:512], E3[:, 0:256], ALU.mult, [qkk, "E3"], ["ke"])
            TT("pool", ke[:, 256:512], qkb[:, 256:512], E3[:, 256:512], ALU.mult, [qkk, "E3"], ["ke"])
            if own:
                pbt, pbk = next_pb()

                def tr2_fn(e, pbt=pbt):
                    ins = None
                    for j in range(4):
                        ins = e.transpose(out=pbt[:, j * 128:(j + 1) * 128], in_=qd[:, j * 128:(j + 1) * 128], identity=identb)
                    for j in range(4):
                        ins = e.transpose(out=pbt[:, 512 + j * 128:512 + (j + 1) * 128], in_=ki[:, j * 128:(j + 1) * 128], identity=identb)
                    return ins
                P.op("pe", tr2_fn, reads=["qd", "ki", "identb"], writes=[pbk])
                CP("act", qdT[:, io, :, :].rearrange("p c t -> p (c t)"), pbt[:, 0:512], [pbk], ["qdT%d" % io])
                CP("dve", kiT.rearrange("p c t -> p (c t)"), pbt[:, 512:1024], [pbk], ["kiT"])
                paX, paXk = next_pf()
                paY, paYk = next_pf()

                def att_fn(e, pa, par, io=io):
                    ins = None
                    p0 = par * 64
                    for dirn in range(2):
                        for pr in range(2):
                            blk = dirn * 2 + pr
                            sl = dirn * 2 + pr
                            ins = e.matmul(pa[:, sl * 128:(sl + 1) * 128], lhsT=kiT[p0:p0 + 64, blk, :], rhs=qdT[p0:p0 + 64, io, blk, :],
                                           start=True, stop=True)
                    return ins
                P.op("pe", lambda e, pa=paX, f=att_fn: f(e, pa, 0), reads=["kiT", "qdT%d" % io], writes=[paXk])
                P.op("pe", lambda e, pa=paY, f=att_fn: f(e, pa, 1), reads=["kiT", "qdT%d" % io], writes=[paYk])
                TT("dve", ez, paX[:, :], maskFB.rearrange("p h c -> p (h c)"), ALU.mult, [paXk, "maskFB"], ["ez"])
                TT("dve", E1, paY[:, :], maskFB.rearrange("p h c -> p (h c)"), ALU.mult, [paYk, "maskFB"], ["E1"])
                av = attnT[:, io, :].rearrange("p (a b c) -> p a b c", a=2, b=2)
                TT("pool", av[:, :, 0, :], ez[:, 0:256].rearrange("p (a c) -> p a c", a=2), ez[:, 256:512].rearrange("p (a c) -> p a c", a=2),
                   ALU.add, ["ez"], ["attnT%d" % io])
                TT("pool", av[:, :, 1, :], E1[:, 0:256].rearrange("p (a c) -> p a c", a=2), E1[:, 256:512].rearrange("p (a c) -> p a c", a=2),
                   ALU.add, ["E1"], ["attnT%d" % io])
            for dirn in range(2):
                if dirn == 0 and i >= T0 + NTO:
                    continue
                if dirn == 1 and left:
                    continue
                pkv, pkvk = next_pf()

                def kv_fn(e, pkv=pkv, dirn=dirn, vt=vt):
                    ins = None
                    for pr in range(2):
                        ins = e.matmul(pkv[:, pr * 256:(pr + 1) * 256], lhsT=ke[:, dirn * 256 + pr * 128: dirn * 256 + (pr + 1) * 128],
                                       rhs=vt[:, pr * 256:(pr + 1) * 256], start=True, stop=True)
                    return ins
                P.op("pe", kv_fn, reads=["ke", vkey], writes=[pkvk])
                if dirn == 0:
                    if own:
                        CP("act", SfT[:, io, :, :].rearrange("p a b -> p (a b)"), Sf.rearrange("p a b -> p (a b)"), ["Sf"], ["SfT%d" % io])
                    for pr in range(2):
                        for hh in range(2):
                            p0 = hh * 64
                            STT(Sf[p0:p0 + 64, pr, :], Sf[p0:p0 + 64, pr, :], dec[p0:p0 + 64, pr:pr + 1],
                                pkv[p0:p0 + 64, pr * 256 + hh * 128: pr * 256 + (hh + 1) * 128], ALU.mult, ALU.add,
                                ["Sf", "dec", pkvk], ["Sf"])
                else:
                    ib = i - T0
                    for pr in range(2):
                        for hh in range(2):
                            p0 = hh * 64
                            CP("act", kvB[p0:p0 + 64, ib, pr, :], pkv[p0:p0 + 64, pr * 256 + hh * 128: pr * 256 + (hh + 1) * 128],
                               [pkvk], ["kvB%d" % ib])
                    CP("dve", decB[:, ib, :], dec[:, 2:4], ["dec"], ["decB%d" % ib])

            def rope(raw, rkey):
                cosb = cst[:, i, 0:8].unsqueeze(1).broadcast_to([128, 8, 8])
                sinb = cst[:, i, 8:16].unsqueeze(1).broadcast_to([128, 8, 8])
                TT("pool", rta, raw[:, :, 0:8], cosb, ALU.mult, [rkey, "cst"], ["rta"])
                TT("pool", rtb, raw[:, :, 8:16], sinb, ALU.mult, [rkey, "cst"], ["rtb"])
                TT("pool", rtc, raw[:, :, 8:16], cosb, ALU.mult, [rkey, "cst"], ["rtc"])
                TT("pool", rtd, raw[:, :, 0:8], sinb, ALU.mult, [rkey, "cst"], ["rtd"])
                TT("dve", raw[:, :, 0:8], rta, rtb, ALU.subtract, ["rta", "rtb"], [rkey])
                TT("dve", raw[:, :, 8:16], rtc, rtd, ALU.add, ["rtc", "rtd"], [rkey])

            def sqnorm(src, col0, skey):
                TT("pool", sqs, src, src, ALU.mult, [skey], ["sqs"])
                P.op("dve", lambda e: e.tensor_reduce(out=nrm[:, col0:col0 + 8], in_=sqs, axis=AX.X, op=ALU.add), reads=["sqs"], writes=["nrm"])
                TT("dve", nmax[:, col0:col0 + 8], nmax[:, col0:col0 + 8], nrm[:, col0:col0 + 8], ALU.max, ["nrm", "nmax"], ["nmax"])

            r0k = HALO + i * 128
            if own:
                rope(aqr[b2], "aqr%d" % b2)
                sqnorm(aqr[b2], 0, "aqr%d" % b2)
                CP("act", qrb[b2], aqr[b2].rearrange("p h d -> p (h d)"), ["aqr%d" % b2], ["qrb%d" % b2])
                DMA("sp", QS[io * 128:(io + 1) * 128, :], qrb[b2], ["qrb%d" % b2], ["QS%d" % io])
            rope(akr[b2], "akr%d" % b2)
            sqnorm(akr[b2], 8, "akr%d" % b2)
            CP("act", krb[b2], akr[b2].rearrange("p h d -> p (h d)"), ["akr%d" % b2], ["krb%d" % b2])
            DMA("sp", KS[r0k:r0k + 128, :], krb[b2], ["krb%d" % b2], ["KS%d" % (r0k // 128)])

        for n_ in range(len(tiles) + 1):
            ra, rb = [], []
            if n_ < len(tiles):
                P.rec = ra
                stream[0] = 0
                S1(tiles[n_])
            if n_ > 0:
                P.rec = rb
                stream[0] = 1
                S2(tiles[n_ - 1])
            P.rec = None
            stream[0] = None
            P.replay_merged(ra, rb)

        if stop_after == "A":
            P.op("sp", None, reads=[k_ for k_ in P.last_w.keys() if k_[:2] in ("QS", "KS", "VS", "GV", "GG")], writes=[])
            P.emit()
            return nc
        for i in range(NTW - 1, T0 - 1, -1):
            ib = i - T0
            if ib < NTO:
                CP("act", SbT[:, ib, :, :].rearrange("p a b -> p (a b)"), Sb.rearrange("p a b -> p (a b)"), ["Sb"], ["SbT%d" % ib])
            if i == T0:
                break
            for pr in range(2):
                STT(Sb[:, pr, :], Sb[:, pr, :], decB[:, ib, pr:pr + 1], kvB[:, ib, pr, :], ALU.mult, ALU.add,
                    ["Sb", "decB%d" % ib, "kvB%d" % ib], ["Sb"])

        P.barrier()
        AR.off = M1
        vb2 = [sb("vb2%d" % i, [128, 512], BF16) for i in range(2)]
        gb2 = [sb("gb2%d" % i, [128, 512], BF16) for i in range(2)]
        osb2 = [sb("osb%d" % i, [128, 512]) for i in range(2)]
        osq2 = [sb("osq%d" % i, [128, 4, 128]) for i in range(2)]
        oms2 = [sb("oms%d" % i, [128, 4]) for i in range(2)]
        sgs2 = [sb("sgs%d" % i, [128, 512]) for i in range(2)]
        ybf2 = [sb("ybf%d" % i, [128, 512]) for i in range(2)]
        mixb = [sb("mixb%d" % i, [128, 512], BF16) for i in range(2)]

        def g2_body(io):
            b2 = io % 2
            osb, osq, oms, sgs, ybf = osb2[b2], osq2[b2], oms2[b2], sgs2[b2], ybf2[b2]
            ok_, qk_, mk_, sk_, yk_ = "osb%d" % b2, "osq%d" % b2, "oms%d" % b2, "sgs%d" % b2, "ybf%d" % b2
            DMA("sp", vb2[b2], GV[io * 128:(io + 1) * 128, :], ["GV%d" % io], ["vb2%d" % b2])
            DMA("sp", gb2[b2], GG[io * 128:(io + 1) * 128, :], ["GG%d" % io], ["gb2%d" % b2])
            poX, poXk = next_pf()
            poY, poYk = next_pf()

            def o_fn(e, po, par):
                ins = None
                p0 = par * 64
                for pr in range(2):
                    h = pr * 2 + par
                    oap = po[:, pr * 128:(pr + 1) * 128]
                    e.matmul(oap, lhsT=attnT[:, io, h * 128:(h + 1) * 128], rhs=vb2[b2][:, h * 128:(h + 1) * 128], start=True, stop=False)
                    e.matmul(oap, lhsT=qdT[p0:p0 + 64, io, pr, :], rhs=SfT[p0:p0 + 64, io, pr, :], start=False, stop=False)
                    ins = e.matmul(oap, lhsT=qdT[p0:p0 + 64, io, 2 + pr, :], rhs=SbT[p0:p0 + 64, io, pr, :], start=False, stop=True)
                return ins
            rk_ = ["attnT%d" % io, "qdT%d" % io, "SfT%d" % io, "SbT%d" % io, "vb2%d" % b2]
            P.op("pe", lambda e: o_fn(e, poX, 0), reads=rk_, writes=[poXk])
            P.op("pe", lambda e: o_fn(e, poY, 1), reads=rk_, writes=[poYk])
            ov = osb.rearrange("p (a b c) -> p a b c", a=2, b=2)
            CP("act", ov[:, :, 0, :], poX[:, 0:256].rearrange("p (a c) -> p a c", a=2), [poXk], [ok_])
            CP("act", ov[:, :, 1, :], poY[:, 0:256].rearrange("p (a c) -> p a c", a=2), [poYk], [ok_])
            TT("pool", osq.rearrange("p h d -> p (h d)"), osb, osb, ALU.mult, [ok_], [qk_])
            P.op("dve", lambda e: e.tensor_reduce(out=oms, in_=osq, axis=AX.X, op=ALU.add), reads=[qk_], writes=[mk_])
            rstd_from_ssq(oms, oms, 128, mk_, mk_)
            ACT(sgs, gb2[b2], AF.Silu, ["gb2%d" % b2], [sk_])
            TT("dve", ybf, osb, gnbc, ALU.mult, [ok_, "gnbc"], [yk_])
            for h in range(4):
                STT(mixb[b2][:, h * 128:(h + 1) * 128], ybf[:, h * 128:(h + 1) * 128], oms[:, h:h + 1], sgs[:, h * 128:(h + 1) * 128],
                    ALU.mult, ALU.mult, [yk_, mk_, sk_], ["mixb%d" % b2])
            DMA("sp", MG[io * 128:(io + 1) * 128, :], mixb[b2], ["mixb%d" % b2], ["MG%d" % io])

        for m in range(0, NTO, 2):
            ra, rb = [], []
            P.rec = ra
            stream[0] = 0
            g2_body(m)
            P.rec = rb
            stream[0] = 1
            g2_body(m + 1)
            P.rec = None
            stream[0] = None
            P.replay_merged(ra, rb)
        MGK = ["MG%d" % i for i in range(NTO)]
        if stop_after == "G2":
            P.op("sp", None, reads=QSK + KSK + VSK + MGK, writes=[])
            P.emit()
            return nc

        P.barrier()
        AR.off = M0
        nm2 = sb("nm2", [128, 2])
        m2 = sb("m2", [2, 2])
        m1 = sb("m1", [1, 4])
        P.op("dve", lambda e: e.tensor_reduce(out=nm2, in_=nmax.rearrange("p (a h) -> p a h", a=2), axis=AX.X, op=ALU.max),
             reads=["nmax"], writes=["nm2"])
        pt, pk = next_pf()
        P.op("pe", lambda e, pt=pt: e.transpose(out=pt[0:2, 0:128], in_=nm2, identity=cm[:, 0, :]), reads=["nm2", "cm"], writes=[pk])
        P.op("dve", lambda e, pt=pt: e.tensor_reduce(out=m2[:, 0:1], in_=pt[0:2, 0:128], axis=AX.X, op=ALU.max), reads=[pk], writes=["m2"])
        pt, pk = next_pf()
        P.op("pe", lambda e, pt=pt: e.transpose(out=pt[0:1, 0:2], in_=m2[:, 0:1], identity=cm[0:2, 0, 0:2]), reads=["m2", "cm"], writes=[pk])
        CP("dve", m1[:, 0:2], pt[0:1, 0:2], [pk], ["m1"])
        TT("dve", m1[:, 2:3], m1[:, 0:1], m1[:, 1:2], ALU.mult, ["m1"], ["m1"])
        ACT(m1[:, 3:4], m1[:, 2:3], AF.Ln, ["m1"], ["m1"])
        ACT(m1[:, 3:4], m1[:, 3:4], AF.Exp, ["m1"], ["m1"], scale=0.5)
        TS("dve", m1[:, 3:4], m1[:, 3:4], -0.125, None, ALU.mult, None, ["m1"], ["m1"])
        pt, pk = next_pf()
        P.op("pe", lambda e, pt=pt: e.matmul(pt[:, 0:1], lhsT=cm[0:1, 7, :], rhs=m1[:, 3:4], start=True, stop=True), reads=["m1", "cm"], writes=[pk])
        CP("dve", negc, pt[:, 0:1], [pk], ["negc"])

        accT = sb("accT", [65, 8, OWN])
        NQ = 8
        qsb2 = [[sb("qsb%d" % i, [128, 512], BF16) for i in range(NQ)] for _ in range(2)]
        ksb2 = [[sb("ksb%d" % i, [128, 512], BF16) for i in range(NQ + 2)] for _ in range(2)]
        vsb2 = [[sb("vsb%d" % i, [128, 8, 65], BF16) for i in range(NQ + 2)] for _ in range(2)]
        qT2 = [[sb("qT%d" % i, [128, 4, 128], BF16) for i in range(NQ)] for _ in range(2)]
        kT2 = [[sb("kT%d" % i, [128, 4, 128], BF16) for i in range(NQ + 2)] for _ in range(2)]
        pex = [sb("pex%d" % i, [128, 384], BF16) for i in range(4)]
        pmk = [sb("pmk%d" % i, [128, 384], BF16) for i in range(4)]
        cnt4 = [0, 0]
        jobs = [(1, 0, 0, 8), (1, 0, 8, 8)] + [(4, r, 0, 4) for r in range(4)] + [(16, r, 0, 1) for r in range(16)]
        for jn, (dd, r, j0, nq) in enumerate(jobs):
            js = jn % 2
            qsb, ksb, vsb, qT, kT = qsb2[js], ksb2[js], vsb2[js], qT2[js], kT2[js]
            QSv = QS.rearrange("(n d) c -> d n c", d=dd)
            KSv = KS.rearrange("(n d) c -> d n c", d=dd)
            VSv = VS.rearrange("(n d) c -> d n c", d=dd)
            accv = accT.rearrange("p h (n d) -> p h d n", d=dd)
            for jq in range(nq):
                n0 = 128 * (j0 + jq)
                DMA("sp", qsb[jq], QSv[r, n0:n0 + 128, :], QSK, [("qsb" + str(js) + "_%d") % jq])
            for kk in range(nq + 2):
                n0 = 2048 // dd + 128 * (j0 + kk - 1)
                DMA("sp", ksb[kk], KSv[r, n0:n0 + 128, :], KSK, [("ksb" + str(js) + "_%d") % kk])
                DMA("sp", vsb[kk].rearrange("p h d -> p (h d)"), VSv[r, n0:n0 + 128, :], VSK, [("vsb" + str(js) + "_%d") % kk])
            tl = [(qsb[jq], ("qsb" + str(js) + "_%d") % jq, qT[jq], ("qT" + str(js) + "_%d") % jq) for jq in range(nq)] + \
                 [(ksb[kk], ("ksb" + str(js) + "_%d") % kk, kT[kk], ("kT" + str(js) + "_%d") % kk) for kk in range(nq + 2)]
            for t0 in range(0, len(tl), 2):
                grp = tl[t0:t0 + 2]
                pbt, pbk = next_pb()

                def trq_fn(e, grp=grp, pbt=pbt):
                    ins = None
                    for gi, (src, _, _, _) in enumerate(grp):
                        for c in range(4):
                            ins = e.transpose(out=pbt[:, gi * 512 + c * 128: gi * 512 + (c + 1) * 128], in_=src[:, c * 128:(c + 1) * 128], identity=identb)
                    return ins
                P.op("pe", trq_fn, reads=[g[1] for g in grp] + ["identb"], writes=[pbk])
                for gi, (_, _, dst, dk) in enumerate(grp):
                    CP("act" if gi == 0 else "dve", dst.rearrange("p c t -> p (c t)"), pbt[:, gi * 512:(gi + 1) * 512], [pbk], [dk])
            def it_body(jq, hg, s_, kT=kT, qT=qT, vsb=vsb, js=js, dd=dd, r=r, j0=j0, accv=accv):
                bufs = []
                for h in range(hg * 4, hg * 4 + 4):
                    p0 = (h % 2) * 64
                    blk = h // 2
                    pS, pSk = next_pf()

                    def s_fn(e, pS=pS, jq=jq, p0=p0, blk=blk, kT=kT, qT=qT):
                        ins = None
                        for sl in range(3):
                            ins = e.matmul(pS[:, sl * 128:(sl + 1) * 128], lhsT=kT[jq + sl][p0:p0 + 64, blk, :], rhs=qT[jq][p0:p0 + 64, blk, :],
                                           start=True, stop=True)
                        return ins
                    P.op("pe", s_fn, reads=[("kT" + str(js) + "_%d") % (jq + sl) for sl in range(3)] + [("qT" + str(js) + "_%d") % jq], writes=[pSk])
                    bi = 2 * s_ + cnt4[s_] % 2
                    cnt4[s_] += 1
                    ACT(pex[bi], pS[:, 0:384], AF.Exp, [pSk, "negc"], ["pex%d" % bi], bias=negc, scale=0.125)
                    TT("pool" if (h % 4 == 3) else "dve", pmk[bi], pex[bi], band, ALU.mult, ["pex%d" % bi, "band"], ["pmk%d" % bi])
                    bufs.append((bi, h))
                    if len(bufs) == 2:
                        pU, pUk = next_pf()

                        def pv_fn(e, pU=pU, jq=jq, bufs=tuple(bufs), vsb=vsb):
                            ins = None
                            for hi, (b_, h_) in enumerate(bufs):
                                for sl in range(3):
                                    ins = e.matmul(pU[0:65, hi * 128:(hi + 1) * 128], lhsT=vsb[jq + sl][:, h_, :], rhs=pmk[b_][:, sl * 128:(sl + 1) * 128],
                                                   start=(sl == 0), stop=(sl == 2))
                            return ins
                        P.op("pe", pv_fn, reads=[("vsb" + str(js) + "_%d") % (jq + sl) for sl in range(3)] + ["pmk%d" % b_ for (b_, _) in bufs], writes=[pUk])
                        n0 = 128 * (j0 + jq)
                        h0 = bufs[0][1]
                        dst = accv[:, h0:h0 + 2, r, n0:n0 + 128]
                        src = pU[0:65, 0:256].rearrange("p (h t) -> p h t", h=2)
                        if dd == 1:
                            CP("dve", dst, src, [pUk], ["accT"])
                        else:
                            TT("dve", dst, src, dst, ALU.add, [pUk, "accT"], ["accT"])
                        bufs = []
            its = [(jq, hg) for jq in range(nq) for hg in range(2)]
            for m in range(0, len(its), 2):
                ra, rb = [], []
                P.rec = ra
                stream[0] = 0
                it_body(its[m][0], its[m][1], 0)
                if m + 1 < len(its):
                    P.rec = rb
                    stream[0] = 1
                    it_body(its[m + 1][0], its[m + 1][1], 1)
                P.rec = None
                stream[0] = None
                P.replay_merged(ra, rb)
        rz = sb("rz", [64, 512])
        otb = [sb("otb%d" % i, [64, 512], BF16) for i in range(2)]
        k2 = 0
        for h in range(8):
            for g in range(4):
                pz, pzk = next_pf()
                P.op("pe", lambda e, pz=pz, h=h, g=g: e.matmul(pz[0:64, :], lhsT=cm[64:65, 7, 0:64], rhs=accT[64:65, h, g * 512:(g + 1) * 512],
                                                                start=True, stop=True), reads=["accT", "cm"], writes=[pzk])
                P.op("dve", lambda e, pz=pz: e.reciprocal(out=rz, in_=pz[0:64, :]), reads=[pzk], writes=["rz"])
                b2 = k2 % 2
                k2 += 1
                TT("pool", otb[b2], accT[0:64, h, g * 512:(g + 1) * 512], rz, ALU.mult, ["accT", "rz"], ["otb%d" % b2])
                DMA("sp", OTS[h, :, g * 512:(g + 1) * 512], otb[b2], ["otb%d" % b2], ["OTS%d_%d" % (h, g)])
        OTK = ["OTS%d_%d" % (h, g) for h in range(8) for g in range(4)]
        if stop_after == "B":
            P.op("sp", None, reads=OTK + MGK, writes=[])
            P.emit()
            return nc

        P.barrier()
        AR.off = M0
        bc_cache = {}

        def bcreg(e):
            if "r" not in bc_cache:
                bc_cache["r"] = e.to_reg(2559)
            return bc_cache["r"]
        CAPG = 640
        NSLOT = 4 * CAPG
        OOB = 4096.0
        u2tok = sb("u2tok", [128, NTO, D], BF16)
        OH = sb("OH", [128, NTO, 4])
        WE = sb("WE", [128, NTO, 8])
        idxf = sb("idxf", [128, NTO])
        idxi = sb("idxi", [128, 2 * NTO], I32)
        goffm = sb("goffm", [128, 4])
        pren = sb("pren", [128, 4])
        M2 = AR.off
        woutG = sb("woutG", [128, 4, D], BF16)
        woutA = sb("woutA", [64, 8, D], BF16)
        DMA("pool", woutG, w_out[0:512, :].rearrange("(c p) n -> p c n", p=128), [], ["woutG"])
        DMA("pool", woutA, w_out[512:1024, :].rearrange("(h p) n -> p h n", p=64), [], ["woutA"])
        for g in range(4):
            P.op("dve", lambda e, g=g: e.memset(goffm[:, g:g + 1], float(g * CAPG) - OOB), writes=["goffm"])
        P.op("dve", lambda e: e.memset(pren, 0.0), writes=["pren"])
        zx = sb("zx", [128, D], BF16)
        zw = sb("zw", [128, 8])
        P.op("pool", lambda e: e.memset(zx, 0.0), writes=["zx"])
        P.op("pool", lambda e: e.memset(zw, 0.0), writes=["zw"])
        for r0 in range(0, NSLOT, 128):
            DMA("sp", XB[r0:r0 + 128, :], zx, ["zx"], ["XB"])
            DMA("sp", WB[r0:r0 + 128, :], zw, ["zw"], ["WB"])
        xo = [sb("xo%d" % i, [128, D]) for i in range(2)]
        mgl = [sb("mgl%d" % i, [128, 512], BF16) for i in range(2)]
        otl = [sb("otl%d" % i, [64, 8, 128], BF16) for i in range(2)]
        mgT2 = [sb("mgT%d" % i, [128, 4, 128], BF16) for i in range(2)]
        h2t = [sb("h2t%d" % i, [128, D]) for i in range(2)]
        u22 = [sb("u2%d" % i, [128, D]) for i in range(2)]
        u2Tf2 = [sb("u2Tf%d" % i, [128, 8, 128]) for i in range(2)]
        junk22 = [sb("junk2%d" % i, [128, D], BF16) for i in range(2)]
        ss2 = sb("ss2", [128, 2])
        rs2 = sb("rs2", [128, 2])
        lg2 = [sb("lg%d" % i, [128, 36]) for i in range(2)]
        sm2 = [sb("sm%d" % i, [128, 64]) for i in range(2)]
        smr = sb("smr", [128, 16])

        def c1_body(io):
            b2 = io % 2
            mgT, u2, u2Tf, junk2, lg, sm = mgT2[b2], u22[b2], u2Tf2[b2], junk22[b2], lg2[b2], sm2[b2]
            mk, uk, lk, sk_ = "mgT%d" % b2, "u2_%d" % b2, "lg%d" % b2, "sm%d" % b2
            DMA("sp", xo[b2], xw[HALO + io * 128: HALO + (io + 1) * 128, :], [], ["xo%d" % b2])
            DMA("sp", mgl[b2], MG[io * 128:(io + 1) * 128, :], ["MG%d" % io], ["mgl%d" % b2])
            DMA("sp", otl[b2], OTS[:, :, io * 128:(io + 1) * 128].rearrange("h p t -> p h t"), OTK, ["otl%d" % b2])
            pbt, pbk = next_pb()

            def trm_fn(e, pbt=pbt, b2=b2):
                ins = None
                for c in range(4):
                    ins = e.transpose(out=pbt[:, c * 128:(c + 1) * 128], in_=mgl[b2][:, c * 128:(c + 1) * 128], identity=identb)
                return ins
            P.op("pe", trm_fn, reads=["mgl%d" % b2, "identb"], writes=[pbk])
            CP("act", mgT.rearrange("p c t -> p (c t)"), pbt[:, 0:512], [pbk], [mk])
            for cg in range(2):
                pt, pk = next_pf()
                pairs = [(mgT[:, c, :], woutG[:, c, cg * 512:(cg + 1) * 512]) for c in range(4)] + \
                        [(otl[b2][:, h, :], woutA[:, h, cg * 512:(cg + 1) * 512]) for h in range(8)]
                mm_group(pt[:, :], pairs, pk, [mk, "otl%d" % b2, "woutG", "woutA"])
                TT("dve", h2t[b2][:, cg * 512:(cg + 1) * 512], pt[:, :], xo[b2][:, cg * 512:(cg + 1) * 512], ALU.add,
                   [pk, "xo%d" % b2], ["h2t%d" % b2])
            DMA("sp", H2[io * 128:(io + 1) * 128, :], h2t[b2], ["h2t%d" % b2], ["H2_%d" % io])
            P.op("act", lambda e: e.activation(out=junk2, in_=h2t[b2], func=AF.Square, accum_out=ss2[:, b2:b2 + 1]),
                 reads=["h2t%d" % b2], writes=["junk2%d" % b2, "ss2%d" % b2])
            rstd_from_ssq(rs2[:, b2:b2 + 1], ss2[:, b2:b2 + 1], D, "ss2%d" % b2, "rs2%d" % b2)
            STT(u2, h2t[b2], rs2[:, b2:b2 + 1], n2bc, ALU.mult, ALU.mult, ["h2t%d" % b2, "rs2%d" % b2, "n2bc"], [uk])
            CP("pool", u2tok[:, io, :], u2, [uk], ["u2tok%d" % io])
            for half in range(2):
                pt, pk = next_pf()

                def tru_fn(e, pt=pt, half=half):
                    ins = None
                    for c in range(4):
                        cc = half * 4 + c
                        ins = e.transpose(out=pt[:, c * 128:(c + 1) * 128], in_=u2[:, cc * 128:(cc + 1) * 128], identity=cm[:, 0, :])
                    return ins
                P.op("pe", tru_fn, reads=[uk, "cm"], writes=[pk])
                CP("act", u2Tf[:, half * 4:half * 4 + 4, :].rearrange("p c t -> p (c t)"), pt[:, :], [pk], ["u2Tf%d_%d" % (b2, half)])
            pr_, prk = next_pf()
            mm_group(pr_[:, 0:36], [(u2Tf[:, c, :], wr[:, c, :]) for c in range(8)], prk, ["u2Tf%d_0" % b2, "u2Tf%d_1" % b2, "wr"])
            TT("dve", lg, pr_[:, 0:36], rbbc, ALU.add, [prk, "rbbc"], [lk])
            gmax, ngmax, gsum, gw = sm[:, 0:1], sm[:, 1:2], sm[:, 2:3], sm[:, 3:4]
            oh = OH[:, io, :]
            ohk = "OH%d" % io
            ge = sm[:, 8:12]
            esel = sm[:, 16:24]
            top8 = sm[:, 24:32]
            d21, w1g, w2g = sm[:, 32:33], sm[:, 33:34], sm[:, 34:35]
            wa = sm[:, 40:48]
            wb_ = sm[:, 48:56]
            P.op("dve", lambda e: e.tensor_reduce(out=gmax, in_=lg[:, 0:4], axis=AX.X, op=ALU.max), reads=[lk], writes=[sk_])
            TS("dve", oh, lg[:, 0:4], gmax, None, ALU.is_equal, None, [lk, sk_], [ohk])
            TS("dve", ngmax, gmax, -1.0, None, ALU.mult, None, [sk_], [sk_])
            ACT(ge, lg[:, 0:4], AF.Exp, [lk, sk_], [sk_], bias=ngmax, scale=1.0)
            P.op("dve", lambda e: e.tensor_reduce(out=gsum, in_=ge, axis=AX.X, op=ALU.add), reads=[sk_], writes=[sk_])
            P.op("dve", lambda e: e.reciprocal(out=gw, in_=gsum), reads=[sk_], writes=[sk_])
            TS("dve", esel, lg[:, 4:12], oh[:, 0:1], None, ALU.mult, None, [lk, ohk], [sk_])
            for g in range(1, 4):
                STT(esel, lg[:, 4 + 8 * g:12 + 8 * g], oh[:, g:g + 1], esel, ALU.mult, ALU.add, [lk, ohk, sk_], [sk_])
            P.op("dve", lambda e: e.max(out=top8, in_=esel), reads=[sk_], writes=[sk_])
            TT("dve", d21, top8[:, 1:2], top8[:, 0:1], ALU.subtract, [sk_], [sk_])
            ACT(d21, d21, AF.Exp, [sk_], [sk_])
            TS("dve", d21, d21, 1.0, None, ALU.add, None, [sk_], [sk_])
            P.op("dve", lambda e: e.reciprocal(out=w1g, in_=d21), reads=[sk_], writes=[sk_])
            TT("dve", w1g, w1g, gw, ALU.mult, [sk_], [sk_])
            TT("dve", w2g, gw, w1g, ALU.subtract, [sk_], [sk_])
            TS("dve", wa, esel, top8[:, 0:1], w1g, ALU.is_equal, ALU.mult, [sk_], [sk_])
            TS("dve", wb_, esel, top8[:, 1:2], w2g, ALU.is_equal, ALU.mult, [sk_], [sk_])
            TT("dve", WE[:, io, :], wa, wb_, ALU.add, [sk_], ["WE%d" % io])

        for m in range(0, NTO, 2):
            ra, rb = [], []
            P.rec = ra
            stream[0] = 0
            c1_body(m)
            P.rec = rb
            stream[0] = 1
            c1_body(m + 1)
            P.rec = None
            stream[0] = None
            P.replay_merged(ra, rb)

        for io in range(NTO):
            oh = OH[:, io, :]
            ohk = "OH%d" % io
            prk_t, prkk = next_pf()
            P.op("pe", lambda e, t=prk_t, io=io: (e.matmul(t[:, 0:4], lhsT=cm[:, 4, :], rhs=OH[:, io, :], start=True, stop=False),
                                                   e.matmul(t[:, 0:4], lhsT=cm[:, 7, :], rhs=pren, start=False, stop=True))[1],
                 reads=[ohk, "pren", "cm"], writes=[prkk])
            rk = smr[:, 0:4]
            okm = smr[:, 4:8]
            TS("dve", rk, prk_t[:, 0:4], -16.0, None, ALU.mult, None, [prkk], ["smr"])
            STT(pren, oh, -1.0 / 16.0, pren, ALU.mult, ALU.add, [ohk, "pren", prkk], ["pren"])
            TS("dve", okm, rk, float(CAPG), None, ALU.is_lt, None, ["smr"], ["smr"])
            TT("dve", okm, okm, oh, ALU.mult, ["smr", ohk], ["smr"])
            TT("dve", rk, rk, goffm, ALU.add, ["smr", "goffm"], ["smr"])
            TT("dve", rk, rk, okm, ALU.mult, ["smr"], ["smr"])
            P.op("dve", lambda e, io=io, rk=rk: e.tensor_reduce(out=idxf[:, io:io + 1], in_=rk, axis=AX.X, op=ALU.add), reads=["smr"], writes=["idxf%d" % io])
            TS("dve", idxf[:, io:io + 1], idxf[:, io:io + 1], OOB, None, ALU.add, None, ["idxf%d" % io], ["idxf%d" % io])
            CP("dve", idxi[:, io:io + 1], idxf[:, io:io + 1], ["idxf%d" % io], ["idxi%d" % io])
            P.op("pool", lambda e, io=io: e.indirect_dma_start(out=XB[:, :], out_offset=bass.IndirectOffsetOnAxis(ap=idxi[:, io:io + 1], axis=0),
                                                               in_=u2tok[:, io, :], in_offset=None, bounds_check=bcreg(e), oob_is_err=False),
                 reads=["u2tok%d" % io, "idxi%d" % io, "XB"], writes=["XBs%d" % io], dma=True)
            P.op("pool", lambda e, io=io: e.indirect_dma_start(out=WB[:, :], out_offset=bass.IndirectOffsetOnAxis(ap=idxi[:, io:io + 1], axis=0),
                                                               in_=WE[:, io, :], in_offset=None, bounds_check=bcreg(e), oob_is_err=False),
                 reads=["WE%d" % io, "idxi%d" % io, "WB"], writes=["WBs%d" % io], dma=True)
        H2K = ["H2_%d" % i for i in range(NTO)]
        XBK = ["XBs%d" % i for i in range(NTO)] + ["XB"]
        WBK = ["WBs%d" % i for i in range(NTO)] + ["WB"]
        if debug:
            DMA("sp", WTD[:, 0:NTO], idxf, ["idxf%d" % i for i in range(NTO)], ["WTD"])
        if stop_after == "C1":
            P.op("sp", None, reads=H2K + XBK + WBK + ["WTD"], writes=[])
            P.emit()
            return nc

        P.barrier()
        AR.off = M2
        NCH = CAPG // 128
        xs = sb("xs", [128, NCH, D], BF16)
        xTg = sb("xTg", [128, 8, CAPG], BF16)
        wsl = sb("wsl", [128, NCH, 8])
        hid = sb("hid", [128, 4, CAPG], BF16)
        yacc = sb("yacc", [128, NCH, D])
        wgb = [sb("wgb%d" % i, [128, 8, 512], BF16) for i in range(2)]
        wub = [sb("wub%d" % i, [128, 8, 512], BF16) for i in range(2)]
        wdb = [sb("wdb%d" % i, [128, 4, D], BF16) for i in range(2)]
        sgb = [sb("sgb%d" % i, [128, 512]) for i in range(2)]

        def load_expert(ex):
            b = ex % 2
            DMA("pool", wgb[b], ewg[ex].rearrange("(c p) n -> p c n", p=128), [], ["wgb%d" % b])
            DMA("pool", wub[b], ewu[ex].rearrange("(c p) n -> p c n", p=128), [], ["wub%d" % b])
            DMA("pool", wdb[b], ewd[ex].rearrange("(c p) n -> p c n", p=128), [], ["wdb%d" % b])
        load_expert(0)
        load_expert(1)
        hid2 = [hid, sb("hidB", [128, 4, CAPG], BF16)]
        kk2 = [0]
        nsl = [(0, 512), (512, CAPG)]

        def GU(ex, hb, XTK):
            b = ex % 2
            for (n0, n1) in nsl:
                for fc in range(4):
                    pg, pgk = next_pf()
                    pu, puk = next_pf()
                    mm_group(pg[:, 0:n1 - n0], [(wgb[b][:, c, fc * 128:(fc + 1) * 128], xTg[:, c, n0:n1]) for c in range(8)], pgk,
                             ["wgb%d" % b] + XTK)
                    mm_group(pu[:, 0:n1 - n0], [(wub[b][:, c, fc * 128:(fc + 1) * 128], xTg[:, c, n0:n1]) for c in range(8)], puk,
                             ["wub%d" % b] + XTK)
                    sb_i = kk2[0] % 2
                    kk2[0] += 1
                    ACT(sgb[sb_i][:, 0:n1 - n0], pg[:, 0:n1 - n0], AF.Silu, [pgk], ["sgb%d" % sb_i])
                    TT("dve", hid2[hb][:, fc, n0:n1], sgb[sb_i][:, 0:n1 - n0], pu[:, 0:n1 - n0], ALU.mult, ["sgb%d" % sb_i, puk],
                       ["hid%d_%d_%d" % (hb, fc, n0)])

        def DN(ex, hb, el):
            b = ex % 2
            HK = ["hid%d_%d_%d" % (hb, fc, n0) for fc in range(4) for (n0, _) in nsl]
            for ch in range(NCH):
                for cg in range(2):
                    py, pyk = next_pf()
                    mm_group(py[:, :], [(hid2[hb][:, fc, ch * 128:(ch + 1) * 128], wdb[b][:, fc, cg * 512:(cg + 1) * 512]) for fc in range(4)], pyk,
                             ["wdb%d" % b] + HK)
                    ya = yacc[:, ch, cg * 512:(cg + 1) * 512]
                    yk = "yacc%d" % ch
                    if el == 0:
                        TS("dve", ya, py[:, :], wsl[:, ch, el:el + 1], None, ALU.mult, None, [pyk, "wsl"], [yk])
                    else:
                        STT(ya, py[:, :], wsl[:, ch, el:el + 1], ya, ALU.mult, ALU.add, [pyk, "wsl", yk], [yk])

        for g in range(4):
            DMA("sp", xs, XB[g * CAPG:(g + 1) * CAPG, :].rearrange("(c p) d -> p c d", p=128), XBK, ["xs"])
            DMA("sp", wsl, WB[g * CAPG:(g + 1) * CAPG, :].rearrange("(c p) d -> p c d", p=128), WBK, ["wsl"])
            for ch in range(NCH):
                pbt, pbk = next_pb()

                def trx_fn(e, pbt=pbt, ch=ch):
                    ins = None
                    for c in range(8):
                        ins = e.transpose(out=pbt[:, c * 128:(c + 1) * 128], in_=xs[:, ch, c * 128:(c + 1) * 128], identity=identb)
                    return ins
                P.op("pe", trx_fn, reads=["xs", "identb"], writes=[pbk])
                CP("act" if ch % 2 else "dve", xTg[:, :, ch * 128:(ch + 1) * 128], pbt[:, :].rearrange("p (c t) -> p c t", c=8), [pbk], ["xTg%d" % ch])
            XTK = ["xTg%d" % ch for ch in range(NCH)]
            GU(g * 8, 0, XTK)
            for el in range(8):
                ex = g * 8 + el
                if ex + 2 < NEXP:
                    b_ = ex % 2
                    DMA("pool", wgb[b_], ewg[ex + 2].rearrange("(c p) n -> p c n", p=128), [], ["wgb%d" % b_])
                    DMA("pool", wub[b_], ewu[ex + 2].rearrange("(c p) n -> p c n", p=128), [], ["wub%d" % b_])
                ra, rb = [], []
                if el + 1 < 8:
                    P.rec = ra
                    stream[0] = 0
                    GU(ex + 1, (el + 1) % 2, XTK)
                P.rec = rb
                stream[0] = 1
                DN(ex, el % 2, el)
                P.rec = None
                stream[0] = None
                P.replay_merged(ra, rb)
                if ex + 2 < NEXP:
                    DMA("pool", wdb[ex % 2], ewd[ex + 2].rearrange("(c p) n -> p c n", p=128), [], ["wdb%d" % (ex % 2)])
            DMA("sp", YB[g * CAPG:(g + 1) * CAPG, :].rearrange("(c p) d -> p c d", p=128), yacc, ["yacc%d" % ch for ch in range(NCH)], ["YB%d" % g])
        YBK = ["YB%d" % g for g in range(4)]
        P.barrier()
        AR.off = M2
        hl = [sb("hl%d" % i, [128, D]) for i in range(2)]
        yg = [sb("yg%d" % i, [128, D]) for i in range(2)]
        ob = [sb("ob%d" % i, [128, D]) for i in range(2)]
        junk3 = sb("junk3", [128, D], BF16)
        ss3 = sb("ss3", [128, 2])
        rs3 = sb("rs3", [128, 2])
        for io in range(NTO):
            b2 = io % 2
            DMA("sp", hl[b2], H2[io * 128:(io + 1) * 128, :], ["H2_%d" % io], ["hl%d" % b2])
            P.op("pool", lambda e, b2=b2: e.memset(yg[b2], 0.0), writes=["yg%d" % b2])
            P.op("pool", lambda e, io=io, b2=b2: e.indirect_dma_start(out=yg[b2], out_offset=None, in_=YB[:, :],
                                                                       in_offset=bass.IndirectOffsetOnAxis(ap=idxi[:, io:io + 1], axis=0),
                                                                       bounds_check=bcreg(e), oob_is_err=False),
                 reads=YBK + ["idxi%d" % io], writes=["yg%d" % b2], dma=True)
            TT("dve", hl[b2], hl[b2], yg[b2], ALU.add, ["hl%d" % b2, "yg%d" % b2], ["hl%d" % b2])
            P.op("act", lambda e, b2=b2: e.activation(out=junk3, in_=hl[b2], func=AF.Square, accum_out=ss3[:, 0:1]),
                 reads=["hl%d" % b2], writes=["junk3", "ss3"])
            rstd_from_ssq(rs3[:, 0:1], ss3[:, 0:1], D, "ss3", "rs3")
            STT(ob[b2], hl[b2], rs3[:, 0:1], fnbc, ALU.mult, ALU.mult, ["hl%d" % b2, "rs3", "fnbc"], ["ob%d" % b2])
            DMA("sp", out_d[io * 128:(io + 1) * 128, :], ob[b2], ["ob%d" % b2], ["OUT%d" % io])
        P.op("sp", None, reads=["OUT%d" % i for i in range(NTO)], writes=[])
        P.emit()
    return nc


def _consts():
    s = np.arange(128)[:, None]
    t = np.arange(128)[None, :]
    cm = np.zeros((128, 8, 128), np.float32)
    cm[:, 0] = (s == t)
    cm[:, 1] = (s <= t) / -16.0
    cm[:, 2] = (s >= t) / -16.0
    cm[:, 3] = (s > t) / -16.0
    cm[:, 4] = (s < t) / -16.0
    cm[:, 5] = (s <= t)
    cm[:, 6] = (s >= t)
    cm[:, 7] = 1.0
    band = np.zeros((128, 384), np.float32)
    band[:, 0:128] = (s >= t + 64)
    band[:, 128:256] = (np.abs(s - t) <= 64)
    band[:, 256:384] = (s <= t - 64)
    return cm, band


def make_in_maps(inputs):
    f = lambda a: np.ascontiguousarray(np.asarray(a, dtype=np.float32))
    x = f(inputs["x"])
    cm, band = _consts()
    wz = np.zeros((33, 512), np.float32)
    wz[0:16, 0:256] = f(inputs["gla_fwd_gate_w"])[0]
    wz[16:32, 256:512] = f(inputs["gla_bwd_gate_w"])[0]
    wz[32, 0:256] = f(inputs["gla_fwd_gate_b"])[0]
    wz[32, 256:512] = f(inputs["gla_bwd_gate_b"])[0]
    vecs = np.zeros((4, D), np.float32)
    vecs[0] = f(inputs["norm1_w"])[0]
    vecs[1] = f(inputs["norm2_w"])[0]
    vecs[2] = f(inputs["final_norm_w"])
    vecs[3] = np.tile(f(inputs["gla_norm_w"])[0], 8)
    wr = np.concatenate([f(inputs["router_group_w"])[0]] + [f(inputs["router_expert_w"])[0, g] for g in range(4)], axis=1)
    rb = np.concatenate([f(inputs["router_group_b"])[0], f(inputs["router_expert_b"])[0].reshape(-1)])[None, :]
    inv = (500000.0 ** (-(np.arange(0, 16, 2, dtype=np.float32) / np.float32(16)))).astype(np.float32)
    shared = dict(cmat=cm, band3=band, w_in=f(inputs["w_in"])[0], wz=wz, vecs=vecs, w_out=f(inputs["w_out"])[0],
                  wr=np.ascontiguousarray(wr), rb=np.ascontiguousarray(rb), ewg=f(inputs["expert_w_gate"])[0],
                  ewu=f(inputs["expert_w_up"])[0], ewd=f(inputs["expert_w_down"])[0])
    maps = []
    for c in range(8):
        b, q = c // 4, c % 4
        s0 = q * OWN
        pos = np.arange(s0 - HALO, s0 + OWN + HALO)
        valid = (pos >= 0) & (pos < S)
        xwin = np.zeros((WIN, D), np.float32)
        xwin[valid] = x[b, pos[valid]]
        ang = (pos.astype(np.float32)[:, None] * inv[None, :]).astype(np.float32)
        cs = np.concatenate([np.cos(ang), np.sin(ang)], axis=1).astype(np.float32)
        cs_t = np.ascontiguousarray(cs.reshape(NTW, 128, 16).transpose(1, 0, 2))
        vcol = np.ascontiguousarray(valid.astype(np.float32).reshape(NTW, 128).T)
        m = dict(shared)
        m.update(xw=xwin, vcol=vcol, cs_t=cs_t)
        maps.append(m)
    return maps


_NC_CACHE = {}


def kernel(**inputs):
    maps = make_in_maps(inputs)
    if "nc" not in _NC_CACHE:
        _NC_CACHE["nc"] = build_program()
    nc = _NC_CACHE["nc"]
    res = run_bass_kernel_spmd(nc, maps, core_ids=list(range(8)))
    out = np.zeros((2, S, D), np.float32)
    for c in range(8):
        b, q = c // 4, c % 4
        out[b, q * OWN:(q + 1) * OWN] = res.results[c]["out"]
    return out
```

```python
import numpy as np
from contextlib import ExitStack
import concourse.bass as bass
import concourse.mybir as mybir
from concourse.bass_utils import run_bass_kernel_spmd

F32 = mybir.dt.float32
BF16 = mybir.dt.bfloat16
I32 = mybir.dt.int32
AF = mybir.ActivationFunctionType
ALU = mybir.AluOpType
AX = mybir.AxisListType

ENGS = ("pe", "act", "dve", "pool", "sp")
EPOCH = 4096
DMA_SLOTS = 8

D = 1024
S = 8192
OWN = 2048
HALO = 1024
WIN = OWN + 2 * HALO
NTW = WIN // 128
T0 = HALO // 128
NTO = OWN // 128
INW = 3104
NEXP = 32
EPS = 1e-6


class Op:
    __slots__ = ("eng", "fn", "dma", "deps", "sig", "sigcount", "dmaidx", "idx")

    def __init__(self, eng, fn, dma):
        self.eng = eng
        self.fn = fn
        self.dma = dma
        self.deps = []
        self.sig = False
        self.sigcount = 0
        self.dmaidx = -1
        self.idx = -1


class Prog:
    def __init__(self, nc):
        self.nc = nc
        self.ops = []
        self.last_w = {}
        self.readers = {}
        self.ndma = {e: 0 for e in ENGS}
        self.bar = None
        self.rec = None

    def barrier(self):
        deps = set()
        for e in ENGS:
            last = None
            nd = 0
            for o in reversed(self.ops):
                if o.eng != e:
                    continue
                if o.dma:
                    if nd < DMA_SLOTS:
                        deps.add(o.idx)
                        nd += 1
                elif last is None:
                    last = o.idx
                    deps.add(o.idx)
                if last is not None and nd >= DMA_SLOTS:
                    break
        b = self.op("sp", None)
        b.deps = sorted(deps | set(b.deps))
        self.bar = b.idx
        return b

    def replay_merged(self, a, b):
        na, nb = len(a), len(b)
        i = j = 0
        while i < na or j < nb:
            if j >= nb or (i < na and i * nb <= j * na):
                self.op(*a[i])
                i += 1
            else:
                self.op(*b[j])
                j += 1

    def op(self, eng, fn, reads=(), writes=(), dma=False):
        if self.rec is not None:
            self.rec.append((eng, fn, list(reads), list(writes), dma))
            return None
        import os as _os
        mx = int(_os.environ.get("DBG_MAXOPS", "0"))
        if mx and len(self.ops) >= mx and fn is not None:
            fn = None
            if dma:
                dma = False
        px = [k_ for k_ in reads if k_[:2] in ("pf", "pb")]
        if px:
            writes = list(writes) + [k_ for k_ in px if k_ not in writes]
            reads = [k_ for k_ in reads if k_ not in px]
        o = Op(eng, fn, dma)
        o.idx = len(self.ops)
        deps = set()
        if self.bar is not None:
            deps.add(self.bar)
        for k in reads:
            w = self.last_w.get(k)
            if w is not None:
                deps.add(w)
        for k in writes:
            w = self.last_w.get(k)
            if w is not None:
                deps.add(w)
            for r in self.readers.get(k, ()):
                deps.add(r)
        deps.discard(o.idx)
        o.deps = sorted(deps)
        for k in writes:
            self.last_w[k] = o.idx
            self.readers[k] = []
        for k in reads:
            if k not in writes:
                self.readers.setdefault(k, []).append(o.idx)
        if dma:
            o.dmaidx = self.ndma[eng]
            self.ndma[eng] += 1
        self.ops.append(o)
        return o

    def emit(self):
        nc = self.nc
        ops = self.ops
        for o in ops:
            for d in o.deps:
                p = ops[d]
                if not p.dma:
                    p.sig = True
        cnt = {e: 0 for e in ENGS}
        for o in ops:
            if o.sig and not o.dma:
                cnt[o.eng] += 1
                o.sigcount = cnt[o.eng]
        nsem = {e: (cnt[e] + EPOCH - 1) // EPOCH for e in ENGS}
        with ExitStack() as es:
            csem = {e: [es.enter_context(nc.semaphore("c_%s_%d" % (e, i))) for i in range(nsem[e])]
                    for e in ENGS}
            dsem = {e: [es.enter_context(nc.semaphore("d_%s_%d" % (e, i)))
                        for i in range(DMA_SLOTS if self.ndma[e] else 0)] for e in ENGS}
            block = es.enter_context(nc.Block())

            def body_for(e):
                def body(eng):
                    waited_c = {x: 0 for x in ENGS}
                    waited_d = {}
                    for o in ops:
                        if o.eng != e:
                            continue
                        need_c = {}
                        need_d = {}
                        for d in o.deps:
                            p = ops[d]
                            if p.dma:
                                slot = p.dmaidx % DMA_SLOTS
                                val = 16 * (p.dmaidx // DMA_SLOTS + 1)
                                key = (p.eng, slot)
                                if waited_d.get(key, 0) < val:
                                    need_d[key] = max(need_d.get(key, 0), val)
                            else:
                                if waited_c[p.eng] < p.sigcount:
                                    need_c[p.eng] = max(need_c.get(p.eng, 0), p.sigcount)
                        if o.dma:
                            slot = o.dmaidx % DMA_SLOTS
                            val = 16 * (o.dmaidx // DMA_SLOTS)
                            key = (e, slot)
                            if val > 0 and waited_d.get(key, 0) < val:
                                need_d[key] = max(need_d.get(key, 0), val)
                        for pe_, c in need_c.items():
                            ep = (c - 1) // EPOCH
                            eng.wait_ge(csem[pe_][ep], (c - 1) % EPOCH + 1)
                            waited_c[pe_] = c
                        for key, val in need_d.items():
                            eng.wait_ge(dsem[key[0]][key[1]], val)
                            waited_d[key] = val
                        ins = o.fn(eng) if o.fn is not None else None
                        if o.dma:
                            ins.then_inc(dsem[e][o.dmaidx % DMA_SLOTS], 16)
                        elif o.sig:
                            if ins is None:
                                ins = eng.nop()
                            ep = (o.sigcount - 1) // EPOCH
                            ins.then_inc(csem[e][ep], 1)
                return body

            block.tensor(body_for("pe"))
            block.scalar(body_for("act"))
            block.vector(body_for("dve"))
            block.gpsimd(body_for("pool"))
            block.sync(body_for("sp"))


class Arena:
    def __init__(self, ap, ncols):
        self.ap = ap
        self.n = ncols
        self.off = 0

    def alloc(self, shape, dt=F32):
        p = shape[0]
        rest = list(shape[1:])
        nel = 1
        for r in rest:
            nel *= r
        ncol = nel if dt in (F32, I32) else (nel + 1) // 2
        ncol += ncol % 2
        assert self.off + ncol <= self.n, "arena overflow: need %d have %d" % (ncol, self.n - self.off)
        v = self.ap[0:p, self.off:self.off + ncol]
        self.off += ncol
        if dt != F32:
            v = v.bitcast(dt)
        if v.shape[1] != nel:
            v = v[:, 0:nel]
        if len(rest) == 2:
            v = v.rearrange("p (a b) -> p a b", a=rest[0])
        elif len(rest) == 3:
            v = v.rearrange("p (a b c) -> p a b c", a=rest[0], b=rest[1])
        return v


def build_program(debug=False, stop_after=None, dbg_tiles=None):
    nc = bass.Bass("TRN2", target_bir_lowering=False)
    P = Prog(nc)
    global LASTP
    LASTP = P

    def din(name, shape, dt=F32):
        return nc.dram_tensor(name, list(shape), dt, kind="ExternalInput").ap()

    def dscr(name, shape, dt):
        kind = "ExternalOutput" if debug else "Internal"
        return nc.dram_tensor(name, list(shape), dt, kind=kind).ap()

    xw = din("xw", [WIN, D])
    vcol = din("vcol", [128, NTW])
    cs_t = din("cs_t", [128, NTW, 16])
    cmat = din("cmat", [128, 8, 128])
    band3 = din("band3", [128, 384])
    w_in = din("w_in", [D, INW])
    wz_d = din("wz", [33, 512])
    vecs = din("vecs", [4, D])
    w_out = din("w_out", [D, D])
    wr_d = din("wr", [D, 36])
    rb_d = din("rb", [1, 36])
    ewg = din("ewg", [NEXP, D, 512])
    ewu = din("ewu", [NEXP, D, 512])
    ewd = din("ewd", [NEXP, 512, D])
    out_d = nc.dram_tensor("out", [OWN, D], F32, kind="ExternalOutput").ap()
    QS = dscr("QS", [OWN, 512], BF16)
    KS = dscr("KS", [WIN + 2 * HALO, 512], BF16)
    VS = dscr("VS", [WIN + 2 * HALO, 520], BF16)
    GV = dscr("GV", [OWN, 512], BF16)
    GG = dscr("GG", [OWN, 512], BF16)
    MG = dscr("MG", [OWN, 512], BF16)
    OTS = dscr("OTS", [8, 64, OWN], BF16)
    H2 = dscr("H2", [OWN, D], F32)
    XB = dscr("XB", [2560, D], BF16)
    WB = dscr("WB", [2560, 8], F32)
    YB = dscr("YB", [2560, D], F32)
    WTD = nc.dram_tensor("WTD", [128, NTO * 32], F32, kind="ExternalOutput").ap() if debug else None

    QSK = ["QS%d" % i for i in range(NTO)]
    KSK = ["KS%d" % i for i in range(48)]
    VSK = ["VS%d" % i for i in range(48)]
    NCOL = 50 * 1024 + 512
    es = ExitStack()
    with es:
        arena_t = es.enter_context(nc.sbuf_tensor("arena", [128, NCOL], F32))
        AR = Arena(arena_t[:], NCOL)
        sb = lambda name, shape, dt=F32: AR.alloc(shape, dt)

        def ps(name, shape, dt=F32):
            return es.enter_context(nc.psum_tensor("p_" + name, list(shape), dt))

        pf = [ps("pf%d" % i, [128, 512]) for i in range(6)]
        pb = [ps("pb%d" % i, [128, 1024], BF16) for i in range(2)]
        pf_rr = [0]
        pb_rr = [0]

        stream = [None]
        srr = [0, 0]

        def next_pf():
            if stream[0] is None:
                i = pf_rr[0] % 6
                pf_rr[0] += 1
            else:
                s_ = stream[0]
                i = 3 * s_ + srr[s_] % 3
                srr[s_] += 1
            return pf[i], "pf%d" % i

        def next_pb():
            if stream[0] is None:
                i = pb_rr[0] % 2
                pb_rr[0] += 1
            else:
                i = stream[0]
            return pb[i], "pb%d" % i

        def mm_group(out_ap, pairs, okey, rkeys):
            def fn(e):
                ins = None
                n = len(pairs)
                for j, (l, r) in enumerate(pairs):
                    ins = e.matmul(out_ap, lhsT=l, rhs=r, start=(j == 0), stop=(j == n - 1))
                return ins
            P.op("pe", fn, reads=rkeys, writes=[okey])

        def ACT(out, in_, func, reads, writes, **kw):
            P.op("act", lambda e: e.activation(out=out, in_=in_, func=func, **kw), reads=reads, writes=writes)

        def TT(eng, out, in0, in1, op, reads, writes):
            P.op(eng, lambda e: e.tensor_tensor(out=out, in0=in0, in1=in1, op=op), reads=reads, writes=writes)

        def STT(out, in0, scalar, in1, op0, op1, reads, writes):
            P.op("dve", lambda e: e.scalar_tensor_tensor(out=out, in0=in0, scalar=scalar, in1=in1, op0=op0, op1=op1),
                 reads=reads, writes=writes)

        def TS(eng, out, in0, s1, s2, op0, op1, reads, writes):
            if op1 is None:
                P.op(eng, lambda e: e.tensor_scalar(out=out, in0=in0, scalar1=s1, scalar2=None, op0=op0), reads=reads, writes=writes)
            else:
                P.op(eng, lambda e: e.tensor_scalar(out=out, in0=in0, scalar1=s1, scalar2=s2, op0=op0, op1=op1), reads=reads, writes=writes)

        def CP(eng, out, in_, reads, writes):
            if eng == "act":
                ACT(out, in_, AF.Copy, reads, writes)
            else:
                P.op(eng, lambda e: e.tensor_copy(out=out, in_=in_), reads=reads, writes=writes)

        def DMA(q, out, in_, reads, writes):
            return P.op(q, lambda e: e.dma_start(out=out, in_=in_), reads=reads, writes=writes, dma=True)

        def rstd_from_ssq(dst, src, n, rk, wk):
            ACT(dst, src, AF.Ln, [rk, "epsc"], [wk], scale=1.0 / n, bias=epsc[0:dst.shape[0], :])
            ACT(dst, dst, AF.Exp, [wk], [wk], scale=-0.5)

        cm = sb("cm", [128, 8, 128])
        identb = sb("identb", [128, 128], BF16)
        band = sb("band", [128, 384], BF16)
        maskFB = sb("maskFB", [128, 4, 128])
        n16col = sb("n16col", [128, 2])
        epsc = sb("epsc", [128, 2])
        onec = sb("onec", [128, 2])
        negc = sb("negc", [128, 2])
        vc = sb("vc", [128, NTW])
        cst = sb("cst", [128, NTW, 16])
        wz = sb("wz", [33, 512])
        n1bc = sb("n1bc", [128, D])
        n2bc = sb("n2bc", [128, D])
        fnbc = sb("fnbc", [128, D])
        gnbc = sb("gnbc", [128, 512])
        rbbc = sb("rbbc", [128, 36])
        wr = sb("wr", [128, 8, 36])
        nmax = sb("nmax", [128, 16])
        n16col = n16col[:, 0:1]
        epsc = epsc[:, 0:1]
        onec = onec[:, 0:1]
        negc = negc[:, 0:1]

        DMA("sp", cm, cmat, [], ["cm"])
        DMA("pool", identb, cmat[:, 0, :], [], ["identb"])
        DMA("pool", band, band3, [], ["band"])
        DMA("sp", vc, vcol, [], ["vc"])
        DMA("sp", cst, cs_t, [], ["cst"])
        DMA("sp", wz, wz_d, [], ["wz"])
        DMA("sp", n1bc, vecs[0:1, :].partition_broadcast(128), [], ["n1bc"])
        DMA("sp", n2bc, vecs[1:2, :].partition_broadcast(128), [], ["n2bc"])
        DMA("sp", fnbc, vecs[2:3, :].partition_broadcast(128), [], ["fnbc"])
        DMA("sp", gnbc, vecs[3:4, 0:512].partition_broadcast(128), [], ["gnbc"])
        DMA("sp", rbbc, rb_d[0:1, :].partition_broadcast(128), [], ["rbbc"])
        DMA("sp", wr, wr_d.rearrange("(c p) n -> p c n", p=128), [], ["wr"])
        P.op("dve", lambda e: e.memset(n16col, -1.0 / 16.0), writes=["n16col"])
        P.op("dve", lambda e: e.memset(epsc, EPS), writes=["epsc"])
        P.op("dve", lambda e: e.memset(onec, 1.0), writes=["onec"])
        P.op("dve", lambda e: e.memset(nmax, 0.0), writes=["nmax"])
        for h in range(4):
            CP("dve", maskFB[:, h, :], cm[:, 5 + h // 2, :], ["cm"], ["maskFB"])
        M0 = AR.off

        attnT = sb("attnT", [128, NTO, 512], BF16)
        qdT = sb("qdT", [128, NTO, 4, 128], BF16)
        SfT = sb("SfT", [128, NTO, 2, 128], BF16)
        SbT = sb("SbT", [128, NTO, 2, 128], BF16)
        M1 = AR.off
        win = sb("win", [128, 8, INW], BF16)
        for c in range(8):
            DMA("pool", win[:, c, :], w_in[c * 128:(c + 1) * 128, :], [], ["win%d" % c])
        winkeys = ["win%d" % c for c in range(8)]
        kvB = sb("kvB", [128, NTW - T0, 2, 128], BF16)
        decB = sb("decB", [128, NTW - T0, 2])
        Sf = sb("Sf", [128, 2, 128])
        Sb = sb("Sb", [128, 2, 128])
        P.op("dve", lambda e: e.memset(Sf, 0.0), writes=["Sf"])
        P.op("dve", lambda e: e.memset(Sb, 0.0), writes=["Sb"])
        zt = sb("zt", [128, 520], BF16)
        P.op("pool", lambda e: e.memset(zt, 0.0), writes=["zt"])
        for blk in range(HALO // 128):
            for base in (0, HALO + WIN):
                r0 = base + blk * 128
                DMA("sp", KS[r0:r0 + 128, :], zt[:, 0:512], ["zt"], ["KS%d" % (r0 // 128)])
                DMA("sp", VS[r0:r0 + 128, :], zt, ["zt"], ["VS%d" % (r0 // 128)])
        xt = [sb("xt%d" % i, [128, D]) for i in range(2)]
        junk = sb("junk", [128, D], BF16)
        xn = [sb("xn%d" % i, [128, D], BF16) for i in range(2)]
        xnT = [sb("xnT%d" % i, [128, 8, 128], BF16) for i in range(2)]
        ssq = sb("ssq", [128, 2])
        rstd = sb("rstd", [128, 2])
        qk = [sb("qk%d" % i, [128, 512]) for i in range(2)]
        vbf = [sb("vbf%d" % i, [128, 512], BF16) for i in range(2)]
        gbf = [sb("gbf%d" % i, [128, 512], BF16) for i in range(2)]
        lr = [sb("lr%d" % i, [128, 32]) for i in range(2)]
        aqr = [sb("aqr%d" % i, [128, 8, 64]) for i in range(2)]
        akr = [sb("akr%d" % i, [128, 8, 64]) for i in range(2)]
        vab = [sb("vab%d" % i, [128, 8, 65], BF16) for i in range(2)]
        lrT = sb("lrT", [33, 128])
        ez = sb("ez", [128, 512])
        spl = sb("spl", [128, 512])
        E1 = sb("E1", [128, 512])
        E2 = sb("E2", [128, 512])
        E3 = sb("E3", [128, 512])
        dec = sb("dec", [128, 4])
        qd = sb("qd", [128, 512], BF16)
        ki = sb("ki", [128, 512], BF16)
        ke = sb("ke", [128, 512], BF16)
        kiT = sb("kiT", [128, 4, 128], BF16)
        qrb = [sb("qrb%d" % i, [128, 512], BF16) for i in range(2)]
        krb = [sb("krb%d" % i, [128, 512], BF16) for i in range(2)]
        rta = sb("rta", [128, 8, 8])
        rtb = sb("rtb", [128, 8, 8])
        rtc = sb("rtc", [128, 8, 8])
        rtd = sb("rtd", [128, 8, 8])
        sqs = sb("sqs", [128, 8, 64])
        nrm = sb("nrm", [128, 16])
        P.op("dve", lambda e: e.memset(lrT[32:33, :], 1.0), writes=["lrT_one"])
        tiles = list(range(NTW) if dbg_tiles is None else dbg_tiles)

        def S1(i):
            own = T0 <= i < T0 + NTO
            io = i - T0
            b2 = i % 2
            xtk, xnk, xnTk = "xt%d" % b2, "xn%d" % b2, "xnT%d" % b2
            if i == tiles[0]:
                DMA("sp", xt[b2], xw[i * 128:(i + 1) * 128, :], [], [xtk])
            if i + 1 < NTW and (dbg_tiles is None):
                DMA("sp", xt[(i + 1) % 2], xw[(i + 1) * 128:(i + 2) * 128, :], [], ["xt%d" % ((i + 1) % 2)])
            sk, rk = "ssq%d" % b2, "rstd%d" % b2
            P.op("act", lambda e, b2=b2: e.activation(out=junk, in_=xt[b2], func=AF.Square, accum_out=ssq[:, b2:b2 + 1]),
                 reads=[xtk], writes=["junk", sk])
            rstd_from_ssq(rstd[:, b2:b2 + 1], ssq[:, b2:b2 + 1], D, sk, rk)
            STT(xn[b2], xt[b2], rstd[:, b2:b2 + 1], n1bc, ALU.mult, ALU.mult, [xtk, rk, "n1bc"], [xnk])
            pbt, pbk = next_pb()

            def tr_fn(e, b2=b2, pbt=pbt):
                ins = None
                for c in range(8):
                    ins = e.transpose(out=pbt[:, c * 128:(c + 1) * 128], in_=xn[b2][:, c * 128:(c + 1) * 128], identity=identb)
                return ins
            P.op("pe", tr_fn, reads=[xnk, "identb"], writes=[pbk])
            CP("act", xnT[b2].rearrange("p c t -> p (c t)"), pbt[:, :], [pbk], [xnTk])

            def proj(c0, c1):
                pt, pk = next_pf()
                n = c1 - c0
                mm_group(pt[:, 0:n], [(xnT[b2][:, c, :], win[:, c, c0:c1]) for c in range(8)], pk, [xnTk] + winkeys)
                return pt, pk

            if own:
                pt, pk = proj(0, 512)
                CP("act", qk[b2], pt[:, 0:512], [pk], ["qk%d" % b2])
            else:
                pt, pk = proj(256, 512)
                CP("act", qk[b2][:, 256:512], pt[:, 0:256], [pk], ["qk%d" % b2])
            pt, pk = proj(512, 1024)
            CP("dve", vbf[b2], pt[:, 0:512], [pk], ["vbf%d" % b2])
            if own:
                DMA("sp", GV[io * 128:(io + 1) * 128, :], vbf[b2], ["vbf%d" % b2], ["GV%d" % io])
                pt, pk = proj(1024, 1536)
                CP("act", gbf[b2], pt[:, 0:512], [pk], ["gbf%d" % b2])
                DMA("sp", GG[io * 128:(io + 1) * 128, :], gbf[b2], ["gbf%d" % b2], ["GG%d" % io])
            pt, pk = proj(1536, 1568)
            CP("dve", lr[b2], pt[:, 0:32], [pk], ["lr%d" % b2])
            if own:
                pt, pk = proj(1568, 2080)
                CP("dve", aqr[b2].rearrange("p h d -> p (h d)"), pt[:, 0:512], [pk], ["aqr%d" % b2])
            pt, pk = proj(2080, 2592)
            CP("act", akr[b2].rearrange("p h d -> p (h d)"), pt[:, 0:512], [pk], ["akr%d" % b2])
            pt, pk = proj(2592, 3104)
            r0k = HALO + i * 128
            CP("act", vab[b2][:, :, 0:64], pt[:, 0:512].rearrange("p (h d) -> p h d", h=8), [pk], ["vab%d" % b2])
            CP("dve", vab[b2][:, :, 64:65], vc[:, i:i + 1].unsqueeze(1).broadcast_to([128, 8, 1]), ["vc"], ["vab%d" % b2])
            DMA("sp", VS[r0k:r0k + 128, :], vab[b2].rearrange("p h d -> p (h d)"), ["vab%d" % b2], ["VS%d" % (r0k // 128)])

        def S2(i):
            own = T0 <= i < T0 + NTO
            left = i < T0
            io = i - T0
            b2 = i % 2
            qkb = qk[b2]
            qkk = "qk%d" % b2
            vkey = "vbf%d" % b2
            vt = vbf[b2]
            pt, pk = next_pf()
            P.op("pe", lambda e, pt=pt: e.transpose(out=pt[0:32, 0:128], in_=lr[b2], identity=cm[:, 0, :]), reads=["lr%d" % b2, "cm"], writes=[pk])
            CP("dve", lrT[0:32, :], pt[0:32, 0:128], [pk], ["lrT"])
            pz, pzk = next_pf()
            P.op("pe", lambda e, pz=pz: e.matmul(pz[:, :], lhsT=lrT, rhs=wz, start=True, stop=True),
                 reads=["lrT", "lrT_one", "wz"], writes=[pzk])
            ACT(ez, pz[:, :], AF.Exp, [pzk], ["ez"], scale=-1.0)
            ACT(spl, ez, AF.Ln, ["ez", "onec"], ["spl"], bias=onec, scale=1.0)
            pbb, pbbk = next_pf()
            P.op("pe", lambda e, pbb=pbb: (e.matmul(pbb[:, 0:256], lhsT=cm[:, 1, :], rhs=spl[:, 0:256], start=True, stop=True),
                                           e.matmul(pbb[:, 256:512], lhsT=cm[:, 2, :], rhs=spl[:, 256:512], start=True, stop=True))[1],
                 reads=["cm", "spl"], writes=[pbbk])
            ACT(E1, pbb[:, :], AF.Exp, [pbbk], ["E1"])
            ACT(E2, pbb[:, :], AF.Exp, [pbbk], ["E2"], scale=-1.0)
            pb3, pb3k = next_pf()
            P.op("pe", lambda e, pb3=pb3: (e.matmul(pb3[:, 0:256], lhsT=cm[:, 3, :], rhs=spl[:, 0:256], start=True, stop=True),
                                           e.matmul(pb3[:, 256:512], lhsT=cm[:, 4, :], rhs=spl[:, 256:512], start=True, stop=True))[1],
                 reads=["cm", "spl"], writes=[pb3k])
            ACT(E3, pb3[:, :], AF.Exp, [pb3k], ["E3"])
            pdc, pdck = next_pf()

            def dec_fn(e, pdc=pdc):
                ins = None
                for j in range(4):
                    ins = e.matmul(pdc[:, j:j + 1], lhsT=spl[:, j * 128:(j + 1) * 128], rhs=n16col, start=True, stop=True)
                return ins
            P.op("pe", dec_fn, reads=["spl", "n16col"], writes=[pdck])
            ACT(dec, pdc[:, 0:4], AF.Exp, [pdck], ["dec"])
            if own:
                STT(qd[:, 0:256], qkb[:, 0:256], 0.125, E1[:, 0:256], ALU.mult, ALU.mult, [qkk, "E1"], ["qd"])
                STT(qd[:, 256:512], qkb[:, 0:256], 0.125, E1[:, 256:512], ALU.mult, ALU.mult, [qkk, "E1"], ["qd"])
                TT("pool", ki[:, 0:256], qkb[:, 256:512], E2[:, 0:256], ALU.mult, [qkk, "E2"], ["ki"])
                TT("pool", ki[:, 256:512], qkb[:, 256:512], E2[:, 256:512], ALU.mult, [qkk, "E2"], ["ki"])
            TT("pool", ke[:, 0:256], qkb[:, 256:512], E3[:, 0:256], ALU.mult, [qkk, "E3"], ["ke"])
            TT("pool", ke[:, 256:512], qkb[:, 256:512], E3[:, 256:512], ALU.mult, [qkk, "E3"], ["ke"])
            if own:
                pbt, pbk = next_pb()

                def tr2_fn(e, pbt=pbt):
                    ins = None
                    for j in range(4):
                        ins = e.transpose(out=pbt[:, j * 128:(j + 1) * 128], in_=qd[:, j * 128:(j + 1) * 128], identity=identb)
                    for j in range(4):
                        ins = e.transpose(out=pbt[:, 512 + j * 128:512 + (j + 1) * 128], in_=ki[:, j * 128:(j + 1) * 128], identity=identb)
                    return ins
                P.op("pe", tr2_fn, reads=["qd", "ki", "identb"], writes=[pbk])
                CP("act", qdT[:, io, :, :].rearrange("p c t -> p (c t)"), pbt[:, 0:512], [pbk], ["qdT%d" % io])
                CP("dve", kiT.rearrange("p c t -> p (c t)"), pbt[:, 512:1024], [pbk], ["kiT"])
                paX, paXk = next_pf()
                paY, paYk = next_pf()

                def att_fn(e, pa, par, io=io):
                    ins = None
                    p0 = par * 64
                    for dirn in range(2):
                        for pr in range(2):
                            blk = dirn * 2 + pr
                            sl = dirn * 2 + pr
                            ins = e.matmul(pa[:, sl * 128:(sl + 1) * 128], lhsT=kiT[p0:p0 + 64, blk, :], rhs=qdT[p0:p0 + 64, io, blk, :],
                                           start=True, stop=True)
                    return ins
                P.op("pe", lambda e, pa=paX, f=att_fn: f(e, pa, 0), reads=["kiT", "qdT%d" % io], writes=[paXk])
                P.op("pe", lambda e, pa=paY, f=att_fn: f(e, pa, 1), reads=["kiT", "qdT%d" % io], writes=[paYk])
                TT("dve", ez, paX[:, :], maskFB.rearrange("p h c -> p (h c)"), ALU.mult, [paXk, "maskFB"], ["ez"])
                TT("dve", E1, paY[:, :], maskFB.rearrange("p h c -> p (h c)"), ALU.mult, [paYk, "maskFB"], ["E1"])
                av = attnT[:, io, :].rearrange("p (a b c) -> p a b c", a=2, b=2)
                TT("pool", av[:, :, 0, :], ez[:, 0:256].rearrange("p (a c) -> p a c", a=2), ez[:, 256:512].rearrange("p (a c) -> p a c", a=2),
                   ALU.add, ["ez"], ["attnT%d" % io])
                TT("pool", av[:, :, 1, :], E1[:, 0:256].rearrange("p (a c) -> p a c", a=2), E1[:, 256:512].rearrange("p (a c) -> p a c", a=2),
                   ALU.add, ["E1"], ["attnT%d" % io])
            for dirn in range(2):
                if dirn == 0 and i >= T0 + NTO:
                    continue
                if dirn == 1 and left:
                    continue
                pkv, pkvk = next_pf()

                def kv_fn(e, pkv=pkv, dirn=dirn, vt=vt):
                    ins = None
                    for pr in range(2):
                        ins = e.matmul(pkv[:, pr * 256:(pr + 1) * 256], lhsT=ke[:, dirn * 256 + pr * 128: dirn * 256 + (pr + 1) * 128],
                                       rhs=vt[:, pr * 256:(pr + 1) * 256], start=True, stop=True)
                    return ins
                P.op("pe", kv_fn, reads=["ke", vkey], writes=[pkvk])
                if dirn == 0:
                    if own:
                        CP("act", SfT[:, io, :, :].rearrange("p a b -> p (a b)"), Sf.rearrange("p a b -> p (a b)"), ["Sf"], ["SfT%d" % io])
                    for pr in range(2):
                        for hh in range(2):
                            p0 = hh * 64
                            STT(Sf[p0:p0 + 64, pr, :], Sf[p0:p0 + 64, pr, :], dec[p0:p0 + 64, pr:pr + 1],
                                pkv[p0:p0 + 64, pr * 256 + hh * 128: pr * 256 + (hh + 1) * 128], ALU.mult, ALU.add,
                                ["Sf", "dec", pkvk], ["Sf"])
                else:
                    ib = i - T0
                    for pr in range(2):
                        for hh in range(2):
                            p0 = hh * 64
                            CP("act", kvB[p0:p0 + 64, ib, pr, :], pkv[p0:p0 + 64, pr * 256 + hh * 128: pr * 256 + (hh + 1) * 128],
                               [pkvk], ["kvB%d" % ib])
                    CP("dve", decB[:, ib, :], dec[:, 2:4], ["dec"], ["decB%d" % ib])

            def rope(raw, rkey):
                cosb = cst[:, i, 0:8].unsqueeze(1).broadcast_to([128, 8, 8])
                sinb = cst[:, i, 8:16].unsqueeze(1).broadcast_to([128, 8, 8])
                TT("pool", rta, raw[:, :, 0:8], cosb, ALU.mult, [rkey, "cst"], ["rta"])
                TT("pool", rtb, raw[:, :, 8:16], sinb, ALU.mult, [rkey, "cst"], ["rtb"])
                TT("pool", rtc, raw[:, :, 8:16], cosb, ALU.mult, [rkey, "cst"], ["rtc"])
                TT("pool", rtd, raw[:, :, 0:8], sinb, ALU.mult, [rkey, "cst"], ["rtd"])
                TT("dve", raw[:, :, 0:8], rta, rtb, ALU.subtract, ["rta", "rtb"], [rkey])
                TT("dve", raw[:, :, 8:16], rtc, rtd, ALU.add, ["rtc", "rtd"], [rkey])

            def sqnorm(src, col0, skey):
                TT("pool", sqs, src, src, ALU.mult, [skey], ["sqs"])
                P.op("dve", lambda e: e.tensor_reduce(out=nrm[:, col0:col0 + 8], in_=sqs, axis=AX.X, op=ALU.add), reads=["sqs"], writes=["nrm"])
                TT("dve", nmax[:, col0:col0 + 8], nmax[:, col0:col0 + 8], nrm[:, col0:col0 + 8], ALU.max, ["nrm", "nmax"], ["nmax"])

            r0k = HALO + i * 128
            if own:
                rope(aqr[b2], "aqr%d" % b2)
                sqnorm(aqr[b2], 0, "aqr%d" % b2)
                CP("act", qrb[b2], aqr[b2].rearrange("p h d -> p (h d)"), ["aqr%d" % b2], ["qrb%d" % b2])
                DMA("sp", QS[io * 128:(io + 1) * 128, :], qrb[b2], ["qrb%d" % b2], ["QS%d" % io])
            rope(akr[b2], "akr%d" % b2)
            sqnorm(akr[b2], 8, "akr%d" % b2)
            CP("act", krb[b2], akr[b2].rearrange("p h d -> p (h d)"), ["akr%d" % b2], ["krb%d" % b2])
            DMA("sp", KS[r0k:r0k + 128, :], krb[b2], ["krb%d" % b2], ["KS%d" % (r0k // 128)])

        for n_ in range(len(tiles) + 1):
            ra, rb = [], []
            if n_ < len(tiles):
                P.rec = ra
                stream[0] = 0
                S1(tiles[n_])
            if n_ > 0:
                P.rec = rb
                stream[0] = 1
                S2(tiles[n_ - 1])
            P.rec = None
            stream[0] = None
            P.replay_merged(ra, rb)

        if stop_after == "A":
            P.op("sp", None, reads=[k_ for k_ in P.last_w.keys() if k_[:2] in ("QS", "KS", "VS", "GV", "GG")], writes=[])
            P.emit()
            return nc
        for i in range(NTW - 1, T0 - 1, -1):
            ib = i - T0
            if ib < NTO:
                CP("act", SbT[:, ib, :, :].rearrange("p a b -> p (a b)"), Sb.rearrange("p a b -> p (a b)"), ["Sb"], ["SbT%d" % ib])
            if i == T0:
                break
            for pr in range(2):
                STT(Sb[:, pr, :], Sb[:, pr, :], decB[:, ib, pr:pr + 1], kvB[:, ib, pr, :], ALU.mult, ALU.add,
                    ["Sb", "decB%d" % ib, "kvB%d" % ib], ["Sb"])

        P.barrier()
        AR.off = M1
        vb2 = [sb("vb2%d" % i, [128, 512], BF16) for i in range(2)]
        gb2 = [sb("gb2%d" % i, [128, 512], BF16) for i in range(2)]
        osb2 = [sb("osb%d" % i, [128, 512]) for i in range(2)]
        osq2 = [sb("osq%d" % i, [128, 4, 128]) for i in range(2)]
        oms2 = [sb("oms%d" % i, [128, 4]) for i in range(2)]
        sgs2 = [sb("sgs%d" % i, [128, 512]) for i in range(2)]
        ybf2 = [sb("ybf%d" % i, [128, 512]) for i in range(2)]
        mixb = [sb("mixb%d" % i, [128, 512], BF16) for i in range(2)]

        def g2_body(io):
            b2 = io % 2
            osb, osq, oms, sgs, ybf = osb2[b2], osq2[b2], oms2[b2], sgs2[b2], ybf2[b2]
            ok_, qk_, mk_, sk_, yk_ = "osb%d" % b2, "osq%d" % b2, "oms%d" % b2, "sgs%d" % b2, "ybf%d" % b2
            DMA("sp", vb2[b2], GV[io * 128:(io + 1) * 128, :], ["GV%d" % io], ["vb2%d" % b2])
            DMA("sp", gb2[b2], GG[io * 128:(io + 1) * 128, :], ["GG%d" % io], ["gb2%d" % b2])
            poX, poXk = next_pf()
            poY, poYk = next_pf()

            def o_fn(e, po, par):
                ins = None
                p0 = par * 64
                for pr in range(2):
                    h = pr * 2 + par
                    oap = po[:, pr * 128:(pr + 1) * 128]
                    e.matmul(oap, lhsT=attnT[:, io, h * 128:(h + 1) * 128], rhs=vb2[b2][:, h * 128:(h + 1) * 128], start=True, stop=False)
                    e.matmul(oap, lhsT=qdT[p0:p0 + 64, io, pr, :], rhs=SfT[p0:p0 + 64, io, pr, :], start=False, stop=False)
                    ins = e.matmul(oap, lhsT=qdT[p0:p0 + 64, io, 2 + pr, :], rhs=SbT[p0:p0 + 64, io, pr, :], start=False, stop=True)
                return ins
            rk_ = ["attnT%d" % io, "qdT%d" % io, "SfT%d" % io, "SbT%d" % io, "vb2%d" % b2]
            P.op("pe", lambda e: o_fn(e, poX, 0), reads=rk_, writes=[poXk])
            P.op("pe", lambda e: o_fn(e, poY, 1), reads=rk_, writes=[poYk])
            ov = osb.rearrange("p (a b c) -> p a b c", a=2, b=2)
            CP("act", ov[:, :, 0, :], poX[:, 0:256].rearrange("p (a c) -> p a c", a=2), [poXk], [ok_])
            CP("act", ov[:, :, 1, :], poY[:, 0:256].rearrange("p (a c) -> p a c", a=2), [poYk], [ok_])
            TT("pool", osq.rearrange("p h d -> p (h d)"), osb, osb, ALU.mult, [ok_], [qk_])
            P.op("dve", lambda e: e.tensor_reduce(out=oms, in_=osq, axis=AX.X, op=ALU.add), reads=[qk_], writes=[mk_])
            rstd_from_ssq(oms, oms, 128, mk_, mk_)
            ACT(sgs, gb2[b2], AF.Silu, ["gb2%d" % b2], [sk_])
            TT("dve", ybf, osb, gnbc, ALU.mult, [ok_, "gnbc"], [yk_])
            for h in range(4):
                STT(mixb[b2][:, h * 128:(h + 1) * 128], ybf[:, h * 128:(h + 1) * 128], oms[:, h:h + 1], sgs[:, h * 128:(h + 1) * 128],
                    ALU.mult, ALU.mult, [yk_, mk_, sk_], ["mixb%d" % b2])
            DMA("sp", MG[io * 128:(io + 1) * 128, :], mixb[b2], ["mixb%d" % b2], ["MG%d" % io])

        for m in range(0, NTO, 2):
            ra, rb = [], []
            P.rec = ra
            stream[0] = 0
            g2_body(m)
            P.rec = rb
            stream[0] = 1
            g2_body(m + 1)
            P.rec = None
            stream[0] = None
            P.replay_merged(ra, rb)
        MGK = ["MG%d" % i for i in range(NTO)]
        if stop_after == "G2":
            P.op("sp", None, reads=QSK + KSK + VSK + MGK, writes=[])
            P.emit()
            return nc

        P.barrier()
        AR.off = M0
        nm2 = sb("nm2", [128, 2])
        m2 = sb("m2", [2, 2])
        m1 = sb("m1", [1, 4])
        P.op("dve", lambda e: e.tensor_reduce(out=nm2, in_=nmax.rearrange("p (a h) -> p a h", a=2), axis=AX.X, op=ALU.max),
             reads=["nmax"], writes=["nm2"])
        pt, pk = next_pf()
        P.op("pe", lambda e, pt=pt: e.transpose(out=pt[0:2, 0:128], in_=nm2, identity=cm[:, 0, :]), reads=["nm2", "cm"], writes=[pk])
        P.op("dve", lambda e, pt=pt: e.tensor_reduce(out=m2[:, 0:1], in_=pt[0:2, 0:128], axis=AX.X, op=ALU.max), reads=[pk], writes=["m2"])
        pt, pk = next_pf()
        P.op("pe", lambda e, pt=pt: e.transpose(out=pt[0:1, 0:2], in_=m2[:, 0:1], identity=cm[0:2, 0, 0:2]), reads=["m2", "cm"], writes=[pk])
        CP("dve", m1[:, 0:2], pt[0:1, 0:2], [pk], ["m1"])
        TT("dve", m1[:, 2:3], m1[:, 0:1], m1[:, 1:2], ALU.mult, ["m1"], ["m1"])
        ACT(m1[:, 3:4], m1[:, 2:3], AF.Ln, ["m1"], ["m1"])
        ACT(m1[:, 3:4], m1[:, 3:4], AF.Exp, ["m1"], ["m1"], scale=0.5)
        TS("dve", m1[:, 3:4], m1[:, 3:4], -0.125, None, ALU.mult, None, ["m1"], ["m1"])
        pt, pk = next_pf()
        P.op("pe", lambda e, pt=pt: e.matmul(pt[:, 0:1], lhsT=cm[0:1, 7, :], rhs=m1[:, 3:4], start=True, stop=True), reads=["m1", "cm"], writes=[pk])
        CP("dve", negc, pt[:, 0:1], [pk], ["negc"])

        accT = sb("accT", [65, 8, OWN])
        NQ = 8
        qsb2 = [[sb("qsb%d" % i, [128, 512], BF16) for i in range(NQ)] for _ in range(2)]
        ksb2 = [[sb("ksb%d" % i, [128, 512], BF16) for i in range(NQ + 2)] for _ in range(2)]
        vsb2 = [[sb("vsb%d" % i, [128, 8, 65], BF16) for i in range(NQ + 2)] for _ in range(2)]
        qT2 = [[sb("qT%d" % i, [128, 4, 128], BF16) for i in range(NQ)] for _ in range(2)]
        kT2 = [[sb("kT%d" % i, [128, 4, 128], BF16) for i in range(NQ + 2)] for _ in range(2)]
        pex = [sb("pex%d" % i, [128, 384], BF16) for i in range(4)]
        pmk = [sb("pmk%d" % i, [128, 384], BF16) for i in range(4)]
        cnt4 = [0, 0]
        jobs = [(1, 0, 0, 8), (1, 0, 8, 8)] + [(4, r, 0, 4) for r in range(4)] + [(16, r, 0, 1) for r in range(16)]
        for jn, (dd, r, j0, nq) in enumerate(jobs):
            js = jn % 2
            qsb, ksb, vsb, qT, kT = qsb2[js], ksb2[js], vsb2[js], qT2[js], kT2[js]
            QSv = QS.rearrange("(n d) c -> d n c", d=dd)
            KSv = KS.rearrange("(n d) c -> d n c", d=dd)
            VSv = VS.rearrange("(n d) c -> d n c", d=dd)
            accv = accT.rearrange("p h (n d) -> p h d n", d=dd)
            for jq in range(nq):
                n0 = 128 * (j0 + jq)
                DMA("sp", qsb[jq], QSv[r, n0:n0 + 128, :], QSK, [("qsb" + str(js) + "_%d") % jq])
            for kk in range(nq + 2):
                n0 = 2048 // dd + 128 * (j0 + kk - 1)
                DMA("sp", ksb[kk], KSv[r, n0:n0 + 128, :], KSK, [("ksb" + str(js) + "_%d") % kk])
                DMA("sp", vsb[kk].rearrange("p h d -> p (h d)"), VSv[r, n0:n0 + 128, :], VSK, [("vsb" + str(js) + "_%d") % kk])
            tl = [(qsb[jq], ("qsb" + str(js) + "_%d") % jq, qT[jq], ("qT" + str(js) + "_%d") % jq) for jq in range(nq)] + \
                 [(ksb[kk], ("ksb" + str(js) + "_%d") % kk, kT[kk], ("kT" + str(js) + "_%d") % kk) for kk in range(nq + 2)]
            for t0 in range(0, len(tl), 2):
                grp = tl[t0:t0 + 2]
                pbt, pbk = next_pb()

                def trq_fn(e, grp=grp, pbt=pbt):
                    ins = None
                    for gi, (src, _, _, _) in enumerate(grp):
                        for c in range(4):
                            ins = e.transpose(out=pbt[:, gi * 512 + c * 128: gi * 512 + (c + 1) * 128], in_=src[:, c * 128:(c + 1) * 128], identity=identb)
                    return ins
                P.op("pe", trq_fn, reads=[g[1] for g in grp] + ["identb"], writes=[pbk])
                for gi, (_, _, dst, dk) in enumerate(grp):
                    CP("act" if gi == 0 else "dve", dst.rearrange("p c t -> p (c t)"), pbt[:, gi * 512:(gi + 1) * 512], [pbk], [dk])
            def it_body(jq, hg, s_, kT=kT, qT=qT, vsb=vsb, js=js, dd=dd, r=r, j0=j0, accv=accv):
                bufs = []
                for h in range(hg * 4, hg * 4 + 4):
                    p0 = (h % 2) * 64
                    blk = h // 2
                    pS, pSk = next_pf()

                    def s_fn(e, pS=pS, jq=jq, p0=p0, blk=blk, kT=kT, qT=qT):
                        ins = None
                        for sl in range(3):
                            ins = e.matmul(pS[:, sl * 128:(sl + 1) * 128], lhsT=kT[jq + sl][p0:p0 + 64, blk, :], rhs=qT[jq][p0:p0 + 64, blk, :],
                                           start=True, stop=True)
                        return ins
                    P.op("pe", s_fn, reads=[("kT" + str(js) + "_%d") % (jq + sl) for sl in range(3)] + [("qT" + str(js) + "_%d") % jq], writes=[pSk])
                    bi = 2 * s_ + cnt4[s_] % 2
                    cnt4[s_] += 1
                    ACT(pex[bi], pS[:, 0:384], AF.Exp, [pSk, "negc"], ["pex%d" % bi], bias=negc, scale=0.125)
                    TT("pool" if (h % 4 == 3) else "dve", pmk[bi], pex[bi], band, ALU.mult, ["pex%d" % bi, "band"], ["pmk%d" % bi])
                    bufs.append((bi, h))
                    if len(bufs) == 2:
                        pU, pUk = next_pf()

                        def pv_fn(e, pU=pU, jq=jq, bufs=tuple(bufs), vsb=vsb):
                            ins = None
                            for hi, (b_, h_) in enumerate(bufs):
                                for sl in range(3):
                                    ins = e.matmul(pU[0:65, hi * 128:(hi + 1) * 128], lhsT=vsb[jq + sl][:, h_, :], rhs=pmk[b_][:, sl * 128:(sl + 1) * 128],
                                                   start=(sl == 0), stop=(sl == 2))
                            return ins
                        P.op("pe", pv_fn, reads=[("vsb" + str(js) + "_%d") % (jq + sl) for sl in range(3)] + ["pmk%d" % b_ for (b_, _) in bufs], writes=[pUk])
                        n0 = 128 * (j0 + jq)
                        h0 = bufs[0][1]
                        dst = accv[:, h0:h0 + 2, r, n0:n0 + 128]
                        src = pU[0:65, 0:256].rearrange("p (h t) -> p h t", h=2)
                        if dd == 1:
                            CP("dve", dst, src, [pUk], ["accT"])
                        else:
                            TT("dve", dst, src, dst, ALU.add, [pUk, "accT"], ["accT"])
                        bufs = []
            its = [(jq, hg) for jq in range(nq) for hg in range(2)]
            for m in range(0, len(its), 2):
                ra, rb = [], []
                P.rec = ra
                stream[0] = 0
                it_body(its[m][0], its[m][1], 0)
                if m + 1 < len(its):
                    P.rec = rb
                    stream[0] = 1
                    it_body(its[m + 1][0], its[m + 1][1], 1)
                P.rec = None
                stream[0] = None
                P.replay_merged(ra, rb)
        rz = sb("rz", [64, 512])
        otb = [sb("otb%d" % i, [64, 512], BF16) for i in range(2)]
        k2 = 0
        for h in range(8):
            for g in range(4):
                pz, pzk = next_pf()
                P.op("pe", lambda e, pz=pz, h=h, g=g: e.matmul(pz[0:64, :], lhsT=cm[64:65, 7, 0:64], rhs=accT[64:65, h, g * 512:(g + 1) * 512],
                                                                start=True, stop=True), reads=["accT", "cm"], writes=[pzk])
                P.op("dve", lambda e, pz=pz: e.reciprocal(out=rz, in_=pz[0:64, :]), reads=[pzk], writes=["rz"])
                b2 = k2 % 2
                k2 += 1
                TT("pool", otb[b2], accT[0:64, h, g * 512:(g + 1) * 512], rz, ALU.mult, ["accT", "rz"], ["otb%d" % b2])
                DMA("sp", OTS[h, :, g * 512:(g + 1) * 512], otb[b2], ["otb%d" % b2], ["OTS%d_%d" % (h, g)])
        OTK = ["OTS%d_%d" % (h, g) for h in range(8) for g in range(4)]
        if stop_after == "B":
            P.op("sp", None, reads=OTK + MGK, writes=[])
            P.emit()
            return nc

        P.barrier()
        AR.off = M0
        bc_cache = {}

        def bcreg(e):
            if "r" not in bc_cache:
                bc_cache["r"] = e.to_reg(2559)
            return bc_cache["r"]
        CAPG = 640
        NSLOT = 4 * CAPG
        OOB = 4096.0
        u2tok = sb("u2tok", [128, NTO, D], BF16)
        OH = sb("OH", [128, NTO, 4])
        WE = sb("WE", [128, NTO, 8])
        idxf = sb("idxf", [128, NTO])
        idxi = sb("idxi", [128, 2 * NTO], I32)
        goffm = sb("goffm", [128, 4])
        pren = sb("pren", [128, 4])
        M2 = AR.off
        woutG = sb("woutG", [128, 4, D], BF16)
        woutA = sb("woutA", [64, 8, D], BF16)
        DMA("pool", woutG, w_out[0:512, :].rearrange("(c p) n -> p c n", p=128), [], ["woutG"])
        DMA("pool", woutA, w_out[512:1024, :].rearrange("(h p) n -> p h n", p=64), [], ["woutA"])
        for g in range(4):
            P.op("dve", lambda e, g=g: e.memset(goffm[:, g:g + 1], float(g * CAPG) - OOB), writes=["goffm"])
        P.op("dve", lambda e: e.memset(pren, 0.0), writes=["pren"])
        zx = sb("zx", [128, D], BF16)
        zw = sb("zw", [128, 8])
        P.op("pool", lambda e: e.memset(zx, 0.0), writes=["zx"])
        P.op("pool", lambda e: e.memset(zw, 0.0), writes=["zw"])
        for r0 in range(0, NSLOT, 128):
            DMA("sp", XB[r0:r0 + 128, :], zx, ["zx"], ["XB"])
            DMA("sp", WB[r0:r0 + 128, :], zw, ["zw"], ["WB"])
        xo = [sb("xo%d" % i, [128, D]) for i in range(2)]
        mgl = [sb("mgl%d" % i, [128, 512], BF16) for i in range(2)]
        otl = [sb("otl%d" % i, [64, 8, 128], BF16) for i in range(2)]
        mgT2 = [sb("mgT%d" % i, [128, 4, 128], BF16) for i in range(2)]
        h2t = [sb("h2t%d" % i, [128, D]) for i in range(2)]
        u22 = [sb("u2%d" % i, [128, D]) for i in range(2)]
        u2Tf2 = [sb("u2Tf%d" % i, [128, 8, 128]) for i in range(2)]
        junk22 = [sb("junk2%d" % i, [128, D], BF16) for i in range(2)]
        ss2 = sb("ss2", [128, 2])
        rs2 = sb("rs2", [128, 2])
        lg2 = [sb("lg%d" % i, [128, 36]) for i in range(2)]
        sm2 = [sb("sm%d" % i, [128, 64]) for i in range(2)]
        smr = sb("smr", [128, 16])

        def c1_body(io):
            b2 = io % 2
            mgT, u2, u2Tf, junk2, lg, sm = mgT2[b2], u22[b2], u2Tf2[b2], junk22[b2], lg2[b2], sm2[b2]
            mk, uk, lk, sk_ = "mgT%d" % b2, "u2_%d" % b2, "lg%d" % b2, "sm%d" % b2
            DMA("sp", xo[b2], xw[HALO + io * 128: HALO + (io + 1) * 128, :], [], ["xo%d" % b2])
            DMA("sp", mgl[b2], MG[io * 128:(io + 1) * 128, :], ["MG%d" % io], ["mgl%d" % b2])
            DMA("sp", otl[b2], OTS[:, :, io * 128:(io + 1) * 128].rearrange("h p t -> p h t"), OTK, ["otl%d" % b2])
            pbt, pbk = next_pb()

            def trm_fn(e, pbt=pbt, b2=b2):
                ins = None
                for c in range(4):
                    ins = e.transpose(out=pbt[:, c * 128:(c + 1) * 128], in_=mgl[b2][:, c * 128:(c + 1) * 128], identity=identb)
                return ins
            P.op("pe", trm_fn, reads=["mgl%d" % b2, "identb"], writes=[pbk])
            CP("act", mgT.rearrange("p c t -> p (c t)"), pbt[:, 0:512], [pbk], [mk])
            for cg in range(2):
                pt, pk = next_pf()
                pairs = [(mgT[:, c, :], woutG[:, c, cg * 512:(cg + 1) * 512]) for c in range(4)] + \
                        [(otl[b2][:, h, :], woutA[:, h, cg * 512:(cg + 1) * 512]) for h in range(8)]
                mm_group(pt[:, :], pairs, pk, [mk, "otl%d" % b2, "woutG", "woutA"])
                TT("dve", h2t[b2][:, cg * 512:(cg + 1) * 512], pt[:, :], xo[b2][:, cg * 512:(cg + 1) * 512], ALU.add,
                   [pk, "xo%d" % b2], ["h2t%d" % b2])
            DMA("sp", H2[io * 128:(io + 1) * 128, :], h2t[b2], ["h2t%d" % b2], ["H2_%d" % io])
            P.op("act", lambda e: e.activation(out=junk2, in_=h2t[b2], func=AF.Square, accum_out=ss2[:, b2:b2 + 1]),
                 reads=["h2t%d" % b2], writes=["junk2%d" % b2, "ss2%d" % b2])
            rstd_from_ssq(rs2[:, b2:b2 + 1], ss2[:, b2:b2 + 1], D, "ss2%d" % b2, "rs2%d" % b2)
            STT(u2, h2t[b2], rs2[:, b2:b2 + 1], n2bc, ALU.mult, ALU.mult, ["h2t%d" % b2, "rs2%d" % b2, "n2bc"], [uk])
            CP("pool", u2tok[:, io, :], u2, [uk], ["u2tok%d" % io])
            for half in range(2):
                pt, pk = next_pf()

                def tru_fn(e, pt=pt, half=half):
                    ins = None
                    for c in range(4):
                        cc = half * 4 + c
                        ins = e.transpose(out=pt[:, c * 128:(c + 1) * 128], in_=u2[:, cc * 128:(cc + 1) * 128], identity=cm[:, 0, :])
                    return ins
                P.op("pe", tru_fn, reads=[uk, "cm"], writes=[pk])
                CP("act", u2Tf[:, half * 4:half * 4 + 4, :].rearrange("p c t -> p (c t)"), pt[:, :], [pk], ["u2Tf%d_%d" % (b2, half)])
            pr_, prk = next_pf()
            mm_group(pr_[:, 0:36], [(u2Tf[:, c, :], wr[:, c, :]) for c in range(8)], prk, ["u2Tf%d_0" % b2, "u2Tf%d_1" % b2, "wr"])
            TT("dve", lg, pr_[:, 0:36], rbbc, ALU.add, [prk, "rbbc"], [lk])
            gmax, ngmax, gsum, gw = sm[:, 0:1], sm[:, 1:2], sm[:, 2:3], sm[:, 3:4]
            oh = OH[:, io, :]
            ohk = "OH%d" % io
            ge = sm[:, 8:12]
            esel = sm[:, 16:24]
            top8 = sm[:, 24:32]
            d21, w1g, w2g = sm[:, 32:33], sm[:, 33:34], sm[:, 34:35]
            wa = sm[:, 40:48]
            wb_ = sm[:, 48:56]
            P.op("dve", lambda e: e.tensor_reduce(out=gmax, in_=lg[:, 0:4], axis=AX.X, op=ALU.max), reads=[lk], writes=[sk_])
            TS("dve", oh, lg[:, 0:4], gmax, None, ALU.is_equal, None, [lk, sk_], [ohk])
            TS("dve", ngmax, gmax, -1.0, None, ALU.mult, None, [sk_], [sk_])
            ACT(ge, lg[:, 0:4], AF.Exp, [lk, sk_], [sk_], bias=ngmax, scale=1.0)
            P.op("dve", lambda e: e.tensor_reduce(out=gsum, in_=ge, axis=AX.X, op=ALU.add), reads=[sk_], writes=[sk_])
            P.op("dve", lambda e: e.reciprocal(out=gw, in_=gsum), reads=[sk_], writes=[sk_])
            TS("dve", esel, lg[:, 4:12], oh[:, 0:1], None, ALU.mult, None, [lk, ohk], [sk_])
            for g in range(1, 4):
                STT(esel, lg[:, 4 + 8 * g:12 + 8 * g], oh[:, g:g + 1], esel, ALU.mult, ALU.add, [lk, ohk, sk_], [sk_])
            P.op("dve", lambda e: e.max(out=top8, in_=esel), reads=[sk_], writes=[sk_])
            TT("dve", d21, top8[:, 1:2], top8[:, 0:1], ALU.subtract, [sk_], [sk_])
            ACT(d21, d21, AF.Exp, [sk_], [sk_])
            TS("dve", d21, d21, 1.0, None, ALU.add, None, [sk_], [sk_])
            P.op("dve", lambda e: e.reciprocal(out=w1g, in_=d21), reads=[sk_], writes=[sk_])
            TT("dve", w1g, w1g, gw, ALU.mult, [sk_], [sk_])
            TT("dve", w2g, gw, w1g, ALU.subtract, [sk_], [sk_])
            TS("dve", wa, esel, top8[:, 0:1], w1g, ALU.is_equal, ALU.mult, [sk_], [sk_])
            TS("dve", wb_, esel, top8[:, 1:2], w2g, ALU.is_equal, ALU.mult, [sk_], [sk_])
            TT("dve", WE[:, io, :], wa, wb_, ALU.add, [sk_], ["WE%d" % io])

        for m in range(0, NTO, 2):
            ra, rb = [], []
            P.rec = ra
            stream[0] = 0
            c1_body(m)
            P.rec = rb
            stream[0] = 1
            c1_body(m + 1)
            P.rec = None
            stream[0] = None
            P.replay_merged(ra, rb)

        for io in range(NTO):
            oh = OH[:, io, :]
            ohk = "OH%d" % io
            prk_t, prkk = next_pf()
            P.op("pe", lambda e, t=prk_t, io=io: (e.matmul(t[:, 0:4], lhsT=cm[:, 4, :], rhs=OH[:, io, :], start=True, stop=False),
                                                   e.matmul(t[:, 0:4], lhsT=cm[:, 7, :], rhs=pren, start=False, stop=True))[1],
                 reads=[ohk, "pren", "cm"], writes=[prkk])
            rk = smr[:, 0:4]
            okm = smr[:, 4:8]
            TS("dve", rk, prk_t[:, 0:4], -16.0, None, ALU.mult, None, [prkk], ["smr"])
            STT(pren, oh, -1.0 / 16.0, pren, ALU.mult, ALU.add, [ohk, "pren", prkk], ["pren"])
            TS("dve", okm, rk, float(CAPG), None, ALU.is_lt, None, ["smr"], ["smr"])
            TT("dve", okm, okm, oh, ALU.mult, ["smr", ohk], ["smr"])
            TT("dve", rk, rk, goffm, ALU.add, ["smr", "goffm"], ["smr"])
            TT("dve", rk, rk, okm, ALU.mult, ["smr"], ["smr"])
            P.op("dve", lambda e, io=io, rk=rk: e.tensor_reduce(out=idxf[:, io:io + 1], in_=rk, axis=AX.X, op=ALU.add), reads=["smr"], writes=["idxf%d" % io])
            TS("dve", idxf[:, io:io + 1], idxf[:, io:io + 1], OOB, None, ALU.add, None, ["idxf%d" % io], ["idxf%d" % io])
            CP("dve", idxi[:, io:io + 1], idxf[:, io:io + 1], ["idxf%d" % io], ["idxi%d" % io])
            P.op("pool", lambda e, io=io: e.indirect_dma_start(out=XB[:, :], out_offset=bass.IndirectOffsetOnAxis(ap=idxi[:, io:io + 1], axis=0),
                                                               in_=u2tok[:, io, :], in_offset=None, bounds_check=bcreg(e), oob_is_err=False),
                 reads=["u2tok%d" % io, "idxi%d" % io, "XB"], writes=["XBs%d" % io], dma=True)
            P.op("pool", lambda e, io=io: e.indirect_dma_start(out=WB[:, :], out_offset=bass.IndirectOffsetOnAxis(ap=idxi[:, io:io + 1], axis=0),
                                                               in_=WE[:, io, :], in_offset=None, bounds_check=bcreg(e), oob_is_err=False),
                 reads=["WE%d" % io, "idxi%d" % io, "WB"], writes=["WBs%d" % io], dma=True)
        H2K = ["H2_%d" % i for i in range(NTO)]
        XBK = ["XBs%d" % i for i in range(NTO)] + ["XB"]
        WBK = ["WBs%d" % i for i in range(NTO)] + ["WB"]
        if debug:
            DMA("sp", WTD[:, 0:NTO], idxf, ["idxf%d" % i for i in range(NTO)], ["WTD"])
        if stop_after == "C1":
            P.op("sp", None, reads=H2K + XBK + WBK + ["WTD"], writes=[])
            P.emit()
            return nc

        P.barrier()
        AR.off = M2
        NCH = CAPG // 128
        xs = sb("xs", [128, NCH, D], BF16)
        xTg = sb("xTg", [128, 8, CAPG], BF16)
        wsl = sb("wsl", [128, NCH, 8])
        hid = sb("hid", [128, 4, CAPG], BF16)
        yacc = sb("yacc", [128, NCH, D])
        wgb = [sb("wgb%d" % i, [128, 8, 512], BF16) for i in range(2)]
        wub = [sb("wub%d" % i, [128, 8, 512], BF16) for i in range(2)]
        wdb = [sb("wdb%d" % i, [128, 4, D], BF16) for i in range(2)]
        sgb = [sb("sgb%d" % i, [128, 512]) for i in range(2)]

        def load_expert(ex):
            b = ex % 2
            DMA("pool", wgb[b], ewg[ex].rearrange("(c p) n -> p c n", p=128), [], ["wgb%d" % b])
            DMA("pool", wub[b], ewu[ex].rearrange("(c p) n -> p c n", p=128), [], ["wub%d" % b])
            DMA("pool", wdb[b], ewd[ex].rearrange("(c p) n -> p c n", p=128), [], ["wdb%d" % b])
        load_expert(0)
        load_expert(1)
        hid2 = [hid, sb("hidB", [128, 4, CAPG], BF16)]
        kk2 = [0]
        nsl = [(0, 512), (512, CAPG)]

        def GU(ex, hb, XTK):
            b = ex % 2
            for (n0, n1) in nsl:
                for fc in range(4):
                    pg, pgk = next_pf()
                    pu, puk = next_pf()
                    mm_group(pg[:, 0:n1 - n0], [(wgb[b][:, c, fc * 128:(fc + 1) * 128], xTg[:, c, n0:n1]) for c in range(8)], pgk,
                             ["wgb%d" % b] + XTK)
                    mm_group(pu[:, 0:n1 - n0], [(wub[b][:, c, fc * 128:(fc + 1) * 128], xTg[:, c, n0:n1]) for c in range(8)], puk,
                             ["wub%d" % b] + XTK)
                    sb_i = kk2[0] % 2
                    kk2[0] += 1
                    ACT(sgb[sb_i][:, 0:n1 - n0], pg[:, 0:n1 - n0], AF.Silu, [pgk], ["sgb%d" % sb_i])
                    TT("dve", hid2[hb][:, fc, n0:n1], sgb[sb_i][:, 0:n1 - n0], pu[:, 0:n1 - n0], ALU.mult, ["sgb%d" % sb_i, puk],
                       ["hid%d_%d_%d" % (hb, fc, n0)])

        def DN(ex, hb, el):
            b = ex % 2
            HK = ["hid%d_%d_%d" % (hb, fc, n0) for fc in range(4) for (n0, _) in nsl]
            for ch in range(NCH):
                for cg in range(2):
                    py, pyk = next_pf()
                    mm_group(py[:, :], [(hid2[hb][:, fc, ch * 128:(ch + 1) * 128], wdb[b][:, fc, cg * 512:(cg + 1) * 512]) for fc in range(4)], pyk,
                             ["wdb%d" % b] + HK)
                    ya = yacc[:, ch, cg * 512:(cg + 1) * 512]
                    yk = "yacc%d" % ch
                    if el == 0:
                        TS("dve", ya, py[:, :], wsl[:, ch, el:el + 1], None, ALU.mult, None, [pyk, "wsl"], [yk])
                    else:
                        STT(ya, py[:, :], wsl[:, ch, el:el + 1], ya, ALU.mult, ALU.add, [pyk, "wsl", yk], [yk])

        for g in range(4):
            DMA("sp", xs, XB[g * CAPG:(g + 1) * CAPG, :].rearrange("(c p) d -> p c d", p=128), XBK, ["xs"])
            DMA("sp", wsl, WB[g * CAPG:(g + 1) * CAPG, :].rearrange("(c p) d -> p c d", p=128), WBK, ["wsl"])
            for ch in range(NCH):
                pbt, pbk = next_pb()

                def trx_fn(e, pbt=pbt, ch=ch):
                    ins = None
                    for c in range(8):
                        ins = e.transpose(out=pbt[:, c * 128:(c + 1) * 128], in_=xs[:, ch, c * 128:(c + 1) * 128], identity=identb)
                    return ins
                P.op("pe", trx_fn, reads=["xs", "identb"], writes=[pbk])
                CP("act" if ch % 2 else "dve", xTg[:, :, ch * 128:(ch + 1) * 128], pbt[:, :].rearrange("p (c t) -> p c t", c=8), [pbk], ["xTg%d" % ch])
            XTK = ["xTg%d" % ch for ch in range(NCH)]
            GU(g * 8, 0, XTK)
            for el in range(8):
                ex = g * 8 + el
                if ex + 2 < NEXP:
                    b_ = ex % 2
                    DMA("pool", wgb[b_], ewg[ex + 2].rearrange("(c p) n -> p c n", p=128), [], ["wgb%d" % b_])
                    DMA("pool", wub[b_], ewu[ex + 2].rearrange("(c p) n -> p c n", p=128), [], ["wub%d" % b_])
                ra, rb = [], []
                if el + 1 < 8:
                    P.rec = ra
                    stream[0] = 0
                    GU(ex + 1, (el + 1) % 2, XTK)
                P.rec = rb
                stream[0] = 1
                DN(ex, el % 2, el)
                P.rec = None
                stream[0] = None
                P.replay_merged(ra, rb)
                if ex + 2 < NEXP:
                    DMA("pool", wdb[ex % 2], ewd[ex + 2].rearrange("(c p) n -> p c n", p=128), [], ["wdb%d" % (ex % 2)])
            DMA("sp", YB[g * CAPG:(g + 1) * CAPG, :].rearrange("(c p) d -> p c d", p=128), yacc, ["yacc%d" % ch for ch in range(NCH)], ["YB%d" % g])
        YBK = ["YB%d" % g for g in range(4)]
        P.barrier()
        AR.off = M2
        hl = [sb("hl%d" % i, [128, D]) for i in range(2)]
        yg = [sb("yg%d" % i, [128, D]) for i in range(2)]
        ob = [sb("ob%d" % i, [128, D]) for i in range(2)]
        junk32 = [sb("junk3%d" % i, [128, D], BF16) for i in range(2)]
        ss3 = sb("ss3", [128, 2])
        rs3 = sb("rs3", [128, 2])

        def fin_body(io):
            b2 = io % 2
            junk3 = junk32[b2]
            DMA("sp", hl[b2], H2[io * 128:(io + 1) * 128, :], ["H2_%d" % io], ["hl%d" % b2])
            P.op("pool", lambda e: e.memset(yg[b2], 0.0), writes=["yg%d" % b2])
            P.op("pool", lambda e: e.indirect_dma_start(out=yg[b2], out_offset=None, in_=YB[:, :],
                                                        in_offset=bass.IndirectOffsetOnAxis(ap=idxi[:, io:io + 1], axis=0),
                                                        bounds_check=bcreg(e), oob_is_err=False),
                 reads=YBK + ["idxi%d" % io], writes=["yg%d" % b2], dma=True)
            TT("dve", hl[b2], hl[b2], yg[b2], ALU.add, ["hl%d" % b2, "yg%d" % b2], ["hl%d" % b2])
            P.op("act", lambda e: e.activation(out=junk3, in_=hl[b2], func=AF.Square, accum_out=ss3[:, b2:b2 + 1]),
                 reads=["hl%d" % b2], writes=["junk3%d" % b2, "ss3%d" % b2])
            rstd_from_ssq(rs3[:, b2:b2 + 1], ss3[:, b2:b2 + 1], D, "ss3%d" % b2, "rs3%d" % b2)
            STT(ob[b2], hl[b2], rs3[:, b2:b2 + 1], fnbc, ALU.mult, ALU.mult, ["hl%d" % b2, "rs3%d" % b2, "fnbc"], ["ob%d" % b2])
            DMA("sp", out_d[io * 128:(io + 1) * 128, :], ob[b2], ["ob%d" % b2], ["OUT%d" % io])

        for m in range(0, NTO, 2):
            ra, rb = [], []
            P.rec = ra
            fin_body(m)
            P.rec = rb
            fin_body(m + 1)
            P.rec = None
            P.replay_merged(ra, rb)
        P.op("sp", None, reads=["OUT%d" % i for i in range(NTO)], writes=[])
        P.emit()
    return nc


def _consts():
    s = np.arange(128)[:, None]
    t = np.arange(128)[None, :]
    cm = np.zeros((128, 8, 128), np.float32)
    cm[:, 0] = (s == t)
    cm[:, 1] = (s <= t) / -16.0
    cm[:, 2] = (s >= t) / -16.0
    cm[:, 3] = (s > t) / -16.0
    cm[:, 4] = (s < t) / -16.0
    cm[:, 5] = (s <= t)
    cm[:, 6] = (s >= t)
    cm[:, 7] = 1.0
    band = np.zeros((128, 384), np.float32)
    band[:, 0:128] = (s >= t + 64)
    band[:, 128:256] = (np.abs(s - t) <= 64)
    band[:, 256:384] = (s <= t - 64)
    return cm, band


def make_in_maps(inputs):
    f = lambda a: np.ascontiguousarray(np.asarray(a, dtype=np.float32))
    x = f(inputs["x"])
    cm, band = _consts()
    wz = np.zeros((33, 512), np.float32)
    wz[0:16, 0:256] = f(inputs["gla_fwd_gate_w"])[0]
    wz[16:32, 256:512] = f(inputs["gla_bwd_gate_w"])[0]
    wz[32, 0:256] = f(inputs["gla_fwd_gate_b"])[0]
    wz[32, 256:512] = f(inputs["gla_bwd_gate_b"])[0]
    vecs = np.zeros((4, D), np.float32)
    vecs[0] = f(inputs["norm1_w"])[0]
    vecs[1] = f(inputs["norm2_w"])[0]
    vecs[2] = f(inputs["final_norm_w"])
    vecs[3] = np.tile(f(inputs["gla_norm_w"])[0], 8)
    wr = np.concatenate([f(inputs["router_group_w"])[0]] + [f(inputs["router_expert_w"])[0, g] for g in range(4)], axis=1)
    rb = np.concatenate([f(inputs["router_group_b"])[0], f(inputs["router_expert_b"])[0].reshape(-1)])[None, :]
    inv = (500000.0 ** (-(np.arange(0, 16, 2, dtype=np.float32) / np.float32(16)))).astype(np.float32)
    shared = dict(cmat=cm, band3=band, w_in=f(inputs["w_in"])[0], wz=wz, vecs=vecs, w_out=f(inputs["w_out"])[0],
                  wr=np.ascontiguousarray(wr), rb=np.ascontiguousarray(rb), ewg=f(inputs["expert_w_gate"])[0],
                  ewu=f(inputs["expert_w_up"])[0], ewd=f(inputs["expert_w_down"])[0])
    maps = []
    for c in range(8):
        b, q = c // 4, c % 4
        s0 = q * OWN
        pos = np.arange(s0 - HALO, s0 + OWN + HALO)
        valid = (pos >= 0) & (pos < S)
        xwin = np.zeros((WIN, D), np.float32)
        xwin[valid] = x[b, pos[valid]]
        ang = (pos.astype(np.float32)[:, None] * inv[None, :]).astype(np.float32)
        cs = np.concatenate([np.cos(ang), np.sin(ang)], axis=1).astype(np.float32)
        cs_t = np.ascontiguousarray(cs.reshape(NTW, 128, 16).transpose(1, 0, 2))
        vcol = np.ascontiguousarray(valid.astype(np.float32).reshape(NTW, 128).T)
        m = dict(shared)
        m.update(xw=xwin, vcol=vcol, cs_t=cs_t)
        maps.append(m)
    return maps


_NC_CACHE = {}


def kernel(**inputs):
    maps = make_in_maps(inputs)
    if "nc" not in _NC_CACHE:
        _NC_CACHE["nc"] = build_program()
    nc = _NC_CACHE["nc"]
    res = run_bass_kernel_spmd(nc, maps, core_ids=list(range(8)))
    out = np.zeros((2, S, D), np.float32)
    for c in range(8):
        b, q = c // 4, c % 4
        out[b, q * OWN:(q + 1) * OWN] = res.results[c]["out"]
    return out
```

```python
import numpy as np
from contextlib import ExitStack
import concourse.bass as bass
import concourse.mybir as mybir
from concourse.bass_utils import run_bass_kernel_spmd

F32 = mybir.dt.float32
BF16 = mybir.dt.bfloat16
I32 = mybir.dt.int32
AF = mybir.ActivationFunctionType
ALU = mybir.AluOpType
AX = mybir.AxisListType

ENGS = ("pe", "act", "dve", "pool", "sp")
EPOCH = 4096
DMA_SLOTS = 8

D = 1024
S = 8192
OWN = 2048
HALO = 1024
WIN = OWN + 2 * HALO
NTW = WIN // 128
T0 = HALO // 128
NTO = OWN // 128
INW = 3104
NEXP = 32
EPS = 1e-6


class Op:
    __slots__ = ("eng", "fn", "dma", "deps", "sig", "sigcount", "dmaidx", "idx")

    def __init__(self, eng, fn, dma):
        self.eng = eng
        self.fn = fn
        self.dma = dma
        self.deps = []
        self.sig = False
        self.sigcount = 0
        self.dmaidx = -1
        self.idx = -1


class Prog:
    def __init__(self, nc):
        self.nc = nc
        self.ops = []
        self.last_w = {}
        self.readers = {}
        self.ndma = {e: 0 for e in ENGS}
        self.bar = None
        self.rec = None

    def barrier(self):
        deps = set()
        for e in ENGS:
            last = None
            nd = 0
            for o in reversed(self.ops):
                if o.eng != e:
                    continue
                if o.dma:
                    if nd < DMA_SLOTS:
                        deps.add(o.idx)
                        nd += 1
                elif last is None:
                    last = o.idx
                    deps.add(o.idx)
                if last is not None and nd >= DMA_SLOTS:
                    break
        b = self.op("sp", None)
        b.deps = sorted(deps | set(b.deps))
        self.bar = b.idx
        return b

    def replay_merged(self, a, b):
        na, nb = len(a), len(b)
        i = j = 0
        while i < na or j < nb:
            if j >= nb or (i < na and i * nb <= j * na):
                self.op(*a[i])
                i += 1
            else:
                self.op(*b[j])
                j += 1

    def op(self, eng, fn, reads=(), writes=(), dma=False):
        if self.rec is not None:
            self.rec.append((eng, fn, list(reads), list(writes), dma))
            return None
        import os as _os
        mx = int(_os.environ.get("DBG_MAXOPS", "0"))
        if mx and len(self.ops) >= mx and fn is not None:
            fn = None
            if dma:
                dma = False
        px = [k_ for k_ in reads if k_[:2] in ("pf", "pb")]
        if px:
            writes = list(writes) + [k_ for k_ in px if k_ not in writes]
            reads = [k_ for k_ in reads if k_ not in px]
        o = Op(eng, fn, dma)
        o.idx = len(self.ops)
        deps = set()
        if self.bar is not None:
            deps.add(self.bar)
        for k in reads:
            w = self.last_w.get(k)
            if w is not None:
                deps.add(w)
        for k in writes:
            w = self.last_w.get(k)
            if w is not None:
                deps.add(w)
            for r in self.readers.get(k, ()):
                deps.add(r)
        deps.discard(o.idx)
        o.deps = sorted(deps)
        for k in writes:
            self.last_w[k] = o.idx
            self.readers[k] = []
        for k in reads:
            if k not in writes:
                self.readers.setdefault(k, []).append(o.idx)
        if dma:
            o.dmaidx = self.ndma[eng]
            self.ndma[eng] += 1
        self.ops.append(o)
        return o

    def emit(self):
        nc = self.nc
        ops = self.ops
        for o in ops:
            for d in o.deps:
                p = ops[d]
                if not p.dma:
                    p.sig = True
        cnt = {e: 0 for e in ENGS}
        for o in ops:
            if o.sig and not o.dma:
                cnt[o.eng] += 1
                o.sigcount = cnt[o.eng]
        nsem = {e: (cnt[e] + EPOCH - 1) // EPOCH for e in ENGS}
        with ExitStack() as es:
            csem = {e: [es.enter_context(nc.semaphore("c_%s_%d" % (e, i))) for i in range(nsem[e])]
                    for e in ENGS}
            dsem = {e: [es.enter_context(nc.semaphore("d_%s_%d" % (e, i)))
                        for i in range(DMA_SLOTS if self.ndma[e] else 0)] for e in ENGS}
            block = es.enter_context(nc.Block())

            def body_for(e):
                def body(eng):
                    waited_c = {x: 0 for x in ENGS}
                    waited_d = {}
                    for o in ops:
                        if o.eng != e:
                            continue
                        need_c = {}
                        need_d = {}
                        for d in o.deps:
                            p = ops[d]
                            if p.dma:
                                slot = p.dmaidx % DMA_SLOTS
                                val = 16 * (p.dmaidx // DMA_SLOTS + 1)
                                key = (p.eng, slot)
                                if waited_d.get(key, 0) < val:
                                    need_d[key] = max(need_d.get(key, 0), val)
                            else:
                                if waited_c[p.eng] < p.sigcount:
                                    need_c[p.eng] = max(need_c.get(p.eng, 0), p.sigcount)
                        if o.dma:
                            slot = o.dmaidx % DMA_SLOTS
                            val = 16 * (o.dmaidx // DMA_SLOTS)
                            key = (e, slot)
                            if val > 0 and waited_d.get(key, 0) < val:
                                need_d[key] = max(need_d.get(key, 0), val)
                        for pe_, c in need_c.items():
                            ep = (c - 1) // EPOCH
                            eng.wait_ge(csem[pe_][ep], (c - 1) % EPOCH + 1)
                            waited_c[pe_] = c
                        for key, val in need_d.items():
                            eng.wait_ge(dsem[key[0]][key[1]], val)
                            waited_d[key] = val
                        ins = o.fn(eng) if o.fn is not None else None
                        if o.dma:
                            ins.then_inc(dsem[e][o.dmaidx % DMA_SLOTS], 16)
                        elif o.sig:
                            if ins is None:
                                ins = eng.nop()
                            ep = (o.sigcount - 1) // EPOCH
                            ins.then_inc(csem[e][ep], 1)
                return body

            block.tensor(body_for("pe"))
            block.scalar(body_for("act"))
            block.vector(body_for("dve"))
            block.gpsimd(body_for("pool"))
            block.sync(body_for("sp"))


class Arena:
    def __init__(self, ap, ncols):
        self.ap = ap
        self.n = ncols
        self.off = 0

    def alloc(self, shape, dt=F32):
        p = shape[0]
        rest = list(shape[1:])
        nel = 1
        for r in rest:
            nel *= r
        ncol = nel if dt in (F32, I32) else (nel + 1) // 2
        ncol += ncol % 2
        assert self.off + ncol <= self.n, "arena overflow: need %d have %d" % (ncol, self.n - self.off)
        v = self.ap[0:p, self.off:self.off + ncol]
        self.off += ncol
        if dt != F32:
            v = v.bitcast(dt)
        if v.shape[1] != nel:
            v = v[:, 0:nel]
        if len(rest) == 2:
            v = v.rearrange("p (a b) -> p a b", a=rest[0])
        elif len(rest) == 3:
            v = v.rearrange("p (a b c) -> p a b c", a=rest[0], b=rest[1])
        return v


def build_program(debug=False, stop_after=None, dbg_tiles=None):
    nc = bass.Bass("TRN2", target_bir_lowering=False)
    P = Prog(nc)
    global LASTP
    LASTP = P

    def din(name, shape, dt=F32):
        return nc.dram_tensor(name, list(shape), dt, kind="ExternalInput").ap()

    def dscr(name, shape, dt):
        kind = "ExternalOutput" if debug else "Internal"
        return nc.dram_tensor(name, list(shape), dt, kind=kind).ap()

    xw = din("xw", [WIN, D])
    vcol = din("vcol", [128, NTW])
    cs_t = din("cs_t", [128, NTW, 16])
    cmat = din("cmat", [128, 8, 128])
    band3 = din("band3", [128, 384])
    w_in = din("w_in", [D, INW])
    wz_d = din("wz", [33, 512])
    vecs = din("vecs", [4, D])
    w_out = din("w_out", [D, D])
    wr_d = din("wr", [D, 36])
    rb_d = din("rb", [1, 36])
    ewg = din("ewg", [NEXP, D, 512])
    ewu = din("ewu", [NEXP, D, 512])
    ewd = din("ewd", [NEXP, 512, D])
    out_d = nc.dram_tensor("out", [OWN, D], F32, kind="ExternalOutput").ap()
    QS = dscr("QS", [OWN, 512], BF16)
    KS = dscr("KS", [WIN + 2 * HALO, 512], BF16)
    VS = dscr("VS", [WIN + 2 * HALO, 520], BF16)
    GV = dscr("GV", [OWN, 512], BF16)
    GG = dscr("GG", [OWN, 512], BF16)
    MG = dscr("MG", [OWN, 512], BF16)
    OTS = dscr("OTS", [8, 64, OWN], BF16)
    H2 = dscr("H2", [OWN, D], F32)
    XB = dscr("XB", [2560, D], BF16)
    WB = dscr("WB", [2560, 8], F32)
    YB = dscr("YB", [2560, D], F32)
    WTD = nc.dram_tensor("WTD", [128, NTO * 32], F32, kind="ExternalOutput").ap() if debug else None

    QSK = ["QS%d" % i for i in range(NTO)]
    KSK = ["KS%d" % i for i in range(48)]
    VSK = ["VS%d" % i for i in range(48)]
    NCOL = 50 * 1024 + 512
    es = ExitStack()
    with es:
        arena_t = es.enter_context(nc.sbuf_tensor("arena", [128, NCOL], F32))
        AR = Arena(arena_t[:], NCOL)
        sb = lambda name, shape, dt=F32: AR.alloc(shape, dt)

        def ps(name, shape, dt=F32):
            return es.enter_context(nc.psum_tensor("p_" + name, list(shape), dt))

        pf = [ps("pf%d" % i, [128, 512]) for i in range(6)]
        pb = [ps("pb%d" % i, [128, 1024], BF16) for i in range(2)]
        pf_rr = [0]
        pb_rr = [0]

        stream = [None]
        srr = [0, 0]

        def next_pf():
            if stream[0] is None:
                i = pf_rr[0] % 6
                pf_rr[0] += 1
            else:
                s_ = stream[0]
                i = 3 * s_ + srr[s_] % 3
                srr[s_] += 1
            return pf[i], "pf%d" % i

        def next_pb():
            if stream[0] is None:
                i = pb_rr[0] % 2
                pb_rr[0] += 1
            else:
                i = stream[0]
            return pb[i], "pb%d" % i

        def mm_group(out_ap, pairs, okey, rkeys):
            def fn(e):
                ins = None
                n = len(pairs)
                for j, (l, r) in enumerate(pairs):
                    ins = e.matmul(out_ap, lhsT=l, rhs=r, start=(j == 0), stop=(j == n - 1))
                return ins
            P.op("pe", fn, reads=rkeys, writes=[okey])

        def ACT(out, in_, func, reads, writes, **kw):
            P.op("act", lambda e: e.activation(out=out, in_=in_, func=func, **kw), reads=reads, writes=writes)

        def TT(eng, out, in0, in1, op, reads, writes):
            P.op(eng, lambda e: e.tensor_tensor(out=out, in0=in0, in1=in1, op=op), reads=reads, writes=writes)

        def STT(out, in0, scalar, in1, op0, op1, reads, writes):
            P.op("dve", lambda e: e.scalar_tensor_tensor(out=out, in0=in0, scalar=scalar, in1=in1, op0=op0, op1=op1),
                 reads=reads, writes=writes)

        def TS(eng, out, in0, s1, s2, op0, op1, reads, writes):
            if op1 is None:
                P.op(eng, lambda e: e.tensor_scalar(out=out, in0=in0, scalar1=s1, scalar2=None, op0=op0), reads=reads, writes=writes)
            else:
                P.op(eng, lambda e: e.tensor_scalar(out=out, in0=in0, scalar1=s1, scalar2=s2, op0=op0, op1=op1), reads=reads, writes=writes)

        def CP(eng, out, in_, reads, writes):
            if eng == "act":
                ACT(out, in_, AF.Copy, reads, writes)
            else:
                P.op(eng, lambda e: e.tensor_copy(out=out, in_=in_), reads=reads, writes=writes)

        def DMA(q, out, in_, reads, writes):
            return P.op(q, lambda e: e.dma_start(out=out, in_=in_), reads=reads, writes=writes, dma=True)

        def rstd_from_ssq(dst, src, n, rk, wk):
            ACT(dst, src, AF.Ln, [rk, "epsc"], [wk], scale=1.0 / n, bias=epsc[0:dst.shape[0], :])
            ACT(dst, dst, AF.Exp, [wk], [wk], scale=-0.5)

        cm = sb("cm", [128, 8, 128])
        identb = sb("identb", [128, 128], BF16)
        band = sb("band", [128, 384], BF16)
        maskFB = sb("maskFB", [128, 4, 128])
        n16col = sb("n16col", [128, 2])
        epsc = sb("epsc", [128, 2])
        onec = sb("onec", [128, 2])
        negc = sb("negc", [128, 2])
        vc = sb("vc", [128, NTW])
        cst = sb("cst", [128, NTW, 16])
        wz = sb("wz", [33, 512])
        n1bc = sb("n1bc", [128, D])
        n2bc = sb("n2bc", [128, D])
        fnbc = sb("fnbc", [128, D])
        gnbc = sb("gnbc", [128, 512])
        rbbc = sb("rbbc", [128, 36])
        wr = sb("wr", [128, 8, 36])
        nmax = sb("nmax", [128, 16])
        n16col = n16col[:, 0:1]
        epsc = epsc[:, 0:1]
        onec = onec[:, 0:1]
        negc = negc[:, 0:1]

        DMA("sp", cm, cmat, [], ["cm"])
        DMA("pool", identb, cmat[:, 0, :], [], ["identb"])
        DMA("pool", band, band3, [], ["band"])
        DMA("sp", vc, vcol, [], ["vc"])
        DMA("sp", cst, cs_t, [], ["cst"])
        DMA("sp", wz, wz_d, [], ["wz"])
        DMA("sp", n1bc, vecs[0:1, :].partition_broadcast(128), [], ["n1bc"])
        DMA("sp", n2bc, vecs[1:2, :].partition_broadcast(128), [], ["n2bc"])
        DMA("sp", fnbc, vecs[2:3, :].partition_broadcast(128), [], ["fnbc"])
        DMA("sp", gnbc, vecs[3:4, 0:512].partition_broadcast(128), [], ["gnbc"])
        DMA("sp", rbbc, rb_d[0:1, :].partition_broadcast(128), [], ["rbbc"])
        DMA("sp", wr, wr_d.rearrange("(c p) n -> p c n", p=128), [], ["wr"])
        P.op("dve", lambda e: e.memset(n16col, -1.0 / 16.0), writes=["n16col"])
        P.op("dve", lambda e: e.memset(epsc, EPS), writes=["epsc"])
        P.op("dve", lambda e: e.memset(onec, 1.0), writes=["onec"])
        P.op("dve", lambda e: e.memset(nmax, 0.0), writes=["nmax"])
        for h in range(4):
            CP("dve", maskFB[:, h, :], cm[:, 5 + h // 2, :], ["cm"], ["maskFB"])
        M0 = AR.off

        attnT = sb("attnT", [128, NTO, 512], BF16)
        qdT = sb("qdT", [128, NTO, 4, 128], BF16)
        SfT = sb("SfT", [128, NTO, 2, 128], BF16)
        SbT = sb("SbT", [128, NTO, 2, 128], BF16)
        M1 = AR.off
        win = sb("win", [128, 8, INW], BF16)
        for c in range(8):
            DMA("pool", win[:, c, :], w_in[c * 128:(c + 1) * 128, :], [], ["win%d" % c])
        winkeys = ["win%d" % c for c in range(8)]
        kvB = sb("kvB", [128, NTW - T0, 2, 128], BF16)
        decB = sb("decB", [128, NTW - T0, 2])
        Sf = sb("Sf", [128, 2, 128])
        Sb = sb("Sb", [128, 2, 128])
        P.op("dve", lambda e: e.memset(Sf, 0.0), writes=["Sf"])
        P.op("dve", lambda e: e.memset(Sb, 0.0), writes=["Sb"])
        zt = sb("zt", [128, 520], BF16)
        P.op("pool", lambda e: e.memset(zt, 0.0), writes=["zt"])
        for blk in range(HALO // 128):
            for base in (0, HALO + WIN):
                r0 = base + blk * 128
                DMA("sp", KS[r0:r0 + 128, :], zt[:, 0:512], ["zt"], ["KS%d" % (r0 // 128)])
                DMA("sp", VS[r0:r0 + 128, :], zt, ["zt"], ["VS%d" % (r0 // 128)])
        xt = [sb("xt%d" % i, [128, D]) for i in range(2)]
        junk = sb("junk", [128, D], BF16)
        xn = [sb("xn%d" % i, [128, D], BF16) for i in range(2)]
        xnT = [sb("xnT%d" % i, [128, 8, 128], BF16) for i in range(2)]
        ssq = sb("ssq", [128, 2])
        rstd = sb("rstd", [128, 2])
        qk = [sb("qk%d" % i, [128, 512]) for i in range(2)]
        vbf = [sb("vbf%d" % i, [128, 512], BF16) for i in range(2)]
        gbf = [sb("gbf%d" % i, [128, 512], BF16) for i in range(2)]
        lr = [sb("lr%d" % i, [128, 32]) for i in range(2)]
        aqr = [sb("aqr%d" % i, [128, 8, 64]) for i in range(2)]
        akr = [sb("akr%d" % i, [128, 8, 64]) for i in range(2)]
        vab = [sb("vab%d" % i, [128, 8, 65], BF16) for i in range(2)]
        lrT = sb("lrT", [33, 128])
        ez = sb("ez", [128, 512])
        spl = sb("spl", [128, 512])
        E1 = sb("E1", [128, 512])
        E2 = sb("E2", [128, 512])
        E3 = sb("E3", [128, 512])
        dec = sb("dec", [128, 4])
        qd = sb("qd", [128, 512], BF16)
        ki = sb("ki", [128, 512], BF16)
        ke = sb("ke", [128, 512], BF16)
        kiT = sb("kiT", [128, 4, 128], BF16)
        qrb = [sb("qrb%d" % i, [128, 512], BF16) for i in range(2)]
        krb = [sb("krb%d" % i, [128, 512], BF16) for i in range(2)]
        rta = sb("rta", [128, 8, 8])
        rtb = sb("rtb", [128, 8, 8])
        rtc = sb("rtc", [128, 8, 8])
        rtd = sb("rtd", [128, 8, 8])
        sqs = sb("sqs", [128, 8, 64])
        nrm = sb("nrm", [128, 16])
        P.op("dve", lambda e: e.memset(lrT[32:33, :], 1.0), writes=["lrT_one"])
        tiles = list(range(NTW) if dbg_tiles is None else dbg_tiles)

        def S1(i):
            own = T0 <= i < T0 + NTO
            io = i - T0
            b2 = i % 2
            xtk, xnk, xnTk = "xt%d" % b2, "xn%d" % b2, "xnT%d" % b2
            if i == tiles[0]:
                DMA("sp", xt[b2], xw[i * 128:(i + 1) * 128, :], [], [xtk])
            if i + 1 < NTW and (dbg_tiles is None):
                DMA("sp", xt[(i + 1) % 2], xw[(i + 1) * 128:(i + 2) * 128, :], [], ["xt%d" % ((i + 1) % 2)])
            sk, rk = "ssq%d" % b2, "rstd%d" % b2
            P.op("act", lambda e, b2=b2: e.activation(out=junk, in_=xt[b2], func=AF.Square, accum_out=ssq[:, b2:b2 + 1]),
                 reads=[xtk], writes=["junk", sk])
            rstd_from_ssq(rstd[:, b2:b2 + 1], ssq[:, b2:b2 + 1], D, sk, rk)
            STT(xn[b2], xt[b2], rstd[:, b2:b2 + 1], n1bc, ALU.mult, ALU.mult, [xtk, rk, "n1bc"], [xnk])
            pbt, pbk = next_pb()

            def tr_fn(e, b2=b2, pbt=pbt):
                ins = None
                for c in range(8):
                    ins = e.transpose(out=pbt[:, c * 128:(c + 1) * 128], in_=xn[b2][:, c * 128:(c + 1) * 128], identity=identb)
                return ins
            P.op("pe", tr_fn, reads=[xnk, "identb"], writes=[pbk])
            CP("act", xnT[b2].rearrange("p c t -> p (c t)"), pbt[:, :], [pbk], [xnTk])

            def proj(c0, c1):
                pt, pk = next_pf()
                n = c1 - c0
                mm_group(pt[:, 0:n], [(xnT[b2][:, c, :], win[:, c, c0:c1]) for c in range(8)], pk, [xnTk] + winkeys)
                return pt, pk

            if own:
                pt, pk = proj(0, 512)
                CP("act", qk[b2], pt[:, 0:512], [pk], ["qk%d" % b2])
            else:
                pt, pk = proj(256, 512)
                CP("act", qk[b2][:, 256:512], pt[:, 0:256], [pk], ["qk%d" % b2])
            pt, pk = proj(512, 1024)
            CP("dve", vbf[b2], pt[:, 0:512], [pk], ["vbf%d" % b2])
            if own:
                DMA("sp", GV[io * 128:(io + 1) * 128, :], vbf[b2], ["vbf%d" % b2], ["GV%d" % io])
                pt, pk = proj(1024, 1536)
                CP("act", gbf[b2], pt[:, 0:512], [pk], ["gbf%d" % b2])
                DMA("sp", GG[io * 128:(io + 1) * 128, :], gbf[b2], ["gbf%d" % b2], ["GG%d" % io])
            pt, pk = proj(1536, 1568)
            CP("dve", lr[b2], pt[:, 0:32], [pk], ["lr%d" % b2])
            if own:
                pt, pk = proj(1568, 2080)
                CP("dve", aqr[b2].rearrange("p h d -> p (h d)"), pt[:, 0:512], [pk], ["aqr%d" % b2])
            pt, pk = proj(2080, 2592)
            CP("act", akr[b2].rearrange("p h d -> p (h d)"), pt[:, 0:512], [pk], ["akr%d" % b2])
            pt, pk = proj(2592, 3104)
            r0k = HALO + i * 128
            CP("act", vab[b2][:, :, 0:64], pt[:, 0:512].rearrange("p (h d) -> p h d", h=8), [pk], ["vab%d" % b2])
            CP("dve", vab[b2][:, :, 64:65], vc[:, i:i + 1].unsqueeze(1).broadcast_to([128, 8, 1]), ["vc"], ["vab%d" % b2])
            DMA("sp", VS[r0k:r0k + 128, :], vab[b2].rearrange("p h d -> p (h d)"), ["vab%d" % b2], ["VS%d" % (r0k // 128)])

        def S2(i):
            own = T0 <= i < T0 + NTO
            left = i < T0
            io = i - T0
            b2 = i % 2
            qkb = qk[b2]
            qkk = "qk%d" % b2
            vkey = "vbf%d" % b2
            vt = vbf[b2]
            pt, pk = next_pf()
            P.op("pe", lambda e, pt=pt: e.transpose(out=pt[0:32, 0:128], in_=lr[b2], identity=cm[:, 0, :]), reads=["lr%d" % b2, "cm"], writes=[pk])
            CP("dve", lrT[0:32, :], pt[0:32, 0:128], [pk], ["lrT"])
            pz, pzk = next_pf()
            P.op("pe", lambda e, pz=pz: e.matmul(pz[:, :], lhsT=lrT, rhs=wz, start=True, stop=True),
                 reads=["lrT", "lrT_one", "wz"], writes=[pzk])
            ACT(ez, pz[:, :], AF.Exp, [pzk], ["ez"], scale=-1.0)
            ACT(spl, ez, AF.Ln, ["ez", "onec"], ["spl"], bias=onec, scale=1.0)
            pbb, pbbk = next_pf()
            P.op("pe", lambda e, pbb=pbb: (e.matmul(pbb[:, 0:256], lhsT=cm[:, 1, :], rhs=spl[:, 0:256], start=True, stop=True),
                                           e.matmul(pbb[:, 256:512], lhsT=cm[:, 2, :], rhs=spl[:, 256:512], start=True, stop=True))[1],
                 reads=["cm", "spl"], writes=[pbbk])
            ACT(E1, pbb[:, :], AF.Exp, [pbbk], ["E1"])
            ACT(E2, pbb[:, :], AF.Exp, [pbbk], ["E2"], scale=-1.0)
            pb3, pb3k = next_pf()
            P.op("pe", lambda e, pb3=pb3: (e.matmul(pb3[:, 0:256], lhsT=cm[:, 3, :], rhs=spl[:, 0:256], start=True, stop=True),
                                           e.matmul(pb3[:, 256:512], lhsT=cm[:, 4, :], rhs=spl[:, 256:512], start=True, stop=True))[1],
                 reads=["cm", "spl"], writes=[pb3k])
            ACT(E3, pb3[:, :], AF.Exp, [pb3k], ["E3"])
            pdc, pdck = next_pf()

            def dec_fn(e, pdc=pdc):
                ins = None
                for j in range(4):
                    ins = e.matmul(pdc[:, j:j + 1], lhsT=spl[:, j * 128:(j + 1) * 128], rhs=n16col, start=True, stop=True)
                return ins
            P.op("pe", dec_fn, reads=["spl", "n16col"], writes=[pdck])
            ACT(dec, pdc[:, 0:4], AF.Exp, [pdck], ["dec"])
            if own:
                STT(qd[:, 0:256], qkb[:, 0:256], 0.125, E1[:, 0:256], ALU.mult, ALU.mult, [qkk, "E1"], ["qd"])
                STT(qd[:, 256:512], qkb[:, 0:256], 0.125, E1[:, 256:512], ALU.mult, ALU.mult, [qkk, "E1"], ["qd"])
                TT("pool", ki[:, 0:256], qkb[:, 256:512], E2[:, 0:256], ALU.mult, [qkk, "E2"], ["ki"])
                TT("pool", ki[:, 256:512], qkb[:, 256:512], E2[:, 256:512], ALU.mult, [qkk, "E2"], ["ki"])
            TT("pool", ke[:, 0:256], qkb[:, 256:512], E3[:, 0:256], ALU.mult, [qkk, "E3"], ["ke"])
            TT("pool", ke[:, 256:512], qkb[:, 256:512], E3[:, 256:512], ALU.mult, [qkk, "E3"], ["ke"])
            if own:
                pbt, pbk = next_pb()

                def tr2_fn(e, pbt=pbt):
                    ins = None
                    for j in range(4):
                        ins = e.transpose(out=pbt[:, j * 128:(j + 1) * 128], in_=qd[:, j * 128:(j + 1) * 128], identity=identb)
                    for j in range(4):
                        ins = e.transpose(out=pbt[:, 512 + j * 128:512 + (j + 1) * 128], in_=ki[:, j * 128:(j + 1) * 128], identity=identb)
                    return ins
                P.op("pe", tr2_fn, reads=["qd", "ki", "identb"], writes=[pbk])
                CP("act", qdT[:, io, :, :].rearrange("p c t -> p (c t)"), pbt[:, 0:512], [pbk], ["qdT%d" % io])
                CP("dve", kiT.rearrange("p c t -> p (c t)"), pbt[:, 512:1024], [pbk], ["kiT"])
                paX, paXk = next_pf()
                paY, paYk = next_pf()

                def att_fn(e, pa, par, io=io):
                    ins = None
                    p0 = par * 64
                    for dirn in range(2):
                        for pr in range(2):
                            blk = dirn * 2 + pr
                            sl = dirn * 2 + pr
                            ins = e.matmul(pa[:, sl * 128:(sl + 1) * 128], lhsT=kiT[p0:p0 + 64, blk, :], rhs=qdT[p0:p0 + 64, io, blk, :],
                                           start=True, stop=True)
                    return ins
                P.op("pe", lambda e, pa=paX, f=att_fn: f(e, pa, 0), reads=["kiT", "qdT%d" % io], writes=[paXk])
                P.op("pe", lambda e, pa=paY, f=att_fn: f(e, pa, 1), reads=["kiT", "qdT%d" % io], writes=[paYk])
                TT("dve", ez, paX[:, :], maskFB.rearrange("p h c -> p (h c)"), ALU.mult, [paXk, "maskFB"], ["ez"])
                TT("dve", E1, paY[:, :], maskFB.rearrange("p h c -> p (h c)"), ALU.mult, [paYk, "maskFB"], ["E1"])
                av = attnT[:, io, :].rearrange("p (a b c) -> p a b c", a=2, b=2)
                TT("pool", av[:, :, 0, :], ez[:, 0:256].rearrange("p (a c) -> p a c", a=2), ez[:, 256:512].rearrange("p (a c) -> p a c", a=2),
                   ALU.add, ["ez"], ["attnT%d" % io])
                TT("pool", av[:, :, 1, :], E1[:, 0:256].rearrange("p (a c) -> p a c", a=2), E1[:, 256:512].rearrange("p (a c) -> p a c", a=2),
                   ALU.add, ["E1"], ["attnT%d" % io])
            for dirn in range(2):
                if dirn == 0 and i >= T0 + NTO:
                    continue
                if dirn == 1 and left:
                    continue
                pkv, pkvk = next_pf()

                def kv_fn(e, pkv=pkv, dirn=dirn, vt=vt):
                    ins = None
                    for pr in range(2):
                        ins = e.matmul(pkv[:, pr * 256:(pr + 1) * 256], lhsT=ke[:, dirn * 256 + pr * 128: dirn * 256 + (pr + 1) * 128],
                                       rhs=vt[:, pr * 256:(pr + 1) * 256], start=True, stop=True)
                    return ins
                P.op("pe", kv_fn, reads=["ke", vkey], writes=[pkvk])
                if dirn == 0:
                    if own:
                        CP("act", SfT[:, io, :, :].rearrange("p a b -> p (a b)"), Sf.rearrange("p a b -> p (a b)"), ["Sf"], ["SfT%d" % io])
                    for pr in range(2):
                        for hh in range(2):
                            p0 = hh * 64
                            STT(Sf[p0:p0 + 64, pr, :], Sf[p0:p0 + 64, pr, :], dec[p0:p0 + 64, pr:pr + 1],
                                pkv[p0:p0 + 64, pr * 256 + hh * 128: pr * 256 + (hh + 1) * 128], ALU.mult, ALU.add,
                                ["Sf", "dec", pkvk], ["Sf"])
                else:
                    ib = i - T0
                    for pr in range(2):
                        for hh in range(2):
                            p0 = hh * 64
                            CP("act", kvB[p0:p0 + 64, ib, pr, :], pkv[p0:p0 + 64, pr * 256 + hh * 128: pr * 256 + (hh + 1) * 128],
                               [pkvk], ["kvB%d" % ib])
                    CP("dve", decB[:, ib, :], dec[:, 2:4], ["dec"], ["decB%d" % ib])

            def rope(raw, rkey):
                cosb = cst[:, i, 0:8].unsqueeze(1).broadcast_to([128, 8, 8])
                sinb = cst[:, i, 8:16].unsqueeze(1).broadcast_to([128, 8, 8])
                TT("pool", rta, raw[:, :, 0:8], cosb, ALU.mult, [rkey, "cst"], ["rta"])
                TT("pool", rtb, raw[:, :, 8:16], sinb, ALU.mult, [rkey, "cst"], ["rtb"])
                TT("pool", rtc, raw[:, :, 8:16], cosb, ALU.mult, [rkey, "cst"], ["rtc"])
                TT("pool", rtd, raw[:, :, 0:8], sinb, ALU.mult, [rkey, "cst"], ["rtd"])
                TT("dve", raw[:, :, 0:8], rta, rtb, ALU.subtract, ["rta", "rtb"], [rkey])
                TT("dve", raw[:, :, 8:16], rtc, rtd, ALU.add, ["rtc", "rtd"], [rkey])

            def sqnorm(src, col0, skey):
                TT("pool", sqs, src, src, ALU.mult, [skey], ["sqs"])
                P.op("dve", lambda e: e.tensor_reduce(out=nrm[:, col0:col0 + 8], in_=sqs, axis=AX.X, op=ALU.add), reads=["sqs"], writes=["nrm"])
                TT("dve", nmax[:, col0:col0 + 8], nmax[:, col0:col0 + 8], nrm[:, col0:col0 + 8], ALU.max, ["nrm", "nmax"], ["nmax"])

            r0k = HALO + i * 128
            if own:
                rope(aqr[b2], "aqr%d" % b2)
                sqnorm(aqr[b2], 0, "aqr%d" % b2)
                CP("act", qrb[b2], aqr[b2].rearrange("p h d -> p (h d)"), ["aqr%d" % b2], ["qrb%d" % b2])
                DMA("sp", QS[io * 128:(io + 1) * 128, :], qrb[b2], ["qrb%d" % b2], ["QS%d" % io])
            rope(akr[b2], "akr%d" % b2)
            sqnorm(akr[b2], 8, "akr%d" % b2)
            CP("act", krb[b2], akr[b2].rearrange("p h d -> p (h d)"), ["akr%d" % b2], ["krb%d" % b2])
            DMA("sp", KS[r0k:r0k + 128, :], krb[b2], ["krb%d" % b2], ["KS%d" % (r0k // 128)])

        for n_ in range(len(tiles) + 1):
            ra, rb = [], []
            if n_ < len(tiles):
                P.rec = ra
                stream[0] = 0
                S1(tiles[n_])
            if n_ > 0:
                P.rec = rb
                stream[0] = 1
                S2(tiles[n_ - 1])
            P.rec = None
            stream[0] = None
            P.replay_merged(ra, rb)

        if stop_after == "A":
            P.op("sp", None, reads=[k_ for k_ in P.last_w.keys() if k_[:2] in ("QS", "KS", "VS", "GV", "GG")], writes=[])
            P.emit()
            return nc
        for i in range(NTW - 1, T0 - 1, -1):
            ib = i - T0
            if ib < NTO:
                CP("act", SbT[:, ib, :, :].rearrange("p a b -> p (a b)"), Sb.rearrange("p a b -> p (a b)"), ["Sb"], ["SbT%d" % ib])
            if i == T0:
                break
            for pr in range(2):
                STT(Sb[:, pr, :], Sb[:, pr, :], decB[:, ib, pr:pr + 1], kvB[:, ib, pr, :], ALU.mult, ALU.add,
                    ["Sb", "decB%d" % ib, "kvB%d" % ib], ["Sb"])

        P.barrier()
        AR.off = M1
        vb2 = [sb("vb2%d" % i, [128, 512], BF16) for i in range(2)]
        gb2 = [sb("gb2%d" % i, [128, 512], BF16) for i in range(2)]
        osb2 = [sb("osb%d" % i, [128, 512]) for i in range(2)]
        osq2 = [sb("osq%d" % i, [128, 4, 128]) for i in range(2)]
        oms2 = [sb("oms%d" % i, [128, 4]) for i in range(2)]
        sgs2 = [sb("sgs%d" % i, [128, 512]) for i in range(2)]
        ybf2 = [sb("ybf%d" % i, [128, 512]) for i in range(2)]
        mixb = [sb("mixb%d" % i, [128, 512], BF16) for i in range(2)]

        def g2_body(io):
            b2 = io % 2
            osb, osq, oms, sgs, ybf = osb2[b2], osq2[b2], oms2[b2], sgs2[b2], ybf2[b2]
            ok_, qk_, mk_, sk_, yk_ = "osb%d" % b2, "osq%d" % b2, "oms%d" % b2, "sgs%d" % b2, "ybf%d" % b2
            DMA("sp", vb2[b2], GV[io * 128:(io + 1) * 128, :], ["GV%d" % io], ["vb2%d" % b2])
            DMA("sp", gb2[b2], GG[io * 128:(io + 1) * 128, :], ["GG%d" % io], ["gb2%d" % b2])
            poX, poXk = next_pf()
            poY, poYk = next_pf()

            def o_fn(e, po, par):
                ins = None
                p0 = par * 64
                for pr in range(2):
                    h = pr * 2 + par
                    oap = po[:, pr * 128:(pr + 1) * 128]
                    e.matmul(oap, lhsT=attnT[:, io, h * 128:(h + 1) * 128], rhs=vb2[b2][:, h * 128:(h + 1) * 128], start=True, stop=False)
                    e.matmul(oap, lhsT=qdT[p0:p0 + 64, io, pr, :], rhs=SfT[p0:p0 + 64, io, pr, :], start=False, stop=False)
                    ins = e.matmul(oap, lhsT=qdT[p0:p0 + 64, io, 2 + pr, :], rhs=SbT[p0:p0 + 64, io, pr, :], start=False, stop=True)
                return ins
            rk_ = ["attnT%d" % io, "qdT%d" % io, "SfT%d" % io, "SbT%d" % io, "vb2%d" % b2]
            P.op("pe", lambda e: o_fn(e, poX, 0), reads=rk_, writes=[poXk])
            P.op("pe", lambda e: o_fn(e, poY, 1), reads=rk_, writes=[poYk])
            ov = osb.rearrange("p (a b c) -> p a b c", a=2, b=2)
            CP("act", ov[:, :, 0, :], poX[:, 0:256].rearrange("p (a c) -> p a c", a=2), [poXk], [ok_])
            CP("act", ov[:, :, 1, :], poY[:, 0:256].rearrange("p (a c) -> p a c", a=2), [poYk], [ok_])
            TT("pool", osq.rearrange("p h d -> p (h d)"), osb, osb, ALU.mult, [ok_], [qk_])
            P.op("dve", lambda e: e.tensor_reduce(out=oms, in_=osq, axis=AX.X, op=ALU.add), reads=[qk_], writes=[mk_])
            rstd_from_ssq(oms, oms, 128, mk_, mk_)
            ACT(sgs, gb2[b2], AF.Silu, ["gb2%d" % b2], [sk_])
            TT("dve", ybf, osb, gnbc, ALU.mult, [ok_, "gnbc"], [yk_])
            for h in range(4):
                STT(mixb[b2][:, h * 128:(h + 1) * 128], ybf[:, h * 128:(h + 1) * 128], oms[:, h:h + 1], sgs[:, h * 128:(h + 1) * 128],
                    ALU.mult, ALU.mult, [yk_, mk_, sk_], ["mixb%d" % b2])
            DMA("sp", MG[io * 128:(io + 1) * 128, :], mixb[b2], ["mixb%d" % b2], ["MG%d" % io])

        for m in range(0, NTO, 2):
            ra, rb = [], []
            P.rec = ra
            stream[0] = 0
            g2_body(m)
            P.rec = rb
            stream[0] = 1
            g2_body(m + 1)
            P.rec = None
            stream[0] = None
            P.replay_merged(ra, rb)
        MGK = ["MG%d" % i for i in range(NTO)]
        if stop_after == "G2":
            P.op("sp", None, reads=QSK + KSK + VSK + MGK, writes=[])
            P.emit()
            return nc

        P.barrier()
        AR.off = M0
        nm2 = sb("nm2", [128, 2])
        m2 = sb("m2", [2, 2])
        m1 = sb("m1", [1, 4])
        P.op("dve", lambda e: e.tensor_reduce(out=nm2, in_=nmax.rearrange("p (a h) -> p a h", a=2), axis=AX.X, op=ALU.max),
             reads=["nmax"], writes=["nm2"])
        pt, pk = next_pf()
        P.op("pe", lambda e, pt=pt: e.transpose(out=pt[0:2, 0:128], in_=nm2, identity=cm[:, 0, :]), reads=["nm2", "cm"], writes=[pk])
        P.op("dve", lambda e, pt=pt: e.tensor_reduce(out=m2[:, 0:1], in_=pt[0:2, 0:128], axis=AX.X, op=ALU.max), reads=[pk], writes=["m2"])
        pt, pk = next_pf()
        P.op("pe", lambda e, pt=pt: e.transpose(out=pt[0:1, 0:2], in_=m2[:, 0:1], identity=cm[0:2, 0, 0:2]), reads=["m2", "cm"], writes=[pk])
        CP("dve", m1[:, 0:2], pt[0:1, 0:2], [pk], ["m1"])
        TT("dve", m1[:, 2:3], m1[:, 0:1], m1[:, 1:2], ALU.mult, ["m1"], ["m1"])
        ACT(m1[:, 3:4], m1[:, 2:3], AF.Ln, ["m1"], ["m1"])
        ACT(m1[:, 3:4], m1[:, 3:4], AF.Exp, ["m1"], ["m1"], scale=0.5)
        TS("dve", m1[:, 3:4], m1[:, 3:4], -0.125, None, ALU.mult, None, ["m1"], ["m1"])
        pt, pk = next_pf()
        P.op("pe", lambda e, pt=pt: e.matmul(pt[:, 0:1], lhsT=cm[0:1, 7, :], rhs=m1[:, 3:4], start=True, stop=True), reads=["m1", "cm"], writes=[pk])
        CP("dve", negc, pt[:, 0:1], [pk], ["negc"])

        accT = sb("accT", [65, 8, OWN])
        NQ = 8
        qsb2 = [[sb("qsb%d" % i, [128, 512], BF16) for i in range(NQ)] for _ in range(2)]
        ksb2 = [[sb("ksb%d" % i, [128, 512], BF16) for i in range(NQ + 2)] for _ in range(2)]
        vsb2 = [[sb("vsb%d" % i, [128, 8, 65], BF16) for i in range(NQ + 2)] for _ in range(2)]
        qT2 = [[sb("qT%d" % i, [128, 4, 128], BF16) for i in range(NQ)] for _ in range(2)]
        kT2 = [[sb("kT%d" % i, [128, 4, 128], BF16) for i in range(NQ + 2)] for _ in range(2)]
        pex = [sb("pex%d" % i, [128, 384], BF16) for i in range(4)]
        pmk = [sb("pmk%d" % i, [128, 384], BF16) for i in range(4)]
        cnt4 = [0, 0]
        jobs = [(1, 0, 0, 8), (1, 0, 8, 8)] + [(4, r, 0, 4) for r in range(4)] + [(16, r, 0, 1) for r in range(16)]
        for jn, (dd, r, j0, nq) in enumerate(jobs):
            js = jn % 2
            qsb, ksb, vsb, qT, kT = qsb2[js], ksb2[js], vsb2[js], qT2[js], kT2[js]
            QSv = QS.rearrange("(n d) c -> d n c", d=dd)
            KSv = KS.rearrange("(n d) c -> d n c", d=dd)
            VSv = VS.rearrange("(n d) c -> d n c", d=dd)
            accv = accT.rearrange("p h (n d) -> p h d n", d=dd)
            for jq in range(nq):
                n0 = 128 * (j0 + jq)
                DMA("sp", qsb[jq], QSv[r, n0:n0 + 128, :], QSK, [("qsb" + str(js) + "_%d") % jq])
            for kk in range(nq + 2):
                n0 = 2048 // dd + 128 * (j0 + kk - 1)
                DMA("sp", ksb[kk], KSv[r, n0:n0 + 128, :], KSK, [("ksb" + str(js) + "_%d") % kk])
                DMA("sp", vsb[kk].rearrange("p h d -> p (h d)"), VSv[r, n0:n0 + 128, :], VSK, [("vsb" + str(js) + "_%d") % kk])
            tl = [(qsb[jq], ("qsb" + str(js) + "_%d") % jq, qT[jq], ("qT" + str(js) + "_%d") % jq) for jq in range(nq)] + \
                 [(ksb[kk], ("ksb" + str(js) + "_%d") % kk, kT[kk], ("kT" + str(js) + "_%d") % kk) for kk in range(nq + 2)]
            for t0 in range(0, len(tl), 2):
                grp = tl[t0:t0 + 2]
                pbt, pbk = next_pb()

                def trq_fn(e, grp=grp, pbt=pbt):
                    ins = None
                    for gi, (src, _, _, _) in enumerate(grp):
                        for c in range(4):
                            ins = e.transpose(out=pbt[:, gi * 512 + c * 128: gi * 512 + (c + 1) * 128], in_=src[:, c * 128:(c + 1) * 128], identity=identb)
                    return ins
                P.op("pe", trq_fn, reads=[g[1] for g in grp] + ["identb"], writes=[pbk])
                for gi, (_, _, dst, dk) in enumerate(grp):
                    CP("act" if gi == 0 else "dve", dst.rearrange("p c t -> p (c t)"), pbt[:, gi * 512:(gi + 1) * 512], [pbk], [dk])
            def it_body(jq, hg, s_, kT=kT, qT=qT, vsb=vsb, js=js, dd=dd, r=r, j0=j0, accv=accv):
                bufs = []
                for h in range(hg * 4, hg * 4 + 4):
                    p0 = (h % 2) * 64
                    blk = h // 2
                    pS, pSk = next_pf()

                    def s_fn(e, pS=pS, jq=jq, p0=p0, blk=blk, kT=kT, qT=qT):
                        ins = None
                        for sl in range(3):
                            ins = e.matmul(pS[:, sl * 128:(sl + 1) * 128], lhsT=kT[jq + sl][p0:p0 + 64, blk, :], rhs=qT[jq][p0:p0 + 64, blk, :],
                                           start=True, stop=True)
                        return ins
                    P.op("pe", s_fn, reads=[("kT" + str(js) + "_%d") % (jq + sl) for sl in range(3)] + [("qT" + str(js) + "_%d") % jq], writes=[pSk])
                    bi = 2 * s_ + cnt4[s_] % 2
                    cnt4[s_] += 1
                    ACT(pex[bi], pS[:, 0:384], AF.Exp, [pSk, "negc"], ["pex%d" % bi], bias=negc, scale=0.125)
                    TT("pool" if (h % 4 == 3) else "dve", pmk[bi], pex[bi], band, ALU.mult, ["pex%d" % bi, "band"], ["pmk%d" % bi])
                    bufs.append((bi, h))
                    if len(bufs) == 2:
                        pU, pUk = next_pf()

                        def pv_fn(e, pU=pU, jq=jq, bufs=tuple(bufs), vsb=vsb):
                            ins = None
                            for hi, (b_, h_) in enumerate(bufs):
                                for sl in range(3):
                                    ins = e.matmul(pU[0:65, hi * 128:(hi + 1) * 128], lhsT=vsb[jq + sl][:, h_, :], rhs=pmk[b_][:, sl * 128:(sl + 1) * 128],
                                                   start=(sl == 0), stop=(sl == 2))
                            return ins
                        P.op("pe", pv_fn, reads=[("vsb" + str(js) + "_%d") % (jq + sl) for sl in range(3)] + ["pmk%d" % b_ for (b_, _) in bufs], writes=[pUk])
                        n0 = 128 * (j0 + jq)
                        h0 = bufs[0][1]
                        dst = accv[:, h0:h0 + 2, r, n0:n0 + 128]
                        src = pU[0:65, 0:256].rearrange("p (h t) -> p h t", h=2)
                        if dd == 1:
                            CP("dve", dst, src, [pUk], ["accT"])
                        else:
                            TT("dve", dst, src, dst, ALU.add, [pUk, "accT"], ["accT"])
                        bufs = []
            its = [(jq, hg) for jq in range(nq) for hg in range(2)]
            for m in range(0, len(its), 2):
                ra, rb = [], []
                P.rec = ra
                stream[0] = 0
                it_body(its[m][0], its[m][1], 0)
                if m + 1 < len(its):
                    P.rec = rb
                    stream[0] = 1
                    it_body(its[m + 1][0], its[m + 1][1], 1)
                P.rec = None
                stream[0] = None
                P.replay_merged(ra, rb)
        rz = sb("rz", [64, 512])
        otb = [sb("otb%d" % i, [64, 512], BF16) for i in range(2)]
        k2 = 0
        for h in range(8):
            for g in range(4):
                pz, pzk = next_pf()
                P.op("pe", lambda e, pz=pz, h=h, g=g: e.matmul(pz[0:64, :], lhsT=cm[64:65, 7, 0:64], rhs=accT[64:65, h, g * 512:(g + 1) * 512],
                                                                start=True, stop=True), reads=["accT", "cm"], writes=[pzk])
                ACT(rz, pz[0:64, :], AF.Ln, [pzk], ["rz"])
                ACT(rz, rz, AF.Exp, ["rz"], ["rz"], scale=-1.0)
                b2 = k2 % 2
                k2 += 1
                TT("pool", otb[b2], accT[0:64, h, g * 512:(g + 1) * 512], rz, ALU.mult, ["accT", "rz"], ["otb%d" % b2])
                DMA("sp", OTS[h, :, g * 512:(g + 1) * 512], otb[b2], ["otb%d" % b2], ["OTS%d_%d" % (h, g)])
        OTK = ["OTS%d_%d" % (h, g) for h in range(8) for g in range(4)]
        if stop_after == "B":
            P.op("sp", None, reads=OTK + MGK, writes=[])
            P.emit()
            return nc

        P.barrier()
        AR.off = M0
        bc_cache = {}

        def bcreg(e):
            if "r" not in bc_cache:
                bc_cache["r"] = e.to_reg(2559)
            return bc_cache["r"]
        CAPG = 640
        NSLOT = 4 * CAPG
        OOB = 4096.0
        u2tok = sb("u2tok", [128, NTO, D], BF16)
        OH = sb("OH", [128, NTO, 4])
        WE = sb("WE", [128, NTO, 8])
        idxf = sb("idxf", [128, NTO])
        idxi = sb("idxi", [128, 2 * NTO], I32)
        goffm = sb("goffm", [128, 4])
        pren = sb("pren", [128, 4])
        M2 = AR.off
        woutG = sb("woutG", [128, 4, D], BF16)
        woutA = sb("woutA", [64, 8, D], BF16)
        DMA("pool", woutG, w_out[0:512, :].rearrange("(c p) n -> p c n", p=128), [], ["woutG"])
        DMA("pool", woutA, w_out[512:1024, :].rearrange("(h p) n -> p h n", p=64), [], ["woutA"])
        for g in range(4):
            P.op("dve", lambda e, g=g: e.memset(goffm[:, g:g + 1], float(g * CAPG) - OOB), writes=["goffm"])
        P.op("dve", lambda e: e.memset(pren, 0.0), writes=["pren"])
        zx = sb("zx", [128, D], BF16)
        zw = sb("zw", [128, 8])
        P.op("pool", lambda e: e.memset(zx, 0.0), writes=["zx"])
        P.op("pool", lambda e: e.memset(zw, 0.0), writes=["zw"])
        for r0 in range(0, NSLOT, 128):
            DMA("sp", XB[r0:r0 + 128, :], zx, ["zx"], ["XB"])
            DMA("sp", WB[r0:r0 + 128, :], zw, ["zw"], ["WB"])
        xo = [sb("xo%d" % i, [128, D]) for i in range(2)]
        mgl = [sb("mgl%d" % i, [128, 512], BF16) for i in range(2)]
        otl = [sb("otl%d" % i, [64, 8, 128], BF16) for i in range(2)]
        mgT2 = [sb("mgT%d" % i, [128, 4, 128], BF16) for i in range(2)]
        h2t = [sb("h2t%d" % i, [128, D]) for i in range(2)]
        u22 = [sb("u2%d" % i, [128, D]) for i in range(2)]
        u2Tf2 = [sb("u2Tf%d" % i, [128, 8, 128]) for i in range(2)]
        junk22 = [sb("junk2%d" % i, [128, D], BF16) for i in range(2)]
        ss2 = sb("ss2", [128, 2])
        rs2 = sb("rs2", [128, 2])
        lg2 = [sb("lg%d" % i, [128, 36]) for i in range(2)]
        sm2 = [sb("sm%d" % i, [128, 64]) for i in range(2)]
        smr = sb("smr", [128, 16])

        def c1_body(io):
            b2 = io % 2
            mgT, u2, u2Tf, junk2, lg, sm = mgT2[b2], u22[b2], u2Tf2[b2], junk22[b2], lg2[b2], sm2[b2]
            mk, uk, lk, sk_ = "mgT%d" % b2, "u2_%d" % b2, "lg%d" % b2, "sm%d" % b2
            DMA("sp", xo[b2], xw[HALO + io * 128: HALO + (io + 1) * 128, :], [], ["xo%d" % b2])
            DMA("sp", mgl[b2], MG[io * 128:(io + 1) * 128, :], ["MG%d" % io], ["mgl%d" % b2])
            DMA("sp", otl[b2], OTS[:, :, io * 128:(io + 1) * 128].rearrange("h p t -> p h t"), OTK, ["otl%d" % b2])
            pbt, pbk = next_pb()

            def trm_fn(e, pbt=pbt, b2=b2):
                ins = None
                for c in range(4):
                    ins = e.transpose(out=pbt[:, c * 128:(c + 1) * 128], in_=mgl[b2][:, c * 128:(c + 1) * 128], identity=identb)
                return ins
            P.op("pe", trm_fn, reads=["mgl%d" % b2, "identb"], writes=[pbk])
            CP("act", mgT.rearrange("p c t -> p (c t)"), pbt[:, 0:512], [pbk], [mk])
            for cg in range(2):
                pt, pk = next_pf()
                pairs = [(mgT[:, c, :], woutG[:, c, cg * 512:(cg + 1) * 512]) for c in range(4)] + \
                        [(otl[b2][:, h, :], woutA[:, h, cg * 512:(cg + 1) * 512]) for h in range(8)]
                mm_group(pt[:, :], pairs, pk, [mk, "otl%d" % b2, "woutG", "woutA"])
                TT("dve", h2t[b2][:, cg * 512:(cg + 1) * 512], pt[:, :], xo[b2][:, cg * 512:(cg + 1) * 512], ALU.add,
                   [pk, "xo%d" % b2], ["h2t%d" % b2])
            DMA("sp", H2[io * 128:(io + 1) * 128, :], h2t[b2], ["h2t%d" % b2], ["H2_%d" % io])
            P.op("act", lambda e: e.activation(out=junk2, in_=h2t[b2], func=AF.Square, accum_out=ss2[:, b2:b2 + 1]),
                 reads=["h2t%d" % b2], writes=["junk2%d" % b2, "ss2%d" % b2])
            rstd_from_ssq(rs2[:, b2:b2 + 1], ss2[:, b2:b2 + 1], D, "ss2%d" % b2, "rs2%d" % b2)
            STT(u2, h2t[b2], rs2[:, b2:b2 + 1], n2bc, ALU.mult, ALU.mult, ["h2t%d" % b2, "rs2%d" % b2, "n2bc"], [uk])
            CP("pool", u2tok[:, io, :], u2, [uk], ["u2tok%d" % io])
            for half in range(2):
                pt, pk = next_pf()

                def tru_fn(e, pt=pt, half=half):
                    ins = None
                    for c in range(4):
                        cc = half * 4 + c
                        ins = e.transpose(out=pt[:, c * 128:(c + 1) * 128], in_=u2[:, cc * 128:(cc + 1) * 128], identity=cm[:, 0, :])
                    return ins
                P.op("pe", tru_fn, reads=[uk, "cm"], writes=[pk])
                CP("act", u2Tf[:, half * 4:half * 4 + 4, :].rearrange("p c t -> p (c t)"), pt[:, :], [pk], ["u2Tf%d_%d" % (b2, half)])
            pr_, prk = next_pf()
            mm_group(pr_[:, 0:36], [(u2Tf[:, c, :], wr[:, c, :]) for c in range(8)], prk, ["u2Tf%d_0" % b2, "u2Tf%d_1" % b2, "wr"])
            TT("dve", lg, pr_[:, 0:36], rbbc, ALU.add, [prk, "rbbc"], [lk])
            gmax, ngmax, gsum, gw = sm[:, 0:1], sm[:, 1:2], sm[:, 2:3], sm[:, 3:4]
            oh = OH[:, io, :]
            ohk = "OH%d" % io
            ge = sm[:, 8:12]
            esel = sm[:, 16:24]
            top8 = sm[:, 24:32]
            d21, w1g, w2g = sm[:, 32:33], sm[:, 33:34], sm[:, 34:35]
            wa = sm[:, 40:48]
            wb_ = sm[:, 48:56]
            P.op("dve", lambda e: e.tensor_reduce(out=gmax, in_=lg[:, 0:4], axis=AX.X, op=ALU.max), reads=[lk], writes=[sk_])
            TS("dve", oh, lg[:, 0:4], gmax, None, ALU.is_equal, None, [lk, sk_], [ohk])
            TS("dve", ngmax, gmax, -1.0, None, ALU.mult, None, [sk_], [sk_])
            ACT(ge, lg[:, 0:4], AF.Exp, [lk, sk_], [sk_], bias=ngmax, scale=1.0)
            P.op("dve", lambda e: e.tensor_reduce(out=gsum, in_=ge, axis=AX.X, op=ALU.add), reads=[sk_], writes=[sk_])
            P.op("dve", lambda e: e.reciprocal(out=gw, in_=gsum), reads=[sk_], writes=[sk_])
            TS("dve", esel, lg[:, 4:12], oh[:, 0:1], None, ALU.mult, None, [lk, ohk], [sk_])
            for g in range(1, 4):
                STT(esel, lg[:, 4 + 8 * g:12 + 8 * g], oh[:, g:g + 1], esel, ALU.mult, ALU.add, [lk, ohk, sk_], [sk_])
            P.op("dve", lambda e: e.max(out=top8, in_=esel), reads=[sk_], writes=[sk_])
            TT("dve", d21, top8[:, 1:2], top8[:, 0:1], ALU.subtract, [sk_], [sk_])
            ACT(d21, d21, AF.Exp, [sk_], [sk_])
            TS("dve", d21, d21, 1.0, None, ALU.add, None, [sk_], [sk_])
            P.op("dve", lambda e: e.reciprocal(out=w1g, in_=d21), reads=[sk_], writes=[sk_])
            TT("dve", w1g, w1g, gw, ALU.mult, [sk_], [sk_])
            TT("dve", w2g, gw, w1g, ALU.subtract, [sk_], [sk_])
            TS("dve", wa, esel, top8[:, 0:1], w1g, ALU.is_equal, ALU.mult, [sk_], [sk_])
            TS("dve", wb_, esel, top8[:, 1:2], w2g, ALU.is_equal, ALU.mult, [sk_], [sk_])
            TT("dve", WE[:, io, :], wa, wb_, ALU.add, [sk_], ["WE%d" % io])

        for m in range(0, NTO, 2):
            ra, rb = [], []
            P.rec = ra
            stream[0] = 0
            c1_body(m)
            P.rec = rb
            stream[0] = 1
            c1_body(m + 1)
            P.rec = None
            stream[0] = None
            P.replay_merged(ra, rb)

        for io in range(NTO):
            oh = OH[:, io, :]
            ohk = "OH%d" % io
            prk_t, prkk = next_pf()
            P.op("pe", lambda e, t=prk_t, io=io: (e.matmul(t[:, 0:4], lhsT=cm[:, 4, :], rhs=OH[:, io, :], start=True, stop=False),
                                                   e.matmul(t[:, 0:4], lhsT=cm[:, 7, :], rhs=pren, start=False, stop=True))[1],
                 reads=[ohk, "pren", "cm"], writes=[prkk])
            rk = smr[:, 0:4]
            okm = smr[:, 4:8]
            TS("dve", rk, prk_t[:, 0:4], -16.0, None, ALU.mult, None, [prkk], ["smr"])
            STT(pren, oh, -1.0 / 16.0, pren, ALU.mult, ALU.add, [ohk, "pren", prkk], ["pren"])
            TS("dve", okm, rk, float(CAPG), None, ALU.is_lt, None, ["smr"], ["smr"])
            TT("dve", okm, okm, oh, ALU.mult, ["smr", ohk], ["smr"])
            TT("dve", rk, rk, goffm, ALU.add, ["smr", "goffm"], ["smr"])
            TT("dve", rk, rk, okm, ALU.mult, ["smr"], ["smr"])
            P.op("dve", lambda e, io=io, rk=rk: e.tensor_reduce(out=idxf[:, io:io + 1], in_=rk, axis=AX.X, op=ALU.add), reads=["smr"], writes=["idxf%d" % io])
            TS("dve", idxf[:, io:io + 1], idxf[:, io:io + 1], OOB, None, ALU.add, None, ["idxf%d" % io], ["idxf%d" % io])
            CP("dve", idxi[:, io:io + 1], idxf[:, io:io + 1], ["idxf%d" % io], ["idxi%d" % io])
            P.op("pool", lambda e, io=io: e.indirect_dma_start(out=XB[:, :], out_offset=bass.IndirectOffsetOnAxis(ap=idxi[:, io:io + 1], axis=0),
                                                               in_=u2tok[:, io, :], in_offset=None, bounds_check=bcreg(e), oob_is_err=False),
                 reads=["u2tok%d" % io, "idxi%d" % io, "XB"], writes=["XBs%d" % io], dma=True)
            P.op("pool", lambda e, io=io: e.indirect_dma_start(out=WB[:, :], out_offset=bass.IndirectOffsetOnAxis(ap=idxi[:, io:io + 1], axis=0),
                                                               in_=WE[:, io, :], in_offset=None, bounds_check=bcreg(e), oob_is_err=False),
                 reads=["WE%d" % io, "idxi%d" % io, "WB"], writes=["WBs%d" % io], dma=True)
        H2K = ["H2_%d" % i for i in range(NTO)]
        XBK = ["XBs%d" % i for i in range(NTO)] + ["XB"]
        WBK = ["WBs%d" % i for i in range(NTO)] + ["WB"]
        if debug:
            DMA("sp", WTD[:, 0:NTO], idxf, ["idxf%d" % i for i in range(NTO)], ["WTD"])
        if stop_after == "C1":
            P.op("sp", None, reads=H2K + XBK + WBK + ["WTD"], writes=[])
            P.emit()
            return nc

        P.barrier()
        AR.off = M2
        NCH = CAPG // 128
        xs = sb("xs", [128, NCH, D], BF16)
        xTg = sb("xTg", [128, 8, CAPG], BF16)
        wsl = sb("wsl", [128, NCH, 8])
        hid = sb("hid", [128, 4, CAPG], BF16)
        yacc = sb("yacc", [128, NCH, D])
        wgb = [sb("wgb%d" % i, [128, 8, 512], BF16) for i in range(2)]
        wub = [sb("wub%d" % i, [128, 8, 512], BF16) for i in range(2)]
        wdb = [sb("wdb%d" % i, [128, 4, D], BF16) for i in range(2)]
        sgb = [sb("sgb%d" % i, [128, 512]) for i in range(2)]

        def load_expert(ex):
            b = ex % 2
            DMA("pool", wgb[b], ewg[ex].rearrange("(c p) n -> p c n", p=128), [], ["wgb%d" % b])
            DMA("pool", wub[b], ewu[ex].rearrange("(c p) n -> p c n", p=128), [], ["wub%d" % b])
            DMA("pool", wdb[b], ewd[ex].rearrange("(c p) n -> p c n", p=128), [], ["wdb%d" % b])
        load_expert(0)
        load_expert(1)
        hid2 = [hid, sb("hidB", [128, 4, CAPG], BF16)]
        kk2 = [0]
        nsl = [(0, 512), (512, CAPG)]

        def GU(ex, hb, XTK):
            b = ex % 2
            for (n0, n1) in nsl:
                for fc in range(4):
                    pg, pgk = next_pf()
                    pu, puk = next_pf()
                    mm_group(pg[:, 0:n1 - n0], [(wgb[b][:, c, fc * 128:(fc + 1) * 128], xTg[:, c, n0:n1]) for c in range(8)], pgk,
                             ["wgb%d" % b] + XTK)
                    mm_group(pu[:, 0:n1 - n0], [(wub[b][:, c, fc * 128:(fc + 1) * 128], xTg[:, c, n0:n1]) for c in range(8)], puk,
                             ["wub%d" % b] + XTK)
                    sb_i = kk2[0] % 2
                    kk2[0] += 1
                    ACT(sgb[sb_i][:, 0:n1 - n0], pg[:, 0:n1 - n0], AF.Silu, [pgk], ["sgb%d" % sb_i])
                    TT("dve", hid2[hb][:, fc, n0:n1], sgb[sb_i][:, 0:n1 - n0], pu[:, 0:n1 - n0], ALU.mult, ["sgb%d" % sb_i, puk],
                       ["hid%d_%d_%d" % (hb, fc, n0)])

        def DN(ex, hb, el):
            b = ex % 2
            HK = ["hid%d_%d_%d" % (hb, fc, n0) for fc in range(4) for (n0, _) in nsl]
            for ch in range(NCH):
                for cg in range(2):
                    py, pyk = next_pf()
                    mm_group(py[:, :], [(hid2[hb][:, fc, ch * 128:(ch + 1) * 128], wdb[b][:, fc, cg * 512:(cg + 1) * 512]) for fc in range(4)], pyk,
                             ["wdb%d" % b] + HK)
                    ya = yacc[:, ch, cg * 512:(cg + 1) * 512]
                    yk = "yacc%d" % ch
                    if el == 0:
                        TS("dve", ya, py[:, :], wsl[:, ch, el:el + 1], None, ALU.mult, None, [pyk, "wsl"], [yk])
                    else:
                        STT(ya, py[:, :], wsl[:, ch, el:el + 1], ya, ALU.mult, ALU.add, [pyk, "wsl", yk], [yk])

        for g in range(4):
            DMA("sp", xs, XB[g * CAPG:(g + 1) * CAPG, :].rearrange("(c p) d -> p c d", p=128), XBK, ["xs"])
            DMA("sp", wsl, WB[g * CAPG:(g + 1) * CAPG, :].rearrange("(c p) d -> p c d", p=128), WBK, ["wsl"])
            for ch in range(NCH):
                pbt, pbk = next_pb()

                def trx_fn(e, pbt=pbt, ch=ch):
                    ins = None
                    for c in range(8):
                        ins = e.transpose(out=pbt[:, c * 128:(c + 1) * 128], in_=xs[:, ch, c * 128:(c + 1) * 128], identity=identb)
                    return ins
                P.op("pe", trx_fn, reads=["xs", "identb"], writes=[pbk])
                CP("act" if ch % 2 else "dve", xTg[:, :, ch * 128:(ch + 1) * 128], pbt[:, :].rearrange("p (c t) -> p c t", c=8), [pbk], ["xTg%d" % ch])
            XTK = ["xTg%d" % ch for ch in range(NCH)]
            GU(g * 8, 0, XTK)
            for el in range(8):
                ex = g * 8 + el
                if ex + 2 < NEXP:
                    b_ = ex % 2
                    DMA("pool", wgb[b_], ewg[ex + 2].rearrange("(c p) n -> p c n", p=128), [], ["wgb%d" % b_])
                    DMA("pool", wub[b_], ewu[ex + 2].rearrange("(c p) n -> p c n", p=128), [], ["wub%d" % b_])
                ra, rb = [], []
                if el + 1 < 8:
                    P.rec = ra
                    stream[0] = 0
                    GU(ex + 1, (el + 1) % 2, XTK)
                P.rec = rb
                stream[0] = 1
                DN(ex, el % 2, el)
                P.rec = None
                stream[0] = None
                P.replay_merged(ra, rb)
                if ex + 2 < NEXP:
                    DMA("pool", wdb[ex % 2], ewd[ex + 2].rearrange("(c p) n -> p c n", p=128), [], ["wdb%d" % (ex % 2)])
            DMA("sp", YB[g * CAPG:(g + 1) * CAPG, :].rearrange("(c p) d -> p c d", p=128), yacc, ["yacc%d" % ch for ch in range(NCH)], ["YB%d" % g])
        YBK = ["YB%d" % g for g in range(4)]
        P.barrier()
        AR.off = M2
        hl = [sb("hl%d" % i, [128, D]) for i in range(2)]
        yg = [sb("yg%d" % i, [128, D]) for i in range(2)]
        ob = [sb("ob%d" % i, [128, D]) for i in range(2)]
        junk32 = [sb("junk3%d" % i, [128, D], BF16) for i in range(2)]
        ss3 = sb("ss3", [128, 2])
        rs3 = sb("rs3", [128, 2])

        def fin_body(io):
            b2 = io % 2
            junk3 = junk32[b2]
            DMA("sp", hl[b2], H2[io * 128:(io + 1) * 128, :], ["H2_%d" % io], ["hl%d" % b2])
            P.op("pool", lambda e: e.memset(yg[b2], 0.0), writes=["yg%d" % b2])
            P.op("pool", lambda e: e.indirect_dma_start(out=yg[b2], out_offset=None, in_=YB[:, :],
                                                        in_offset=bass.IndirectOffsetOnAxis(ap=idxi[:, io:io + 1], axis=0),
                                                        bounds_check=bcreg(e), oob_is_err=False),
                 reads=YBK + ["idxi%d" % io], writes=["yg%d" % b2], dma=True)
            TT("dve", hl[b2], hl[b2], yg[b2], ALU.add, ["hl%d" % b2, "yg%d" % b2], ["hl%d" % b2])
            P.op("act", lambda e: e.activation(out=junk3, in_=hl[b2], func=AF.Square, accum_out=ss3[:, b2:b2 + 1]),
                 reads=["hl%d" % b2], writes=["junk3%d" % b2, "ss3%d" % b2])
            rstd_from_ssq(rs3[:, b2:b2 + 1], ss3[:, b2:b2 + 1], D, "ss3%d" % b2, "rs3%d" % b2)
            STT(ob[b2], hl[b2], rs3[:, b2:b2 + 1], fnbc, ALU.mult, ALU.mult, ["hl%d" % b2, "rs3%d" % b2, "fnbc"], ["ob%d" % b2])
            DMA("sp", out_d[io * 128:(io + 1) * 128, :], ob[b2], ["ob%d" % b2], ["OUT%d" % io])

        for m in range(0, NTO, 2):
            ra, rb = [], []
            P.rec = ra
            fin_body(m)
            P.rec = rb
            fin_body(m + 1)
            P.rec = None
            P.replay_merged(ra, rb)
        P.op("sp", None, reads=["OUT%d" % i for i in range(NTO)], writes=[])
        P.emit()
    return nc


def _consts():
    s = np.arange(128)[:, None]
    t = np.arange(128)[None, :]
    cm = np.zeros((128, 8, 128), np.float32)
    cm[:, 0] = (s == t)
    cm[:, 1] = (s <= t) / -16.0
    cm[:, 2] = (s >= t) / -16.0
    cm[:, 3] = (s > t) / -16.0
    cm[:, 4] = (s < t) / -16.0
    cm[:, 5] = (s <= t)
    cm[:, 6] = (s >= t)
    cm[:, 7] = 1.0
    band = np.zeros((128, 384), np.float32)
    band[:, 0:128] = (s >= t + 64)
    band[:, 128:256] = (np.abs(s - t) <= 64)
    band[:, 256:384] = (s <= t - 64)
    return cm, band


def make_in_maps(inputs):
    f = lambda a: np.ascontiguousarray(np.asarray(a, dtype=np.float32))
    x = f(inputs["x"])
    cm, band = _consts()
    wz = np.zeros((33, 512), np.float32)
    wz[0:16, 0:256] = f(inputs["gla_fwd_gate_w"])[0]
    wz[16:32, 256:512] = f(inputs["gla_bwd_gate_w"])[0]
    wz[32, 0:256] = f(inputs["gla_fwd_gate_b"])[0]
    wz[32, 256:512] = f(inputs["gla_bwd_gate_b"])[0]
    vecs = np.zeros((4, D), np.float32)
    vecs[0] = f(inputs["norm1_w"])[0]
    vecs[1] = f(inputs["norm2_w"])[0]
    vecs[2] = f(inputs["final_norm_w"])
    vecs[3] = np.tile(f(inputs["gla_norm_w"])[0], 8)
    wr = np.concatenate([f(inputs["router_group_w"])[0]] + [f(inputs["router_expert_w"])[0, g] for g in range(4)], axis=1)
    rb = np.concatenate([f(inputs["router_group_b"])[0], f(inputs["router_expert_b"])[0].reshape(-1)])[None, :]
    inv = (500000.0 ** (-(np.arange(0, 16, 2, dtype=np.float32) / np.float32(16)))).astype(np.float32)
    shared = dict(cmat=cm, band3=band, w_in=f(inputs["w_in"])[0], wz=wz, vecs=vecs, w_out=f(inputs["w_out"])[0],
                  wr=np.ascontiguousarray(wr), rb=np.ascontiguousarray(rb), ewg=f(inputs["expert_w_gate"])[0],
                  ewu=f(inputs["expert_w_up"])[0], ewd=f(inputs["expert_w_down"])[0])
    maps = []
    for c in range(8):
        b, q = c // 4, c % 4
        s0 = q * OWN
        pos = np.arange(s0 - HALO, s0 + OWN + HALO)
        valid = (pos >= 0) & (pos < S)
        xwin = np.zeros((WIN, D), np.float32)
        xwin[valid] = x[b, pos[valid]]
        ang = (pos.astype(np.float32)[:, None] * inv[None, :]).astype(np.float32)
        cs = np.concatenate([np.cos(ang), np.sin(ang)], axis=1).astype(np.float32)
        cs_t = np.ascontiguousarray(cs.reshape(NTW, 128, 16).transpose(1, 0, 2))
        vcol = np.ascontiguousarray(valid.astype(np.float32).reshape(NTW, 128).T)
        m = dict(shared)
        m.update(xw=xwin, vcol=vcol, cs_t=cs_t)
        maps.append(m)
    return maps


_NC_CACHE = {}


def kernel(**inputs):
    maps = make_in_maps(inputs)
    if "nc" not in _NC_CACHE:
        _NC_CACHE["nc"] = build_program()
    nc = _NC_CACHE["nc"]
    res = run_bass_kernel_spmd(nc, maps, core_ids=list(range(8)))
    out = np.zeros((2, S, D), np.float32)
    for c in range(8):
        b, q = c // 4, c % 4
        out[b, q * OWN:(q + 1) * OWN] = res.results[c]["out"]
    return out
```
